# Optimizing a Trainium2 kernel written in Bass

```python
import jax, jax.numpy as jnp
from jax import lax
import numpy as np

D_MODEL = 2048
BATCH = 2
SEQ = 8192
DEPTH = 1

EPS = 1e-6
M_HEADS = 4
M_HEAD_DIM = D_MODEL // 2 // M_HEADS
M_WIDTH = M_HEADS * M_HEAD_DIM
M_CHUNK = 64
CONV_W = 4
A_GROUPS = 4
A_HPG = 4
A_HEADS = A_GROUPS * A_HPG
A_HEAD_DIM = 64
A_WIDTH = A_HEADS * A_HEAD_DIM
CMP_BLOCK = 32
CMP_STRIDE = 16
CMP_HIDDEN = 256
SEL_BLOCK = 64
SEL_TOP = 16
WINDOW = 512
Q_BLOCK = 128
FORCE_SCORE = 1e4
P_HEADS = 8
P_NKEYS = 128
P_EXPERTS = P_NKEYS * P_NKEYS
P_TOPK = 16
P_QDIM = 256
P_HALF = P_QDIM // 2
P_TOK_BLOCK = 128
MV_OFF = 2 * M_WIDTH
MO_OFF = 3 * M_WIDTH
MIF_OFF = 4 * M_WIDTH
AQ_OFF = MIF_OFF + 2 * M_HEADS
AKV_OFF = AQ_OFF + A_WIDTH
AG_OFF = AKV_OFF + 6 * A_GROUPS * A_HEAD_DIM
IN_COLS = AG_OFF + 3 * A_HEADS
D_MIX = M_WIDTH + A_WIDTH

kernel_name = 'hybrid_mlstm_nsa_peer'


def rmsnorm(a, g):
    a32 = a.astype(jnp.float32)
    r = a32 * lax.rsqrt(jnp.mean(a32 * a32, axis=-1, keepdims=True) + EPS)
    return (r * g.astype(jnp.float32)).astype(a.dtype)


def masked_softmax(s, mask):
    s = jnp.where(mask, s, -jnp.inf)
    m = jnp.max(s, axis=-1, keepdims=True)
    m = jnp.where(jnp.isfinite(m), m, 0.0)
    e = jnp.exp(s - m)
    return e / jnp.maximum(jnp.sum(e, axis=-1, keepdims=True), 1e-30)


def causal_conv(a, w):
    return lax.conv_general_dilated(a, w[:, None, :], window_strides=(1,), padding=[(CONV_W - 1, 0)],
                                    dimension_numbers=('NWC', 'WIO', 'NWC'), feature_group_count=a.shape[-1])


def mlstm_chunkwise(q, k, v, ig, lf):
    b_, s_, h_, d_ = q.shape
    nc = s_ // M_CHUNK

    def chunks(a):
        a = a.reshape((b_, nc, M_CHUNK, h_) + a.shape[3:])
        return jnp.moveaxis(jnp.moveaxis(a, 1, 0), 2, 3)

    causal = jnp.tril(jnp.ones((M_CHUNK, M_CHUNK), dtype=bool))

    def step(carry, inp):
        c_st, n_st, m_st = carry
        qc, kc, vc, ic, fc = inp
        b = jnp.cumsum(fc, axis=-1)
        dmat = jnp.where(causal, b[..., :, None] - b[..., None, :] + ic[..., None, :], -jnp.inf)
        m_inter = b + m_st[..., None]
        m_t = jnp.maximum(m_inter, jnp.max(dmat, axis=-1))
        w = jnp.exp(dmat - m_t[..., None]) * jnp.einsum('bhtd,bhsd->bhts', qc, kc)
        a_inter = jnp.exp(m_inter - m_t)
        num = jnp.einsum('bhts,bhsd->bhtd', w, vc) + a_inter[..., None] * jnp.einsum('bhtd,bhde->bhte', qc, c_st)
        den = jnp.sum(w, axis=-1) + a_inter * jnp.einsum('bhtd,bhd->bht', qc, n_st)
        h = num / jnp.maximum(jnp.abs(den), jnp.exp(-m_t))[..., None]
        b_last = b[..., -1]
        a_s = b_last[..., None] - b + ic
        m_new = jnp.maximum(b_last + m_st, jnp.max(a_s, axis=-1))
        w_s = jnp.exp(a_s - m_new[..., None])
        decay = jnp.exp(b_last + m_st - m_new)
        c_new = decay[..., None, None] * c_st + jnp.einsum('bhs,bhsd,bhse->bhde', w_s, kc, vc)
        n_new = decay[..., None] * n_st + jnp.einsum('bhs,bhsd->bhd', w_s, kc)
        return (c_new, n_new, m_new), h

    init = (jnp.zeros((b_, h_, d_, d_), jnp.float32), jnp.zeros((b_, h_, d_), jnp.float32),
            jnp.zeros((b_, h_), jnp.float32))
    _, hs = lax.scan(step, init, (chunks(q), chunks(k), chunks(v), chunks(ig), chunks(lf)))
    return jnp.moveaxis(hs, 0, 1).transpose(0, 1, 3, 2, 4).reshape(b_, s_, h_, d_)


def nsa(qg, k_c, v_c, k_sel, v_sel, k_win, v_win, gates, pos_k, pos_v, k_w1, k_w2, v_w1, v_w2, kcmp_g):
    f32 = jnp.float32
    b_, s_ = qg.shape[:2]
    n_cmp = (s_ - CMP_BLOCK) // CMP_STRIDE + 1
    n_blk = s_ // SEL_BLOCK
    n_sel = min(SEL_TOP, n_blk)
    scale = A_HEAD_DIM ** -0.5
    cmp_idx = np.arange(n_cmp)[:, None] * CMP_STRIDE + np.arange(CMP_BLOCK)[None, :]
    cmp_end = jnp.asarray(cmp_idx[:, -1], f32)
    starts = np.arange(n_cmp)[:, None] * CMP_STRIDE
    blk_starts = np.arange(n_blk)[None, :] * SEL_BLOCK
    overlap = jnp.asarray((starts < blk_starts + SEL_BLOCK) & (starts + CMP_BLOCK > blk_starts), f32)
    slopes = jnp.asarray(2.0 ** (-8.0 * (np.arange(A_HEADS) + 1) / A_HEADS), f32).reshape(A_GROUPS, A_HPG)
    sl = slopes[None, :, :, None, None]

    def compress(a, pos, w1, w2):
        blk = a[:, cmp_idx] + pos[:, None, :]
        blk = jnp.swapaxes(blk, 2, 3).reshape(b_, n_cmp, A_GROUPS, CMP_BLOCK * A_HEAD_DIM)
        return jax.nn.gelu(blk @ w1) @ w2

    k_cmp = rmsnorm(compress(k_c, pos_k, k_w1, k_w2), kcmp_g)
    v_cmp = compress(v_c, pos_v, v_w1, v_w2)
    ks_blocks = k_sel.reshape(b_, n_blk, SEL_BLOCK, A_GROUPS, A_HEAD_DIM).transpose(0, 3, 1, 2, 4)
    vs_blocks = v_sel.reshape(b_, n_blk, SEL_BLOCK, A_GROUPS, A_HEAD_DIM).transpose(0, 3, 1, 2, 4)
    pad = ((0, 0), (WINDOW, 0), (0, 0), (0, 0))
    kw_pad = jnp.pad(k_win, pad)
    vw_pad = jnp.pad(v_win, pad)
    gather = jax.vmap(jax.vmap(lambda tab, ix: tab[ix]))
    blk_ids = jnp.arange(n_blk)

    def block_fn(qb):
        q0 = qb * Q_BLOCK
        t = q0 + jnp.arange(Q_BLOCK)
        qq = lax.dynamic_slice_in_dim(qg, q0, Q_BLOCK, axis=1)
        gt = lax.dynamic_slice_in_dim(gates, q0, Q_BLOCK, axis=1)
        dist = t.astype(f32)[:, None] - cmp_end[None, :]
        s = jnp.einsum('btghd,bigd->bghti', qq, k_cmp, preferred_element_type=f32) * scale - sl * dist
        p_cmp = masked_softmax(s, dist >= 0)
        o_cmp = jnp.einsum('bghti,bigd->btghd', p_cmp, v_cmp)
        imp = jnp.einsum('bghti,ij->bgtj', p_cmp, overlap)
        cur = (t // SEL_BLOCK)[:, None]
        forced = (blk_ids[None, :] == 0) | (blk_ids[None, :] == cur) | (blk_ids[None, :] == cur - 1)
        imp = jnp.where(forced, FORCE_SCORE, jnp.where(blk_ids[None, :] <= cur, imp, -FORCE_SCORE))
        _, sel = lax.top_k(imp, n_sel)
        k_g = gather(ks_blocks, sel).reshape(b_, A_GROUPS, Q_BLOCK, n_sel * SEL_BLOCK, A_HEAD_DIM)
        v_g = gather(vs_blocks, sel).reshape(b_, A_GROUPS, Q_BLOCK, n_sel * SEL_BLOCK, A_HEAD_DIM)
        pos = (sel[..., None] * SEL_BLOCK + jnp.arange(SEL_BLOCK)).reshape(b_, A_GROUPS, Q_BLOCK, n_sel * SEL_BLOCK)
        dist = (t[None, None, :, None] - pos)[:, :, None]
        s = jnp.einsum('btghd,bgtjd->bghtj', qq, k_g, preferred_element_type=f32) * scale - sl * dist.astype(f32)
        p = masked_softmax(s, dist >= 0)
        o_sel = jnp.einsum('bghtj,bgtjd->btghd', p, v_g)
        kw = lax.dynamic_slice_in_dim(kw_pad, q0, Q_BLOCK + WINDOW, axis=1)
        vw = lax.dynamic_slice_in_dim(vw_pad, q0, Q_BLOCK + WINDOW, axis=1)
        kpos = q0 - WINDOW + jnp.arange(Q_BLOCK + WINDOW)
        dist = t[:, None] - kpos[None, :]
        mask = (kpos[None, :] >= 0) & (dist >= 0) & (dist < WINDOW)
        s = jnp.einsum('btghd,bsgd->bghts', qq, kw, preferred_element_type=f32) * scale - sl * dist.astype(f32)
        p = masked_softmax(s, mask)
        o_win = jnp.einsum('bghts,bsgd->btghd', p, vw)
        return gt[..., 0:1] * o_cmp + gt[..., 1:2] * o_sel + gt[..., 2:3] * o_win

    outs = lax.map(block_fn, jnp.arange(s_ // Q_BLOCK))
    return jnp.moveaxis(outs, 0, 1).reshape(b_, s_, A_WIDTH)


def peer(h, wq, subkeys, u_tab, v_tab):
    b_, s_, d_ = h.shape
    n_tok = b_ * s_
    xt = h.reshape(n_tok, d_)
    q = (xt @ wq).reshape(n_tok, P_HEADS, 2, P_HALF)
    sc = jnp.einsum('nhpd,pkd->nhpk', q, subkeys, preferred_element_type=jnp.float32)
    s1, i1 = lax.top_k(sc[:, :, 0], P_TOPK)
    s2, i2 = lax.top_k(sc[:, :, 1], P_TOPK)
    cand = (s1[..., :, None] + s2[..., None, :]).reshape(n_tok, P_HEADS, P_TOPK * P_TOPK)
    cidx = (i1[..., :, None] * P_NKEYS + i2[..., None, :]).reshape(n_tok, P_HEADS, P_TOPK * P_TOPK)
    top, sel = lax.top_k(cand, P_TOPK)
    eidx = jnp.take_along_axis(cidx, sel, axis=-1)
    gate = jax.nn.softmax(top, axis=-1)
    nb = n_tok // P_TOK_BLOCK

    def blk(args):
        xb, eb, gb = args
        a = jnp.einsum('td,thkd->thk', xb, u_tab[eb], preferred_element_type=jnp.float32)
        w = gb * jax.nn.gelu(a)
        return jnp.einsum('thk,thkd->td', w, v_tab[eb])

    y = lax.map(blk, (xt.reshape(nb, P_TOK_BLOCK, d_), eidx.reshape(nb, P_TOK_BLOCK, P_HEADS, P_TOPK),
                      gate.reshape(nb, P_TOK_BLOCK, P_HEADS, P_TOPK)))
    return y.reshape(b_, s_, d_)


def setup_inputs(seed: int = 0) -> dict:
    key = jax.random.key(seed)
    ks = jax.random.split(key, 24)
    L = DEPTH
    f32 = jnp.float32

    def nrm(k, shape, scale):
        return jax.random.normal(k, shape, f32) * scale

    return {
        'x': nrm(ks[0], (BATCH, SEQ, D_MODEL), 1.0),
        'c': nrm(ks[1], (BATCH, D_MODEL), 1.0),
        'w_mod': nrm(ks[2], (L, D_MODEL, 6 * D_MODEL), 0.5 * D_MODEL ** -0.5),
        'b_mod': nrm(ks[3], (L, 6 * D_MODEL), 0.02),
        'norm1_g': 1.0 + nrm(ks[4], (L, D_MODEL), 0.05),
        'norm2_g': 1.0 + nrm(ks[5], (L, D_MODEL), 0.05),
        'w_in': nrm(ks[6], (L, D_MODEL, IN_COLS), D_MODEL ** -0.5),
        'conv_qk': nrm(ks[7], (L, CONV_W, 2 * M_WIDTH), CONV_W ** -0.5),
        'b_igate': nrm(ks[8], (L, M_HEADS), 0.1),
        'b_fgate': jnp.linspace(3.0, 6.0, M_HEADS, dtype=f32)[None, :] + nrm(ks[9], (L, M_HEADS), 0.1),
        'mlstm_norm_g': 1.0 + nrm(ks[10], (L, M_WIDTH), 0.05),
        'qn_g': 1.0 + nrm(ks[11], (L, A_HEAD_DIM), 0.05),
        'kn_g': 1.0 + nrm(ks[12], (L, 3, A_HEAD_DIM), 0.05),
        'cmp_pos_k': nrm(ks[13], (L, CMP_BLOCK, A_HEAD_DIM), 0.2),
        'cmp_pos_v': nrm(ks[14], (L, CMP_BLOCK, A_HEAD_DIM), 0.2),
        'cmp_k_w1': nrm(ks[15], (L, CMP_BLOCK * A_HEAD_DIM, CMP_HIDDEN), (CMP_BLOCK * A_HEAD_DIM) ** -0.5),
        'cmp_k_w2': nrm(ks[16], (L, CMP_HIDDEN, A_HEAD_DIM), CMP_HIDDEN ** -0.5),
        'cmp_v_w1': nrm(ks[17], (L, CMP_BLOCK * A_HEAD_DIM, CMP_HIDDEN), (CMP_BLOCK * A_HEAD_DIM) ** -0.5),
        'cmp_v_w2': nrm(ks[18], (L, CMP_HIDDEN, A_HEAD_DIM), CMP_HIDDEN ** -0.5),
        'w_out': nrm(ks[19], (L, D_MIX, D_MODEL), D_MIX ** -0.5),
        'peer_wq': nrm(ks[20], (L, D_MODEL, P_HEADS * P_QDIM), D_MODEL ** -0.5),
        'peer_subkeys': nrm(ks[21], (L, 2, P_NKEYS, P_HALF), P_HALF ** -0.5),
        'peer_u': nrm(ks[22], (L, P_EXPERTS, D_MODEL), D_MODEL ** -0.5),
        'peer_v': nrm(ks[23], (L, P_EXPERTS, D_MODEL), 1.0),
    }


def reference(x, c, w_mod, b_mod, norm1_g, norm2_g, w_in, conv_qk, b_igate, b_fgate, mlstm_norm_g,
              qn_g, kn_g, cmp_pos_k, cmp_pos_v, cmp_k_w1, cmp_k_w2, cmp_v_w1, cmp_v_w2, w_out,
              peer_wq, peer_subkeys, peer_u, peer_v):
    f32 = jnp.float32
    b_, s_, _ = x.shape
    for l in range(DEPTH):
        mod = (jax.nn.silu(c) @ w_mod[l] + b_mod[l]).reshape(b_, 6, 1, D_MODEL)
        shift1, scale1, gate1, shift2, scale2, gate2 = (mod[:, i] for i in range(6))
        h = rmsnorm(x, norm1_g[l]) * (1.0 + scale1) + shift1
        z = h @ w_in[l]
        qk = jax.nn.silu(causal_conv(z[..., :MV_OFF], conv_qk[l]))
        mshape = (b_, s_, M_HEADS, M_HEAD_DIM)
        mq = qk[..., :M_WIDTH].reshape(mshape).astype(f32)
        mk = qk[..., M_WIDTH:].reshape(mshape).astype(f32) * (M_HEAD_DIM ** -0.5)
        mv = z[..., MV_OFF:MO_OFF].reshape(mshape).astype(f32)
        mo = jax.nn.sigmoid(z[..., MO_OFF:MIF_OFF].astype(f32)).reshape(mshape)
        ig = (z[..., MIF_OFF:MIF_OFF + M_HEADS] + b_igate[l]).astype(f32)
        lf = jax.nn.log_sigmoid((z[..., MIF_OFF + M_HEADS:AQ_OFF] + b_fgate[l]).astype(f32))
        hm = mlstm_chunkwise(mq, mk, mv, ig, lf)
        hm = (rmsnorm(hm, mlstm_norm_g[l].reshape(M_HEADS, M_HEAD_DIM)) * mo).reshape(b_, s_, M_WIDTH)
        aq = rmsnorm(z[..., AQ_OFF:AKV_OFF].reshape(b_, s_, A_HEADS, A_HEAD_DIM), qn_g[l])
        aq = aq.reshape(b_, s_, A_GROUPS, A_HPG, A_HEAD_DIM)
        kv = z[..., AKV_OFF:AG_OFF].reshape(b_, s_, 6, A_GROUPS, A_HEAD_DIM)
        gates = jax.nn.sigmoid(z[..., AG_OFF:IN_COLS].astype(f32)).reshape(b_, s_, A_GROUPS, A_HPG, 3)
        ha = nsa(aq, kv[:, :, 0], kv[:, :, 1], rmsnorm(kv[:, :, 2], kn_g[l, 1]), kv[:, :, 3],
                 rmsnorm(kv[:, :, 4], kn_g[l, 2]), kv[:, :, 5], gates, cmp_pos_k[l], cmp_pos_v[l],
                 cmp_k_w1[l], cmp_k_w2[l], cmp_v_w1[l], cmp_v_w2[l], kn_g[l, 0])
        y = jnp.concatenate([hm, ha], axis=-1).astype(x.dtype) @ w_out[l]
        x = x + gate1 * y
        h2 = rmsnorm(x, norm2_g[l]) * (1.0 + scale2) + shift2
        x = x + gate2 * peer(h2, peer_wq[l], peer_subkeys[l], peer_u[l], peer_v[l]).astype(x.dtype)
    return x
```

```python
import numpy as np
from contextlib import ExitStack
import concourse.bass as bass
import concourse.mybir as mybir
from concourse.bass_utils import run_bass_kernel_spmd

F32 = mybir.dt.float32
BF16 = mybir.dt.bfloat16
AF = mybir.ActivationFunctionType
ALU = mybir.AluOpType
AX = mybir.AxisListType

D = 2048
S = 8192
NB = 16
EPS = 1e-6
import os
NDS = int(os.environ.get("NDS", "24"))


class Trk:
    __slots__ = ("w", "r")

    def __init__(self):
        self.w = None
        self.r = {}


class KB:
    def __init__(self, nc, es):
        self.nc = nc
        self.eng = {"pe": nc.tensor, "act": nc.scalar, "dve": nc.vector, "pool": nc.gpsimd, "sp": nc.sync}
        self.sem = {k: es.enter_context(nc.semaphore("s_" + k)) for k in self.eng}
        self.cnt = {k: 0 for k in self.eng}
        self.seen = {k: {} for k in self.eng}
        self.dsem = [es.enter_context(nc.semaphore("dq%d" % i)) for i in range(NDS)]
        self.duse = [0] * NDS
        self.dnext = 0
        self.ccsem = es.enter_context(nc.semaphore("ccs"))
        self.log = {k: [] for k in self.eng}

    def _wait(self, e, tok):
        if tok is None:
            return
        key, sem, val = tok
        if self.seen[e].get(key, 0) >= val:
            return
        self.eng[e].wait_ge(sem, val)
        self.log[e].append(("w", key, val))
        self.seen[e][key] = val

    def _deps(self, e, reads, writes):
        for b in reads:
            if b.w is not None and not (e == "pe" and b.w[0] == "pe"):
                self._wait(e, b.w)
        for b in writes:
            if b.w is not None and not (e == "pe" and b.w[0] == "pe"):
                self._wait(e, b.w)
            for t in b.r.values():
                if not (e == "pe" and t[0] == "pe"):
                    self._wait(e, t)

    def _mark(self, tok, reads, writes):
        for b in reads:
            b.r[tok[0]] = tok
        for b in writes:
            b.w = tok
            b.r = {}

    def op(self, e, fn, reads=(), writes=(), sig=True):
        self._deps(e, reads, writes)
        inst = fn(self.eng[e])
        if sig:
            self.cnt[e] += 1
            inst.then_inc(self.sem[e], 1)
            self.log[e].append(("i", e, 1))
            tok = (e, self.sem[e], self.cnt[e])
        else:
            tok = (e, self.sem[e], self.cnt[e] + 1)
        self._mark(tok, reads, writes)
        return tok

    def dma(self, out, in_, reads=(), writes=(), q="sp", **kw):
        i = self.dnext
        self.dnext = (i + 1) % NDS
        if self.duse[i] > 0:
            self._wait(q, (("d", i), self.dsem[i], 16 * self.duse[i]))
        self._deps(q, reads, writes)
        inst = self.eng[q].dma_start(out=out, in_=in_, **kw)
        self.duse[i] += 1
        inst.then_inc(self.dsem[i], 16)
        self.log[q].append(("i", ("d", i), 16))
        tok = (("d", i), self.dsem[i], 16 * self.duse[i])
        self._mark(tok, reads, writes)
        return tok

    def check(self):
        sems = {}
        pc = {k: 0 for k in self.log}
        prog = True
        while prog:
            prog = False
            for e, lg in self.log.items():
                while pc[e] < len(lg):
                    kind, key, val = lg[pc[e]]
                    if kind == "w":
                        if sems.get(key, 0) < val:
                            break
                    else:
                        sems[key] = sems.get(key, 0) + val
                    pc[e] += 1
                    prog = True
        stuck = {e: (pc[e], len(lg), lg[pc[e]] if pc[e] < len(lg) else None) for e, lg in self.log.items()}
        ok = all(pc[e] == len(lg) for e, lg in self.log.items())
        return ok, stuck, sems

    def barrier(self):
        for e in self.eng:
            for e2 in self.eng:
                if e2 != e and self.cnt[e2] > 0:
                    self._wait(e, (e2, self.sem[e2], self.cnt[e2]))
            for i in range(NDS):
                if self.duse[i] > 0:
                    self._wait(e, (("d", i), self.dsem[i], 16 * self.duse[i]))


def _bf(a):
    import ml_dtypes
    return np.asarray(a, dtype=np.float32).astype(ml_dtypes.bfloat16)


def build(dbg=False, phases=99, nb=NB, parts='abcde'):
    nc = bass.Bass("TRN2", target_bir_lowering=False)
    es = ExitStack()
    kb = KB(nc, es)

    def din(name, shape, dt=F32):
        return nc.dram_tensor(name, list(shape), dt, kind="ExternalInput").ap()

    def dscr(name, shape, dt=F32):
        return nc.dram_tensor(name, list(shape), dt, kind=("ExternalOutput" if dbg else "Internal")).ap()

    xT = din("xT", [D, S])
    c_col = din("c_col", [128, 16])
    w_mod = din("w_mod", [D, 6 * D])
    b_mod = din("b_mod", [1, 6 * D])
    g1_col = din("g1_col", [128, 16])
    w_in = din("w_in", [D, 1680])
    convw = din("convw", [128, 4, 4])
    qk_g = din("qk_g", [128, 4])
    mng_row = din("mng_row", [1, 256])
    ones_bf = din("ones_bf", [128, 128], BF16)
    blk64_bf = din("blk64_bf", [128, 128], BF16)
    ident_f = din("ident_f", [128, 128])

    s_q = dscr("s_q", [256, S])
    s_k = dscr("s_k", [256, S])
    s_aq = dscr("s_aq", [256, S], BF16)
    s_kv = dscr("s_kv", [128, S])
    s_kk = dscr("s_kk", [128, S], BF16)
    s_if = dscr("s_if", [2, S])
    s_v = dscr("s_v", [S, 256])
    s_go = dscr("s_go", [S, 256])
    s_vv = dscr("s_vv", [S, 128], BF16)
    s_gt = dscr("s_gt", [S, 12])

    ps = []
    pst = []
    for i in range(8):
        ps.append(es.enter_context(nc.psum_tensor("ps%d" % i, [128, 512], F32)))
        pst.append(Trk())

    def sb(name, shape, dt=F32):
        return es.enter_context(nc.sbuf_tensor(name, list(shape), dt))

    ones_b = sb("ones_b", [128, 128], BF16); t_ones_b = Trk()
    blk64 = sb("blk64", [128, 128], BF16); t_blk64 = Trk()
    ident = sb("ident", [128, 128]); t_ident = Trk()
    ones_f = sb("ones_f", [128, 128]); t_ones_f = Trk()
    kb.dma(ones_b[:], ones_bf, writes=[t_ones_b])
    kb.dma(blk64[:], blk64_bf, writes=[t_blk64])
    kb.dma(ident[:], ident_f, writes=[t_ident])
    kb.op("dve", lambda e: e.memset(ones_f[:], 1.0), writes=[t_ones_f])

    s_mod = dscr("s_mod", [1, 6 * D]); t_smod = Trk()
    csil = sb("csil", [128, 16]); t_csil = Trk()
    ccol = sb("ccol", [128, 16]); t_ccol = Trk()
    g1c = sb("g1c", [128, 16]); t_g1c = Trk()
    gs1 = sb("gs1", [128, 16]); t_gs1 = Trk()
    sh1 = sb("sh1", [128, 16]); t_sh1 = Trk()
    kb.dma(ccol[:], c_col, writes=[t_ccol])
    kb.dma(g1c[:], g1_col, writes=[t_g1c])
    kb.op("act", lambda e: e.activation(out=csil[:], in_=ccol[:], func=AF.Silu), reads=[t_ccol], writes=[t_csil])
    with ExitStack() as p0:
        wm = [p0.enter_context(nc.sbuf_tensor("wm%d" % i, [128, 16, 512], F32)) for i in range(2)]
        modrow = p0.enter_context(nc.sbuf_tensor("modrow", [1, 6 * D], F32)); t_modrow = Trk()
        bmod_sb = p0.enter_context(nc.sbuf_tensor("bmod_sb", [1, 6 * D], F32)); t_bmod = Trk()
        kb.dma(bmod_sb[:], b_mod, writes=[t_bmod])
        t_wm = [Trk(), Trk()]
        wmv = w_mod.rearrange("(k p) n -> p k n", p=128)
        for n in range(24):
            bi = n % 2
            kb.dma(wm[bi][:, 0:8, :], wmv[:, 0:8, n * 512:(n + 1) * 512], writes=[t_wm[bi]])
            kb.dma(wm[bi][:, 8:16, :], wmv[:, 8:16, n * 512:(n + 1) * 512], writes=[t_wm[bi]])
            pb = n % 2
            for k in range(16):
                kb.op("pe", lambda e, k=k, bi=bi, pb=pb: e.matmul(ps[pb][0:1, :], lhsT=csil[:, k:k + 1], rhs=wm[bi][:, k, :],
                                                                  start=(k == 0), stop=(k == 15)),
                      reads=[t_csil, t_wm[bi]], writes=[pst[pb]], sig=(k == 15))
            kb.op("dve", lambda e, n=n, pb=pb: e.tensor_tensor(out=modrow[0:1, n * 512:(n + 1) * 512], in0=ps[pb][0:1, :],
                                                               in1=bmod_sb[0:1, n * 512:(n + 1) * 512], op=ALU.add),
                  reads=[pst[pb], t_bmod], writes=[t_modrow])
        for which, dst, t_dst in ((0, sh1, t_sh1), (1, gs1, t_gs1)):
            for k in range(16):
                off = which * D + k * 128
                kb.op("pe", lambda e, off=off, k=k: e.matmul(ps[2][:, k:k + 1], lhsT=modrow[0:1, off:off + 128], rhs=ones_f[0:1, 0:1],
                                                             start=True, stop=True),
                      reads=[t_modrow, t_ones_f], writes=[pst[2]], sig=(k == 15))
            if which == 0:
                kb.op("dve", lambda e: e.tensor_copy(out=sh1[:], in_=ps[2][:, 0:16]), reads=[pst[2]], writes=[t_sh1])
            else:
                kb.op("dve", lambda e: e.scalar_tensor_tensor(out=gs1[:], in0=ps[2][:, 0:16], scalar=1.0, in1=g1c[:],
                                                              op0=ALU.add, op1=ALU.mult),
                      reads=[pst[2], t_g1c], writes=[t_gs1])
        kb.dma(s_mod, modrow[:], reads=[t_modrow], writes=[t_smod])
        kb.barrier()

    t_scr = {n: Trk() for n in ("q", "k", "aq", "kv", "kk", "if", "v", "go", "vv", "gt")}
    if phases >= 1:
        with ExitStack() as p1:
            def sb1(name, shape, dt=F32):
                return p1.enter_context(nc.sbuf_tensor(name, list(shape), dt))
            wb = sb1("wb", [128, 16, 1680], BF16); t_wb = Trk()
            cw = sb1("cw", [128, 4, 4]); t_cw = Trk()
            qkg = sb1("qkg", [128, 4]); t_qkg = Trk()
            mng = sb1("mng", [128, 256]); t_mng = Trk()
            mngr = sb1("mngr", [1, 256]); t_mngr = Trk()
            kb.dma(cw[:], convw, writes=[t_cw])
            kb.dma(qkg[:], qk_g, writes=[t_qkg])
            kb.dma(mngr[:], mng_row, writes=[t_mngr])
            kb.op("pe", lambda e: e.matmul(ps[3][:, 0:256], lhsT=ones_f[0:1, :], rhs=mngr[0:1, :], start=True, stop=True),
                  reads=[t_ones_f, t_mngr], writes=[pst[3]])
            kb.op("dve", lambda e: e.tensor_copy(out=mng[:], in_=ps[3][:, 0:256]), reads=[pst[3]], writes=[t_mng])
            wst = [sb1("wst%d" % i, [128, 1680]) for i in range(2)]; t_wst = [Trk(), Trk()]
            wiv = w_in.rearrange("(k p) n -> p k n", p=128)
            for k in range(16):
                bi = k % 2
                kb.dma(wst[bi][:], wiv[:, k, :], writes=[t_wst[bi]])
                kb.op("dve", lambda e, k=k, bi=bi: e.tensor_copy(out=wb[:, k, :], in_=wst[bi][:]),
                      reads=[t_wst[bi]], writes=[t_wb])
            xt = [sb1("xt%d" % i, [128, 16, 512]) for i in range(2)]; t_xt = [Trk(), Trk()]
            xsq = sb1("xsq", [128, 16, 512], BF16); t_xsq = Trk()
            hT = sb1("hT", [128, 16, 512], BF16); t_hT = Trk()
            rstd = sb1("rstd", [128, 512]); t_rstd = Trk()
            tmp = [sb1("tmp%d" % i, [128, 512]) for i in range(2)]; t_tmp = [Trk(), Trk()]
            zr = [sb1("zr%d" % i, [128, 3 + 512]) for i in range(4)]; t_zr = [Trk() for _ in range(4)]
            acc = [sb1("acc%d" % i, [128, 512]) for i in range(2)]; t_acc = [Trk(), Trk()]
            ofm = [sb1("ofm%d" % i, [128, 512]) for i in range(2)]; t_ofm = [Trk(), Trk()]
            obf = [sb1("obf%d" % i, [128, 512], BF16) for i in range(2)]; t_obf = [Trk(), Trk()]
            sqb = sb1("sqb", [128, 512], BF16); t_sqb = Trk()
            rs2 = sb1("rs2", [128, 512]); t_rs2 = Trk()
            otm = [sb1("otm%d" % i, [128, 512]) for i in range(2)]; t_otm = [Trk(), Trk()]
            otb = [sb1("otb%d" % i, [128, 128], BF16) for i in range(2)]; t_otb = [Trk(), Trk()]
            otg = [sb1("otg%d" % i, [128, 12]) for i in range(2)]; t_otg = [Trk(), Trk()]
            for i in range(4):
                kb.op("dve", lambda e, i=i: e.memset(zr[i][:, 0:3], 0.0), writes=[t_zr[i]])
            xv = xT.rearrange("(k p) t -> p k t", p=128)
            cnt2 = [0]

            def rot2():
                cnt2[0] += 1
                return cnt2[0] % 2

            def load_x(n):
                bi = n % 2
                for h in range(4):
                    kb.dma(xt[bi][:, 4 * h:4 * h + 4, :], xv[:, 4 * h:4 * h + 4, n * 512:(n + 1) * 512], writes=[t_xt[bi]])

            load_x(0)
            for n in range(nb):
                bi = n % 2
                t0 = n * 512
                if n + 1 < nb:
                    load_x(n + 1)
                for k in range(16):
                    kb.op("act" if k % 2 else "dve",
                          (lambda e, k=k, bi=bi: e.activation(out=xsq[:, k, :], in_=xt[bi][:, k, :], func=AF.Square)) if k % 2 else
                          (lambda e, k=k, bi=bi: e.tensor_tensor(out=xsq[:, k, :], in0=xt[bi][:, k, :], in1=xt[bi][:, k, :], op=ALU.mult)),
                          reads=[t_xt[bi]], writes=[t_xsq])
                for k in range(16):
                    kb.op("pe", lambda e, k=k: e.matmul(ps[0][:, :], lhsT=ones_b[:], rhs=xsq[:, k, :], start=(k == 0), stop=(k == 15)),
                          reads=[t_ones_b, t_xsq], writes=[pst[0]], sig=(k == 15))
                kb.op("act", lambda e: e.activation(out=rstd[:], in_=ps[0][:, :], func=AF.Sqrt, scale=1.0 / D, bias=EPS),
                      reads=[pst[0]], writes=[t_rstd])
                kb.op("dve", lambda e: e.reciprocal(out=rstd[:], in_=rstd[:]), reads=[t_rstd], writes=[t_rstd])
                for k in range(16):
                    tb = k % 2
                    kb.op("dve", lambda e, k=k, tb=tb, bi=bi: e.tensor_tensor(out=tmp[tb][:], in0=xt[bi][:, k, :], in1=rstd[:], op=ALU.mult),
                          reads=[t_xt[bi], t_rstd], writes=[t_tmp[tb]])
                    kb.op("act", lambda e, k=k, tb=tb: e.activation(out=hT[:, k, :], in_=tmp[tb][:], func=AF.Identity,
                                                                    scale=gs1[:, k:k + 1], bias=sh1[:, k:k + 1]),
                          reads=[t_tmp[tb], t_gs1, t_sh1], writes=[t_hT])
                for ct in range(9):
                    if not ({0: 'a', 1: 'a', 2: 'a', 3: 'a', 4: 'b', 5: 'b', 7: 'b', 6: 'c', 8: 'd'}[ct] in parts):
                        continue
                    c0 = ct * 128
                    cn = 128 if ct < 8 else 2
                    pb = 1 + (ct % 2)
                    for k in range(16):
                        kb.op("pe", lambda e, k=k, c0=c0, cn=cn, pb=pb: e.matmul(ps[pb][0:cn, :], lhsT=wb[:, k, c0:c0 + cn], rhs=hT[:, k, :],
                                                                                 start=(k == 0), stop=(k == 15)),
                              reads=[t_wb, t_hT], writes=[pst[pb]], sig=(k == 15))
                    if ct < 4:
                        z = zr[ct]; tz = t_zr[ct]
                        kb.op("act", lambda e, z=z, pb=pb: e.activation(out=z[:, 3:515], in_=ps[pb][:, :], func=AF.Copy),
                              reads=[pst[pb]], writes=[tz])
                        ab = rot2()
                        a = acc[ab]; ta = t_acc[ab]
                        kb.op("dve", lambda e, z=z, a=a, ct=ct: e.tensor_scalar(out=a[:], in0=z[:, 3:515], scalar1=cw[:, ct, 3:4], scalar2=None,
                                                                                 op0=ALU.mult), reads=[tz, t_cw], writes=[ta])
                        for j in range(3):
                            kb.op("dve", lambda e, z=z, a=a, ct=ct, j=j: e.scalar_tensor_tensor(out=a[:], in0=z[:, j:j + 512], scalar=cw[:, ct, j:j + 1],
                                                                                               in1=a[:], op0=ALU.mult, op1=ALU.add),
                                  reads=[tz, t_cw, ta], writes=[ta])
                        ob = rot2()
                        kb.op("act", lambda e, a=a, ob=ob: e.activation(out=ofm[ob][:], in_=a[:], func=AF.Silu), reads=[ta], writes=[t_ofm[ob]])
                        dst = s_q if ct < 2 else s_k
                        r0 = (ct % 2) * 128
                        kb.dma(dst[r0:r0 + 128, t0:t0 + 512], ofm[ob][:], reads=[t_ofm[ob]], writes=[t_scr["q" if ct < 2 else "k"]])
                        kb.op("dve", lambda e, z=z: e.tensor_copy(out=z[:, 0:3], in_=z[:, 512:515]), reads=[tz], writes=[tz])
                    elif ct in (4, 5, 7):
                        kb.op("act", lambda e, pb=pb: e.activation(out=sqb[:], in_=ps[pb][:, :], func=AF.Square), reads=[pst[pb]], writes=[t_sqb])
                        kb.op("pe", lambda e: e.matmul(ps[3][:, :], lhsT=blk64[:], rhs=sqb[:], start=True, stop=True),
                              reads=[t_blk64, t_sqb], writes=[pst[3]])
                        kb.op("act", lambda e: e.activation(out=rs2[:], in_=ps[3][:, :], func=AF.Sqrt, scale=1.0 / 64, bias=EPS),
                              reads=[pst[3]], writes=[t_rs2])
                        kb.op("dve", lambda e: e.reciprocal(out=rs2[:], in_=rs2[:]), reads=[t_rs2], writes=[t_rs2])
                        ob = rot2()
                        gcol = 0 if ct in (4, 5) else 3
                        kb.op("dve", lambda e, pb=pb, ob=ob, gcol=gcol: e.scalar_tensor_tensor(out=obf[ob][:], in0=ps[pb][:, :], scalar=qkg[:, gcol:gcol + 1],
                                                                                              in1=rs2[:], op0=ALU.mult, op1=ALU.mult),
                              reads=[pst[pb], t_rs2, t_qkg], writes=[t_obf[ob]])
                        if ct == 7:
                            kb.dma(s_kk[:, t0:t0 + 512], obf[ob][:], reads=[t_obf[ob]], writes=[t_scr["kk"]])
                        else:
                            r0 = (ct - 4) * 128
                            kb.dma(s_aq[r0:r0 + 128, t0:t0 + 512], obf[ob][:], reads=[t_obf[ob]], writes=[t_scr["aq"]])
                    elif ct == 6:
                        ob = rot2()
                        kb.op("act", lambda e, pb=pb, ob=ob: e.activation(out=ofm[ob][:], in_=ps[pb][:, :], func=AF.Copy), reads=[pst[pb]], writes=[t_ofm[ob]])
                        kb.dma(s_kv[:, t0:t0 + 512], ofm[ob][:], reads=[t_ofm[ob]], writes=[t_scr["kv"]])
                    else:
                        ob = rot2()
                        kb.op("act", lambda e, pb=pb, ob=ob: e.activation(out=ofm[ob][0:2, :], in_=ps[pb][0:2, :], func=AF.Copy), reads=[pst[pb]], writes=[t_ofm[ob]])
                        kb.dma(s_if[:, t0:t0 + 512], ofm[ob][0:2, :], reads=[t_ofm[ob]], writes=[t_scr["if"]])
                for ts in range(int(os.environ.get('NTS', '4')) if 'e' in parts else 0):
                    tt = t0 + ts * 128
                    pb = 4 + (ts % 2)
                    for k in range(16):
                        kb.op("pe", lambda e, k=k, ts=ts, pb=pb: e.matmul(ps[pb][:, :], lhsT=hT[:, k, ts * 128:(ts + 1) * 128], rhs=wb[:, k, 1026:1538],
                                                                          start=(k == 0), stop=(k == 15)),
                              reads=[t_wb, t_hT], writes=[pst[pb]], sig=(k == 15))
                    pb2 = 6 + (ts % 2)
                    for k in range(16):
                        kb.op("pe", lambda e, k=k, ts=ts, pb2=pb2: e.matmul(ps[pb2][:, 0:140], lhsT=hT[:, k, ts * 128:(ts + 1) * 128], rhs=wb[:, k, 1538:1678],
                                                                            start=(k == 0), stop=(k == 15)),
                              reads=[t_wb, t_hT], writes=[pst[pb2]], sig=(k == 15))
                    ob = ts % 2
                    kb.op("dve", lambda e, pb=pb, ob=ob: e.tensor_copy(out=otm[ob][:, 0:256], in_=ps[pb][:, 0:256]), reads=[pst[pb]], writes=[t_otm[ob]])
                    kb.op("act", lambda e, pb=pb, ob=ob: e.activation(out=otm[ob][:, 256:512], in_=ps[pb][:, 256:512], func=AF.Sigmoid),
                          reads=[pst[pb]], writes=[t_otm[ob]])
                    SK = os.environ.get('SKIP', '')
                    if 'P' not in SK:
                        kb.op("dve", lambda e, ob=ob: e.tensor_tensor(out=otm[ob][:, 256:512], in0=otm[ob][:, 256:512], in1=mng[:], op=ALU.mult),
                              reads=[t_otm[ob], t_mng], writes=[t_otm[ob]])
                    if 'S' not in SK:
                        kb.dma(s_v[tt:tt + 128, :], otm[ob][:, 0:256], reads=[t_otm[ob]], writes=[t_scr["v"]])
                        kb.dma(s_go[tt:tt + 128, :], otm[ob][:, 256:512], reads=[t_otm[ob]], writes=[t_scr["go"]])
                    kb.op("dve", lambda e, pb2=pb2, ob=ob: e.tensor_copy(out=otb[ob][:], in_=ps[pb2][:, 0:128]), reads=[pst[pb2]], writes=[t_otb[ob]])
                    kb.op("act", lambda e, pb2=pb2, ob=ob: e.activation(out=otg[ob][:], in_=ps[pb2][:, 128:140], func=AF.Sigmoid),
                          reads=[pst[pb2]], writes=[t_otg[ob]])
                    if 'V' not in os.environ.get('SKIP', ''):
                        kb.dma(s_vv[tt:tt + 128, :], otb[ob][:], reads=[t_otb[ob]], writes=[t_scr["vv"]])
                    if 'G' not in os.environ.get('SKIP', ''):
                        kb.dma(s_gt[tt:tt + 128, :], otg[ob][:], reads=[t_otg[ob]], writes=[t_scr["gt"]])
            kb.barrier()


    mixT = [nc.dram_tensor("mixT%d" % i, [512, 1024], BF16, kind="Internal").ap() for i in range(8)]; t_mixT = Trk()
    if phases >= 2:
        bif_in = din("bif", [128, 2])
        ut_in = din("ut_f", [128, 128])
        identb_in = din("ident_b", [128, 128], BF16)
        with ExitStack() as p2:
            def sb2(name, shape, dt=F32):
                return p2.enter_context(nc.sbuf_tensor(name, list(shape), dt))
            NCH = 64
            bif = sb2("bif_sb", [128, 2]); t_bif = Trk()
            ut = sb2("ut_sb", [128, 128]); t_ut = Trk()
            identb = sb2("identb", [128, 128], BF16); t_identb = Trk()
            kb.dma(bif[:], bif_in, writes=[t_bif])
            kb.dma(ut[:], ut_in, writes=[t_ut])
            kb.dma(identb[:], identb_in, writes=[t_identb])
            rows = sb2("rows", [64, 2, 128]); t_rows = Trk()
            kb.dma(rows[:, 0, :], s_if[0:1, :].rearrange("o (c p) -> (o c) p", p=128), reads=[t_scr["if"]], writes=[t_rows])
            kb.dma(rows[:, 1, :], s_if[1:2, :].rearrange("o (c p) -> (o c) p", p=128), reads=[t_scr["if"]], writes=[t_rows])
            cols = sb2("cols", [128, 12, 64]); t_cols = Trk()
            nbf = sb2("nbf", [128, 1]); t_nbf = Trk()
            zero64 = sb2("zero64", [128, 128]); t_zero = Trk()
            kb.op("dve", lambda e: e.memset(zero64[:], 0.0), writes=[t_zero])
            kb.op("dve", lambda e: e.tensor_scalar(out=nbf[:], in0=bif[:, 1:2], scalar1=-1.0, scalar2=None, op0=ALU.mult), reads=[t_bif], writes=[t_nbf])
            for w in range(2):
                kb.op("pe", lambda e, w=w: e.matmul(ps[w][:, 0:64], lhsT=rows[:, w, :], rhs=ident[0:64, 0:64], start=True, stop=True),
                      reads=[t_rows, t_ident], writes=[pst[w]])
            kb.op("dve", lambda e: e.tensor_scalar(out=cols[:, 0, :], in0=ps[0][:, 0:64], scalar1=bif[:, 0:1], scalar2=None, op0=ALU.add),
                  reads=[pst[0], t_bif], writes=[t_cols])
            kb.op("act", lambda e: e.activation(out=cols[:, 11, :], in_=ps[1][:, 0:64], func=AF.Exp, scale=-1.0, bias=nbf[:, 0:1]),
                  reads=[pst[1], t_nbf], writes=[t_cols])
            kb.op("act", lambda e: e.activation(out=cols[:, 1, :], in_=cols[:, 11, :], func=AF.Ln, scale=1.0, bias=1.0), reads=[t_cols], writes=[t_cols])
            kb.op("dve", lambda e: e.tensor_scalar(out=cols[:, 1, :], in0=cols[:, 1, :], scalar1=-1.0, scalar2=None, op0=ALU.mult), reads=[t_cols], writes=[t_cols])
            kb.op("pe", lambda e: e.matmul(ps[2][:, 0:64], lhsT=ut[:], rhs=cols[:, 1, :], start=True, stop=True), reads=[t_ut, t_cols], writes=[pst[2]])
            kb.op("pe", lambda e: e.matmul(ps[3][:, 0:64], lhsT=ones_f[:], rhs=cols[:, 1, :], start=True, stop=True), reads=[t_ones_f, t_cols], writes=[pst[3]])
            kb.op("dve", lambda e: e.tensor_copy(out=cols[:, 11, :], in_=ps[3][:, 0:64]), reads=[pst[3]], writes=[t_cols])
            kb.op("dve", lambda e: e.tensor_tensor_scan(out=cols[:, 2, :], data0=cols[:, 11, :], data1=zero64[:, 0:64], initial=0.0, op0=ALU.add, op1=ALU.add),
                  reads=[t_cols, t_zero], writes=[t_cols])
            kb.op("dve", lambda e: e.tensor_tensor(out=cols[:, 2, :], in0=cols[:, 2, :], in1=cols[:, 11, :], op=ALU.subtract), reads=[t_cols], writes=[t_cols])
            kb.op("dve", lambda e: e.tensor_tensor(out=cols[:, 2, :], in0=cols[:, 2, :], in1=ps[2][:, 0:64], op=ALU.add), reads=[t_cols, pst[2]], writes=[t_cols])
            kb.op("dve", lambda e: e.tensor_tensor(out=cols[:, 3, :], in0=cols[:, 0, :], in1=cols[:, 2, :], op=ALU.subtract), reads=[t_cols], writes=[t_cols])
            grow = sb2("grow", [64, 128]); t_grow = Trk()
            cmrow = sb2("cmrow", [64, 128]); t_cmrow = Trk()
            kb.op("pe", lambda e: e.matmul(ps[4][0:64, 0:128], lhsT=cols[:, 3, :], rhs=ident[:, :], start=True, stop=True), reads=[t_cols, t_ident], writes=[pst[4]])
            kb.op("dve", lambda e: e.tensor_copy(out=grow[:], in_=ps[4][0:64, 0:128]), reads=[pst[4]], writes=[t_grow])
            kb.op("dve", lambda e: e.tensor_tensor_scan(out=cmrow[:], data0=grow[:], data1=grow[:], initial=-1e30, op0=ALU.max, op1=ALU.max),
                  reads=[t_grow], writes=[t_cmrow])
            mrow = sb2("mrow", [1, 3, 64]); t_mrow = Trk()
            kb.op("pe", lambda e: e.matmul(ps[5][0:1, 0:64], lhsT=cmrow[:, 127:128], rhs=ident[0:64, 0:64], start=True, stop=True),
                  reads=[t_cmrow, t_ident], writes=[pst[5]])
            kb.op("dve", lambda e: e.tensor_copy(out=mrow[0:1, 0, :], in_=ps[5][0:1, 0:64]), reads=[pst[5]], writes=[t_mrow])
            kb.op("dve", lambda e: e.tensor_tensor_scan(out=mrow[0:1, 1, :], data0=mrow[0:1, 0, :], data1=mrow[0:1, 0, :], initial=0.0, op0=ALU.max, op1=ALU.max),
                  reads=[t_mrow], writes=[t_mrow])
            kb.op("dve", lambda e: e.memset(mrow[0:1, 2, 0:1], 0.0), reads=[t_mrow], writes=[t_mrow])
            kb.op("dve", lambda e: e.tensor_copy(out=mrow[0:1, 2, 1:64], in_=mrow[0:1, 1, 0:63]), reads=[t_mrow], writes=[t_mrow])
            kb.op("pe", lambda e: e.matmul(ps[6][:, 0:128], lhsT=ones_f[0:1, :], rhs=mrow[0:1, 1:3, :].rearrange("o a c -> o (a c)"), start=True, stop=True),
                  reads=[t_ones_f, t_mrow], writes=[pst[6]])
            kb.op("dve", lambda e: e.tensor_copy(out=cols[:, 6, :], in_=ps[6][:, 0:64]), reads=[pst[6]], writes=[t_cols])
            kb.op("dve", lambda e: e.tensor_copy(out=cols[:, 5, :], in_=ps[6][:, 64:128]), reads=[pst[6]], writes=[t_cols])
            kb.op("pe", lambda e: e.matmul(ps[7][:, 0:64], lhsT=cmrow[:, :], rhs=ident[0:64, 0:64], start=True, stop=True), reads=[t_cmrow, t_ident], writes=[pst[7]])
            kb.op("dve", lambda e: e.tensor_tensor(out=cols[:, 4, :], in0=ps[7][:, 0:64], in1=cols[:, 5, :], op=ALU.max), reads=[pst[7], t_cols], writes=[t_cols])
            kb.op("dve", lambda e: e.tensor_tensor(out=cols[:, 11, :], in0=cols[:, 3, :], in1=cols[:, 5, :], op=ALU.subtract), reads=[t_cols], writes=[t_cols])
            kb.op("act", lambda e: e.activation(out=cols[:, 7, :], in_=cols[:, 11, :], func=AF.Exp), reads=[t_cols], writes=[t_cols])
            kb.op("dve", lambda e: e.tensor_scalar(out=cols[:, 7, :], in0=cols[:, 7, :], scalar1=1.0 / 16, scalar2=None, op0=ALU.mult), reads=[t_cols], writes=[t_cols])
            kb.op("dve", lambda e: e.tensor_tensor(out=cols[:, 11, :], in0=cols[:, 5, :], in1=cols[:, 4, :], op=ALU.subtract), reads=[t_cols], writes=[t_cols])
            kb.op("act", lambda e: e.activation(out=cols[:, 8, :], in_=cols[:, 11, :], func=AF.Exp), reads=[t_cols], writes=[t_cols])
            kb.op("dve", lambda e: e.tensor_tensor(out=cols[:, 11, :], in0=cols[:, 2, :], in1=cols[:, 4, :], op=ALU.add), reads=[t_cols], writes=[t_cols])
            kb.op("act", lambda e: e.activation(out=cols[:, 9, :], in_=cols[:, 11, :], func=AF.Exp, scale=-1.0), reads=[t_cols], writes=[t_cols])
            kb.op("dve", lambda e: e.tensor_tensor(out=cols[:, 11, :], in0=cols[:, 5, :], in1=cols[:, 6, :], op=ALU.subtract), reads=[t_cols], writes=[t_cols])
            kb.op("act", lambda e: e.activation(out=cols[:, 10, :], in_=cols[:, 11, :], func=AF.Exp), reads=[t_cols], writes=[t_cols])
            if dbg:
                d_cols = nc.dram_tensor("d_cols", [128, 12, 64], F32, kind="ExternalOutput").ap()
                kb.dma(d_cols, cols[:], reads=[t_cols])

            qT = [sb2("qT%d" % i, [128, 2, 128]) for i in range(2)]; t_qT = [Trk(), Trk()]
            kT = [sb2("kT%d" % i, [128, 2, 128]) for i in range(2)]; t_kT = [Trk(), Trk()]
            va = [sb2("va%d" % i, [128, 257]) for i in range(2)]; t_va = [Trk(), Trk()]
            go = [sb2("go%d" % i, [128, 256]) for i in range(2)]; t_go = [Trk(), Trk()]
            kp = sb2("kp", [128, 256]); t_kp = Trk()
            wT = sb2("wT", [128, 128]); t_wT = Trk()
            St = [sb2("St%d" % i, [128, 2, 257]) for i in range(2)]; t_St = [Trk(), Trk()]
            sc = sb2("sc", [128, 8]); t_sc = Trk()
            junk = sb2("junk", [128, 256]); t_junk = Trk()
            hmf = sb2("hmf", [128, 256], BF16); t_hmf = Trk()
            hmT = [sb2("hmT%d" % i, [128, 2, 128], BF16) for i in range(2)]; t_hmT = [Trk(), Trk()]
            for i in range(2):
                kb.op("dve", lambda e, i=i: e.memset(va[i][:, 256:257], 1.0), writes=[t_va[i]])
            kb.op("dve", lambda e: e.memset(St[0][:], 0.0), writes=[t_St[0]])
            qv = s_q.rearrange("(a p) t -> p a t", p=128)
            kv_ = s_k.rearrange("(a p) t -> p a t", p=128)

            def load_chunk(c):
                bi = c % 2
                kb.dma(qT[bi][:], qv[:, :, c * 128:(c + 1) * 128], reads=[t_scr["q"]], writes=[t_qT[bi]])
                kb.dma(kT[bi][:], kv_[:, :, c * 128:(c + 1) * 128], reads=[t_scr["k"]], writes=[t_kT[bi]])
                kb.dma(va[bi][:, 0:256], s_v[c * 128:(c + 1) * 128, :], reads=[t_scr["v"]], writes=[t_va[bi]])
                kb.dma(go[bi][:], s_go[c * 128:(c + 1) * 128, :], reads=[t_scr["go"]], writes=[t_go[bi]])

            nch = int(os.environ.get("NCH", "64"))
            load_chunk(0)
            for c in range(nch):
                bi = c % 2
                so = St[c % 2]; tso = t_St[c % 2]
                sn = St[(c + 1) % 2]; tsn = t_St[(c + 1) % 2]
                if c + 1 < nch:
                    load_chunk(c + 1)
                for dc in range(2):
                    kb.op("pe", lambda e, dc=dc, bi=bi: e.matmul(ps[0][:, dc * 128:(dc + 1) * 128], lhsT=kT[bi][:, dc, :], rhs=ident[:, :], start=True, stop=True),
                          reads=[t_kT[bi], t_ident], writes=[pst[0]], sig=(dc == 1))
                kb.op("dve", lambda e, c=c: e.tensor_scalar(out=kp[:], in0=ps[0][:, 0:256], scalar1=cols[:, 7, c:c + 1], scalar2=None, op0=ALU.mult),
                      reads=[pst[0], t_cols], writes=[t_kp])
                for dc in range(2):
                    kb.op("pe", lambda e, dc=dc, bi=bi: e.matmul(ps[1][:, 0:128], lhsT=kT[bi][:, dc, :], rhs=qT[bi][:, dc, :], start=(dc == 0), stop=(dc == 1)),
                          reads=[t_kT[bi], t_qT[bi]], writes=[pst[1]], sig=(dc == 1))
                kb.op("dve", lambda e, c=c: e.scalar_tensor_tensor(out=wT[:], in0=ps[1][:, 0:128], scalar=cols[:, 7, c:c + 1], in1=ut[:], op0=ALU.mult, op1=ALU.mult),
                      reads=[pst[1], t_cols, t_ut], writes=[t_wT])
                kb.op("pe", lambda e, bi=bi: e.matmul(ps[2][:, 0:257], lhsT=wT[:], rhs=va[bi][:], start=True, stop=False),
                      reads=[t_wT, t_va[bi]], writes=[pst[2]], sig=False)
                for dc in range(2):
                    kb.op("pe", lambda e, dc=dc, bi=bi, so=so: e.matmul(ps[2][:, 0:257], lhsT=qT[bi][:, dc, :], rhs=so[:, dc, :], start=False, stop=(dc == 1)),
                          reads=[t_qT[bi], tso], writes=[pst[2]], sig=(dc == 1))
                for dc in range(2):
                    pb = 3 + dc
                    kb.op("pe", lambda e, dc=dc, bi=bi, pb=pb: e.matmul(ps[pb][:, 0:257], lhsT=kp[:, dc * 128:(dc + 1) * 128], rhs=va[bi][:], start=True, stop=False),
                          reads=[t_kp, t_va[bi]], writes=[pst[pb]], sig=False)
                    kb.op("pe", lambda e, dc=dc, pb=pb, so=so: e.matmul(ps[pb][:, 0:257], lhsT=ident[:, :], rhs=so[:, dc, :], start=False, stop=True),
                          reads=[t_ident, tso], writes=[pst[pb]])
                    kb.op("act" if dc else "dve",
                          (lambda e, dc=dc, pb=pb, sn=sn, c=c: e.activation(out=sn[:, dc, :], in_=ps[pb][:, 0:257], func=AF.Copy, scale=cols[:, 10, c:c + 1])) if dc else
                          (lambda e, dc=dc, pb=pb, sn=sn, c=c: e.tensor_scalar(out=sn[:, dc, :], in0=ps[pb][:, 0:257], scalar1=cols[:, 10, c:c + 1], scalar2=None, op0=ALU.mult)),
                          reads=[pst[pb], t_cols], writes=[tsn])
                kb.op("act", lambda e, c=c: e.activation(out=sc[:, 0:1], in_=ps[2][:, 256:257], func=AF.Abs, scale=cols[:, 8, c:c + 1]),
                      reads=[pst[2], t_cols], writes=[t_sc])
                kb.op("dve", lambda e, c=c: e.tensor_tensor(out=sc[:, 1:2], in0=sc[:, 0:1], in1=cols[:, 9, c:c + 1], op=ALU.max), reads=[t_sc, t_cols], writes=[t_sc])
                kb.op("dve", lambda e: e.reciprocal(out=sc[:, 2:3], in_=sc[:, 1:2]), reads=[t_sc], writes=[t_sc])
                kb.op("dve", lambda e, c=c: e.tensor_tensor(out=sc[:, 3:4], in0=sc[:, 2:3], in1=cols[:, 8, c:c + 1], op=ALU.mult), reads=[t_sc, t_cols], writes=[t_sc])
                kb.op("act", lambda e: e.activation(out=junk[:], in_=ps[2][:, 0:256], func=AF.Square, accum_out=sc[:, 4:5]), reads=[pst[2]], writes=[t_junk, t_sc])
                kb.op("dve", lambda e: e.scalar_tensor_tensor(out=sc[:, 5:6], in0=sc[:, 3:4], scalar=sc[:, 3:4], in1=sc[:, 4:5], op0=ALU.mult, op1=ALU.mult),
                      reads=[t_sc], writes=[t_sc])
                kb.op("act", lambda e: e.activation(out=sc[:, 5:6], in_=sc[:, 5:6], func=AF.Sqrt, scale=1.0 / 256, bias=EPS), reads=[t_sc], writes=[t_sc])
                kb.op("dve", lambda e: e.reciprocal(out=sc[:, 6:7], in_=sc[:, 5:6]), reads=[t_sc], writes=[t_sc])
                kb.op("dve", lambda e: e.tensor_tensor(out=sc[:, 7:8], in0=sc[:, 6:7], in1=sc[:, 3:4], op=ALU.mult), reads=[t_sc], writes=[t_sc])
                kb.op("dve", lambda e, bi=bi: e.scalar_tensor_tensor(out=hmf[:], in0=ps[2][:, 0:256], scalar=sc[:, 7:8], in1=go[bi][:], op0=ALU.mult, op1=ALU.mult),
                      reads=[pst[2], t_sc, t_go[bi]], writes=[t_hmf])
                ob = c % 2
                for dc in range(2):
                    kb.op("pe", lambda e, dc=dc: e.matmul(ps[5][:, dc * 128:(dc + 1) * 128], lhsT=hmf[:, dc * 128:(dc + 1) * 128], rhs=identb[:, :], start=True, stop=True),
                          reads=[t_hmf, t_identb], writes=[pst[5]], sig=(dc == 1))
                kb.op("act", lambda e, ob=ob: e.activation(out=hmT[ob][:].rearrange("p a t -> p (a t)"), in_=ps[5][:, 0:256], func=AF.Copy), reads=[pst[5]], writes=[t_hmT[ob]])
                kb.dma(mixT[c // 8][0:256, (c % 8) * 128:(c % 8 + 1) * 128].rearrange("(a p) t -> p a t", p=128), hmT[ob][:], reads=[t_hmT[ob]], writes=[t_mixT])
            kb.barrier()

    if phases >= 3:
        tb_sw_in = din("tb_sw", [128, 64, 4])
        tb_c_in = din("tb_c", [128, 64, 4])
        cmask_in = din("cmask", [128, 17, 128], BF16)
        caus_in = din("caus_add", [128, 128], BF16)
        acaus_in = din("acaus_add", [128, 128], BF16)
        ovl_in = din("ovl", [128, 4, 128], BF16)
        expT_in = din("expT", [128, S], BF16)
        tkeep_in = din("t_keep", [128, 255])
        tadd_in = din("t_add", [128, 255])
        w1k_in = din("w1k", [2048, 256]); w1v_in = din("w1v", [2048, 256])
        w2k_in = din("w2k", [256, 64]); w2v_in = din("w2v", [256, 64])
        posk_in = din("posk", [128, 16]); posv_in = din("posv", [128, 16])
        kng0_in = din("kng0", [64, 1])
        with ExitStack() as p3:
            def sb3(name, shape, dt=F32):
                return p3.enter_context(nc.sbuf_tensor(name, list(shape), dt))
            QT = sb3("QT", [128, 4, S], BF16); t_QT = Trk()
            KK = sb3("KK", [128, S], BF16); t_KK = Trk()
            Vs = sb3("Vs", [128, 64, 65], BF16); t_Vs = Trk()
            Vw = sb3("Vw", [128, 64, 65], BF16); t_Vw = Trk()
            expT = sb3("expT_sb", [128, S], BF16); t_expT = Trk()
            tb_sw = sb3("tb_sw_sb", [128, 64, 4]); t_tbsw = Trk()
            tb_c = sb3("tb_c_sb", [128, 64, 4]); t_tbc = Trk()
            cmask = sb3("cmask_sb", [128, 17, 128], BF16); t_cmask = Trk()
            caus = sb3("caus_sb", [128, 128], BF16); t_caus = Trk()
            acaus = sb3("acaus_sb", [128, 128], BF16); t_acaus = Trk()
            ovl = sb3("ovl_sb", [128, 4, 128], BF16); t_ovl = Trk()
            tkeep = sb3("tkeep_sb", [128, 255]); t_tkeep = Trk()
            tadd = sb3("tadd_sb", [128, 255]); t_tadd = Trk()
            gts = sb3("gts", [128, 64, 12]); t_gts = Trk()
            kcT = sb3("kcT", [64, 512], BF16); t_kcT = Trk()
            vca = sb3("vca", [128, 4, 65], BF16); t_vca = Trk()
            identb3 = sb3("identb3", [128, 128], BF16); t_identb3 = Trk()
            aqv = s_aq.rearrange("(h d) t -> d h t", d=64)
            for hh in range(2):
                for h in range(4):
                    kb.dma(QT[hh * 64:(hh + 1) * 64, h, :], aqv[:, h, :], reads=[t_scr["aq"]], writes=[t_QT])
            kb.dma(KK[:, 0:4096], s_kk[:, 0:4096], reads=[t_scr["kk"]], writes=[t_KK])
            kb.dma(KK[:, 4096:S], s_kk[:, 4096:S], reads=[t_scr["kk"]], writes=[t_KK])
            kb.dma(expT[:], expT_in, writes=[t_expT])
            kb.dma(tb_sw[:], tb_sw_in, writes=[t_tbsw]); kb.dma(tb_c[:], tb_c_in, writes=[t_tbc])
            kb.dma(cmask[:], cmask_in, writes=[t_cmask]); kb.dma(caus[:], caus_in, writes=[t_caus]); kb.dma(acaus[:], acaus_in, writes=[t_acaus])
            kb.dma(ovl[:], ovl_in, writes=[t_ovl]); kb.dma(tkeep[:], tkeep_in, writes=[t_tkeep]); kb.dma(tadd[:], tadd_in, writes=[t_tadd])
            kb.dma(identb3[:], identb_in if phases >= 2 else din("ident_b", [128, 128], BF16), writes=[t_identb3])
            kb.dma(gts[:], s_gt.rearrange("(q p) c -> p q c", p=128), reads=[t_scr["gt"]], writes=[t_gts])
            with ExitStack() as p3a:
                def sb3a(name, shape, dt=F32):
                    return p3a.enter_context(nc.sbuf_tensor(name, list(shape), dt))
                vtmp = sb3a("vtmp", [128, 64, 128], BF16); t_vtmp = Trk()
                kb.dma(vtmp[:], s_vv.rearrange("(q p) c -> p q c", p=128), reads=[t_scr["vv"]], writes=[t_vtmp])
                kb.op("dve", lambda e: e.tensor_copy(out=Vs[:, :, 0:64], in_=vtmp[:, :, 0:64]), reads=[t_vtmp], writes=[t_Vs])
                kb.op("dve", lambda e: e.tensor_copy(out=Vw[:, :, 0:64], in_=vtmp[:, :, 64:128]), reads=[t_vtmp], writes=[t_Vw])
                kb.op("dve", lambda e: e.memset(Vs[:, :, 64:65], 1.0), writes=[t_Vs])
                kb.op("dve", lambda e: e.memset(Vw[:, :, 64:65], 1.0), writes=[t_Vw])
                kb.op("dve", lambda e: e.memset(vca[:, :, 64:65], 1.0), writes=[t_vca])
                w1f = sb3a("w1f", [128, 16, 256]); t_w1f = Trk()
                w1b = sb3a("w1b", [128, 16, 256], BF16); t_w1b = Trk()
                w2f = sb3a("w2f", [128, 2, 64]); t_w2f = Trk()
                w2b = sb3a("w2b", [128, 2, 64], BF16); t_w2b = Trk()
                posf = sb3a("posf", [128, 16]); t_posf = Trk()
                bcol = sb3a("bcol", [128, 2]); t_bcol = Trk()
                kng0 = sb3a("kng0_sb", [64, 1]); t_kng0 = Trk()
                a2f = sb3a("a2f", [128, 2048]); t_a2f = Trk()
                a2b = sb3a("a2b", [128, S], BF16); t_a2b = Trk()
                hid = sb3a("hid", [128, 2, 512], BF16); t_hid = Trk()
                csq = sb3a("csq", [64, 512], BF16); t_csq = Trk()
                crs = sb3a("crs", [64, 512]); t_crs = Trk()
                kb.dma(kng0[:], kng0_in, writes=[t_kng0])
                kb.op("dve", lambda e: e.memset(hid[:], 0.0), writes=[t_hid])
                kb.op("dve", lambda e: e.memset(kcT[:], 0.0), writes=[t_kcT])
                for which in range(2):
                    w1_in = w1k_in if which == 0 else w1v_in
                    w2_in = w2k_in if which == 0 else w2v_in
                    pos_in = posk_in if which == 0 else posv_in
                    r0 = 0 if which == 0 else 64
                    kb.dma(w1f[:, 0:8, :], w1_in.rearrange("(k p) n -> p k n", p=128)[:, 0:8, :], writes=[t_w1f])
                    kb.dma(w1f[:, 8:16, :], w1_in.rearrange("(k p) n -> p k n", p=128)[:, 8:16, :], writes=[t_w1f])
                    kb.dma(w2f[:], w2_in.rearrange("(k p) n -> p k n", p=128), writes=[t_w2f])
                    kb.dma(posf[:], pos_in, writes=[t_posf])
                    kb.op("dve", lambda e: e.tensor_copy(out=w1b[:], in_=w1f[:]), reads=[t_w1f], writes=[t_w1b])
                    kb.op("dve", lambda e: e.tensor_copy(out=w2b[:], in_=w2f[:]), reads=[t_w2f], writes=[t_w2b])
                    for q4 in range(4):
                        c0 = q4 * 2048
                        kb.dma(a2f[0:64, :], s_kv[r0:r0 + 64, c0:c0 + 2048], reads=[t_scr["kv"]], writes=[t_a2f])
                        if q4 < 3:
                            kb.dma(a2f[64:128, :], s_kv[r0:r0 + 64, c0 + 1:c0 + 2049], reads=[t_scr["kv"]], writes=[t_a2f])
                        else:
                            kb.dma(a2f[64:128, 0:2047], s_kv[r0:r0 + 64, c0 + 1:c0 + 2048], reads=[t_scr["kv"]], writes=[t_a2f])
                        kb.op("dve", lambda e, c0=c0: e.tensor_copy(out=a2b[:, c0:c0 + 2048], in_=a2f[:]), reads=[t_a2f], writes=[t_a2b])
                    for hc in range(2):
                        for jj in range(16):
                            kb.op("pe", lambda e, hc=hc, jj=jj: e.matmul(ps[7][:, hc:hc + 1], lhsT=w1f[:, jj, hc * 128:(hc + 1) * 128], rhs=posf[:, jj:jj + 1],
                                                                          start=(jj == 0), stop=(jj == 15)),
                                  reads=[t_w1f, t_posf], writes=[pst[7]], sig=(jj == 15))
                    kb.op("dve", lambda e: e.tensor_copy(out=bcol[:], in_=ps[7][:, 0:2]), reads=[pst[7]], writes=[t_bcol])
                    a2v = a2b[:].rearrange("p (i s) -> p i s", s=16)
                    for hc in range(2):
                        for jj in range(16):
                            j0 = 2 * jj
                            rhs = a2v[:, 0:511, j0] if j0 < 16 else a2v[:, 1:512, j0 - 16]
                            kb.op("pe", lambda e, hc=hc, jj=jj, rhs=rhs: e.matmul(ps[hc][:, 0:511], lhsT=w1b[:, jj, hc * 128:(hc + 1) * 128], rhs=rhs,
                                                                                  start=(jj == 0), stop=(jj == 15)),
                                  reads=[t_w1b, t_a2b], writes=[pst[hc]], sig=(jj == 15))
                        kb.op("act", lambda e, hc=hc: e.activation(out=hid[:, hc, 0:511], in_=ps[hc][:, 0:511], func=AF.Gelu_apprx_tanh, bias=bcol[:, hc:hc + 1]),
                              reads=[pst[hc], t_bcol], writes=[t_hid])
                    if which == 0:
                        for hc in range(2):
                            kb.op("pe", lambda e, hc=hc: e.matmul(ps[2][0:64, 0:511], lhsT=w2b[:, hc, :], rhs=hid[:, hc, 0:511], start=(hc == 0), stop=(hc == 1)),
                                  reads=[t_w2b, t_hid], writes=[pst[2]], sig=(hc == 1))
                        kb.op("act", lambda e: e.activation(out=csq[:, 0:511], in_=ps[2][0:64, 0:511], func=AF.Square), reads=[pst[2]], writes=[t_csq])
                        kb.op("pe", lambda e: e.matmul(ps[3][0:64, 0:511], lhsT=blk64[0:64, 0:64], rhs=csq[:, 0:511], start=True, stop=True),
                              reads=[t_blk64, t_csq], writes=[pst[3]])
                        kb.op("act", lambda e: e.activation(out=crs[:, 0:511], in_=ps[3][0:64, 0:511], func=AF.Sqrt, scale=1.0 / 64, bias=EPS), reads=[pst[3]], writes=[t_crs])
                        kb.op("dve", lambda e: e.reciprocal(out=crs[:, 0:511], in_=crs[:, 0:511]), reads=[t_crs], writes=[t_crs])
                        kb.op("dve", lambda e: e.scalar_tensor_tensor(out=kcT[:, 0:511], in0=ps[2][0:64, 0:511], scalar=kng0[:, 0:1], in1=crs[:, 0:511],
                                                                      op0=ALU.mult, op1=ALU.mult), reads=[pst[2], t_kng0, t_crs], writes=[t_kcT])
                    else:
                        for it in range(4):
                            for hc in range(2):
                                kb.op("pe", lambda e, hc=hc, it=it: e.matmul(ps[4][:, it * 64:(it + 1) * 64], lhsT=hid[:, hc, it * 128:(it + 1) * 128], rhs=w2b[:, hc, :],
                                                                             start=(hc == 0), stop=(hc == 1)),
                                      reads=[t_hid, t_w2b], writes=[pst[4]], sig=(hc == 1 and it == 3))
                        kb.op("dve", lambda e: e.tensor_copy(out=vca[:, :, 0:64], in_=ps[4][:, 0:256].rearrange("p (a d) -> p a d", d=64)), reads=[pst[4]], writes=[t_vca])
                if dbg:
                    d_kcT = nc.dram_tensor("d_kcT", [64, 512], BF16, kind="ExternalOutput").ap()
                    d_vca = nc.dram_tensor("d_vca", [128, 4, 65], BF16, kind="ExternalOutput").ap()
                    kb.dma(d_kcT, kcT[:], reads=[t_kcT]); kb.dma(d_vca, vca[:], reads=[t_vca])
                kb.barrier()

            pT = [sb3("pT%d" % i, [128, 512], BF16) for i in range(3)]; t_pT = [Trk() for _ in range(3)]
            oT = sb3("oT", [65, 3, 512]); t_oT = Trk()
            zsc = sb3("zsc", [128, 24]); t_zsc = Trk()
            impn = sb3("impn", [128, 128]); t_impn = Trk()
            impw = sb3("impw", [128, 128]); t_impw = Trk()
            m8 = sb3("m8", [128, 16]); t_m8 = Trk()
            selb = sb3("selb", [128, 128], BF16); t_selb = Trk()
            selT = sb3("selT", [128, 128], BF16); t_selT = Trk()
            haf = sb3("haf", [128, 256]); t_haf = Trk()
            hab = sb3("hab", [128, 256], BF16); t_hab = Trk()
            haT = [sb3("haT%d" % i, [128, 2, 128], BF16) for i in range(2)]; t_haT = [Trk(), Trk()]
            if dbg:
                d_imp = nc.dram_tensor("d_imp", [S, 128], F32, kind="ExternalOutput").ap()
                d_sel = nc.dram_tensor("d_sel", [S, 128], BF16, kind="ExternalOutput").ap()
            pcount = [0]
            NEG = -30000.0

            pend = []

            def flush_pairs():
                while pend:
                    pend.pop(0)()

            def tile_pair(lhsK, rhsQ, masks, bias_tab, bidx, Vaug, obank, first, last, imp_it=None):
                i = pcount[0] % 2
                pi = pcount[0] % 3
                pcount[0] += 1
                sps = ps[i]; tsp = pst[i]
                nm = len(masks)
                kb.op("pe", lambda e: e.matmul(sps[:, :], lhsT=lhsK[0], rhs=rhsQ[0], start=True, stop=(nm == 0)),
                      reads=[lhsK[1], rhsQ[1]], writes=[tsp], sig=(nm == 0))
                for mi, (ml, mr, mt) in enumerate(masks):
                    for h in range(4):
                        lastm = (mi == nm - 1 and h == 3)
                        kb.op("pe", lambda e, ml=ml, mr=mr, h=h, lastm=lastm: e.matmul(sps[:, h * 128:(h + 1) * 128], lhsT=ml, rhs=mr, start=False, stop=lastm),
                              reads=mt, writes=[tsp], sig=lastm)
                for h in range(4):
                    kb.op("act", lambda e, h=h: e.activation(out=pT[pi][:, h * 128:(h + 1) * 128], in_=sps[:, h * 128:(h + 1) * 128], func=AF.Exp,
                                                             bias=bias_tab[0][:, bidx, h:h + 1]),
                          reads=[tsp, bias_tab[1]], writes=[t_pT[pi]])

                def stage_b():
                    kb.op("pe", lambda e: e.matmul(ps[obank][0:65, :], lhsT=Vaug[0], rhs=pT[pi][:, :], start=first, stop=last),
                          reads=[Vaug[1], t_pT[pi]], writes=[pst[obank]], sig=last)
                    if imp_it is not None:
                        it, nit = imp_it
                        for h in range(4):
                            kb.op("pe", lambda e, h=h, it=it: e.matmul(ps[5][:, h * 128:(h + 1) * 128], lhsT=pT[pi][:, h * 128:(h + 1) * 128], rhs=ovl[:, it, :],
                                                                       start=(it == 0), stop=(it == nit - 1)),
                                  reads=[t_pT[pi], t_ovl], writes=[pst[5]], sig=(it == nit - 1 and h == 3))
                while len(pend) > 1:
                    pend.pop(0)()
                pend.append(stage_b)
                if len(pend) > 1:
                    pend.pop(0)()

            nqt = int(os.environ.get("NQT", "64"))
            for qt in range(nqt):
                q0 = qt * 128
                qv_lo = (QT[0:64, :, q0:q0 + 128], t_QT)
                qv_hi = (QT[64:128, :, q0:q0 + 128], t_QT)
                nit = (8 * qt + 6) // 128 + 1
                for it in range(nit):
                    m = qt - 16 * it
                    masks = []
                    if m <= 16:
                        masks.append((identb3[:, :], cmask[:, m, :], [t_identb3, t_cmask]))
                    tile_pair((kcT[0:64, it * 128:(it + 1) * 128], t_kcT), qv_lo, masks, (tb_c, t_tbc), qt - 16 * it, (vca[:, it, :], t_vca), 2,
                              it == 0, it == nit - 1, imp_it=(it, nit))
                kts = [kt for kt in range(qt - 4, qt + 1) if kt >= 0]
                for n_, kt in enumerate(kts):
                    masks = []
                    if kt == qt:
                        masks.append((identb3[:, :], caus[:, :], [t_identb3, t_caus]))
                    if kt == qt - 4:
                        masks.append((identb3[:, :], acaus[:, :], [t_identb3, t_acaus]))
                    tile_pair((KK[64:128, kt * 128:(kt + 1) * 128], t_KK), qv_hi, masks, (tb_sw, t_tbsw), qt - kt, (Vw[:, kt, :], t_Vw), 4,
                              n_ == 0, n_ == len(kts) - 1)
                flush_pairs()
                kb.op("dve", lambda e: e.tensor_copy(out=oT[:, 0, :], in_=ps[2][0:65, :]), reads=[pst[2]], writes=[t_oT])
                for h in range(4):
                    kb.op("pe", lambda e, h=h: e.matmul(ps[6][:, h * 65:(h + 1) * 65], lhsT=oT[0:65, 0, h * 128:(h + 1) * 128], rhs=ident[0:65, 0:65], start=True, stop=True),
                          reads=[t_oT, t_ident], writes=[pst[6]], sig=(h == 3))
                kb.op("dve", lambda e: e.tensor_scalar(out=zsc[:, 0:4], in0=ps[6][:, 0:260].rearrange("p (h c) -> p h c", c=65)[:, :, 64], scalar1=1e-30, scalar2=None, op0=ALU.max),
                      reads=[pst[6]], writes=[t_zsc])
                kb.op("dve", lambda e: e.reciprocal(out=zsc[:, 0:4], in_=zsc[:, 0:4]), reads=[t_zsc], writes=[t_zsc])
                kb.op("dve", lambda e: e.tensor_scalar(out=impn[:], in0=ps[5][:, 0:128], scalar1=zsc[:, 0:1], scalar2=None, op0=ALU.mult), reads=[pst[5], t_zsc], writes=[t_impn])
                for h in range(1, 4):
                    kb.op("dve", lambda e, h=h: e.scalar_tensor_tensor(out=impn[:], in0=ps[5][:, h * 128:(h + 1) * 128], scalar=zsc[:, h:h + 1], in1=impn[:], op0=ALU.mult, op1=ALU.add),
                          reads=[pst[5], t_zsc, t_impn], writes=[t_impn])
                if dbg:
                    kb.dma(d_imp[q0:q0 + 128, :], impn[:], reads=[t_impn])
                o0 = 127 - 2 * qt
                kb.op("dve", lambda e, o0=o0: e.tensor_tensor(out=impn[:], in0=impn[:], in1=tkeep[:, o0:o0 + 128], op=ALU.mult), reads=[t_impn, t_tkeep], writes=[t_impn])
                kb.op("dve", lambda e, o0=o0: e.tensor_tensor(out=impn[:], in0=impn[:], in1=tadd[:, o0:o0 + 128], op=ALU.add), reads=[t_impn, t_tadd], writes=[t_impn])
                kb.op("dve", lambda e: e.memset(impn[:, 0:1], 1e4), reads=[t_impn], writes=[t_impn])
                kb.op("dve", lambda e: e.max(out=m8[:, 0:8], in_=impn[:]), reads=[t_impn], writes=[t_m8])
                kb.op("dve", lambda e: e.match_replace(out=impw[:], in_to_replace=m8[:, 0:8], in_values=impn[:], imm_value=-3e38), reads=[t_impn, t_m8], writes=[t_impw])
                kb.op("dve", lambda e: e.max(out=m8[:, 8:16], in_=impw[:]), reads=[t_impw], writes=[t_m8])
                kb.op("dve", lambda e: e.tensor_scalar(out=selb[:], in0=impn[:], scalar1=m8[:, 15:16], scalar2=None, op0=ALU.is_ge), reads=[t_impn, t_m8], writes=[t_selb])
                if dbg:
                    kb.dma(d_sel[q0:q0 + 128, :], selb[:], reads=[t_selb])
                kb.op("pe", lambda e: e.matmul(ps[7][:, 0:128], lhsT=selb[:, :], rhs=identb3[:, :], start=True, stop=True), reads=[t_selb, t_identb3], writes=[pst[7]])
                kb.op("dve", lambda e: e.tensor_scalar(out=selT[:], in0=ps[7][:, 0:128], scalar1=-1.0, scalar2=-NEG, op0=ALU.add, op1=ALU.mult),
                      reads=[pst[7]], writes=[t_selT])
                for kt in range(qt + 1):
                    if kt == qt:
                        masks = [(identb3[:, :], caus[:, :], [t_identb3, t_caus])]
                    else:
                        masks = [(expT[:, kt * 128:(kt + 1) * 128], selT[:, :], [t_expT, t_selT])]
                    tile_pair((KK[0:64, kt * 128:(kt + 1) * 128], t_KK), qv_lo, masks, (tb_sw, t_tbsw), qt - kt, (Vs[:, kt, :], t_Vs), 3, kt == 0, kt == qt)
                flush_pairs()
                kb.op("dve", lambda e: e.tensor_copy(out=oT[:, 1, :], in_=ps[3][0:65, :]), reads=[pst[3]], writes=[t_oT])
                kb.op("dve", lambda e: e.tensor_copy(out=oT[:, 2, :], in_=ps[4][0:65, :]), reads=[pst[4]], writes=[t_oT])
                for br in (1, 2):
                    for h in range(4):
                        col = 260 + ((br - 1) * 4 + h) * 65
                        pbk, cc = (6, col) if col + 65 <= 512 else (7, col - 455 + 128)
                        kb.op("pe", lambda e, h=h, br=br, pbk=pbk, cc=cc: e.matmul(ps[pbk][:, cc:cc + 65], lhsT=oT[0:65, br, h * 128:(h + 1) * 128], rhs=ident[0:65, 0:65], start=True, stop=True),
                              reads=[t_oT, t_ident], writes=[pst[pbk]])

                def oslot(br, h):
                    if br == 0:
                        return 6, h * 65
                    col = 260 + ((br - 1) * 4 + h) * 65
                    return (6, col) if col + 65 <= 512 else (7, col - 455 + 128)
                for br in (1, 2):
                    for h in range(4):
                        pbk, cc = oslot(br, h)
                        kb.op("dve", lambda e, br=br, h=h, pbk=pbk, cc=cc: e.tensor_scalar(out=zsc[:, br * 4 + h:br * 4 + h + 1], in0=ps[pbk][:, cc + 64:cc + 65], scalar1=1e-30, scalar2=None, op0=ALU.max),
                              reads=[pst[pbk]], writes=[t_zsc])
                kb.op("dve", lambda e: e.reciprocal(out=zsc[:, 4:12], in_=zsc[:, 4:12]), reads=[t_zsc], writes=[t_zsc])
                for br in range(3):
                    kb.op("dve", lambda e, br=br, qt=qt: e.tensor_tensor(out=zsc[:, 12 + br * 4:16 + br * 4], in0=zsc[:, br * 4:br * 4 + 4],
                                                                         in1=gts[:, qt, :].rearrange("p (h b) -> p h b", b=3)[:, :, br], op=ALU.mult),
                          reads=[t_zsc, t_gts], writes=[t_zsc])
                for h in range(4):
                    for br in range(3):
                        pbk, cc = oslot(br, h)
                        if br == 0:
                            kb.op("dve", lambda e, h=h, br=br, pbk=pbk, cc=cc: e.tensor_scalar(out=haf[:, h * 64:(h + 1) * 64], in0=ps[pbk][:, cc:cc + 64],
                                                                                               scalar1=zsc[:, 12 + br * 4 + h:13 + br * 4 + h], scalar2=None, op0=ALU.mult),
                                  reads=[pst[pbk], t_zsc], writes=[t_haf])
                        else:
                            kb.op("dve", lambda e, h=h, br=br, pbk=pbk, cc=cc: e.scalar_tensor_tensor(out=haf[:, h * 64:(h + 1) * 64], in0=ps[pbk][:, cc:cc + 64],
                                                                                                      scalar=zsc[:, 12 + br * 4 + h:13 + br * 4 + h], in1=haf[:, h * 64:(h + 1) * 64],
                                                                                                      op0=ALU.mult, op1=ALU.add),
                                  reads=[pst[pbk], t_zsc, t_haf], writes=[t_haf])
                kb.op("dve", lambda e: e.tensor_copy(out=hab[:], in_=haf[:]), reads=[t_haf], writes=[t_hab])
                ob = qt % 2
                for dc in range(2):
                    kb.op("pe", lambda e, dc=dc: e.matmul(ps[5][:, dc * 128:(dc + 1) * 128], lhsT=hab[:, dc * 128:(dc + 1) * 128], rhs=identb3[:, :], start=True, stop=True),
                          reads=[t_hab, t_identb3], writes=[pst[5]], sig=(dc == 1))
                kb.op("dve", lambda e, ob=ob: e.tensor_copy(out=haT[ob][:].rearrange("p a t -> p (a t)"), in_=ps[5][:, 0:256]), reads=[pst[5]], writes=[t_haT[ob]])
                kb.dma(mixT[qt // 8][256:512, (qt % 8) * 128:(qt % 8 + 1) * 128].rearrange("(a p) t -> p a t", p=128), haT[ob][:], reads=[t_haT[ob]], writes=[t_mixT])
            kb.barrier()
    if dbg:
        d_mixT = nc.dram_tensor("d_mixT", [512, S], BF16, kind="ExternalOutput").ap()
        for i in range(8):
            kb.dma(d_mixT[:, i * 1024:(i + 1) * 1024], mixT[i], reads=[t_mixT])


    if phases >= 4:
        selm_in = din("selm", [128, 4])
        x_own = din("x_own", [2048, D])
        w_out_in = din("w_out_p", [D, D])
        g2_row = din("g2_row", [1, D])
        wq_in = din("wq", [D, D])
        skT_in = din("skT", [128, 2, 128])
        uT_in = din("uT", [D, 16384])
        v_in = din("v_tab", [16384, D])
        y_out = nc.dram_tensor("y", [2048, D], F32, kind="ExternalOutput").ap()
        mixG = [nc.dram_tensor("mixG%d" % i, [2048, 1024], BF16, kind="Internal").ap() for i in range(8)]; t_mixG = Trk()
        s_x1 = dscr("s_x1", [2048, D]); t_sx1 = Trk()
        s_h2T = dscr("s_h2T", [D, 2048], BF16); t_sh2T = Trk()
        kb.barrier()
        for i in range(8):
            kb.op("pool", lambda e, i=i: e.collective_compute("AllGather", ALU.bypass, replica_groups=[[0, 1, 2, 3], [4, 5, 6, 7]],
                                                              ins=[mixT[i]], outs=[mixG[i]]), reads=[t_mixT], writes=[t_mixG])
        with ExitStack() as p45:
            def sb45(name, shape, dt=F32):
                return p45.enter_context(nc.sbuf_tensor(name, list(shape), dt))
            g2tb = sb45("g2tb", [128, D]); t_g2tb = Trk()
            identb4 = sb45("identb4", [128, 128], BF16); t_identb4 = Trk()
            kb.dma(identb4[:], din("ident_b2", [128, 128], BF16), writes=[t_identb4])
            with ExitStack() as p4:
                def sb4(name, shape, dt=F32):
                    return p4.enter_context(nc.sbuf_tensor(name, list(shape), dt))
                rowst = sb4("rowst", [1, D]); t_rowst = Trk()
                g1b = sb4("g1b", [128, D]); t_g1b = Trk()
                sh2b = sb4("sh2b", [128, D]); t_sh2b = Trk()
                gs2b = sb4("gs2b", [128, D]); t_gs2b = Trk()
                selm = sb4("selm_sb", [128, 4]); t_selm = Trk()
                kb.dma(selm[:], selm_in, writes=[t_selm])

                def bcast_row(src_ap, dst, t_dst, src_reads=()):
                    kb.dma(rowst[:], src_ap, reads=list(src_reads), writes=[t_rowst])
                    for n in range(4):
                        kb.op("pe", lambda e, n=n: e.matmul(ps[n][:, :], lhsT=ones_f[0:1, :], rhs=rowst[0:1, n * 512:(n + 1) * 512], start=True, stop=True),
                              reads=[t_ones_f, t_rowst], writes=[pst[n]])
                        kb.op("dve", lambda e, n=n: e.tensor_copy(out=dst[:, n * 512:(n + 1) * 512], in_=ps[n][:, :]), reads=[pst[n]], writes=[t_dst])
                bcast_row(s_mod[0:1, 2 * D:3 * D], g1b, t_g1b, [t_smod])
                bcast_row(s_mod[0:1, 3 * D:4 * D], sh2b, t_sh2b, [t_smod])
                bcast_row(s_mod[0:1, 5 * D:6 * D], g2tb, t_g2tb, [t_smod])
                bcast_row(s_mod[0:1, 4 * D:5 * D], gs2b, t_gs2b, [t_smod])
                kb.dma(rowst[:], g2_row, writes=[t_rowst])
                for n in range(4):
                    kb.op("pe", lambda e, n=n: e.matmul(ps[n][:, :], lhsT=ones_f[0:1, :], rhs=rowst[0:1, n * 512:(n + 1) * 512], start=True, stop=True),
                          reads=[t_ones_f, t_rowst], writes=[pst[n]])
                    kb.op("dve", lambda e, n=n: e.scalar_tensor_tensor(out=gs2b[:, n * 512:(n + 1) * 512], in0=gs2b[:, n * 512:(n + 1) * 512], scalar=1.0, in1=ps[n][:, :],
                                                                       op0=ALU.add, op1=ALU.mult), reads=[pst[n], t_gs2b], writes=[t_gs2b])
                wo = sb4("wo", [128, 16, D], BF16); t_wo = Trk()
                for fc in range(16):
                    kb.dma(wo[:, fc, :], w_out_in[fc * 128:(fc + 1) * 128, :], writes=[t_wo], q="pool")
                sl = [sb4("sl%d" % i, [128, 16, 128], BF16) for i in range(4)]; t_sl = [Trk() for _ in range(4)]
                mixo = sb4("mixo", [128, 16, 128], BF16); t_mixo = Trk()
                xin = [sb4("xin%d" % i, [128, D]) for i in range(2)]; t_xin = [Trk(), Trk()]
                tmpy = sb4("tmpy", [128, D]); t_tmpy = Trk()
                h2b = sb4("h2b", [128, D], BF16); t_h2b = Trk()
                h2Tt = [sb4("h2Tt%d" % i, [128, 16, 128], BF16) for i in range(2)]; t_h2Tt = [Trk(), Trk()]
                nsc = sb4("nsc", [128, 4]); t_nsc = Trk()
                mgv = [mg.rearrange("(f p) t -> p f t", p=128) for mg in mixG]
                for tt in range(16):
                    bi = tt % 2
                    kb.dma(xin[bi][:], x_own[tt * 128:(tt + 1) * 128, :], writes=[t_xin[bi]])
                    for s_ in range(4):
                        kb.dma(sl[s_][:], mgv[2 * s_ + tt // 8][:, :, (tt % 8) * 128:(tt % 8 + 1) * 128], reads=[t_mixG], writes=[t_sl[s_]])
                    kb.op("dve", lambda e: e.tensor_scalar(out=mixo[:], in0=sl[0][:], scalar1=selm[:, 0:1], scalar2=None, op0=ALU.mult), reads=[t_sl[0], t_selm], writes=[t_mixo])
                    for s_ in range(1, 4):
                        kb.op("dve", lambda e, s_=s_: e.scalar_tensor_tensor(out=mixo[:], in0=sl[s_][:], scalar=selm[:, s_:s_ + 1], in1=mixo[:], op0=ALU.mult, op1=ALU.add),
                              reads=[t_sl[s_], t_selm, t_mixo], writes=[t_mixo])
                    for n in range(4):
                        for fc in range(16):
                            kb.op("pe", lambda e, n=n, fc=fc: e.matmul(ps[n][:, :], lhsT=mixo[:, fc, :], rhs=wo[:, fc, n * 512:(n + 1) * 512], start=(fc == 0), stop=(fc == 15)),
                                  reads=[t_mixo, t_wo], writes=[pst[n]], sig=(fc == 15))
                        kb.op("dve", lambda e, n=n: e.tensor_tensor(out=tmpy[:, n * 512:(n + 1) * 512], in0=ps[n][:, :], in1=g1b[:, n * 512:(n + 1) * 512], op=ALU.mult),
                              reads=[pst[n], t_g1b], writes=[t_tmpy])
                        kb.op("dve", lambda e, n=n, bi=bi: e.tensor_tensor(out=xin[bi][:, n * 512:(n + 1) * 512], in0=xin[bi][:, n * 512:(n + 1) * 512], in1=tmpy[:, n * 512:(n + 1) * 512], op=ALU.add),
                              reads=[t_xin[bi], t_tmpy], writes=[t_xin[bi]])
                    kb.dma(s_x1[tt * 128:(tt + 1) * 128, :], xin[bi][:], reads=[t_xin[bi]], writes=[t_sx1])
                    kb.op("act", lambda e, bi=bi: e.activation(out=tmpy[:], in_=xin[bi][:], func=AF.Square, accum_out=nsc[:, 0:1]), reads=[t_xin[bi]], writes=[t_tmpy, t_nsc])
                    kb.op("act", lambda e: e.activation(out=nsc[:, 1:2], in_=nsc[:, 0:1], func=AF.Sqrt, scale=1.0 / D, bias=EPS), reads=[t_nsc], writes=[t_nsc])
                    kb.op("dve", lambda e: e.reciprocal(out=nsc[:, 2:3], in_=nsc[:, 1:2]), reads=[t_nsc], writes=[t_nsc])
                    kb.op("dve", lambda e, bi=bi: e.scalar_tensor_tensor(out=tmpy[:], in0=xin[bi][:], scalar=nsc[:, 2:3], in1=gs2b[:], op0=ALU.mult, op1=ALU.mult),
                          reads=[t_xin[bi], t_nsc, t_gs2b], writes=[t_tmpy])
                    kb.op("dve", lambda e: e.tensor_tensor(out=h2b[:], in0=tmpy[:], in1=sh2b[:], op=ALU.add), reads=[t_tmpy, t_sh2b], writes=[t_h2b])
                    for qd in range(4):
                        pb = 4 + qd
                        for k in range(4):
                            dc = qd * 4 + k
                            kb.op("pe", lambda e, dc=dc, k=k, pb=pb: e.matmul(ps[pb][:, k * 128:(k + 1) * 128], lhsT=h2b[:, dc * 128:(dc + 1) * 128], rhs=identb4[:, :], start=True, stop=True),
                                  reads=[t_h2b, t_identb4], writes=[pst[pb]], sig=(k == 3))
                        kb.op("act", lambda e, qd=qd, pb=pb, bi=bi: e.activation(out=h2Tt[bi][:, qd * 4:(qd + 1) * 4, :].rearrange("p a t -> p (a t)"), in_=ps[pb][:, :], func=AF.Copy),
                              reads=[pst[pb]], writes=[t_h2Tt[bi]])
                    kb.dma(s_h2T[:, tt * 128:(tt + 1) * 128].rearrange("(a p) t -> p a t", p=128), h2Tt[bi][:], reads=[t_h2Tt[bi]], writes=[t_sh2T])
                kb.barrier()

            if phases >= 5:
                with ExitStack() as p5:
                    def sb5(name, shape, dt=F32):
                        return p5.enter_context(nc.sbuf_tensor(name, list(shape), dt))
                    skT = sb5("skT_sb", [128, 2, 128], BF16); t_skT = Trk()
                    kb.dma(skT[:], skT_in, writes=[t_skT], q="pool")
                    h2g = sb5("h2g", [128, 16, 512], BF16); t_h2g = Trk()
                    s1m = sb5("s1m", [128, 4, 8, 128]); t_s1m = Trk()
                    s2t = sb5("s2t", [128, 4, 8, 128]); t_s2t = Trk()
                    tau = sb5("tau", [128, 4, 8]); t_tau = Trk()
                    Ysb = sb5("Ysb", [128, 4, D]); t_Ysb = Trk()
                    wqs = [sb5("wqs%d" % i, [128, 16, 128], BF16) for i in range(2)]; t_wqs = [Trk(), Trk()]
                    sct = sb5("sct", [128, 16, 128]); t_sct = Trk()
                    wk = sb5("wk", [128, 256]); t_wk = Trk()
                    tv = sb5("tv", [128, 16, 16]); t_tv = Trk()
                    cand = sb5("cand", [128, 8, 256]); t_cand = Trk()
                    cw2 = sb5("cw2", [128, 256]); t_cw2 = Trk()
                    m24 = sb5("m24", [128, 8, 24]); t_m24 = Trk()
                    hs = sb5("hs", [128, 8, 8]); t_hs = Trk()
                    ex = sb5("ex", [128, 8, 256]); t_ex = Trk()
                    Ub = [sb5("Ub%d" % i, [128, 16, 256], BF16) for i in range(2)]; t_Ub = [Trk(), Trk()]
                    Vb = [sb5("Vb%d" % i, [128, 2, D], BF16) for i in range(2)]; t_Vb = [Trk(), Trk()]
                    Lt = [sb5("Lt0", [128, 8, 256]), cand]; t_Lt = [Trk(), t_cand]
                    Et = [sb5("Et%d" % i, [128, 8, 256], BF16) for i in range(2)]; t_Et = [Trk(), Trk()]
                    Wt = [sb5("Wt%d" % i, [128, 8, 256], BF16) for i in range(2)]; t_Wt = [Trk(), Trk()]
                    glT = [sb5("glT%d" % i, [128, 2, 512], BF16) for i in range(2)]; t_glT = [Trk(), Trk()]
                    _exb = ex[:].rearrange("p h e -> p (h e)").bitcast(BF16)
                    Yt = [_exb[:, 0:D], _exb[:, D:2 * D]]; t_Yt = [t_ex, t_ex]
                    GT = [sb5("GT%d" % i, [128, 2, 128], BF16) for i in range(2)]; t_GT = [Trk(), Trk()]
                    x1t = Lt[0][:].rearrange("p h e -> p (h e)"); t_x1t = t_Lt[0]
                    h2v = s_h2T.rearrange("(a p) t -> p a t", p=128)
                    wqv = wq_in.rearrange("(a p) n -> p a n", p=128)
                    uTv = uT_in.rearrange("(a p) e -> p a e", p=128)
                    vv_ = v_in.rearrange("(c p) d -> p c d", p=128)
                    ngrp = int(os.environ.get("NGRP", "4"))
                    net = int(os.environ.get("NET", "64"))
                    for g in range(ngrp):
                        kb.dma(h2g[:, 0:8, :], h2v[:, 0:8, g * 512:(g + 1) * 512], reads=[t_sh2T], writes=[t_h2g])
                        kb.dma(h2g[:, 8:16, :], h2v[:, 8:16, g * 512:(g + 1) * 512], reads=[t_sh2T], writes=[t_h2g])
                        for tl in range(4):
                            pass
                        qall = p5.enter_context(nc.sbuf_tensor("qall%d" % g, [128, 16, 512], BF16)) if g == 0 else qall
                        t_qall = Trk() if g == 0 else t_qall
                        for ch in range(16):
                            wi = ch % 2
                            kb.dma(wqs[wi][:], wqv[:, :, ch * 128:(ch + 1) * 128], writes=[t_wqs[wi]], q="pool")
                            pb = ch % 2
                            for dc in range(16):
                                kb.op("pe", lambda e, dc=dc, wi=wi, pb=pb: e.matmul(ps[pb][:, :], lhsT=wqs[wi][:, dc, :], rhs=h2g[:, dc, :], start=(dc == 0), stop=(dc == 15)),
                                      reads=[t_wqs[wi], t_h2g], writes=[pst[pb]], sig=(dc == 15))
                            kb.op("act", lambda e, ch=ch, pb=pb: e.activation(out=qall[:, ch, :], in_=ps[pb][:, :], func=AF.Copy), reads=[pst[pb]], writes=[t_qall])
                        for tl in range(4):
                            for qd in range(4):
                                pb = 2 + (qd % 2)
                                for k in range(4):
                                    ch = qd * 4 + k
                                    kb.op("pe", lambda e, ch=ch, k=k, pb=pb, tl=tl: e.matmul(ps[pb][:, k * 128:(k + 1) * 128], lhsT=qall[:, ch, tl * 128:(tl + 1) * 128], rhs=skT[:, ch % 2, :],
                                                                                             start=True, stop=True),
                                          reads=[t_qall, t_skT], writes=[pst[pb]], sig=(k == 3))
                                kb.op("dve", lambda e, qd=qd, pb=pb: e.tensor_copy(out=sct[:, qd * 4:(qd + 1) * 4, :].rearrange("p a k -> p (a k)"), in_=ps[pb][:, :]),
                                      reads=[pst[pb]], writes=[t_sct])
                            for ch in range(16):
                                kb.op("dve", lambda e, ch=ch: e.max(out=tv[:, ch, 0:8], in_=sct[:, ch, :]), reads=[t_sct], writes=[t_tv])
                                kb.op("dve", lambda e, ch=ch: e.match_replace(out=wk[:, 0:128], in_to_replace=tv[:, ch, 0:8], in_values=sct[:, ch, :], imm_value=-3e38),
                                      reads=[t_sct, t_tv], writes=[t_wk])
                                kb.op("dve", lambda e, ch=ch: e.max(out=tv[:, ch, 8:16], in_=wk[:, 0:128]), reads=[t_wk], writes=[t_tv])
                            tvv = tv[:].rearrange("p (h two) a -> p h two a", two=2)
                            kb.op("dve", lambda e: e.tensor_tensor(out=cand[:].rearrange("p h (a b) -> p h a b", b=16),
                                                                   in0=tvv[:, :, 0, :].unsqueeze(3).broadcast_to([128, 8, 16, 16]),
                                                                   in1=tvv[:, :, 1, :].unsqueeze(2).broadcast_to([128, 8, 16, 16]), op=ALU.add),
                                  reads=[t_tv], writes=[t_cand])
                            for h in range(8):
                                kb.op("dve", lambda e, h=h: e.max(out=m24[:, h, 0:8], in_=cand[:, h, :]), reads=[t_cand], writes=[t_m24])
                                kb.op("dve", lambda e, h=h: e.match_replace(out=cw2[:], in_to_replace=m24[:, h, 0:8], in_values=cand[:, h, :], imm_value=-3e38),
                                      reads=[t_cand, t_m24], writes=[t_cw2])
                                kb.op("dve", lambda e, h=h: e.max(out=m24[:, h, 8:16], in_=cw2[:]), reads=[t_cw2], writes=[t_m24])
                                kb.op("dve", lambda e, h=h: e.match_replace(out=cw2[:], in_to_replace=m24[:, h, 8:16], in_values=cw2[:], imm_value=-3e38),
                                      reads=[t_cw2, t_m24], writes=[t_cw2])
                                kb.op("dve", lambda e, h=h: e.max(out=m24[:, h, 16:24], in_=cw2[:]), reads=[t_cw2], writes=[t_m24])
                            kb.op("dve", lambda e: e.tensor_copy(out=hs[:, :, 0], in_=m24[:, :, 0]), reads=[t_m24], writes=[t_hs])
                            kb.op("dve", lambda e: e.tensor_tensor(out=hs[:, :, 1], in0=m24[:, :, 15], in1=m24[:, :, 16], op=ALU.add), reads=[t_m24], writes=[t_hs])
                            kb.op("dve", lambda e: e.tensor_scalar(out=hs[:, :, 1], in0=hs[:, :, 1], scalar1=0.5, scalar2=None, op0=ALU.mult), reads=[t_hs], writes=[t_hs])
                            kb.op("dve", lambda e: e.tensor_tensor(out=ex[:], in0=cand[:], in1=hs[:, :, 0:1].broadcast_to([128, 8, 256]), op=ALU.subtract), reads=[t_cand, t_hs], writes=[t_ex])
                            kb.op("act", lambda e: e.activation(out=ex[:], in_=ex[:], func=AF.Exp), reads=[t_ex], writes=[t_ex])
                            kb.op("dve", lambda e: e.tensor_tensor(out=cand[:], in0=cand[:], in1=hs[:, :, 1:2].broadcast_to([128, 8, 256]), op=ALU.is_ge), reads=[t_cand, t_hs], writes=[t_cand])
                            kb.op("dve", lambda e: e.tensor_tensor(out=ex[:], in0=ex[:], in1=cand[:], op=ALU.mult), reads=[t_cand, t_ex], writes=[t_ex])
                            kb.op("dve", lambda e: e.tensor_reduce(out=hs[:, :, 2], in_=ex[:], axis=AX.X, op=ALU.add), reads=[t_ex], writes=[t_hs])
                            kb.op("act", lambda e: e.activation(out=hs[:, :, 3], in_=hs[:, :, 2], func=AF.Ln), reads=[t_hs], writes=[t_hs])
                            kb.op("dve", lambda e: e.tensor_tensor(out=hs[:, :, 4], in0=hs[:, :, 0], in1=hs[:, :, 3], op=ALU.add), reads=[t_hs], writes=[t_hs])
                            sv = sct[:].rearrange("p (h two) k -> p h two k", two=2)
                            kb.op("dve", lambda e, tl=tl: e.tensor_tensor(out=s1m[:, tl, :, :], in0=sv[:, :, 0, :], in1=hs[:, :, 4:5].broadcast_to([128, 8, 128]), op=ALU.subtract),
                                  reads=[t_sct, t_hs], writes=[t_s1m])
                            kb.op("dve", lambda e, tl=tl: e.tensor_copy(out=s2t[:, tl, :, :], in_=sv[:, :, 1, :]), reads=[t_sct], writes=[t_s2t])
                            kb.op("dve", lambda e, tl=tl: e.tensor_tensor(out=tau[:, tl, :], in0=hs[:, :, 1], in1=hs[:, :, 4], op=ALU.subtract), reads=[t_hs], writes=[t_tau])
                        kb.op("dve", lambda e: e.memset(Ysb[:], 0.0), writes=[t_Ysb])

                        def load_u(et):
                            bi = et % 2
                            kb.dma(Ub[bi][:, 0:8, :], uTv[:, 0:8, et * 256:(et + 1) * 256], writes=[t_Ub[bi]], q="pool")
                            kb.dma(Ub[bi][:, 8:16, :], uTv[:, 8:16, et * 256:(et + 1) * 256], writes=[t_Ub[bi]], q="pool")

                        def load_v(et):
                            bi = et % 2
                            kb.dma(Vb[bi][:], vv_[:, et * 2:et * 2 + 2, :], writes=[t_Vb[bi]], q="pool")

                        def emit_a(et):
                            bi = et % 2
                            for ec in range(2):
                                for dc in range(16):
                                    kb.op("pe", lambda e, dc=dc, ec=ec, bi=bi: e.matmul(ps[ec][:, :], lhsT=Ub[bi][:, dc, ec * 128:(ec + 1) * 128], rhs=h2g[:, dc, :],
                                                                                        start=(dc == 0), stop=(dc == 15)),
                                          reads=[t_Ub[bi], t_h2g], writes=[pst[ec]], sig=(dc == 15))
                                kb.op("act", lambda e, ec=ec, bi=bi: e.activation(out=glT[bi][:, ec, :], in_=ps[ec][:, :], func=AF.Gelu_apprx_tanh),
                                      reads=[pst[ec]], writes=[t_glT[bi]])
                        load_u(0)
                        if net > 1:
                            load_u(1)
                        load_v(0)
                        emit_a(0)
                        pairs = [(et, tl) for et in range(net) for tl in range(4)]
                        NP = len(pairs)

                        def st_L(n):
                            et, tl = pairs[n]; k = n % 2
                            kb.op("dve", lambda e: e.tensor_tensor(out=Lt[k][:].rearrange("p h (a k) -> p h a k", k=128),
                                                                   in0=s1m[:, tl, :, 2 * et:2 * et + 2].unsqueeze(3).broadcast_to([128, 8, 2, 128]),
                                                                   in1=s2t[:, tl, :, :].unsqueeze(2).broadcast_to([128, 8, 2, 128]), op=ALU.add),
                                  reads=[t_s1m, t_s2t], writes=[t_Lt[k]])
                            kb.op("act", lambda e: e.activation(out=Et[k][:], in_=Lt[k][:], func=AF.Exp), reads=[t_Lt[k]], writes=[t_Et[k]])

                        def st_C(n):
                            et, tl = pairs[n]; k = n % 2
                            for h in range(8):
                                kb.op("dve", lambda e, h=h: e.scalar_tensor_tensor(out=Wt[k][:, h, :], in0=Lt[k][:, h, :], scalar=tau[:, tl, h:h + 1], in1=Et[k][:, h, :],
                                                                                   op0=ALU.is_ge, op1=ALU.mult),
                                      reads=[t_Lt[k], t_tau, t_Et[k]], writes=[t_Wt[k]])
                            pw = 2 + k
                            for ec in range(2):
                                for h in range(8):
                                    kb.op("pe", lambda e, ec=ec, h=h: e.matmul(ps[pw][:, ec * 128:(ec + 1) * 128], lhsT=Wt[k][:, h, ec * 128:(ec + 1) * 128], rhs=identb4[:, :],
                                                                               start=(h == 0), stop=(h == 7)),
                                          reads=[t_Wt[k], t_identb4], writes=[pst[pw]], sig=(ec == 1 and h == 7))

                        def st_G(n):
                            et, tl = pairs[n]; k = n % 2; bi = et % 2; pw = 2 + k
                            if tl == 0:
                                if et + 1 < net:
                                    load_v(et + 1)
                                    emit_a(et + 1)
                                if et + 2 < net:
                                    load_u(et + 2)
                            kb.op("dve", lambda e: e.tensor_tensor(out=GT[k][:], in0=glT[bi][:, :, tl * 128:(tl + 1) * 128],
                                                                   in1=ps[pw][:, 0:256].rearrange("p (a t) -> p a t", t=128), op=ALU.mult),
                                  reads=[t_glT[bi], pst[pw]], writes=[t_GT[k]])
                            for nn in range(4):
                                pb = 4 + nn
                                for ec in range(2):
                                    kb.op("pe", lambda e, ec=ec, nn=nn, pb=pb: e.matmul(ps[pb][:, :], lhsT=GT[k][:, ec, :], rhs=Vb[bi][:, ec, nn * 512:(nn + 1) * 512],
                                                                                        start=(ec == 0), stop=(ec == 1)),
                                          reads=[t_GT[k], t_Vb[bi]], writes=[pst[pb]], sig=(ec == 1))

                        def st_Y(n):
                            et, tl = pairs[n]; k = n % 2
                            for nn in range(4):
                                pb = 4 + nn
                                kb.op("act", lambda e, nn=nn, pb=pb: e.activation(out=Yt[k][:, nn * 512:(nn + 1) * 512], in_=ps[pb][:, :], func=AF.Copy),
                                      reads=[pst[pb]], writes=[t_Yt[k]])
                            kb.op("pool", lambda e: e.tensor_tensor(out=Ysb[:, tl, :], in0=Ysb[:, tl, :], in1=Yt[k], op=ALU.add),
                                  reads=[t_Ysb, t_Yt[k]], writes=[t_Ysb])
                        for n in range(-2, NP):
                            if 0 <= n:
                                st_G(n)
                            if 0 <= n + 2 < NP:
                                st_L(n + 2)
                            if 0 <= n + 1 < NP:
                                st_C(n + 1)
                            if 0 <= n:
                                st_Y(n)
                        for tl in range(4):
                            r0 = (g * 4 + tl) * 128
                            kb.dma(x1t, s_x1[r0:r0 + 128, :], reads=[t_sx1], writes=[t_x1t])
                            kb.op("dve", lambda e, tl=tl: e.tensor_tensor(out=Ysb[:, tl, :], in0=Ysb[:, tl, :], in1=g2tb[:], op=ALU.mult), reads=[t_Ysb, t_g2tb], writes=[t_Ysb])
                            kb.op("dve", lambda e, tl=tl: e.tensor_tensor(out=x1t, in0=x1t, in1=Ysb[:, tl, :], op=ALU.add), reads=[t_Ysb, t_x1t], writes=[t_x1t])
                            kb.dma(y_out[r0:r0 + 128, :], x1t, reads=[t_x1t])
                    kb.barrier()

    for _i in range(int(os.environ.get('EXTRA', '0'))):
        kb.dma(s_mod[0:1, 0:128], ident[0:1, :], reads=[t_ident])
    kb.barrier()
    ok, stuck, sems = kb.check()
    if not ok or os.environ.get('KBV'):
        print('SYNC CHECK ok=%s' % ok, stuck, {k: v for k, v in kb.cnt.items()})
    assert ok, 'sync deadlock'
    es.close()
    return nc


def _consts():
    ones = np.ones((128, 128), np.float32)
    blk = np.zeros((128, 128), np.float32)
    blk[:64, :64] = 1.0
    blk[64:, 64:] = 1.0
    ut = np.triu(np.ones((128, 128), np.float32))
    NEG = -30000.0
    il = np.arange(128)
    cmask = np.zeros((128, 17, 128), np.float32)
    for m in range(17):
        ok = (il[None, :] + 128 * m) >= (16 * il[:, None] + 31)
        cmask[:, m, :] = np.where(ok, 0.0, NEG)
    caus = np.where(il[:, None] <= il[None, :], 0.0, NEG).astype(np.float32)
    acaus = np.where(il[:, None] > il[None, :], 0.0, NEG).astype(np.float32)
    i_abs = (np.arange(4)[None, :, None] * 128 + il[:, None, None])
    blkk = np.arange(128)[None, None, :]
    ovl = ((16 * i_abs < 64 * blkk + 64) & (16 * i_abs + 32 > 64 * blkk)).astype(np.float32)
    expT = (np.arange(S)[None, :] // 64 == il[:, None]).astype(np.float32)
    o = np.arange(255)[None, :] - 127
    cur_rel = (il[:, None] >= 64).astype(np.int64)
    rel = o - cur_rel
    t_keep = (rel < -1).astype(np.float32)
    t_add = np.where(rel > 0, -1e4, np.where(rel >= -1, 1e4, 0.0)).astype(np.float32)
    return {"ones_bf": _bf(ones), "blk64_bf": _bf(blk), "ident_f": np.eye(128, dtype=np.float32),
            "ut_f": ut, "ident_b": _bf(np.eye(128, dtype=np.float32)),
            "cmask": _bf(cmask), "caus_add": _bf(caus), "acaus_add": _bf(acaus), "ovl": _bf(ovl), "expT": _bf(expT),
            "t_keep": np.ascontiguousarray(np.broadcast_to(t_keep, (128, 255))).astype(np.float32), "t_add": t_add}


def make_in_maps(inp):
    f = lambda a: np.ascontiguousarray(np.asarray(a, dtype=np.float32))
    x = f(inp["x"]); c = f(inp["c"])
    w_in = f(inp["w_in"])[0]
    conv = f(inp["conv_qk"])[0]
    qn_g = f(inp["qn_g"])[0]; kn_g = f(inp["kn_g"])[0]
    mng = f(inp["mlstm_norm_g"])[0]
    g1 = f(inp["norm1_g"])[0]
    cst = _consts()
    maps = []
    xTs = [np.ascontiguousarray(x[b].T) for b in range(2)]
    w_out = f(inp["w_out"])[0]
    rows = []
    for fc in range(16):
        r, part, half = fc // 4, (fc % 4) // 2, fc % 2
        base = part * 1024 + r * 256 + half * 128
        rows += list(range(base, base + 128))
    w_out_p = np.ascontiguousarray(w_out[rows])
    wq = f(inp["peer_wq"])[0]
    skT = np.ascontiguousarray(f(inp["peer_subkeys"])[0].transpose(2, 0, 1))
    uT = np.ascontiguousarray(f(inp["peer_u"])[0].T)
    vtab = f(inp["peer_v"])[0]
    for core in range(8):
        b, j = divmod(core, 4)
        cols = []
        cols += list(range(j * 256, j * 256 + 256))
        cols += list(range(1024 + j * 256, 1024 + j * 256 + 256))
        AQ = 4096 + 8
        cols += list(range(AQ + j * 256, AQ + j * 256 + 256))
        AKV = AQ + 1024
        for br in (0, 1, 2, 4):
            cols += list(range(AKV + br * 256 + j * 64, AKV + br * 256 + j * 64 + 64))
        cols += [4096 + j, 4096 + 4 + j]
        cols += list(range(2048 + j * 256, 2048 + j * 256 + 256))
        cols += list(range(3072 + j * 256, 3072 + j * 256 + 256))
        for br in (3, 5):
            cols += list(range(AKV + br * 256 + j * 64, AKV + br * 256 + j * 64 + 64))
        AG = AKV + 1536
        cols += list(range(AG + j * 12, AG + j * 12 + 12))
        assert len(cols) == 1678
        wj = np.zeros((D, 1680), np.float32)
        wj[:, :1678] = w_in[:, cols]
        cwt = np.zeros((128, 4, 4), np.float32)
        for t in range(4):
            base = (j * 256 + (t % 2) * 128) if t < 2 else (1024 + j * 256 + (t % 2) * 128)
            cwt[:, t, :] = conv[:, base:base + 128].T
        qkg = np.zeros((128, 4), np.float32)
        qkg[:, 0] = np.tile(qn_g, 2) * 0.125
        qkg[:, 3] = np.concatenate([kn_g[1], kn_g[2]])
        m = {
            "xT": xTs[b], "c_col": np.ascontiguousarray(c[b].reshape(16, 128).T),
            "w_mod": f(inp["w_mod"])[0], "b_mod": f(inp["b_mod"]),
            "g1_col": np.ascontiguousarray(g1.reshape(16, 128).T),
            "w_in": wj, "convw": cwt, "qk_g": qkg,
            "mng_row": np.ascontiguousarray(mng[j * 256:(j + 1) * 256][None, :]),
            "bif": np.ascontiguousarray(np.tile(np.array([[f(inp["b_igate"])[0, j], f(inp["b_fgate"])[0, j]]], np.float32), (128, 1))),
        }
        slopes = np.array([2.0 ** (-8.0 * (4 * j + hh + 1) / 16) for hh in range(4)], np.float64)
        kl = np.arange(128, dtype=np.float64)
        dl = np.arange(64, dtype=np.float64)
        m["tb_sw"] = (slopes[None, None, :] * (kl[:, None, None] - 64 - 128 * dl[None, :, None])).astype(np.float32)
        m["tb_c"] = (slopes[None, None, :] * (16 * kl[:, None, None] - 33 - 128 * dl[None, :, None])).astype(np.float32)
        m["w1k"] = f(inp["cmp_k_w1"])[0]; m["w1v"] = f(inp["cmp_v_w1"])[0]
        m["w2k"] = f(inp["cmp_k_w2"])[0]; m["w2v"] = f(inp["cmp_v_w2"])[0]
        for nm, key in (("posk", "cmp_pos_k"), ("posv", "cmp_pos_v")):
            pos = f(inp[key])[0]
            m[nm] = np.ascontiguousarray(pos.reshape(16, 2, 64).transpose(1, 2, 0).reshape(128, 16))
        m["kng0"] = np.ascontiguousarray(kn_g[0][:, None])
        sm = np.zeros((128, 4), np.float32); sm[:, j] = 1.0
        m["selm"] = sm
        m["x_own"] = np.ascontiguousarray(x[b, j * 2048:(j + 1) * 2048])
        m["w_out_p"] = w_out_p
        m["g2_row"] = f(inp["norm2_g"])
        m["wq"] = wq
        m["skT"] = skT
        m["uT"] = uT
        m["v_tab"] = vtab
        m["ident_b2"] = cst["ident_b"]
        m.update(cst)
        maps.append(m)
    return maps


def kernel(**inputs):
    nc = build()
    maps = make_in_maps(inputs)
    res = run_bass_kernel_spmd(nc, maps, core_ids=list(range(8)))
    out = np.zeros((2, S, D), np.float32)
    for core in range(8):
        b, j = divmod(core, 4)
        out[b, j * 2048:(j + 1) * 2048] = res.results[core]["y"]
    return out
```

```python
import numpy as np
from contextlib import ExitStack
import concourse.bass as bass
import concourse.mybir as mybir
from concourse.bass_utils import run_bass_kernel_spmd

F32 = mybir.dt.float32
BF16 = mybir.dt.bfloat16
AF = mybir.ActivationFunctionType
ALU = mybir.AluOpType
AX = mybir.AxisListType

D = 2048
S = 8192
NB = 16
EPS = 1e-6
import os
NDS = int(os.environ.get("NDS", "24"))


class Trk:
    __slots__ = ("w", "r")

    def __init__(self):
        self.w = None
        self.r = {}


class KB:
    def __init__(self, nc, es):
        self.nc = nc
        self.eng = {"pe": nc.tensor, "act": nc.scalar, "dve": nc.vector, "pool": nc.gpsimd, "sp": nc.sync}
        self.sem = {k: es.enter_context(nc.semaphore("s_" + k)) for k in self.eng}
        self.cnt = {k: 0 for k in self.eng}
        self.seen = {k: {} for k in self.eng}
        self.dsem = [es.enter_context(nc.semaphore("dq%d" % i)) for i in range(NDS)]
        self.duse = [0] * NDS
        self.dnext = 0
        self.ccsem = es.enter_context(nc.semaphore("ccs"))
        self.log = {k: [] for k in self.eng}

    def _wait(self, e, tok):
        if tok is None:
            return
        key, sem, val = tok
        if self.seen[e].get(key, 0) >= val:
            return
        self.eng[e].wait_ge(sem, val)
        self.log[e].append(("w", key, val))
        self.seen[e][key] = val

    def _deps(self, e, reads, writes):
        for b in reads:
            if b.w is not None and not (e == "pe" and b.w[0] == "pe"):
                self._wait(e, b.w)
        for b in writes:
            if b.w is not None and not (e == "pe" and b.w[0] == "pe"):
                self._wait(e, b.w)
            for t in b.r.values():
                if not (e == "pe" and t[0] == "pe"):
                    self._wait(e, t)

    def _mark(self, tok, reads, writes):
        for b in reads:
            b.r[tok[0]] = tok
        for b in writes:
            b.w = tok
            b.r = {}

    def op(self, e, fn, reads=(), writes=(), sig=True):
        self._deps(e, reads, writes)
        inst = fn(self.eng[e])
        if sig:
            self.cnt[e] += 1
            inst.then_inc(self.sem[e], 1)
            self.log[e].append(("i", e, 1))
            tok = (e, self.sem[e], self.cnt[e])
        else:
            tok = (e, self.sem[e], self.cnt[e] + 1)
        self._mark(tok, reads, writes)
        return tok

    def dma(self, out, in_, reads=(), writes=(), q="sp", **kw):
        i = self.dnext
        self.dnext = (i + 1) % NDS
        if self.duse[i] > 0:
            self._wait(q, (("d", i), self.dsem[i], 16 * self.duse[i]))
        self._deps(q, reads, writes)
        inst = self.eng[q].dma_start(out=out, in_=in_, **kw)
        self.duse[i] += 1
        inst.then_inc(self.dsem[i], 16)
        self.log[q].append(("i", ("d", i), 16))
        tok = (("d", i), self.dsem[i], 16 * self.duse[i])
        self._mark(tok, reads, writes)
        return tok

    def check(self):
        sems = {}
        pc = {k: 0 for k in self.log}
        prog = True
        while prog:
            prog = False
            for e, lg in self.log.items():
                while pc[e] < len(lg):
                    kind, key, val = lg[pc[e]]
                    if kind == "w":
                        if sems.get(key, 0) < val:
                            break
                    else:
                        sems[key] = sems.get(key, 0) + val
                    pc[e] += 1
                    prog = True
        stuck = {e: (pc[e], len(lg), lg[pc[e]] if pc[e] < len(lg) else None) for e, lg in self.log.items()}
        ok = all(pc[e] == len(lg) for e, lg in self.log.items())
        return ok, stuck, sems

    def barrier(self):
        for e in self.eng:
            for e2 in self.eng:
                if e2 != e and self.cnt[e2] > 0:
                    self._wait(e, (e2, self.sem[e2], self.cnt[e2]))
            for i in range(NDS):
                if self.duse[i] > 0:
                    self._wait(e, (("d", i), self.dsem[i], 16 * self.duse[i]))


def _bf(a):
    import ml_dtypes
    return np.asarray(a, dtype=np.float32).astype(ml_dtypes.bfloat16)


def build(dbg=False, phases=99, nb=NB, parts='abcde'):
    nc = bass.Bass("TRN2", target_bir_lowering=False)
    es = ExitStack()
    kb = KB(nc, es)

    def din(name, shape, dt=F32):
        return nc.dram_tensor(name, list(shape), dt, kind="ExternalInput").ap()

    def dscr(name, shape, dt=F32):
        return nc.dram_tensor(name, list(shape), dt, kind=("ExternalOutput" if dbg else "Internal")).ap()

    xT = din("xT", [D, S])
    c_col = din("c_col", [128, 16])
    w_mod = din("w_mod", [D, 6 * D])
    b_mod = din("b_mod", [1, 6 * D])
    g1_col = din("g1_col", [128, 16])
    w_in = din("w_in", [D, 1680])
    convw = din("convw", [128, 4, 4])
    qk_g = din("qk_g", [128, 4])
    mng_row = din("mng_row", [1, 256])
    ones_bf = din("ones_bf", [128, 128], BF16)
    blk64_bf = din("blk64_bf", [128, 128], BF16)
    ident_f = din("ident_f", [128, 128])

    s_q = dscr("s_q", [256, S])
    s_k = dscr("s_k", [256, S])
    s_aq = dscr("s_aq", [256, S], BF16)
    s_kv = dscr("s_kv", [128, S])
    s_kk = dscr("s_kk", [128, S], BF16)
    s_if = dscr("s_if", [2, S])
    s_v = dscr("s_v", [S, 256])
    s_go = dscr("s_go", [S, 256])
    s_vv = dscr("s_vv", [S, 128], BF16)
    s_gt = dscr("s_gt", [S, 12])

    ps = []
    pst = []
    for i in range(4):
        ps.append(es.enter_context(nc.psum_tensor("ps%d" % i, [128, 512], F32)))
        pst.append(Trk())
    psY = es.enter_context(nc.psum_tensor("psY", [128, 2048], F32))
    for i in range(4):
        ps.append(psY[:, i * 512:(i + 1) * 512])
        pst.append(Trk())

    def sb(name, shape, dt=F32):
        return es.enter_context(nc.sbuf_tensor(name, list(shape), dt))

    ones_b = sb("ones_b", [128, 128], BF16); t_ones_b = Trk()
    blk64 = sb("blk64", [128, 128], BF16); t_blk64 = Trk()
    ident = sb("ident", [128, 128]); t_ident = Trk()
    ones_f = sb("ones_f", [128, 128]); t_ones_f = Trk()
    kb.dma(ones_b[:], ones_bf, writes=[t_ones_b])
    kb.dma(blk64[:], blk64_bf, writes=[t_blk64])
    kb.dma(ident[:], ident_f, writes=[t_ident])
    kb.op("dve", lambda e: e.memset(ones_f[:], 1.0), writes=[t_ones_f])

    s_mod = dscr("s_mod", [1, 6 * D]); t_smod = Trk()
    csil = sb("csil", [128, 16]); t_csil = Trk()
    ccol = sb("ccol", [128, 16]); t_ccol = Trk()
    g1c = sb("g1c", [128, 16]); t_g1c = Trk()
    gs1 = sb("gs1", [128, 16]); t_gs1 = Trk()
    sh1 = sb("sh1", [128, 16]); t_sh1 = Trk()
    kb.dma(ccol[:], c_col, writes=[t_ccol])
    kb.dma(g1c[:], g1_col, writes=[t_g1c])
    kb.op("act", lambda e: e.activation(out=csil[:], in_=ccol[:], func=AF.Silu), reads=[t_ccol], writes=[t_csil])
    with ExitStack() as p0:
        wm = [p0.enter_context(nc.sbuf_tensor("wm%d" % i, [128, 16, 512], F32)) for i in range(2)]
        modrow = p0.enter_context(nc.sbuf_tensor("modrow", [1, 6 * D], F32)); t_modrow = Trk()
        bmod_sb = p0.enter_context(nc.sbuf_tensor("bmod_sb", [1, 6 * D], F32)); t_bmod = Trk()
        kb.dma(bmod_sb[:], b_mod, writes=[t_bmod])
        t_wm = [Trk(), Trk()]
        wmv = w_mod.rearrange("(k p) n -> p k n", p=128)
        for n in range(24):
            bi = n % 2
            kb.dma(wm[bi][:, 0:8, :], wmv[:, 0:8, n * 512:(n + 1) * 512], writes=[t_wm[bi]])
            kb.dma(wm[bi][:, 8:16, :], wmv[:, 8:16, n * 512:(n + 1) * 512], writes=[t_wm[bi]])
            pb = n % 2
            for k in range(16):
                kb.op("pe", lambda e, k=k, bi=bi, pb=pb: e.matmul(ps[pb][0:1, :], lhsT=csil[:, k:k + 1], rhs=wm[bi][:, k, :],
                                                                  start=(k == 0), stop=(k == 15)),
                      reads=[t_csil, t_wm[bi]], writes=[pst[pb]], sig=(k == 15))
            kb.op("dve", lambda e, n=n, pb=pb: e.tensor_tensor(out=modrow[0:1, n * 512:(n + 1) * 512], in0=ps[pb][0:1, :],
                                                               in1=bmod_sb[0:1, n * 512:(n + 1) * 512], op=ALU.add),
                  reads=[pst[pb], t_bmod], writes=[t_modrow])
        for which, dst, t_dst in ((0, sh1, t_sh1), (1, gs1, t_gs1)):
            for k in range(16):
                off = which * D + k * 128
                kb.op("pe", lambda e, off=off, k=k: e.matmul(ps[2][:, k:k + 1], lhsT=modrow[0:1, off:off + 128], rhs=ones_f[0:1, 0:1],
                                                             start=True, stop=True),
                      reads=[t_modrow, t_ones_f], writes=[pst[2]], sig=(k == 15))
            if which == 0:
                kb.op("dve", lambda e: e.tensor_copy(out=sh1[:], in_=ps[2][:, 0:16]), reads=[pst[2]], writes=[t_sh1])
            else:
                kb.op("dve", lambda e: e.scalar_tensor_tensor(out=gs1[:], in0=ps[2][:, 0:16], scalar=1.0, in1=g1c[:],
                                                              op0=ALU.add, op1=ALU.mult),
                      reads=[pst[2], t_g1c], writes=[t_gs1])
        kb.dma(s_mod, modrow[:], reads=[t_modrow], writes=[t_smod])
        kb.barrier()

    t_scr = {n: Trk() for n in ("q", "k", "aq", "kv", "kk", "if", "v", "go", "vv", "gt")}
    if phases >= 1:
        with ExitStack() as p1:
            def sb1(name, shape, dt=F32):
                return p1.enter_context(nc.sbuf_tensor(name, list(shape), dt))
            wb = sb1("wb", [128, 16, 1680], BF16); t_wb = Trk()
            cw = sb1("cw", [128, 4, 4]); t_cw = Trk()
            qkg = sb1("qkg", [128, 4]); t_qkg = Trk()
            mng = sb1("mng", [128, 256]); t_mng = Trk()
            mngr = sb1("mngr", [1, 256]); t_mngr = Trk()
            kb.dma(cw[:], convw, writes=[t_cw])
            kb.dma(qkg[:], qk_g, writes=[t_qkg])
            kb.dma(mngr[:], mng_row, writes=[t_mngr])
            kb.op("pe", lambda e: e.matmul(ps[3][:, 0:256], lhsT=ones_f[0:1, :], rhs=mngr[0:1, :], start=True, stop=True),
                  reads=[t_ones_f, t_mngr], writes=[pst[3]])
            kb.op("dve", lambda e: e.tensor_copy(out=mng[:], in_=ps[3][:, 0:256]), reads=[pst[3]], writes=[t_mng])
            wst = [sb1("wst%d" % i, [128, 1680]) for i in range(2)]; t_wst = [Trk(), Trk()]
            wiv = w_in.rearrange("(k p) n -> p k n", p=128)
            for k in range(16):
                bi = k % 2
                kb.dma(wst[bi][:], wiv[:, k, :], writes=[t_wst[bi]])
                kb.op("dve", lambda e, k=k, bi=bi: e.tensor_copy(out=wb[:, k, :], in_=wst[bi][:]),
                      reads=[t_wst[bi]], writes=[t_wb])
            xt = [sb1("xt%d" % i, [128, 16, 512]) for i in range(2)]; t_xt = [Trk(), Trk()]
            xsq = sb1("xsq", [128, 16, 512], BF16); t_xsq = [Trk() for _ in range(16)]
            hT = sb1("hT", [128, 16, 512], BF16); t_hT = [Trk() for _ in range(16)]
            rstd = sb1("rstd", [128, 512]); t_rstd = Trk()
            tmp = [sb1("tmp%d" % i, [128, 512]) for i in range(2)]; t_tmp = [Trk(), Trk()]
            zr = [sb1("zr%d" % i, [128, 3 + 512]) for i in range(4)]; t_zr = [Trk() for _ in range(4)]
            acc = [sb1("acc%d" % i, [128, 512]) for i in range(2)]; t_acc = [Trk(), Trk()]
            ofm = [sb1("ofm%d" % i, [128, 512]) for i in range(2)]; t_ofm = [Trk(), Trk()]
            obf = [sb1("obf%d" % i, [128, 512], BF16) for i in range(2)]; t_obf = [Trk(), Trk()]
            sqb = sb1("sqb", [128, 512], BF16); t_sqb = Trk()
            rs2 = sb1("rs2", [128, 512]); t_rs2 = Trk()
            otm = [sb1("otm%d" % i, [128, 512]) for i in range(2)]; t_otm = [Trk(), Trk()]
            otb = [sb1("otb%d" % i, [128, 128], BF16) for i in range(2)]; t_otb = [Trk(), Trk()]
            otg = [sb1("otg%d" % i, [128, 12]) for i in range(2)]; t_otg = [Trk(), Trk()]
            for i in range(4):
                kb.op("dve", lambda e, i=i: e.memset(zr[i][:, 0:3], 0.0), writes=[t_zr[i]])
            xv = xT.rearrange("(k p) t -> p k t", p=128)
            cnt2 = [0]

            def rot2():
                cnt2[0] += 1
                return cnt2[0] % 2

            def load_x(n):
                bi = n % 2
                for h in range(4):
                    kb.dma(xt[bi][:, 4 * h:4 * h + 4, :], xv[:, 4 * h:4 * h + 4, n * 512:(n + 1) * 512], writes=[t_xt[bi]])

            load_x(0)
            for n in range(nb):
                bi = n % 2
                t0 = n * 512
                if n + 1 < nb:
                    load_x(n + 1)
                for k in range(16):
                    kb.op("act" if k % 2 else "dve",
                          (lambda e, k=k, bi=bi: e.activation(out=xsq[:, k, :], in_=xt[bi][:, k, :], func=AF.Square)) if k % 2 else
                          (lambda e, k=k, bi=bi: e.tensor_tensor(out=xsq[:, k, :], in0=xt[bi][:, k, :], in1=xt[bi][:, k, :], op=ALU.mult)),
                          reads=[t_xt[bi]], writes=[t_xsq[k]])
                for k in range(16):
                    kb.op("pe", lambda e, k=k: e.matmul(ps[0][:, :], lhsT=ones_b[:], rhs=xsq[:, k, :], start=(k == 0), stop=(k == 15)),
                          reads=[t_ones_b, t_xsq[k]], writes=[pst[0]], sig=(k == 15))
                kb.op("act", lambda e: e.activation(out=rstd[:], in_=ps[0][:, :], func=AF.Sqrt, scale=1.0 / D, bias=EPS),
                      reads=[pst[0]], writes=[t_rstd])
                kb.op("dve", lambda e: e.reciprocal(out=rstd[:], in_=rstd[:]), reads=[t_rstd], writes=[t_rstd])
                for k in range(16):
                    tb = k % 2
                    kb.op("dve", lambda e, k=k, tb=tb, bi=bi: e.tensor_tensor(out=tmp[tb][:], in0=xt[bi][:, k, :], in1=rstd[:], op=ALU.mult),
                          reads=[t_xt[bi], t_rstd], writes=[t_tmp[tb]])
                    kb.op("act", lambda e, k=k, tb=tb: e.activation(out=hT[:, k, :], in_=tmp[tb][:], func=AF.Identity,
                                                                    scale=gs1[:, k:k + 1], bias=sh1[:, k:k + 1]),
                          reads=[t_tmp[tb], t_gs1, t_sh1], writes=[t_hT[k]])
                for ct in range(9):
                    if not ({0: 'a', 1: 'a', 2: 'a', 3: 'a', 4: 'b', 5: 'b', 7: 'b', 6: 'c', 8: 'd'}[ct] in parts):
                        continue
                    c0 = ct * 128
                    cn = 128 if ct < 8 else 2
                    pb = 1 + (ct % 2)
                    for k in range(16):
                        kb.op("pe", lambda e, k=k, c0=c0, cn=cn, pb=pb: e.matmul(ps[pb][0:cn, :], lhsT=wb[:, k, c0:c0 + cn], rhs=hT[:, k, :],
                                                                                 start=(k == 0), stop=(k == 15)),
                              reads=[t_wb, t_hT[k]], writes=[pst[pb]], sig=(k == 15))
                    if ct < 4:
                        z = zr[ct]; tz = t_zr[ct]
                        kb.op("act", lambda e, z=z, pb=pb: e.activation(out=z[:, 3:515], in_=ps[pb][:, :], func=AF.Copy),
                              reads=[pst[pb]], writes=[tz])
                        ab = rot2()
                        a = acc[ab]; ta = t_acc[ab]
                        kb.op("dve", lambda e, z=z, a=a, ct=ct: e.tensor_scalar(out=a[:], in0=z[:, 3:515], scalar1=cw[:, ct, 3:4], scalar2=None,
                                                                                 op0=ALU.mult), reads=[tz, t_cw], writes=[ta])
                        for j in range(3):
                            kb.op("dve", lambda e, z=z, a=a, ct=ct, j=j: e.scalar_tensor_tensor(out=a[:], in0=z[:, j:j + 512], scalar=cw[:, ct, j:j + 1],
                                                                                               in1=a[:], op0=ALU.mult, op1=ALU.add),
                                  reads=[tz, t_cw, ta], writes=[ta])
                        ob = rot2()
                        kb.op("act", lambda e, a=a, ob=ob: e.activation(out=ofm[ob][:], in_=a[:], func=AF.Silu), reads=[ta], writes=[t_ofm[ob]])
                        dst = s_q if ct < 2 else s_k
                        r0 = (ct % 2) * 128
                        kb.dma(dst[r0:r0 + 128, t0:t0 + 512], ofm[ob][:], reads=[t_ofm[ob]], writes=[t_scr["q" if ct < 2 else "k"]])
                        kb.op("dve", lambda e, z=z: e.tensor_copy(out=z[:, 0:3], in_=z[:, 512:515]), reads=[tz], writes=[tz])
                    elif ct in (4, 5, 7):
                        kb.op("act", lambda e, pb=pb: e.activation(out=sqb[:], in_=ps[pb][:, :], func=AF.Square), reads=[pst[pb]], writes=[t_sqb])
                        kb.op("pe", lambda e: e.matmul(ps[3][:, :], lhsT=blk64[:], rhs=sqb[:], start=True, stop=True),
                              reads=[t_blk64, t_sqb], writes=[pst[3]])
                        kb.op("act", lambda e: e.activation(out=rs2[:], in_=ps[3][:, :], func=AF.Sqrt, scale=1.0 / 64, bias=EPS),
                              reads=[pst[3]], writes=[t_rs2])
                        kb.op("dve", lambda e: e.reciprocal(out=rs2[:], in_=rs2[:]), reads=[t_rs2], writes=[t_rs2])
                        ob = rot2()
                        gcol = 0 if ct in (4, 5) else 3
                        kb.op("dve", lambda e, pb=pb, ob=ob, gcol=gcol: e.scalar_tensor_tensor(out=obf[ob][:], in0=ps[pb][:, :], scalar=qkg[:, gcol:gcol + 1],
                                                                                              in1=rs2[:], op0=ALU.mult, op1=ALU.mult),
                              reads=[pst[pb], t_rs2, t_qkg], writes=[t_obf[ob]])
                        if ct == 7:
                            kb.dma(s_kk[:, t0:t0 + 512], obf[ob][:], reads=[t_obf[ob]], writes=[t_scr["kk"]])
                        else:
                            r0 = (ct - 4) * 128
                            kb.dma(s_aq[r0:r0 + 128, t0:t0 + 512], obf[ob][:], reads=[t_obf[ob]], writes=[t_scr["aq"]])
                    elif ct == 6:
                        ob = rot2()
                        kb.op("act", lambda e, pb=pb, ob=ob: e.activation(out=ofm[ob][:], in_=ps[pb][:, :], func=AF.Copy), reads=[pst[pb]], writes=[t_ofm[ob]])
                        kb.dma(s_kv[:, t0:t0 + 512], ofm[ob][:], reads=[t_ofm[ob]], writes=[t_scr["kv"]])
                    else:
                        ob = rot2()
                        kb.op("act", lambda e, pb=pb, ob=ob: e.activation(out=ofm[ob][0:2, :], in_=ps[pb][0:2, :], func=AF.Copy), reads=[pst[pb]], writes=[t_ofm[ob]])
                        kb.dma(s_if[:, t0:t0 + 512], ofm[ob][0:2, :], reads=[t_ofm[ob]], writes=[t_scr["if"]])
                for ts in range(int(os.environ.get('NTS', '4')) if 'e' in parts else 0):
                    tt = t0 + ts * 128
                    pb = 4 + (ts % 2)
                    for k in range(16):
                        kb.op("pe", lambda e, k=k, ts=ts, pb=pb: e.matmul(ps[pb][:, :], lhsT=hT[:, k, ts * 128:(ts + 1) * 128], rhs=wb[:, k, 1026:1538],
                                                                          start=(k == 0), stop=(k == 15)),
                              reads=[t_wb, t_hT[k]], writes=[pst[pb]], sig=(k == 15))
                    pb2 = 6 + (ts % 2)
                    for k in range(16):
                        kb.op("pe", lambda e, k=k, ts=ts, pb2=pb2: e.matmul(ps[pb2][:, 0:140], lhsT=hT[:, k, ts * 128:(ts + 1) * 128], rhs=wb[:, k, 1538:1678],
                                                                            start=(k == 0), stop=(k == 15)),
                              reads=[t_wb, t_hT[k]], writes=[pst[pb2]], sig=(k == 15))
                    ob = ts % 2
                    kb.op("dve", lambda e, pb=pb, ob=ob: e.tensor_copy(out=otm[ob][:, 0:256], in_=ps[pb][:, 0:256]), reads=[pst[pb]], writes=[t_otm[ob]])
                    kb.op("act", lambda e, pb=pb, ob=ob: e.activation(out=otm[ob][:, 256:512], in_=ps[pb][:, 256:512], func=AF.Sigmoid),
                          reads=[pst[pb]], writes=[t_otm[ob]])
                    SK = os.environ.get('SKIP', '')
                    if 'P' not in SK:
                        kb.op("dve", lambda e, ob=ob: e.tensor_tensor(out=otm[ob][:, 256:512], in0=otm[ob][:, 256:512], in1=mng[:], op=ALU.mult),
                              reads=[t_otm[ob], t_mng], writes=[t_otm[ob]])
                    if 'S' not in SK:
                        kb.dma(s_v[tt:tt + 128, :], otm[ob][:, 0:256], reads=[t_otm[ob]], writes=[t_scr["v"]])
                        kb.dma(s_go[tt:tt + 128, :], otm[ob][:, 256:512], reads=[t_otm[ob]], writes=[t_scr["go"]])
                    kb.op("dve", lambda e, pb2=pb2, ob=ob: e.tensor_copy(out=otb[ob][:], in_=ps[pb2][:, 0:128]), reads=[pst[pb2]], writes=[t_otb[ob]])
                    kb.op("act", lambda e, pb2=pb2, ob=ob: e.activation(out=otg[ob][:], in_=ps[pb2][:, 128:140], func=AF.Sigmoid),
                          reads=[pst[pb2]], writes=[t_otg[ob]])
                    if 'V' not in os.environ.get('SKIP', ''):
                        kb.dma(s_vv[tt:tt + 128, :], otb[ob][:], reads=[t_otb[ob]], writes=[t_scr["vv"]])
                    if 'G' not in os.environ.get('SKIP', ''):
                        kb.dma(s_gt[tt:tt + 128, :], otg[ob][:], reads=[t_otg[ob]], writes=[t_scr["gt"]])
            kb.barrier()


    mixT = [nc.dram_tensor("mixT%d" % i, [512, 1024], BF16, kind="Internal").ap() for i in range(8)]; t_mixT = Trk()
    if phases >= 2:
        bif_in = din("bif", [128, 2])
        ut_in = din("ut_f", [128, 128])
        identb_in = din("ident_b", [128, 128], BF16)
        with ExitStack() as p2:
            def sb2(name, shape, dt=F32):
                return p2.enter_context(nc.sbuf_tensor(name, list(shape), dt))
            NCH = 64
            bif = sb2("bif_sb", [128, 2]); t_bif = Trk()
            ut = sb2("ut_sb", [128, 128]); t_ut = Trk()
            identb = sb2("identb", [128, 128], BF16); t_identb = Trk()
            kb.dma(bif[:], bif_in, writes=[t_bif])
            kb.dma(ut[:], ut_in, writes=[t_ut])
            kb.dma(identb[:], identb_in, writes=[t_identb])
            rows = sb2("rows", [64, 2, 128]); t_rows = Trk()
            kb.dma(rows[:, 0, :], s_if[0:1, :].rearrange("o (c p) -> (o c) p", p=128), reads=[t_scr["if"]], writes=[t_rows])
            kb.dma(rows[:, 1, :], s_if[1:2, :].rearrange("o (c p) -> (o c) p", p=128), reads=[t_scr["if"]], writes=[t_rows])
            cols = sb2("cols", [128, 12, 64]); t_cols = Trk()
            nbf = sb2("nbf", [128, 1]); t_nbf = Trk()
            zero64 = sb2("zero64", [128, 128]); t_zero = Trk()
            kb.op("dve", lambda e: e.memset(zero64[:], 0.0), writes=[t_zero])
            kb.op("dve", lambda e: e.tensor_scalar(out=nbf[:], in0=bif[:, 1:2], scalar1=-1.0, scalar2=None, op0=ALU.mult), reads=[t_bif], writes=[t_nbf])
            for w in range(2):
                kb.op("pe", lambda e, w=w: e.matmul(ps[w][:, 0:64], lhsT=rows[:, w, :], rhs=ident[0:64, 0:64], start=True, stop=True),
                      reads=[t_rows, t_ident], writes=[pst[w]])
            kb.op("dve", lambda e: e.tensor_scalar(out=cols[:, 0, :], in0=ps[0][:, 0:64], scalar1=bif[:, 0:1], scalar2=None, op0=ALU.add),
                  reads=[pst[0], t_bif], writes=[t_cols])
            kb.op("act", lambda e: e.activation(out=cols[:, 11, :], in_=ps[1][:, 0:64], func=AF.Exp, scale=-1.0, bias=nbf[:, 0:1]),
                  reads=[pst[1], t_nbf], writes=[t_cols])
            kb.op("act", lambda e: e.activation(out=cols[:, 1, :], in_=cols[:, 11, :], func=AF.Ln, scale=1.0, bias=1.0), reads=[t_cols], writes=[t_cols])
            kb.op("dve", lambda e: e.tensor_scalar(out=cols[:, 1, :], in0=cols[:, 1, :], scalar1=-1.0, scalar2=None, op0=ALU.mult), reads=[t_cols], writes=[t_cols])
            kb.op("pe", lambda e: e.matmul(ps[2][:, 0:64], lhsT=ut[:], rhs=cols[:, 1, :], start=True, stop=True), reads=[t_ut, t_cols], writes=[pst[2]])
            kb.op("pe", lambda e: e.matmul(ps[3][:, 0:64], lhsT=ones_f[:], rhs=cols[:, 1, :], start=True, stop=True), reads=[t_ones_f, t_cols], writes=[pst[3]])
            kb.op("dve", lambda e: e.tensor_copy(out=cols[:, 11, :], in_=ps[3][:, 0:64]), reads=[pst[3]], writes=[t_cols])
            kb.op("dve", lambda e: e.tensor_tensor_scan(out=cols[:, 2, :], data0=cols[:, 11, :], data1=zero64[:, 0:64], initial=0.0, op0=ALU.add, op1=ALU.add),
                  reads=[t_cols, t_zero], writes=[t_cols])
            kb.op("dve", lambda e: e.tensor_tensor(out=cols[:, 2, :], in0=cols[:, 2, :], in1=cols[:, 11, :], op=ALU.subtract), reads=[t_cols], writes=[t_cols])
            kb.op("dve", lambda e: e.tensor_tensor(out=cols[:, 2, :], in0=cols[:, 2, :], in1=ps[2][:, 0:64], op=ALU.add), reads=[t_cols, pst[2]], writes=[t_cols])
            kb.op("dve", lambda e: e.tensor_tensor(out=cols[:, 3, :], in0=cols[:, 0, :], in1=cols[:, 2, :], op=ALU.subtract), reads=[t_cols], writes=[t_cols])
            grow = sb2("grow", [64, 128]); t_grow = Trk()
            cmrow = sb2("cmrow", [64, 128]); t_cmrow = Trk()
            kb.op("pe", lambda e: e.matmul(ps[4][0:64, 0:128], lhsT=cols[:, 3, :], rhs=ident[:, :], start=True, stop=True), reads=[t_cols, t_ident], writes=[pst[4]])
            kb.op("dve", lambda e: e.tensor_copy(out=grow[:], in_=ps[4][0:64, 0:128]), reads=[pst[4]], writes=[t_grow])
            kb.op("dve", lambda e: e.tensor_tensor_scan(out=cmrow[:], data0=grow[:], data1=grow[:], initial=-1e30, op0=ALU.max, op1=ALU.max),
                  reads=[t_grow], writes=[t_cmrow])
            mrow = sb2("mrow", [1, 3, 64]); t_mrow = Trk()
            kb.op("pe", lambda e: e.matmul(ps[5][0:1, 0:64], lhsT=cmrow[:, 127:128], rhs=ident[0:64, 0:64], start=True, stop=True),
                  reads=[t_cmrow, t_ident], writes=[pst[5]])
            kb.op("dve", lambda e: e.tensor_copy(out=mrow[0:1, 0, :], in_=ps[5][0:1, 0:64]), reads=[pst[5]], writes=[t_mrow])
            kb.op("dve", lambda e: e.tensor_tensor_scan(out=mrow[0:1, 1, :], data0=mrow[0:1, 0, :], data1=mrow[0:1, 0, :], initial=0.0, op0=ALU.max, op1=ALU.max),
                  reads=[t_mrow], writes=[t_mrow])
            kb.op("dve", lambda e: e.memset(mrow[0:1, 2, 0:1], 0.0), reads=[t_mrow], writes=[t_mrow])
            kb.op("dve", lambda e: e.tensor_copy(out=mrow[0:1, 2, 1:64], in_=mrow[0:1, 1, 0:63]), reads=[t_mrow], writes=[t_mrow])
            kb.op("pe", lambda e: e.matmul(ps[6][:, 0:128], lhsT=ones_f[0:1, :], rhs=mrow[0:1, 1:3, :].rearrange("o a c -> o (a c)"), start=True, stop=True),
                  reads=[t_ones_f, t_mrow], writes=[pst[6]])
            kb.op("dve", lambda e: e.tensor_copy(out=cols[:, 6, :], in_=ps[6][:, 0:64]), reads=[pst[6]], writes=[t_cols])
            kb.op("dve", lambda e: e.tensor_copy(out=cols[:, 5, :], in_=ps[6][:, 64:128]), reads=[pst[6]], writes=[t_cols])
            kb.op("pe", lambda e: e.matmul(ps[7][:, 0:64], lhsT=cmrow[:, :], rhs=ident[0:64, 0:64], start=True, stop=True), reads=[t_cmrow, t_ident], writes=[pst[7]])
            kb.op("dve", lambda e: e.tensor_tensor(out=cols[:, 4, :], in0=ps[7][:, 0:64], in1=cols[:, 5, :], op=ALU.max), reads=[pst[7], t_cols], writes=[t_cols])
            kb.op("dve", lambda e: e.tensor_tensor(out=cols[:, 11, :], in0=cols[:, 3, :], in1=cols[:, 5, :], op=ALU.subtract), reads=[t_cols], writes=[t_cols])
            kb.op("act", lambda e: e.activation(out=cols[:, 7, :], in_=cols[:, 11, :], func=AF.Exp), reads=[t_cols], writes=[t_cols])
            kb.op("dve", lambda e: e.tensor_scalar(out=cols[:, 7, :], in0=cols[:, 7, :], scalar1=1.0 / 16, scalar2=None, op0=ALU.mult), reads=[t_cols], writes=[t_cols])
            kb.op("dve", lambda e: e.tensor_tensor(out=cols[:, 11, :], in0=cols[:, 5, :], in1=cols[:, 4, :], op=ALU.subtract), reads=[t_cols], writes=[t_cols])
            kb.op("act", lambda e: e.activation(out=cols[:, 8, :], in_=cols[:, 11, :], func=AF.Exp), reads=[t_cols], writes=[t_cols])
            kb.op("dve", lambda e: e.tensor_tensor(out=cols[:, 11, :], in0=cols[:, 2, :], in1=cols[:, 4, :], op=ALU.add), reads=[t_cols], writes=[t_cols])
            kb.op("act", lambda e: e.activation(out=cols[:, 9, :], in_=cols[:, 11, :], func=AF.Exp, scale=-1.0), reads=[t_cols], writes=[t_cols])
            kb.op("dve", lambda e: e.tensor_tensor(out=cols[:, 11, :], in0=cols[:, 5, :], in1=cols[:, 6, :], op=ALU.subtract), reads=[t_cols], writes=[t_cols])
            kb.op("act", lambda e: e.activation(out=cols[:, 10, :], in_=cols[:, 11, :], func=AF.Exp), reads=[t_cols], writes=[t_cols])
            if dbg:
                d_cols = nc.dram_tensor("d_cols", [128, 12, 64], F32, kind="ExternalOutput").ap()
                kb.dma(d_cols, cols[:], reads=[t_cols])

            qT = [sb2("qT%d" % i, [128, 2, 128]) for i in range(2)]; t_qT = [Trk(), Trk()]
            kT = [sb2("kT%d" % i, [128, 2, 128]) for i in range(2)]; t_kT = [Trk(), Trk()]
            va = [sb2("va%d" % i, [128, 257]) for i in range(2)]; t_va = [Trk(), Trk()]
            go = [sb2("go%d" % i, [128, 256]) for i in range(2)]; t_go = [Trk(), Trk()]
            kp = sb2("kp", [128, 256]); t_kp = Trk()
            wT = sb2("wT", [128, 128]); t_wT = Trk()
            St = [sb2("St%d" % i, [128, 2, 257]) for i in range(2)]; t_St = [Trk(), Trk()]
            sc = sb2("sc", [128, 8]); t_sc = Trk()
            junk = sb2("junk", [128, 256]); t_junk = Trk()
            hmf = sb2("hmf", [128, 256], BF16); t_hmf = Trk()
            hmT = [sb2("hmT%d" % i, [128, 2, 128], BF16) for i in range(2)]; t_hmT = [Trk(), Trk()]
            for i in range(2):
                kb.op("dve", lambda e, i=i: e.memset(va[i][:, 256:257], 1.0), writes=[t_va[i]])
            kb.op("dve", lambda e: e.memset(St[0][:], 0.0), writes=[t_St[0]])
            qv = s_q.rearrange("(a p) t -> p a t", p=128)
            kv_ = s_k.rearrange("(a p) t -> p a t", p=128)

            def load_chunk(c):
                bi = c % 2
                kb.dma(qT[bi][:], qv[:, :, c * 128:(c + 1) * 128], reads=[t_scr["q"]], writes=[t_qT[bi]])
                kb.dma(kT[bi][:], kv_[:, :, c * 128:(c + 1) * 128], reads=[t_scr["k"]], writes=[t_kT[bi]])
                kb.dma(va[bi][:, 0:256], s_v[c * 128:(c + 1) * 128, :], reads=[t_scr["v"]], writes=[t_va[bi]])
                kb.dma(go[bi][:], s_go[c * 128:(c + 1) * 128, :], reads=[t_scr["go"]], writes=[t_go[bi]])

            nch = int(os.environ.get("NCH", "64"))
            load_chunk(0)
            for c in range(nch):
                bi = c % 2
                so = St[c % 2]; tso = t_St[c % 2]
                sn = St[(c + 1) % 2]; tsn = t_St[(c + 1) % 2]
                if c + 1 < nch:
                    load_chunk(c + 1)
                for dc in range(2):
                    kb.op("pe", lambda e, dc=dc, bi=bi: e.matmul(ps[0][:, dc * 128:(dc + 1) * 128], lhsT=kT[bi][:, dc, :], rhs=ident[:, :], start=True, stop=True),
                          reads=[t_kT[bi], t_ident], writes=[pst[0]], sig=(dc == 1))
                kb.op("dve", lambda e, c=c: e.tensor_scalar(out=kp[:], in0=ps[0][:, 0:256], scalar1=cols[:, 7, c:c + 1], scalar2=None, op0=ALU.mult),
                      reads=[pst[0], t_cols], writes=[t_kp])
                for dc in range(2):
                    kb.op("pe", lambda e, dc=dc, bi=bi: e.matmul(ps[1][:, 0:128], lhsT=kT[bi][:, dc, :], rhs=qT[bi][:, dc, :], start=(dc == 0), stop=(dc == 1)),
                          reads=[t_kT[bi], t_qT[bi]], writes=[pst[1]], sig=(dc == 1))
                kb.op("dve", lambda e, c=c: e.scalar_tensor_tensor(out=wT[:], in0=ps[1][:, 0:128], scalar=cols[:, 7, c:c + 1], in1=ut[:], op0=ALU.mult, op1=ALU.mult),
                      reads=[pst[1], t_cols, t_ut], writes=[t_wT])
                kb.op("pe", lambda e, bi=bi: e.matmul(ps[2][:, 0:257], lhsT=wT[:], rhs=va[bi][:], start=True, stop=False),
                      reads=[t_wT, t_va[bi]], writes=[pst[2]], sig=False)
                for dc in range(2):
                    kb.op("pe", lambda e, dc=dc, bi=bi, so=so: e.matmul(ps[2][:, 0:257], lhsT=qT[bi][:, dc, :], rhs=so[:, dc, :], start=False, stop=(dc == 1)),
                          reads=[t_qT[bi], tso], writes=[pst[2]], sig=(dc == 1))
                for dc in range(2):
                    pb = 3 + dc
                    kb.op("pe", lambda e, dc=dc, bi=bi, pb=pb: e.matmul(ps[pb][:, 0:257], lhsT=kp[:, dc * 128:(dc + 1) * 128], rhs=va[bi][:], start=True, stop=False),
                          reads=[t_kp, t_va[bi]], writes=[pst[pb]], sig=False)
                    kb.op("pe", lambda e, dc=dc, pb=pb, so=so: e.matmul(ps[pb][:, 0:257], lhsT=ident[:, :], rhs=so[:, dc, :], start=False, stop=True),
                          reads=[t_ident, tso], writes=[pst[pb]])
                    kb.op("act" if dc else "dve",
                          (lambda e, dc=dc, pb=pb, sn=sn, c=c: e.activation(out=sn[:, dc, :], in_=ps[pb][:, 0:257], func=AF.Copy, scale=cols[:, 10, c:c + 1])) if dc else
                          (lambda e, dc=dc, pb=pb, sn=sn, c=c: e.tensor_scalar(out=sn[:, dc, :], in0=ps[pb][:, 0:257], scalar1=cols[:, 10, c:c + 1], scalar2=None, op0=ALU.mult)),
                          reads=[pst[pb], t_cols], writes=[tsn])
                kb.op("act", lambda e, c=c: e.activation(out=sc[:, 0:1], in_=ps[2][:, 256:257], func=AF.Abs, scale=cols[:, 8, c:c + 1]),
                      reads=[pst[2], t_cols], writes=[t_sc])
                kb.op("dve", lambda e, c=c: e.tensor_tensor(out=sc[:, 1:2], in0=sc[:, 0:1], in1=cols[:, 9, c:c + 1], op=ALU.max), reads=[t_sc, t_cols], writes=[t_sc])
                kb.op("dve", lambda e: e.reciprocal(out=sc[:, 2:3], in_=sc[:, 1:2]), reads=[t_sc], writes=[t_sc])
                kb.op("dve", lambda e, c=c: e.tensor_tensor(out=sc[:, 3:4], in0=sc[:, 2:3], in1=cols[:, 8, c:c + 1], op=ALU.mult), reads=[t_sc, t_cols], writes=[t_sc])
                kb.op("act", lambda e: e.activation(out=junk[:], in_=ps[2][:, 0:256], func=AF.Square, accum_out=sc[:, 4:5]), reads=[pst[2]], writes=[t_junk, t_sc])
                kb.op("dve", lambda e: e.scalar_tensor_tensor(out=sc[:, 5:6], in0=sc[:, 3:4], scalar=sc[:, 3:4], in1=sc[:, 4:5], op0=ALU.mult, op1=ALU.mult),
                      reads=[t_sc], writes=[t_sc])
                kb.op("act", lambda e: e.activation(out=sc[:, 5:6], in_=sc[:, 5:6], func=AF.Sqrt, scale=1.0 / 256, bias=EPS), reads=[t_sc], writes=[t_sc])
                kb.op("dve", lambda e: e.reciprocal(out=sc[:, 6:7], in_=sc[:, 5:6]), reads=[t_sc], writes=[t_sc])
                kb.op("dve", lambda e: e.tensor_tensor(out=sc[:, 7:8], in0=sc[:, 6:7], in1=sc[:, 3:4], op=ALU.mult), reads=[t_sc], writes=[t_sc])
                kb.op("dve", lambda e, bi=bi: e.scalar_tensor_tensor(out=hmf[:], in0=ps[2][:, 0:256], scalar=sc[:, 7:8], in1=go[bi][:], op0=ALU.mult, op1=ALU.mult),
                      reads=[pst[2], t_sc, t_go[bi]], writes=[t_hmf])
                ob = c % 2
                for dc in range(2):
                    kb.op("pe", lambda e, dc=dc: e.matmul(ps[5][:, dc * 128:(dc + 1) * 128], lhsT=hmf[:, dc * 128:(dc + 1) * 128], rhs=identb[:, :], start=True, stop=True),
                          reads=[t_hmf, t_identb], writes=[pst[5]], sig=(dc == 1))
                kb.op("act", lambda e, ob=ob: e.activation(out=hmT[ob][:].rearrange("p a t -> p (a t)"), in_=ps[5][:, 0:256], func=AF.Copy), reads=[pst[5]], writes=[t_hmT[ob]])
                kb.dma(mixT[c // 8][0:256, (c % 8) * 128:(c % 8 + 1) * 128].rearrange("(a p) t -> p a t", p=128), hmT[ob][:], reads=[t_hmT[ob]], writes=[t_mixT])
            kb.barrier()

    if phases >= 3:
        tb_sw_in = din("tb_sw", [128, 64, 4])
        tb_c_in = din("tb_c", [128, 64, 4])
        cmask_in = din("cmask", [128, 17, 128], BF16)
        caus_in = din("caus_add", [128, 128], BF16)
        acaus_in = din("acaus_add", [128, 128], BF16)
        ovl_in = din("ovl", [128, 4, 128], BF16)
        expT_in = din("expT", [128, S], BF16)
        tkeep_in = din("t_keep", [128, 255])
        tadd_in = din("t_add", [128, 255])
        w1k_in = din("w1k", [2048, 256]); w1v_in = din("w1v", [2048, 256])
        w2k_in = din("w2k", [256, 64]); w2v_in = din("w2v", [256, 64])
        posk_in = din("posk", [128, 16]); posv_in = din("posv", [128, 16])
        kng0_in = din("kng0", [64, 1])
        with ExitStack() as p3:
            def sb3(name, shape, dt=F32):
                return p3.enter_context(nc.sbuf_tensor(name, list(shape), dt))
            QT = sb3("QT", [128, 4, S], BF16); t_QT = Trk()
            KK = sb3("KK", [128, S], BF16); t_KK = Trk()
            Vs = sb3("Vs", [128, 64, 65], BF16); t_Vs = Trk()
            Vw = sb3("Vw", [128, 64, 65], BF16); t_Vw = Trk()
            expT = sb3("expT_sb", [128, S], BF16); t_expT = Trk()
            tb_sw = sb3("tb_sw_sb", [128, 64, 4]); t_tbsw = Trk()
            tb_c = sb3("tb_c_sb", [128, 64, 4]); t_tbc = Trk()
            cmask = sb3("cmask_sb", [128, 17, 128], BF16); t_cmask = Trk()
            caus = sb3("caus_sb", [128, 128], BF16); t_caus = Trk()
            acaus = sb3("acaus_sb", [128, 128], BF16); t_acaus = Trk()
            ovl = sb3("ovl_sb", [128, 4, 128], BF16); t_ovl = Trk()
            tkeep = sb3("tkeep_sb", [128, 255]); t_tkeep = Trk()
            tadd = sb3("tadd_sb", [128, 255]); t_tadd = Trk()
            gts = sb3("gts", [128, 64, 12]); t_gts = Trk()
            kcT = sb3("kcT", [64, 512], BF16); t_kcT = Trk()
            vca = sb3("vca", [128, 4, 65], BF16); t_vca = Trk()
            identb3 = sb3("identb3", [128, 128], BF16); t_identb3 = Trk()
            aqv = s_aq.rearrange("(h d) t -> d h t", d=64)
            for hh in range(2):
                for h in range(4):
                    kb.dma(QT[hh * 64:(hh + 1) * 64, h, :], aqv[:, h, :], reads=[t_scr["aq"]], writes=[t_QT])
            kb.dma(KK[:, 0:4096], s_kk[:, 0:4096], reads=[t_scr["kk"]], writes=[t_KK])
            kb.dma(KK[:, 4096:S], s_kk[:, 4096:S], reads=[t_scr["kk"]], writes=[t_KK])
            kb.dma(expT[:], expT_in, writes=[t_expT])
            kb.dma(tb_sw[:], tb_sw_in, writes=[t_tbsw]); kb.dma(tb_c[:], tb_c_in, writes=[t_tbc])
            kb.dma(cmask[:], cmask_in, writes=[t_cmask]); kb.dma(caus[:], caus_in, writes=[t_caus]); kb.dma(acaus[:], acaus_in, writes=[t_acaus])
            kb.dma(ovl[:], ovl_in, writes=[t_ovl]); kb.dma(tkeep[:], tkeep_in, writes=[t_tkeep]); kb.dma(tadd[:], tadd_in, writes=[t_tadd])
            kb.dma(identb3[:], identb_in if phases >= 2 else din("ident_b", [128, 128], BF16), writes=[t_identb3])
            kb.dma(gts[:], s_gt.rearrange("(q p) c -> p q c", p=128), reads=[t_scr["gt"]], writes=[t_gts])
            with ExitStack() as p3a:
                def sb3a(name, shape, dt=F32):
                    return p3a.enter_context(nc.sbuf_tensor(name, list(shape), dt))
                vtmp = sb3a("vtmp", [128, 64, 128], BF16); t_vtmp = Trk()
                kb.dma(vtmp[:], s_vv.rearrange("(q p) c -> p q c", p=128), reads=[t_scr["vv"]], writes=[t_vtmp])
                kb.op("dve", lambda e: e.tensor_copy(out=Vs[:, :, 0:64], in_=vtmp[:, :, 0:64]), reads=[t_vtmp], writes=[t_Vs])
                kb.op("dve", lambda e: e.tensor_copy(out=Vw[:, :, 0:64], in_=vtmp[:, :, 64:128]), reads=[t_vtmp], writes=[t_Vw])
                kb.op("dve", lambda e: e.memset(Vs[:, :, 64:65], 1.0), writes=[t_Vs])
                kb.op("dve", lambda e: e.memset(Vw[:, :, 64:65], 1.0), writes=[t_Vw])
                kb.op("dve", lambda e: e.memset(vca[:, :, 64:65], 1.0), writes=[t_vca])
                w1f = sb3a("w1f", [128, 16, 256]); t_w1f = Trk()
                w1b = sb3a("w1b", [128, 16, 256], BF16); t_w1b = Trk()
                w2f = sb3a("w2f", [128, 2, 64]); t_w2f = Trk()
                w2b = sb3a("w2b", [128, 2, 64], BF16); t_w2b = Trk()
                posf = sb3a("posf", [128, 16]); t_posf = Trk()
                bcol = sb3a("bcol", [128, 2]); t_bcol = Trk()
                kng0 = sb3a("kng0_sb", [64, 1]); t_kng0 = Trk()
                a2f = sb3a("a2f", [128, 2048]); t_a2f = Trk()
                a2b = sb3a("a2b", [128, S], BF16); t_a2b = Trk()
                hid = sb3a("hid", [128, 2, 512], BF16); t_hid = Trk()
                csq = sb3a("csq", [64, 512], BF16); t_csq = Trk()
                crs = sb3a("crs", [64, 512]); t_crs = Trk()
                kb.dma(kng0[:], kng0_in, writes=[t_kng0])
                kb.op("dve", lambda e: e.memset(hid[:], 0.0), writes=[t_hid])
                kb.op("dve", lambda e: e.memset(kcT[:], 0.0), writes=[t_kcT])
                for which in range(2):
                    w1_in = w1k_in if which == 0 else w1v_in
                    w2_in = w2k_in if which == 0 else w2v_in
                    pos_in = posk_in if which == 0 else posv_in
                    r0 = 0 if which == 0 else 64
                    kb.dma(w1f[:, 0:8, :], w1_in.rearrange("(k p) n -> p k n", p=128)[:, 0:8, :], writes=[t_w1f])
                    kb.dma(w1f[:, 8:16, :], w1_in.rearrange("(k p) n -> p k n", p=128)[:, 8:16, :], writes=[t_w1f])
                    kb.dma(w2f[:], w2_in.rearrange("(k p) n -> p k n", p=128), writes=[t_w2f])
                    kb.dma(posf[:], pos_in, writes=[t_posf])
                    kb.op("dve", lambda e: e.tensor_copy(out=w1b[:], in_=w1f[:]), reads=[t_w1f], writes=[t_w1b])
                    kb.op("dve", lambda e: e.tensor_copy(out=w2b[:], in_=w2f[:]), reads=[t_w2f], writes=[t_w2b])
                    for q4 in range(4):
                        c0 = q4 * 2048
                        kb.dma(a2f[0:64, :], s_kv[r0:r0 + 64, c0:c0 + 2048], reads=[t_scr["kv"]], writes=[t_a2f])
                        if q4 < 3:
                            kb.dma(a2f[64:128, :], s_kv[r0:r0 + 64, c0 + 1:c0 + 2049], reads=[t_scr["kv"]], writes=[t_a2f])
                        else:
                            kb.dma(a2f[64:128, 0:2047], s_kv[r0:r0 + 64, c0 + 1:c0 + 2048], reads=[t_scr["kv"]], writes=[t_a2f])
                        kb.op("dve", lambda e, c0=c0: e.tensor_copy(out=a2b[:, c0:c0 + 2048], in_=a2f[:]), reads=[t_a2f], writes=[t_a2b])
                    for hc in range(2):
                        for jj in range(16):
                            kb.op("pe", lambda e, hc=hc, jj=jj: e.matmul(ps[7][:, hc:hc + 1], lhsT=w1f[:, jj, hc * 128:(hc + 1) * 128], rhs=posf[:, jj:jj + 1],
                                                                          start=(jj == 0), stop=(jj == 15)),
                                  reads=[t_w1f, t_posf], writes=[pst[7]], sig=(jj == 15))
                    kb.op("dve", lambda e: e.tensor_copy(out=bcol[:], in_=ps[7][:, 0:2]), reads=[pst[7]], writes=[t_bcol])
                    a2v = a2b[:].rearrange("p (i s) -> p i s", s=16)
                    for hc in range(2):
                        for jj in range(16):
                            j0 = 2 * jj
                            rhs = a2v[:, 0:511, j0] if j0 < 16 else a2v[:, 1:512, j0 - 16]
                            kb.op("pe", lambda e, hc=hc, jj=jj, rhs=rhs: e.matmul(ps[hc][:, 0:511], lhsT=w1b[:, jj, hc * 128:(hc + 1) * 128], rhs=rhs,
                                                                                  start=(jj == 0), stop=(jj == 15)),
                                  reads=[t_w1b, t_a2b], writes=[pst[hc]], sig=(jj == 15))
                        kb.op("act", lambda e, hc=hc: e.activation(out=hid[:, hc, 0:511], in_=ps[hc][:, 0:511], func=AF.Gelu_apprx_tanh, bias=bcol[:, hc:hc + 1]),
                              reads=[pst[hc], t_bcol], writes=[t_hid])
                    if which == 0:
                        for hc in range(2):
                            kb.op("pe", lambda e, hc=hc: e.matmul(ps[2][0:64, 0:511], lhsT=w2b[:, hc, :], rhs=hid[:, hc, 0:511], start=(hc == 0), stop=(hc == 1)),
                                  reads=[t_w2b, t_hid], writes=[pst[2]], sig=(hc == 1))
                        kb.op("act", lambda e: e.activation(out=csq[:, 0:511], in_=ps[2][0:64, 0:511], func=AF.Square), reads=[pst[2]], writes=[t_csq])
                        kb.op("pe", lambda e: e.matmul(ps[3][0:64, 0:511], lhsT=blk64[0:64, 0:64], rhs=csq[:, 0:511], start=True, stop=True),
                              reads=[t_blk64, t_csq], writes=[pst[3]])
                        kb.op("act", lambda e: e.activation(out=crs[:, 0:511], in_=ps[3][0:64, 0:511], func=AF.Sqrt, scale=1.0 / 64, bias=EPS), reads=[pst[3]], writes=[t_crs])
                        kb.op("dve", lambda e: e.reciprocal(out=crs[:, 0:511], in_=crs[:, 0:511]), reads=[t_crs], writes=[t_crs])
                        kb.op("dve", lambda e: e.scalar_tensor_tensor(out=kcT[:, 0:511], in0=ps[2][0:64, 0:511], scalar=kng0[:, 0:1], in1=crs[:, 0:511],
                                                                      op0=ALU.mult, op1=ALU.mult), reads=[pst[2], t_kng0, t_crs], writes=[t_kcT])
                    else:
                        for it in range(4):
                            for hc in range(2):
                                kb.op("pe", lambda e, hc=hc, it=it: e.matmul(ps[4][:, it * 64:(it + 1) * 64], lhsT=hid[:, hc, it * 128:(it + 1) * 128], rhs=w2b[:, hc, :],
                                                                             start=(hc == 0), stop=(hc == 1)),
                                      reads=[t_hid, t_w2b], writes=[pst[4]], sig=(hc == 1 and it == 3))
                        kb.op("dve", lambda e: e.tensor_copy(out=vca[:, :, 0:64], in_=ps[4][:, 0:256].rearrange("p (a d) -> p a d", d=64)), reads=[pst[4]], writes=[t_vca])
                if dbg:
                    d_kcT = nc.dram_tensor("d_kcT", [64, 512], BF16, kind="ExternalOutput").ap()
                    d_vca = nc.dram_tensor("d_vca", [128, 4, 65], BF16, kind="ExternalOutput").ap()
                    kb.dma(d_kcT, kcT[:], reads=[t_kcT]); kb.dma(d_vca, vca[:], reads=[t_vca])
                kb.barrier()

            pT = [sb3("pT%d" % i, [128, 512], BF16) for i in range(3)]; t_pT = [[Trk() for _ in range(4)] for _ in range(3)]
            oT = sb3("oT", [65, 3, 512]); t_oT = Trk()
            zsc = sb3("zsc", [128, 24]); t_zsc = Trk()
            impn = sb3("impn", [128, 128]); t_impn = Trk()
            impw = sb3("impw", [128, 128]); t_impw = Trk()
            m8 = sb3("m8", [128, 16]); t_m8 = Trk()
            selb = sb3("selb", [128, 128], BF16); t_selb = Trk()
            selT = sb3("selT", [128, 128], BF16); t_selT = Trk()
            haf = sb3("haf", [128, 256]); t_haf = Trk()
            hab = sb3("hab", [128, 256], BF16); t_hab = Trk()
            haT = [sb3("haT%d" % i, [128, 2, 128], BF16) for i in range(2)]; t_haT = [Trk(), Trk()]
            if dbg:
                d_imp = nc.dram_tensor("d_imp", [S, 128], F32, kind="ExternalOutput").ap()
                d_sel = nc.dram_tensor("d_sel", [S, 128], BF16, kind="ExternalOutput").ap()
            pcount = [0]
            NEG = -30000.0

            pend = []

            def flush_pairs():
                while pend:
                    pend.pop(0)()

            def tile_pair(lhsK, rhsQ, masks, bias_tab, bidx, Vaug, obank, first, last, imp_it=None):
                i = pcount[0] % 2
                pi = pcount[0] % 3
                pcount[0] += 1
                sps = ps[i]; tsp = pst[i]
                nm = len(masks)
                kb.op("pe", lambda e: e.matmul(sps[:, :], lhsT=lhsK[0], rhs=rhsQ[0], start=True, stop=(nm == 0)),
                      reads=[lhsK[1], rhsQ[1]], writes=[tsp], sig=(nm == 0))
                for mi, (ml, mr, mt) in enumerate(masks):
                    for h in range(4):
                        lastm = (mi == nm - 1 and h == 3)
                        kb.op("pe", lambda e, ml=ml, mr=mr, h=h, lastm=lastm: e.matmul(sps[:, h * 128:(h + 1) * 128], lhsT=ml, rhs=mr, start=False, stop=lastm),
                              reads=mt, writes=[tsp], sig=lastm)
                for h in range(4):
                    kb.op("act", lambda e, h=h: e.activation(out=pT[pi][:, h * 128:(h + 1) * 128], in_=sps[:, h * 128:(h + 1) * 128], func=AF.Exp,
                                                             bias=bias_tab[0][:, bidx, h:h + 1]),
                          reads=[tsp, bias_tab[1]], writes=[t_pT[pi][h]])

                def stage_b():
                    kb.op("pe", lambda e: e.matmul(ps[obank][0:65, :], lhsT=Vaug[0], rhs=pT[pi][:, :], start=first, stop=last),
                          reads=[Vaug[1]] + t_pT[pi], writes=[pst[obank]], sig=last)
                    if imp_it is not None:
                        it, nit = imp_it
                        for h in range(4):
                            kb.op("pe", lambda e, h=h, it=it: e.matmul(ps[5][:, h * 128:(h + 1) * 128], lhsT=pT[pi][:, h * 128:(h + 1) * 128], rhs=ovl[:, it, :],
                                                                       start=(it == 0), stop=(it == nit - 1)),
                                  reads=[t_pT[pi][h], t_ovl], writes=[pst[5]], sig=(it == nit - 1 and h == 3))
                while len(pend) > 1:
                    pend.pop(0)()
                pend.append(stage_b)
                if len(pend) > 1:
                    pend.pop(0)()

            nqt = int(os.environ.get("NQT", "64"))
            for qt in range(nqt):
                q0 = qt * 128
                qv_lo = (QT[0:64, :, q0:q0 + 128], t_QT)
                qv_hi = (QT[64:128, :, q0:q0 + 128], t_QT)
                nit = (8 * qt + 6) // 128 + 1
                for it in range(nit):
                    m = qt - 16 * it
                    masks = []
                    if m <= 16:
                        masks.append((identb3[:, :], cmask[:, m, :], [t_identb3, t_cmask]))
                    tile_pair((kcT[0:64, it * 128:(it + 1) * 128], t_kcT), qv_lo, masks, (tb_c, t_tbc), qt - 16 * it, (vca[:, it, :], t_vca), 2,
                              it == 0, it == nit - 1, imp_it=(it, nit))
                kts = [kt for kt in range(qt - 4, qt + 1) if kt >= 0]
                for n_, kt in enumerate(kts):
                    masks = []
                    if kt == qt:
                        masks.append((identb3[:, :], caus[:, :], [t_identb3, t_caus]))
                    if kt == qt - 4:
                        masks.append((identb3[:, :], acaus[:, :], [t_identb3, t_acaus]))
                    tile_pair((KK[64:128, kt * 128:(kt + 1) * 128], t_KK), qv_hi, masks, (tb_sw, t_tbsw), qt - kt, (Vw[:, kt, :], t_Vw), 4,
                              n_ == 0, n_ == len(kts) - 1)
                flush_pairs()
                kb.op("dve", lambda e: e.tensor_copy(out=oT[:, 0, :], in_=ps[2][0:65, :]), reads=[pst[2]], writes=[t_oT])
                for h in range(4):
                    kb.op("pe", lambda e, h=h: e.matmul(ps[6][:, h * 65:(h + 1) * 65], lhsT=oT[0:65, 0, h * 128:(h + 1) * 128], rhs=ident[0:65, 0:65], start=True, stop=True),
                          reads=[t_oT, t_ident], writes=[pst[6]], sig=(h == 3))
                kb.op("dve", lambda e: e.tensor_scalar(out=zsc[:, 0:4], in0=ps[6][:, 0:260].rearrange("p (h c) -> p h c", c=65)[:, :, 64], scalar1=1e-30, scalar2=None, op0=ALU.max),
                      reads=[pst[6]], writes=[t_zsc])
                kb.op("dve", lambda e: e.reciprocal(out=zsc[:, 0:4], in_=zsc[:, 0:4]), reads=[t_zsc], writes=[t_zsc])
                kb.op("dve", lambda e: e.tensor_scalar(out=impn[:], in0=ps[5][:, 0:128], scalar1=zsc[:, 0:1], scalar2=None, op0=ALU.mult), reads=[pst[5], t_zsc], writes=[t_impn])
                for h in range(1, 4):
                    kb.op("dve", lambda e, h=h: e.scalar_tensor_tensor(out=impn[:], in0=ps[5][:, h * 128:(h + 1) * 128], scalar=zsc[:, h:h + 1], in1=impn[:], op0=ALU.mult, op1=ALU.add),
                          reads=[pst[5], t_zsc, t_impn], writes=[t_impn])
                if dbg:
                    kb.dma(d_imp[q0:q0 + 128, :], impn[:], reads=[t_impn])
                o0 = 127 - 2 * qt
                kb.op("dve", lambda e, o0=o0: e.tensor_tensor(out=impn[:], in0=impn[:], in1=tkeep[:, o0:o0 + 128], op=ALU.mult), reads=[t_impn, t_tkeep], writes=[t_impn])
                kb.op("dve", lambda e, o0=o0: e.tensor_tensor(out=impn[:], in0=impn[:], in1=tadd[:, o0:o0 + 128], op=ALU.add), reads=[t_impn, t_tadd], writes=[t_impn])
                kb.op("dve", lambda e: e.memset(impn[:, 0:1], 1e4), reads=[t_impn], writes=[t_impn])
                kb.op("dve", lambda e: e.max(out=m8[:, 0:8], in_=impn[:]), reads=[t_impn], writes=[t_m8])
                kb.op("dve", lambda e: e.match_replace(out=impw[:], in_to_replace=m8[:, 0:8], in_values=impn[:], imm_value=-3e38), reads=[t_impn, t_m8], writes=[t_impw])
                kb.op("dve", lambda e: e.max(out=m8[:, 8:16], in_=impw[:]), reads=[t_impw], writes=[t_m8])
                kb.op("dve", lambda e: e.tensor_scalar(out=selb[:], in0=impn[:], scalar1=m8[:, 15:16], scalar2=None, op0=ALU.is_ge), reads=[t_impn, t_m8], writes=[t_selb])
                if dbg:
                    kb.dma(d_sel[q0:q0 + 128, :], selb[:], reads=[t_selb])
                kb.op("pe", lambda e: e.matmul(ps[7][:, 0:128], lhsT=selb[:, :], rhs=identb3[:, :], start=True, stop=True), reads=[t_selb, t_identb3], writes=[pst[7]])
                kb.op("dve", lambda e: e.tensor_scalar(out=selT[:], in0=ps[7][:, 0:128], scalar1=-1.0, scalar2=-NEG, op0=ALU.add, op1=ALU.mult),
                      reads=[pst[7]], writes=[t_selT])
                for kt in range(qt + 1):
                    if kt == qt:
                        masks = [(identb3[:, :], caus[:, :], [t_identb3, t_caus])]
                    else:
                        masks = [(expT[:, kt * 128:(kt + 1) * 128], selT[:, :], [t_expT, t_selT])]
                    tile_pair((KK[0:64, kt * 128:(kt + 1) * 128], t_KK), qv_lo, masks, (tb_sw, t_tbsw), qt - kt, (Vs[:, kt, :], t_Vs), 3, kt == 0, kt == qt)
                flush_pairs()
                kb.op("dve", lambda e: e.tensor_copy(out=oT[:, 1, :], in_=ps[3][0:65, :]), reads=[pst[3]], writes=[t_oT])
                kb.op("dve", lambda e: e.tensor_copy(out=oT[:, 2, :], in_=ps[4][0:65, :]), reads=[pst[4]], writes=[t_oT])
                for br in (1, 2):
                    for h in range(4):
                        col = 260 + ((br - 1) * 4 + h) * 65
                        pbk, cc = (6, col) if col + 65 <= 512 else (7, col - 455 + 128)
                        kb.op("pe", lambda e, h=h, br=br, pbk=pbk, cc=cc: e.matmul(ps[pbk][:, cc:cc + 65], lhsT=oT[0:65, br, h * 128:(h + 1) * 128], rhs=ident[0:65, 0:65], start=True, stop=True),
                              reads=[t_oT, t_ident], writes=[pst[pbk]])

                def oslot(br, h):
                    if br == 0:
                        return 6, h * 65
                    col = 260 + ((br - 1) * 4 + h) * 65
                    return (6, col) if col + 65 <= 512 else (7, col - 455 + 128)
                for br in (1, 2):
                    for h in range(4):
                        pbk, cc = oslot(br, h)
                        kb.op("dve", lambda e, br=br, h=h, pbk=pbk, cc=cc: e.tensor_scalar(out=zsc[:, br * 4 + h:br * 4 + h + 1], in0=ps[pbk][:, cc + 64:cc + 65], scalar1=1e-30, scalar2=None, op0=ALU.max),
                              reads=[pst[pbk]], writes=[t_zsc])
                kb.op("dve", lambda e: e.reciprocal(out=zsc[:, 4:12], in_=zsc[:, 4:12]), reads=[t_zsc], writes=[t_zsc])
                for br in range(3):
                    kb.op("dve", lambda e, br=br, qt=qt: e.tensor_tensor(out=zsc[:, 12 + br * 4:16 + br * 4], in0=zsc[:, br * 4:br * 4 + 4],
                                                                         in1=gts[:, qt, :].rearrange("p (h b) -> p h b", b=3)[:, :, br], op=ALU.mult),
                          reads=[t_zsc, t_gts], writes=[t_zsc])
                for h in range(4):
                    for br in range(3):
                        pbk, cc = oslot(br, h)
                        if br == 0:
                            kb.op("dve", lambda e, h=h, br=br, pbk=pbk, cc=cc: e.tensor_scalar(out=haf[:, h * 64:(h + 1) * 64], in0=ps[pbk][:, cc:cc + 64],
                                                                                               scalar1=zsc[:, 12 + br * 4 + h:13 + br * 4 + h], scalar2=None, op0=ALU.mult),
                                  reads=[pst[pbk], t_zsc], writes=[t_haf])
                        else:
                            kb.op("dve", lambda e, h=h, br=br, pbk=pbk, cc=cc: e.scalar_tensor_tensor(out=haf[:, h * 64:(h + 1) * 64], in0=ps[pbk][:, cc:cc + 64],
                                                                                                      scalar=zsc[:, 12 + br * 4 + h:13 + br * 4 + h], in1=haf[:, h * 64:(h + 1) * 64],
                                                                                                      op0=ALU.mult, op1=ALU.add),
                                  reads=[pst[pbk], t_zsc, t_haf], writes=[t_haf])
                kb.op("dve", lambda e: e.tensor_copy(out=hab[:], in_=haf[:]), reads=[t_haf], writes=[t_hab])
                ob = qt % 2
                for dc in range(2):
                    kb.op("pe", lambda e, dc=dc: e.matmul(ps[5][:, dc * 128:(dc + 1) * 128], lhsT=hab[:, dc * 128:(dc + 1) * 128], rhs=identb3[:, :], start=True, stop=True),
                          reads=[t_hab, t_identb3], writes=[pst[5]], sig=(dc == 1))
                kb.op("dve", lambda e, ob=ob: e.tensor_copy(out=haT[ob][:].rearrange("p a t -> p (a t)"), in_=ps[5][:, 0:256]), reads=[pst[5]], writes=[t_haT[ob]])
                kb.dma(mixT[qt // 8][256:512, (qt % 8) * 128:(qt % 8 + 1) * 128].rearrange("(a p) t -> p a t", p=128), haT[ob][:], reads=[t_haT[ob]], writes=[t_mixT])
            kb.barrier()
    if dbg:
        d_mixT = nc.dram_tensor("d_mixT", [512, S], BF16, kind="ExternalOutput").ap()
        for i in range(8):
            kb.dma(d_mixT[:, i * 1024:(i + 1) * 1024], mixT[i], reads=[t_mixT])


    if phases >= 4:
        selm_in = din("selm", [128, 4])
        x_own = din("x_own", [2048, D])
        w_out_in = din("w_out_p", [D, D])
        g2_row = din("g2_row", [1, D])
        wq_in = din("wq", [D, D])
        skT_in = din("skT", [128, 2, 128])
        uT_in = din("uT", [D, 16384])
        v_in = din("v_tab", [16384, D])
        y_out = nc.dram_tensor("y", [2048, D], F32, kind="ExternalOutput").ap()
        mixG = [nc.dram_tensor("mixG%d" % i, [2048, 1024], BF16, kind="Internal").ap() for i in range(8)]; t_mixG = Trk()
        s_x1 = dscr("s_x1", [2048, D]); t_sx1 = Trk()
        s_h2T = dscr("s_h2T", [D, 2048], BF16); t_sh2T = Trk()
        kb.barrier()
        for i in range(8):
            kb.op("pool", lambda e, i=i: e.collective_compute("AllGather", ALU.bypass, replica_groups=[[0, 1, 2, 3], [4, 5, 6, 7]],
                                                              ins=[mixT[i]], outs=[mixG[i]]), reads=[t_mixT], writes=[t_mixG])
        with ExitStack() as p45:
            def sb45(name, shape, dt=F32):
                return p45.enter_context(nc.sbuf_tensor(name, list(shape), dt))
            g2tb = sb45("g2tb", [128, D]); t_g2tb = Trk()
            identb4 = sb45("identb4", [128, 128], BF16); t_identb4 = Trk()
            kb.dma(identb4[:], din("ident_b2", [128, 128], BF16), writes=[t_identb4])
            with ExitStack() as p4:
                def sb4(name, shape, dt=F32):
                    return p4.enter_context(nc.sbuf_tensor(name, list(shape), dt))
                rowst = sb4("rowst", [1, D]); t_rowst = Trk()
                g1b = sb4("g1b", [128, D]); t_g1b = Trk()
                sh2b = sb4("sh2b", [128, D]); t_sh2b = Trk()
                gs2b = sb4("gs2b", [128, D]); t_gs2b = Trk()
                selm = sb4("selm_sb", [128, 4]); t_selm = Trk()
                kb.dma(selm[:], selm_in, writes=[t_selm])

                def bcast_row(src_ap, dst, t_dst, src_reads=()):
                    kb.dma(rowst[:], src_ap, reads=list(src_reads), writes=[t_rowst])
                    for n in range(4):
                        kb.op("pe", lambda e, n=n: e.matmul(ps[n][:, :], lhsT=ones_f[0:1, :], rhs=rowst[0:1, n * 512:(n + 1) * 512], start=True, stop=True),
                              reads=[t_ones_f, t_rowst], writes=[pst[n]])
                        kb.op("dve", lambda e, n=n: e.tensor_copy(out=dst[:, n * 512:(n + 1) * 512], in_=ps[n][:, :]), reads=[pst[n]], writes=[t_dst])
                bcast_row(s_mod[0:1, 2 * D:3 * D], g1b, t_g1b, [t_smod])
                bcast_row(s_mod[0:1, 3 * D:4 * D], sh2b, t_sh2b, [t_smod])
                bcast_row(s_mod[0:1, 5 * D:6 * D], g2tb, t_g2tb, [t_smod])
                bcast_row(s_mod[0:1, 4 * D:5 * D], gs2b, t_gs2b, [t_smod])
                kb.dma(rowst[:], g2_row, writes=[t_rowst])
                for n in range(4):
                    kb.op("pe", lambda e, n=n: e.matmul(ps[n][:, :], lhsT=ones_f[0:1, :], rhs=rowst[0:1, n * 512:(n + 1) * 512], start=True, stop=True),
                          reads=[t_ones_f, t_rowst], writes=[pst[n]])
                    kb.op("dve", lambda e, n=n: e.scalar_tensor_tensor(out=gs2b[:, n * 512:(n + 1) * 512], in0=gs2b[:, n * 512:(n + 1) * 512], scalar=1.0, in1=ps[n][:, :],
                                                                       op0=ALU.add, op1=ALU.mult), reads=[pst[n], t_gs2b], writes=[t_gs2b])
                wo = sb4("wo", [128, 16, D], BF16); t_wo = Trk()
                for fc in range(16):
                    kb.dma(wo[:, fc, :], w_out_in[fc * 128:(fc + 1) * 128, :], writes=[t_wo], q="pool")
                sl = [sb4("sl%d" % i, [128, 16, 128], BF16) for i in range(4)]; t_sl = [Trk() for _ in range(4)]
                mixo = sb4("mixo", [128, 16, 128], BF16); t_mixo = Trk()
                xin = [sb4("xin%d" % i, [128, D]) for i in range(2)]; t_xin = [Trk(), Trk()]
                tmpy = sb4("tmpy", [128, D]); t_tmpy = Trk()
                h2b = sb4("h2b", [128, D], BF16); t_h2b = Trk()
                h2Tt = [sb4("h2Tt%d" % i, [128, 16, 128], BF16) for i in range(2)]; t_h2Tt = [Trk(), Trk()]
                nsc = sb4("nsc", [128, 4]); t_nsc = Trk()
                mgv = [mg.rearrange("(f p) t -> p f t", p=128) for mg in mixG]
                for tt in range(16):
                    bi = tt % 2
                    kb.dma(xin[bi][:], x_own[tt * 128:(tt + 1) * 128, :], writes=[t_xin[bi]])
                    for s_ in range(4):
                        kb.dma(sl[s_][:], mgv[2 * s_ + tt // 8][:, :, (tt % 8) * 128:(tt % 8 + 1) * 128], reads=[t_mixG], writes=[t_sl[s_]])
                    kb.op("dve", lambda e: e.tensor_scalar(out=mixo[:], in0=sl[0][:], scalar1=selm[:, 0:1], scalar2=None, op0=ALU.mult), reads=[t_sl[0], t_selm], writes=[t_mixo])
                    for s_ in range(1, 4):
                        kb.op("dve", lambda e, s_=s_: e.scalar_tensor_tensor(out=mixo[:], in0=sl[s_][:], scalar=selm[:, s_:s_ + 1], in1=mixo[:], op0=ALU.mult, op1=ALU.add),
                              reads=[t_sl[s_], t_selm, t_mixo], writes=[t_mixo])
                    for n in range(4):
                        for fc in range(16):
                            kb.op("pe", lambda e, n=n, fc=fc: e.matmul(ps[n][:, :], lhsT=mixo[:, fc, :], rhs=wo[:, fc, n * 512:(n + 1) * 512], start=(fc == 0), stop=(fc == 15)),
                                  reads=[t_mixo, t_wo], writes=[pst[n]], sig=(fc == 15))
                        kb.op("dve", lambda e, n=n: e.tensor_tensor(out=tmpy[:, n * 512:(n + 1) * 512], in0=ps[n][:, :], in1=g1b[:, n * 512:(n + 1) * 512], op=ALU.mult),
                              reads=[pst[n], t_g1b], writes=[t_tmpy])
                        kb.op("dve", lambda e, n=n, bi=bi: e.tensor_tensor(out=xin[bi][:, n * 512:(n + 1) * 512], in0=xin[bi][:, n * 512:(n + 1) * 512], in1=tmpy[:, n * 512:(n + 1) * 512], op=ALU.add),
                              reads=[t_xin[bi], t_tmpy], writes=[t_xin[bi]])
                    kb.dma(s_x1[tt * 128:(tt + 1) * 128, :], xin[bi][:], reads=[t_xin[bi]], writes=[t_sx1])
                    kb.op("act", lambda e, bi=bi: e.activation(out=tmpy[:], in_=xin[bi][:], func=AF.Square, accum_out=nsc[:, 0:1]), reads=[t_xin[bi]], writes=[t_tmpy, t_nsc])
                    kb.op("act", lambda e: e.activation(out=nsc[:, 1:2], in_=nsc[:, 0:1], func=AF.Sqrt, scale=1.0 / D, bias=EPS), reads=[t_nsc], writes=[t_nsc])
                    kb.op("dve", lambda e: e.reciprocal(out=nsc[:, 2:3], in_=nsc[:, 1:2]), reads=[t_nsc], writes=[t_nsc])
                    kb.op("dve", lambda e, bi=bi: e.scalar_tensor_tensor(out=tmpy[:], in0=xin[bi][:], scalar=nsc[:, 2:3], in1=gs2b[:], op0=ALU.mult, op1=ALU.mult),
                          reads=[t_xin[bi], t_nsc, t_gs2b], writes=[t_tmpy])
                    kb.op("dve", lambda e: e.tensor_tensor(out=h2b[:], in0=tmpy[:], in1=sh2b[:], op=ALU.add), reads=[t_tmpy, t_sh2b], writes=[t_h2b])
                    for qd in range(4):
                        pb = 4 + qd
                        for k in range(4):
                            dc = qd * 4 + k
                            kb.op("pe", lambda e, dc=dc, k=k, pb=pb: e.matmul(ps[pb][:, k * 128:(k + 1) * 128], lhsT=h2b[:, dc * 128:(dc + 1) * 128], rhs=identb4[:, :], start=True, stop=True),
                                  reads=[t_h2b, t_identb4], writes=[pst[pb]], sig=(k == 3))
                        kb.op("act", lambda e, qd=qd, pb=pb, bi=bi: e.activation(out=h2Tt[bi][:, qd * 4:(qd + 1) * 4, :].rearrange("p a t -> p (a t)"), in_=ps[pb][:, :], func=AF.Copy),
                              reads=[pst[pb]], writes=[t_h2Tt[bi]])
                    kb.dma(s_h2T[:, tt * 128:(tt + 1) * 128].rearrange("(a p) t -> p a t", p=128), h2Tt[bi][:], reads=[t_h2Tt[bi]], writes=[t_sh2T])
                kb.barrier()

            if phases >= 5:
                with ExitStack() as p5:
                    def sb5(name, shape, dt=F32):
                        return p5.enter_context(nc.sbuf_tensor(name, list(shape), dt))
                    skT = sb5("skT_sb", [128, 2, 128], BF16); t_skT = Trk()
                    kb.dma(skT[:], skT_in, writes=[t_skT], q="pool")
                    h2g = sb5("h2g", [128, 16, 512], BF16); t_h2g = Trk()
                    s1m = sb5("s1m", [128, 4, 8, 128]); t_s1m = Trk()
                    s2t = sb5("s2t", [128, 4, 8, 128]); t_s2t = Trk()
                    tau = sb5("tau", [128, 4, 8]); t_tau = Trk()
                    Ysb = sb5("Ysb", [128, 4, D]); t_Ysb = [Trk() for _ in range(4)]
                    wqs = [sb5("wqs%d" % i, [128, 16, 128], BF16) for i in range(2)]; t_wqs = [Trk(), Trk()]
                    sct = sb5("sct", [128, 16, 128]); t_sct = Trk()
                    wk = sb5("wk", [128, 256]); t_wk = Trk()
                    tv = sb5("tv", [128, 16, 16]); t_tv = Trk()
                    cand = sb5("cand", [128, 8, 256]); t_cand = Trk()
                    cw2 = sb5("cw2", [128, 256]); t_cw2 = Trk()
                    m24 = sb5("m24", [128, 8, 24]); t_m24 = Trk()
                    hs = sb5("hs", [128, 8, 8]); t_hs = Trk()
                    ex = sb5("ex", [128, 8, 256]); t_ex = Trk()
                    Ub = [sb5("Ub%d" % i, [128, 16, 256], BF16) for i in range(2)]; t_Ub = [Trk(), Trk()]
                    Vb = [sb5("Vb%d" % i, [128, 2, D], BF16) for i in range(2)]; t_Vb = [Trk(), Trk()]
                    Lt = [sb5("Lt0", [128, 8, 256]), cand]; t_Lt = [Trk(), t_cand]
                    Et = [sb5("Et%d" % i, [128, 8, 256], BF16) for i in range(2)]; t_Et = [Trk(), Trk()]
                    Wt = [sb5("Wt%d" % i, [128, 8, 256], BF16) for i in range(2)]; t_Wt = [[Trk() for _ in range(8)] for _ in range(2)]
                    glT = [sb5("glT%d" % i, [128, 2, 512], BF16) for i in range(2)]; t_glT = [Trk(), Trk()]
                    GT = [sb5("GT%d" % i, [128, 2, 128], BF16) for i in range(2)]; t_GT = [Trk(), Trk()]
                    x1t = Lt[0][:].rearrange("p h e -> p (h e)"); t_x1t = t_Lt[0]
                    h2v = s_h2T.rearrange("(a p) t -> p a t", p=128)
                    wqv = wq_in.rearrange("(a p) n -> p a n", p=128)
                    uTv = uT_in.rearrange("(a p) e -> p a e", p=128)
                    vv_ = v_in.rearrange("(c p) d -> p c d", p=128)
                    ngrp = int(os.environ.get("NGRP", "4"))
                    net = int(os.environ.get("NET", "64"))
                    for g in range(ngrp):
                        kb.dma(h2g[:, 0:8, :], h2v[:, 0:8, g * 512:(g + 1) * 512], reads=[t_sh2T], writes=[t_h2g])
                        kb.dma(h2g[:, 8:16, :], h2v[:, 8:16, g * 512:(g + 1) * 512], reads=[t_sh2T], writes=[t_h2g])
                        for tl in range(4):
                            pass
                        qall = p5.enter_context(nc.sbuf_tensor("qall%d" % g, [128, 16, 512], BF16)) if g == 0 else qall
                        t_qall = Trk() if g == 0 else t_qall
                        for ch in range(16):
                            wi = ch % 2
                            kb.dma(wqs[wi][:], wqv[:, :, ch * 128:(ch + 1) * 128], writes=[t_wqs[wi]], q="pool")
                            pb = ch % 2
                            for dc in range(16):
                                kb.op("pe", lambda e, dc=dc, wi=wi, pb=pb: e.matmul(ps[pb][:, :], lhsT=wqs[wi][:, dc, :], rhs=h2g[:, dc, :], start=(dc == 0), stop=(dc == 15)),
                                      reads=[t_wqs[wi], t_h2g], writes=[pst[pb]], sig=(dc == 15))
                            kb.op("act", lambda e, ch=ch, pb=pb: e.activation(out=qall[:, ch, :], in_=ps[pb][:, :], func=AF.Copy), reads=[pst[pb]], writes=[t_qall])
                        for tl in range(4):
                            for qd in range(4):
                                pb = 2 + (qd % 2)
                                for k in range(4):
                                    ch = qd * 4 + k
                                    kb.op("pe", lambda e, ch=ch, k=k, pb=pb, tl=tl: e.matmul(ps[pb][:, k * 128:(k + 1) * 128], lhsT=qall[:, ch, tl * 128:(tl + 1) * 128], rhs=skT[:, ch % 2, :],
                                                                                             start=True, stop=True),
                                          reads=[t_qall, t_skT], writes=[pst[pb]], sig=(k == 3))
                                kb.op("dve", lambda e, qd=qd, pb=pb: e.tensor_copy(out=sct[:, qd * 4:(qd + 1) * 4, :].rearrange("p a k -> p (a k)"), in_=ps[pb][:, :]),
                                      reads=[pst[pb]], writes=[t_sct])
                            for ch in range(16):
                                kb.op("dve", lambda e, ch=ch: e.max(out=tv[:, ch, 0:8], in_=sct[:, ch, :]), reads=[t_sct], writes=[t_tv])
                                kb.op("dve", lambda e, ch=ch: e.match_replace(out=wk[:, 0:128], in_to_replace=tv[:, ch, 0:8], in_values=sct[:, ch, :], imm_value=-3e38),
                                      reads=[t_sct, t_tv], writes=[t_wk])
                                kb.op("dve", lambda e, ch=ch: e.max(out=tv[:, ch, 8:16], in_=wk[:, 0:128]), reads=[t_wk], writes=[t_tv])
                            tvv = tv[:].rearrange("p (h two) a -> p h two a", two=2)
                            kb.op("dve", lambda e: e.tensor_tensor(out=cand[:].rearrange("p h (a b) -> p h a b", b=16),
                                                                   in0=tvv[:, :, 0, :].unsqueeze(3).broadcast_to([128, 8, 16, 16]),
                                                                   in1=tvv[:, :, 1, :].unsqueeze(2).broadcast_to([128, 8, 16, 16]), op=ALU.add),
                                  reads=[t_tv], writes=[t_cand])
                            for h in range(8):
                                kb.op("dve", lambda e, h=h: e.max(out=m24[:, h, 0:8], in_=cand[:, h, :]), reads=[t_cand], writes=[t_m24])
                                kb.op("dve", lambda e, h=h: e.match_replace(out=cw2[:], in_to_replace=m24[:, h, 0:8], in_values=cand[:, h, :], imm_value=-3e38),
                                      reads=[t_cand, t_m24], writes=[t_cw2])
                                kb.op("dve", lambda e, h=h: e.max(out=m24[:, h, 8:16], in_=cw2[:]), reads=[t_cw2], writes=[t_m24])
                                kb.op("dve", lambda e, h=h: e.match_replace(out=cw2[:], in_to_replace=m24[:, h, 8:16], in_values=cw2[:], imm_value=-3e38),
                                      reads=[t_cw2, t_m24], writes=[t_cw2])
                                kb.op("dve", lambda e, h=h: e.max(out=m24[:, h, 16:24], in_=cw2[:]), reads=[t_cw2], writes=[t_m24])
                            kb.op("dve", lambda e: e.tensor_copy(out=hs[:, :, 0], in_=m24[:, :, 0]), reads=[t_m24], writes=[t_hs])
                            kb.op("dve", lambda e: e.tensor_tensor(out=hs[:, :, 1], in0=m24[:, :, 15], in1=m24[:, :, 16], op=ALU.add), reads=[t_m24], writes=[t_hs])
                            kb.op("dve", lambda e: e.tensor_scalar(out=hs[:, :, 1], in0=hs[:, :, 1], scalar1=0.5, scalar2=None, op0=ALU.mult), reads=[t_hs], writes=[t_hs])
                            kb.op("dve", lambda e: e.tensor_tensor(out=ex[:], in0=cand[:], in1=hs[:, :, 0:1].broadcast_to([128, 8, 256]), op=ALU.subtract), reads=[t_cand, t_hs], writes=[t_ex])
                            kb.op("act", lambda e: e.activation(out=ex[:], in_=ex[:], func=AF.Exp), reads=[t_ex], writes=[t_ex])
                            kb.op("dve", lambda e: e.tensor_tensor(out=cand[:], in0=cand[:], in1=hs[:, :, 1:2].broadcast_to([128, 8, 256]), op=ALU.is_ge), reads=[t_cand, t_hs], writes=[t_cand])
                            kb.op("dve", lambda e: e.tensor_tensor(out=ex[:], in0=ex[:], in1=cand[:], op=ALU.mult), reads=[t_cand, t_ex], writes=[t_ex])
                            kb.op("dve", lambda e: e.tensor_reduce(out=hs[:, :, 2], in_=ex[:], axis=AX.X, op=ALU.add), reads=[t_ex], writes=[t_hs])
                            kb.op("act", lambda e: e.activation(out=hs[:, :, 3], in_=hs[:, :, 2], func=AF.Ln), reads=[t_hs], writes=[t_hs])
                            kb.op("dve", lambda e: e.tensor_tensor(out=hs[:, :, 4], in0=hs[:, :, 0], in1=hs[:, :, 3], op=ALU.add), reads=[t_hs], writes=[t_hs])
                            sv = sct[:].rearrange("p (h two) k -> p h two k", two=2)
                            kb.op("dve", lambda e, tl=tl: e.tensor_tensor(out=s1m[:, tl, :, :], in0=sv[:, :, 0, :], in1=hs[:, :, 4:5].broadcast_to([128, 8, 128]), op=ALU.subtract),
                                  reads=[t_sct, t_hs], writes=[t_s1m])
                            kb.op("dve", lambda e, tl=tl: e.tensor_copy(out=s2t[:, tl, :, :], in_=sv[:, :, 1, :]), reads=[t_sct], writes=[t_s2t])
                            kb.op("dve", lambda e, tl=tl: e.tensor_tensor(out=tau[:, tl, :], in0=hs[:, :, 1], in1=hs[:, :, 4], op=ALU.subtract), reads=[t_hs], writes=[t_tau])
                        kb.op("dve", lambda e: e.memset(Ysb[:], 0.0), writes=t_Ysb)

                        def load_u(et):
                            bi = et % 2
                            kb.dma(Ub[bi][:, 0:8, :], uTv[:, 0:8, et * 256:(et + 1) * 256], writes=[t_Ub[bi]], q="pool")
                            kb.dma(Ub[bi][:, 8:16, :], uTv[:, 8:16, et * 256:(et + 1) * 256], writes=[t_Ub[bi]], q="pool")

                        def load_v(et):
                            bi = et % 2
                            kb.dma(Vb[bi][:], vv_[:, et * 2:et * 2 + 2, :], writes=[t_Vb[bi]], q="pool")

                        def emit_a(et):
                            bi = et % 2
                            for ec in range(2):
                                for dc in range(16):
                                    kb.op("pe", lambda e, dc=dc, ec=ec, bi=bi: e.matmul(ps[ec][:, :], lhsT=Ub[bi][:, dc, ec * 128:(ec + 1) * 128], rhs=h2g[:, dc, :],
                                                                                        start=(dc == 0), stop=(dc == 15)),
                                          reads=[t_Ub[bi], t_h2g], writes=[pst[ec]], sig=(dc == 15))
                                kb.op("act", lambda e, ec=ec, bi=bi: e.activation(out=glT[bi][:, ec, :], in_=ps[ec][:, :], func=AF.Gelu_apprx_tanh),
                                      reads=[pst[ec]], writes=[t_glT[bi]])
                        load_u(0)
                        if net > 1:
                            load_u(1)
                        load_v(0)
                        emit_a(0)
                        pairs = [(et, tl) for et in range(net) for tl in range(4)]
                        NP = len(pairs)

                        def st_L(n):
                            et, tl = pairs[n]; k = n % 2
                            kb.op("dve", lambda e: e.tensor_tensor(out=Lt[k][:].rearrange("p h (a k) -> p h a k", k=128),
                                                                   in0=s1m[:, tl, :, 2 * et:2 * et + 2].unsqueeze(3).broadcast_to([128, 8, 2, 128]),
                                                                   in1=s2t[:, tl, :, :].unsqueeze(2).broadcast_to([128, 8, 2, 128]), op=ALU.add),
                                  reads=[t_s1m, t_s2t], writes=[t_Lt[k]])
                            kb.op("act", lambda e: e.activation(out=Et[k][:], in_=Lt[k][:], func=AF.Exp), reads=[t_Lt[k]], writes=[t_Et[k]])

                        def st_C(n):
                            et, tl = pairs[n]; k = n % 2
                            for h in range(8):
                                kb.op("dve", lambda e, h=h: e.scalar_tensor_tensor(out=Wt[k][:, h, :], in0=Lt[k][:, h, :], scalar=tau[:, tl, h:h + 1], in1=Et[k][:, h, :],
                                                                                   op0=ALU.is_ge, op1=ALU.mult),
                                      reads=[t_Lt[k], t_tau, t_Et[k]], writes=[t_Wt[k][h]])
                            pw = 2 + k
                            for ec in range(2):
                                for h in range(8):
                                    kb.op("pe", lambda e, ec=ec, h=h: e.matmul(ps[pw][:, ec * 128:(ec + 1) * 128], lhsT=Wt[k][:, h, ec * 128:(ec + 1) * 128], rhs=identb4[:, :],
                                                                               start=(h == 0), stop=(h == 7)),
                                          reads=[t_Wt[k][h], t_identb4], writes=[pst[pw]], sig=(ec == 1 and h == 7))

                        def st_G(n):
                            et, tl = pairs[n]; k = n % 2; bi = et % 2; pw = 2 + k
                            if tl == 0:
                                if et + 1 < net:
                                    load_v(et + 1)
                                    emit_a(et + 1)
                                if et + 2 < net:
                                    load_u(et + 2)
                            kb.op("dve", lambda e: e.tensor_tensor(out=GT[k][:], in0=glT[bi][:, :, tl * 128:(tl + 1) * 128],
                                                                   in1=ps[pw][:, 0:256].rearrange("p (a t) -> p a t", t=128), op=ALU.mult),
                                  reads=[t_glT[bi], pst[pw]], writes=[t_GT[k]])
                            for nn in range(4):
                                pb = 4 + nn
                                for ec in range(2):
                                    kb.op("pe", lambda e, ec=ec, nn=nn, pb=pb: e.matmul(ps[pb][:, :], lhsT=GT[k][:, ec, :], rhs=Vb[bi][:, ec, nn * 512:(nn + 1) * 512],
                                                                                        start=(ec == 0), stop=(ec == 1)),
                                          reads=[t_GT[k], t_Vb[bi]], writes=[pst[pb]], sig=(ec == 1))

                        def st_Y(n):
                            et, tl = pairs[n]
                            kb.op("dve", lambda e: e.tensor_tensor(out=Ysb[:, tl, :], in0=Ysb[:, tl, :], in1=psY[:, :], op=ALU.add),
                                  reads=[t_Ysb[tl], pst[4], pst[5], pst[6], pst[7]], writes=[t_Ysb[tl]])
                        for n in range(-2, NP):
                            if 0 <= n:
                                st_G(n)
                            if 0 <= n + 2 < NP:
                                st_L(n + 2)
                            if 0 <= n + 1 < NP:
                                st_C(n + 1)
                            if 0 <= n:
                                st_Y(n)
                        for tl in range(4):
                            r0 = (g * 4 + tl) * 128
                            kb.dma(x1t, s_x1[r0:r0 + 128, :], reads=[t_sx1], writes=[t_x1t])
                            kb.op("dve", lambda e, tl=tl: e.tensor_tensor(out=Ysb[:, tl, :], in0=Ysb[:, tl, :], in1=g2tb[:], op=ALU.mult), reads=[t_Ysb[tl], t_g2tb], writes=[t_Ysb[tl]])
                            kb.op("dve", lambda e, tl=tl: e.tensor_tensor(out=x1t, in0=x1t, in1=Ysb[:, tl, :], op=ALU.add), reads=[t_Ysb[tl], t_x1t], writes=[t_x1t])
                            kb.dma(y_out[r0:r0 + 128, :], x1t, reads=[t_x1t])
                    kb.barrier()

    for _i in range(int(os.environ.get('EXTRA', '0'))):
        kb.dma(s_mod[0:1, 0:128], ident[0:1, :], reads=[t_ident])
    kb.barrier()
    ok, stuck, sems = kb.check()
    if not ok or os.environ.get('KBV'):
        print('SYNC CHECK ok=%s' % ok, stuck, {k: v for k, v in kb.cnt.items()})
    assert ok, 'sync deadlock'
    es.close()
    return nc


def _consts():
    ones = np.ones((128, 128), np.float32)
    blk = np.zeros((128, 128), np.float32)
    blk[:64, :64] = 1.0
    blk[64:, 64:] = 1.0
    ut = np.triu(np.ones((128, 128), np.float32))
    NEG = -30000.0
    il = np.arange(128)
    cmask = np.zeros((128, 17, 128), np.float32)
    for m in range(17):
        ok = (il[None, :] + 128 * m) >= (16 * il[:, None] + 31)
        cmask[:, m, :] = np.where(ok, 0.0, NEG)
    caus = np.where(il[:, None] <= il[None, :], 0.0, NEG).astype(np.float32)
    acaus = np.where(il[:, None] > il[None, :], 0.0, NEG).astype(np.float32)
    i_abs = (np.arange(4)[None, :, None] * 128 + il[:, None, None])
    blkk = np.arange(128)[None, None, :]
    ovl = ((16 * i_abs < 64 * blkk + 64) & (16 * i_abs + 32 > 64 * blkk)).astype(np.float32)
    expT = (np.arange(S)[None, :] // 64 == il[:, None]).astype(np.float32)
    o = np.arange(255)[None, :] - 127
    cur_rel = (il[:, None] >= 64).astype(np.int64)
    rel = o - cur_rel
    t_keep = (rel < -1).astype(np.float32)
    t_add = np.where(rel > 0, -1e4, np.where(rel >= -1, 1e4, 0.0)).astype(np.float32)
    return {"ones_bf": _bf(ones), "blk64_bf": _bf(blk), "ident_f": np.eye(128, dtype=np.float32),
            "ut_f": ut, "ident_b": _bf(np.eye(128, dtype=np.float32)),
            "cmask": _bf(cmask), "caus_add": _bf(caus), "acaus_add": _bf(acaus), "ovl": _bf(ovl), "expT": _bf(expT),
            "t_keep": np.ascontiguousarray(np.broadcast_to(t_keep, (128, 255))).astype(np.float32), "t_add": t_add}


def make_in_maps(inp):
    f = lambda a: np.ascontiguousarray(np.asarray(a, dtype=np.float32))
    x = f(inp["x"]); c = f(inp["c"])
    w_in = f(inp["w_in"])[0]
    conv = f(inp["conv_qk"])[0]
    qn_g = f(inp["qn_g"])[0]; kn_g = f(inp["kn_g"])[0]
    mng = f(inp["mlstm_norm_g"])[0]
    g1 = f(inp["norm1_g"])[0]
    cst = _consts()
    maps = []
    xTs = [np.ascontiguousarray(x[b].T) for b in range(2)]
    w_out = f(inp["w_out"])[0]
    rows = []
    for fc in range(16):
        r, part, half = fc // 4, (fc % 4) // 2, fc % 2
        base = part * 1024 + r * 256 + half * 128
        rows += list(range(base, base + 128))
    w_out_p = np.ascontiguousarray(w_out[rows])
    wq = f(inp["peer_wq"])[0]
    skT = np.ascontiguousarray(f(inp["peer_subkeys"])[0].transpose(2, 0, 1))
    uT = np.ascontiguousarray(f(inp["peer_u"])[0].T)
    vtab = f(inp["peer_v"])[0]
    for core in range(8):
        b, j = divmod(core, 4)
        cols = []
        cols += list(range(j * 256, j * 256 + 256))
        cols += list(range(1024 + j * 256, 1024 + j * 256 + 256))
        AQ = 4096 + 8
        cols += list(range(AQ + j * 256, AQ + j * 256 + 256))
        AKV = AQ + 1024
        for br in (0, 1, 2, 4):
            cols += list(range(AKV + br * 256 + j * 64, AKV + br * 256 + j * 64 + 64))
        cols += [4096 + j, 4096 + 4 + j]
        cols += list(range(2048 + j * 256, 2048 + j * 256 + 256))
        cols += list(range(3072 + j * 256, 3072 + j * 256 + 256))
        for br in (3, 5):
            cols += list(range(AKV + br * 256 + j * 64, AKV + br * 256 + j * 64 + 64))
        AG = AKV + 1536
        cols += list(range(AG + j * 12, AG + j * 12 + 12))
        assert len(cols) == 1678
        wj = np.zeros((D, 1680), np.float32)
        wj[:, :1678] = w_in[:, cols]
        cwt = np.zeros((128, 4, 4), np.float32)
        for t in range(4):
            base = (j * 256 + (t % 2) * 128) if t < 2 else (1024 + j * 256 + (t % 2) * 128)
            cwt[:, t, :] = conv[:, base:base + 128].T
        qkg = np.zeros((128, 4), np.float32)
        qkg[:, 0] = np.tile(qn_g, 2) * 0.125
        qkg[:, 3] = np.concatenate([kn_g[1], kn_g[2]])
        m = {
            "xT": xTs[b], "c_col": np.ascontiguousarray(c[b].reshape(16, 128).T),
            "w_mod": f(inp["w_mod"])[0], "b_mod": f(inp["b_mod"]),
            "g1_col": np.ascontiguousarray(g1.reshape(16, 128).T),
            "w_in": wj, "convw": cwt, "qk_g": qkg,
            "mng_row": np.ascontiguousarray(mng[j * 256:(j + 1) * 256][None, :]),
            "bif": np.ascontiguousarray(np.tile(np.array([[f(inp["b_igate"])[0, j], f(inp["b_fgate"])[0, j]]], np.float32), (128, 1))),
        }
        slopes = np.array([2.0 ** (-8.0 * (4 * j + hh + 1) / 16) for hh in range(4)], np.float64)
        kl = np.arange(128, dtype=np.float64)
        dl = np.arange(64, dtype=np.float64)
        m["tb_sw"] = (slopes[None, None, :] * (kl[:, None, None] - 64 - 128 * dl[None, :, None])).astype(np.float32)
        m["tb_c"] = (slopes[None, None, :] * (16 * kl[:, None, None] - 33 - 128 * dl[None, :, None])).astype(np.float32)
        m["w1k"] = f(inp["cmp_k_w1"])[0]; m["w1v"] = f(inp["cmp_v_w1"])[0]
        m["w2k"] = f(inp["cmp_k_w2"])[0]; m["w2v"] = f(inp["cmp_v_w2"])[0]
        for nm, key in (("posk", "cmp_pos_k"), ("posv", "cmp_pos_v")):
            pos = f(inp[key])[0]
            m[nm] = np.ascontiguousarray(pos.reshape(16, 2, 64).transpose(1, 2, 0).reshape(128, 16))
        m["kng0"] = np.ascontiguousarray(kn_g[0][:, None])
        sm = np.zeros((128, 4), np.float32); sm[:, j] = 1.0
        m["selm"] = sm
        m["x_own"] = np.ascontiguousarray(x[b, j * 2048:(j + 1) * 2048])
        m["w_out_p"] = w_out_p
        m["g2_row"] = f(inp["norm2_g"])
        m["wq"] = wq
        m["skT"] = skT
        m["uT"] = uT
        m["v_tab"] = vtab
        m["ident_b2"] = cst["ident_b"]
        m.update(cst)
        maps.append(m)
    return maps


def kernel(**inputs):
    nc = build()
    maps = make_in_maps(inputs)
    res = run_bass_kernel_spmd(nc, maps, core_ids=list(range(8)))
    out = np.zeros((2, S, D), np.float32)
    for core in range(8):
        b, j = divmod(core, 4)
        out[b, j * 2048:(j + 1) * 2048] = res.results[core]["y"]
    return out
```

```python
import numpy as np
from contextlib import ExitStack
import concourse.bass as bass
import concourse.mybir as mybir
from concourse.bass_utils import run_bass_kernel_spmd

F32 = mybir.dt.float32
BF16 = mybir.dt.bfloat16
AF = mybir.ActivationFunctionType
ALU = mybir.AluOpType
AX = mybir.AxisListType

D = 2048
S = 8192
NB = 16
EPS = 1e-6
import os
NDS = int(os.environ.get("NDS", "24"))


class Trk:
    __slots__ = ("w", "r")

    def __init__(self):
        self.w = None
        self.r = {}


class KB:
    def __init__(self, nc, es):
        self.nc = nc
        self.eng = {"pe": nc.tensor, "act": nc.scalar, "dve": nc.vector, "pool": nc.gpsimd, "sp": nc.sync}
        self.sem = {k: es.enter_context(nc.semaphore("s_" + k)) for k in self.eng}
        self.cnt = {k: 0 for k in self.eng}
        self.seen = {k: {} for k in self.eng}
        self.dsem = [es.enter_context(nc.semaphore("dq%d" % i)) for i in range(NDS)]
        self.duse = [0] * NDS
        self.dnext = 0
        self.ccsem = es.enter_context(nc.semaphore("ccs"))
        self.log = {k: [] for k in self.eng}

    def _wait(self, e, tok):
        if tok is None:
            return
        key, sem, val = tok
        if self.seen[e].get(key, 0) >= val:
            return
        self.eng[e].wait_ge(sem, val)
        self.log[e].append(("w", key, val))
        self.seen[e][key] = val

    def _deps(self, e, reads, writes):
        for b in reads:
            if b.w is not None and not (e == "pe" and b.w[0] == "pe"):
                self._wait(e, b.w)
        for b in writes:
            if b.w is not None and not (e == "pe" and b.w[0] == "pe"):
                self._wait(e, b.w)
            for t in b.r.values():
                if not (e == "pe" and t[0] == "pe"):
                    self._wait(e, t)

    def _mark(self, tok, reads, writes):
        for b in reads:
            b.r[tok[0]] = tok
        for b in writes:
            b.w = tok
            b.r = {}

    def op(self, e, fn, reads=(), writes=(), sig=True):
        self._deps(e, reads, writes)
        inst = fn(self.eng[e])
        if sig:
            self.cnt[e] += 1
            inst.then_inc(self.sem[e], 1)
            self.log[e].append(("i", e, 1))
            tok = (e, self.sem[e], self.cnt[e])
        else:
            tok = (e, self.sem[e], self.cnt[e] + 1)
        self._mark(tok, reads, writes)
        return tok

    def dma(self, out, in_, reads=(), writes=(), q="sp", **kw):
        i = self.dnext
        self.dnext = (i + 1) % NDS
        if self.duse[i] > 0:
            self._wait(q, (("d", i), self.dsem[i], 16 * self.duse[i]))
        self._deps(q, reads, writes)
        inst = self.eng[q].dma_start(out=out, in_=in_, **kw)
        self.duse[i] += 1
        inst.then_inc(self.dsem[i], 16)
        self.log[q].append(("i", ("d", i), 16))
        tok = (("d", i), self.dsem[i], 16 * self.duse[i])
        self._mark(tok, reads, writes)
        return tok

    def check(self):
        sems = {}
        pc = {k: 0 for k in self.log}
        prog = True
        while prog:
            prog = False
            for e, lg in self.log.items():
                while pc[e] < len(lg):
                    kind, key, val = lg[pc[e]]
                    if kind == "w":
                        if sems.get(key, 0) < val:
                            break
                    else:
                        sems[key] = sems.get(key, 0) + val
                    pc[e] += 1
                    prog = True
        stuck = {e: (pc[e], len(lg), lg[pc[e]] if pc[e] < len(lg) else None) for e, lg in self.log.items()}
        ok = all(pc[e] == len(lg) for e, lg in self.log.items())
        return ok, stuck, sems

    def barrier(self):
        for e in self.eng:
            for e2 in self.eng:
                if e2 != e and self.cnt[e2] > 0:
                    self._wait(e, (e2, self.sem[e2], self.cnt[e2]))
            for i in range(NDS):
                if self.duse[i] > 0:
                    self._wait(e, (("d", i), self.dsem[i], 16 * self.duse[i]))


def _bf(a):
    import ml_dtypes
    return np.asarray(a, dtype=np.float32).astype(ml_dtypes.bfloat16)


def build(dbg=False, phases=99, nb=NB, parts='abcde'):
    nc = bass.Bass("TRN2", target_bir_lowering=False)
    es = ExitStack()
    kb = KB(nc, es)

    def din(name, shape, dt=F32):
        return nc.dram_tensor(name, list(shape), dt, kind="ExternalInput").ap()

    def dscr(name, shape, dt=F32):
        return nc.dram_tensor(name, list(shape), dt, kind=("ExternalOutput" if dbg else "Internal")).ap()

    xT = din("xT", [D, S])
    c_col = din("c_col", [128, 16])
    w_mod = din("w_mod", [D, 6 * D])
    b_mod = din("b_mod", [1, 6 * D])
    g1_col = din("g1_col", [128, 16])
    w_in = din("w_in", [D, 1680])
    convw = din("convw", [128, 4, 4])
    qk_g = din("qk_g", [128, 4])
    mng_row = din("mng_row", [1, 256])
    ones_bf = din("ones_bf", [128, 128], BF16)
    blk64_bf = din("blk64_bf", [128, 128], BF16)
    ident_f = din("ident_f", [128, 128])

    s_q = dscr("s_q", [256, S])
    s_k = dscr("s_k", [256, S])
    s_aq = dscr("s_aq", [256, S], BF16)
    s_kv = dscr("s_kv", [128, S])
    s_kk = dscr("s_kk", [128, S], BF16)
    s_if = dscr("s_if", [2, S])
    s_v = dscr("s_v", [S, 256])
    s_go = dscr("s_go", [S, 256])
    s_vv = dscr("s_vv", [S, 128], BF16)
    s_gt = dscr("s_gt", [S, 12])

    ps = []
    pst = []
    for i in range(4):
        ps.append(es.enter_context(nc.psum_tensor("ps%d" % i, [128, 512], F32)))
        pst.append(Trk())
    psY = es.enter_context(nc.psum_tensor("psY", [128, 2048], F32))
    for i in range(4):
        ps.append(psY[:, i * 512:(i + 1) * 512])
        pst.append(Trk())

    def sb(name, shape, dt=F32):
        return es.enter_context(nc.sbuf_tensor(name, list(shape), dt))

    ones_b = sb("ones_b", [128, 128], BF16); t_ones_b = Trk()
    blk64 = sb("blk64", [128, 128], BF16); t_blk64 = Trk()
    ident = sb("ident", [128, 128]); t_ident = Trk()
    ones_f = sb("ones_f", [128, 128]); t_ones_f = Trk()
    kb.dma(ones_b[:], ones_bf, writes=[t_ones_b])
    kb.dma(blk64[:], blk64_bf, writes=[t_blk64])
    kb.dma(ident[:], ident_f, writes=[t_ident])
    kb.op("dve", lambda e: e.memset(ones_f[:], 1.0), writes=[t_ones_f])

    s_mod = dscr("s_mod", [1, 6 * D]); t_smod = Trk()
    csil = sb("csil", [128, 16]); t_csil = Trk()
    ccol = sb("ccol", [128, 16]); t_ccol = Trk()
    g1c = sb("g1c", [128, 16]); t_g1c = Trk()
    gs1 = sb("gs1", [128, 16]); t_gs1 = Trk()
    sh1 = sb("sh1", [128, 16]); t_sh1 = Trk()
    kb.dma(ccol[:], c_col, writes=[t_ccol])
    kb.dma(g1c[:], g1_col, writes=[t_g1c])
    kb.op("act", lambda e: e.activation(out=csil[:], in_=ccol[:], func=AF.Silu), reads=[t_ccol], writes=[t_csil])
    with ExitStack() as p0:
        wm = [p0.enter_context(nc.sbuf_tensor("wm%d" % i, [128, 16, 512], F32)) for i in range(2)]
        modrow = p0.enter_context(nc.sbuf_tensor("modrow", [1, 6 * D], F32)); t_modrow = Trk()
        bmod_sb = p0.enter_context(nc.sbuf_tensor("bmod_sb", [1, 6 * D], F32)); t_bmod = Trk()
        kb.dma(bmod_sb[:], b_mod, writes=[t_bmod])
        t_wm = [Trk(), Trk()]
        wmv = w_mod.rearrange("(k p) n -> p k n", p=128)
        for n in range(24):
            bi = n % 2
            kb.dma(wm[bi][:, 0:8, :], wmv[:, 0:8, n * 512:(n + 1) * 512], writes=[t_wm[bi]])
            kb.dma(wm[bi][:, 8:16, :], wmv[:, 8:16, n * 512:(n + 1) * 512], writes=[t_wm[bi]])
            pb = n % 2
            for k in range(16):
                kb.op("pe", lambda e, k=k, bi=bi, pb=pb: e.matmul(ps[pb][0:1, :], lhsT=csil[:, k:k + 1], rhs=wm[bi][:, k, :],
                                                                  start=(k == 0), stop=(k == 15)),
                      reads=[t_csil, t_wm[bi]], writes=[pst[pb]], sig=(k == 15))
            kb.op("dve", lambda e, n=n, pb=pb: e.tensor_tensor(out=modrow[0:1, n * 512:(n + 1) * 512], in0=ps[pb][0:1, :],
                                                               in1=bmod_sb[0:1, n * 512:(n + 1) * 512], op=ALU.add),
                  reads=[pst[pb], t_bmod], writes=[t_modrow])
        for which, dst, t_dst in ((0, sh1, t_sh1), (1, gs1, t_gs1)):
            for k in range(16):
                off = which * D + k * 128
                kb.op("pe", lambda e, off=off, k=k: e.matmul(ps[2][:, k:k + 1], lhsT=modrow[0:1, off:off + 128], rhs=ones_f[0:1, 0:1],
                                                             start=True, stop=True),
                      reads=[t_modrow, t_ones_f], writes=[pst[2]], sig=(k == 15))
            if which == 0:
                kb.op("dve", lambda e: e.tensor_copy(out=sh1[:], in_=ps[2][:, 0:16]), reads=[pst[2]], writes=[t_sh1])
            else:
                kb.op("dve", lambda e: e.scalar_tensor_tensor(out=gs1[:], in0=ps[2][:, 0:16], scalar=1.0, in1=g1c[:],
                                                              op0=ALU.add, op1=ALU.mult),
                      reads=[pst[2], t_g1c], writes=[t_gs1])
        kb.dma(s_mod, modrow[:], reads=[t_modrow], writes=[t_smod])
        kb.barrier()

    t_scr = {n: Trk() for n in ("q", "k", "aq", "kv", "kk", "if", "v", "go", "vv", "gt")}
    if phases >= 1:
        with ExitStack() as p1:
            def sb1(name, shape, dt=F32):
                return p1.enter_context(nc.sbuf_tensor(name, list(shape), dt))
            wb = sb1("wb", [128, 16, 1680], BF16); t_wb = Trk()
            cw = sb1("cw", [128, 4, 4]); t_cw = Trk()
            qkg = sb1("qkg", [128, 4]); t_qkg = Trk()
            mng = sb1("mng", [128, 256]); t_mng = Trk()
            mngr = sb1("mngr", [1, 256]); t_mngr = Trk()
            kb.dma(cw[:], convw, writes=[t_cw])
            kb.dma(qkg[:], qk_g, writes=[t_qkg])
            kb.dma(mngr[:], mng_row, writes=[t_mngr])
            kb.op("pe", lambda e: e.matmul(ps[3][:, 0:256], lhsT=ones_f[0:1, :], rhs=mngr[0:1, :], start=True, stop=True),
                  reads=[t_ones_f, t_mngr], writes=[pst[3]])
            kb.op("dve", lambda e: e.tensor_copy(out=mng[:], in_=ps[3][:, 0:256]), reads=[pst[3]], writes=[t_mng])
            wst = [sb1("wst%d" % i, [128, 1680]) for i in range(2)]; t_wst = [Trk(), Trk()]
            wiv = w_in.rearrange("(k p) n -> p k n", p=128)
            for k in range(16):
                bi = k % 2
                kb.dma(wst[bi][:], wiv[:, k, :], writes=[t_wst[bi]])
                kb.op("dve", lambda e, k=k, bi=bi: e.tensor_copy(out=wb[:, k, :], in_=wst[bi][:]),
                      reads=[t_wst[bi]], writes=[t_wb])
            xt = [sb1("xt%d" % i, [128, 16, 512]) for i in range(2)]; t_xt = [Trk(), Trk()]
            xsq = sb1("xsq", [128, 16, 512], BF16); t_xsq = [Trk() for _ in range(16)]
            hT = sb1("hT", [128, 16, 512], BF16); t_hT = [Trk() for _ in range(16)]
            rstd = sb1("rstd", [128, 512]); t_rstd = Trk()
            tmp = [sb1("tmp%d" % i, [128, 512]) for i in range(2)]; t_tmp = [Trk(), Trk()]
            zr = [sb1("zr%d" % i, [128, 3 + 512]) for i in range(4)]; t_zr = [Trk() for _ in range(4)]
            acc = [sb1("acc%d" % i, [128, 512]) for i in range(2)]; t_acc = [Trk(), Trk()]
            ofm = [sb1("ofm%d" % i, [128, 512]) for i in range(2)]; t_ofm = [Trk(), Trk()]
            obf = [sb1("obf%d" % i, [128, 512], BF16) for i in range(2)]; t_obf = [Trk(), Trk()]
            sqb = sb1("sqb", [128, 512], BF16); t_sqb = Trk()
            rs2 = sb1("rs2", [128, 512]); t_rs2 = Trk()
            otm = [sb1("otm%d" % i, [128, 512]) for i in range(2)]; t_otm = [Trk(), Trk()]
            otb = [sb1("otb%d" % i, [128, 128], BF16) for i in range(2)]; t_otb = [Trk(), Trk()]
            otg = [sb1("otg%d" % i, [128, 12]) for i in range(2)]; t_otg = [Trk(), Trk()]
            for i in range(4):
                kb.op("dve", lambda e, i=i: e.memset(zr[i][:, 0:3], 0.0), writes=[t_zr[i]])
            xv = xT.rearrange("(k p) t -> p k t", p=128)
            cnt2 = [0]

            def rot2():
                cnt2[0] += 1
                return cnt2[0] % 2

            def load_x(n):
                bi = n % 2
                for h in range(4):
                    kb.dma(xt[bi][:, 4 * h:4 * h + 4, :], xv[:, 4 * h:4 * h + 4, n * 512:(n + 1) * 512], writes=[t_xt[bi]])

            load_x(0)
            for n in range(nb):
                bi = n % 2
                t0 = n * 512
                if n + 1 < nb:
                    load_x(n + 1)
                for k in range(16):
                    kb.op("act" if k % 2 else "dve",
                          (lambda e, k=k, bi=bi: e.activation(out=xsq[:, k, :], in_=xt[bi][:, k, :], func=AF.Square)) if k % 2 else
                          (lambda e, k=k, bi=bi: e.tensor_tensor(out=xsq[:, k, :], in0=xt[bi][:, k, :], in1=xt[bi][:, k, :], op=ALU.mult)),
                          reads=[t_xt[bi]], writes=[t_xsq[k]])
                for k in range(16):
                    kb.op("pe", lambda e, k=k: e.matmul(ps[0][:, :], lhsT=ones_b[:], rhs=xsq[:, k, :], start=(k == 0), stop=(k == 15)),
                          reads=[t_ones_b, t_xsq[k]], writes=[pst[0]], sig=(k == 15))
                kb.op("act", lambda e: e.activation(out=rstd[:], in_=ps[0][:, :], func=AF.Sqrt, scale=1.0 / D, bias=EPS),
                      reads=[pst[0]], writes=[t_rstd])
                kb.op("dve", lambda e: e.reciprocal(out=rstd[:], in_=rstd[:]), reads=[t_rstd], writes=[t_rstd])
                for k in range(16):
                    tb = k % 2
                    kb.op("dve", lambda e, k=k, tb=tb, bi=bi: e.tensor_tensor(out=tmp[tb][:], in0=xt[bi][:, k, :], in1=rstd[:], op=ALU.mult),
                          reads=[t_xt[bi], t_rstd], writes=[t_tmp[tb]])
                    kb.op("act", lambda e, k=k, tb=tb: e.activation(out=hT[:, k, :], in_=tmp[tb][:], func=AF.Identity,
                                                                    scale=gs1[:, k:k + 1], bias=sh1[:, k:k + 1]),
                          reads=[t_tmp[tb], t_gs1, t_sh1], writes=[t_hT[k]])
                for ct in range(9):
                    if not ({0: 'a', 1: 'a', 2: 'a', 3: 'a', 4: 'b', 5: 'b', 7: 'b', 6: 'c', 8: 'd'}[ct] in parts):
                        continue
                    c0 = ct * 128
                    cn = 128 if ct < 8 else 2
                    pb = 1 + (ct % 2)
                    for k in range(16):
                        kb.op("pe", lambda e, k=k, c0=c0, cn=cn, pb=pb: e.matmul(ps[pb][0:cn, :], lhsT=wb[:, k, c0:c0 + cn], rhs=hT[:, k, :],
                                                                                 start=(k == 0), stop=(k == 15)),
                              reads=[t_wb, t_hT[k]], writes=[pst[pb]], sig=(k == 15))
                    if ct < 4:
                        z = zr[ct]; tz = t_zr[ct]
                        kb.op("act", lambda e, z=z, pb=pb: e.activation(out=z[:, 3:515], in_=ps[pb][:, :], func=AF.Copy),
                              reads=[pst[pb]], writes=[tz])
                        ab = rot2()
                        a = acc[ab]; ta = t_acc[ab]
                        kb.op("dve", lambda e, z=z, a=a, ct=ct: e.tensor_scalar(out=a[:], in0=z[:, 3:515], scalar1=cw[:, ct, 3:4], scalar2=None,
                                                                                 op0=ALU.mult), reads=[tz, t_cw], writes=[ta])
                        for j in range(3):
                            kb.op("dve", lambda e, z=z, a=a, ct=ct, j=j: e.scalar_tensor_tensor(out=a[:], in0=z[:, j:j + 512], scalar=cw[:, ct, j:j + 1],
                                                                                               in1=a[:], op0=ALU.mult, op1=ALU.add),
                                  reads=[tz, t_cw, ta], writes=[ta])
                        ob = rot2()
                        kb.op("act", lambda e, a=a, ob=ob: e.activation(out=ofm[ob][:], in_=a[:], func=AF.Silu), reads=[ta], writes=[t_ofm[ob]])
                        dst = s_q if ct < 2 else s_k
                        r0 = (ct % 2) * 128
                        kb.dma(dst[r0:r0 + 128, t0:t0 + 512], ofm[ob][:], reads=[t_ofm[ob]], writes=[t_scr["q" if ct < 2 else "k"]])
                        kb.op("dve", lambda e, z=z: e.tensor_copy(out=z[:, 0:3], in_=z[:, 512:515]), reads=[tz], writes=[tz])
                    elif ct in (4, 5, 7):
                        kb.op("act", lambda e, pb=pb: e.activation(out=sqb[:], in_=ps[pb][:, :], func=AF.Square), reads=[pst[pb]], writes=[t_sqb])
                        kb.op("pe", lambda e: e.matmul(ps[3][:, :], lhsT=blk64[:], rhs=sqb[:], start=True, stop=True),
                              reads=[t_blk64, t_sqb], writes=[pst[3]])
                        kb.op("act", lambda e: e.activation(out=rs2[:], in_=ps[3][:, :], func=AF.Sqrt, scale=1.0 / 64, bias=EPS),
                              reads=[pst[3]], writes=[t_rs2])
                        kb.op("dve", lambda e: e.reciprocal(out=rs2[:], in_=rs2[:]), reads=[t_rs2], writes=[t_rs2])
                        ob = rot2()
                        gcol = 0 if ct in (4, 5) else 3
                        kb.op("dve", lambda e, pb=pb, ob=ob, gcol=gcol: e.scalar_tensor_tensor(out=obf[ob][:], in0=ps[pb][:, :], scalar=qkg[:, gcol:gcol + 1],
                                                                                              in1=rs2[:], op0=ALU.mult, op1=ALU.mult),
                              reads=[pst[pb], t_rs2, t_qkg], writes=[t_obf[ob]])
                        if ct == 7:
                            kb.dma(s_kk[:, t0:t0 + 512], obf[ob][:], reads=[t_obf[ob]], writes=[t_scr["kk"]])
                        else:
                            r0 = (ct - 4) * 128
                            kb.dma(s_aq[r0:r0 + 128, t0:t0 + 512], obf[ob][:], reads=[t_obf[ob]], writes=[t_scr["aq"]])
                    elif ct == 6:
                        ob = rot2()
                        kb.op("act", lambda e, pb=pb, ob=ob: e.activation(out=ofm[ob][:], in_=ps[pb][:, :], func=AF.Copy), reads=[pst[pb]], writes=[t_ofm[ob]])
                        kb.dma(s_kv[:, t0:t0 + 512], ofm[ob][:], reads=[t_ofm[ob]], writes=[t_scr["kv"]])
                    else:
                        ob = rot2()
                        kb.op("act", lambda e, pb=pb, ob=ob: e.activation(out=ofm[ob][0:2, :], in_=ps[pb][0:2, :], func=AF.Copy), reads=[pst[pb]], writes=[t_ofm[ob]])
                        kb.dma(s_if[:, t0:t0 + 512], ofm[ob][0:2, :], reads=[t_ofm[ob]], writes=[t_scr["if"]])
                for ts in range(int(os.environ.get('NTS', '4')) if 'e' in parts else 0):
                    tt = t0 + ts * 128
                    pb = 4 + (ts % 2)
                    for k in range(16):
                        kb.op("pe", lambda e, k=k, ts=ts, pb=pb: e.matmul(ps[pb][:, :], lhsT=hT[:, k, ts * 128:(ts + 1) * 128], rhs=wb[:, k, 1026:1538],
                                                                          start=(k == 0), stop=(k == 15)),
                              reads=[t_wb, t_hT[k]], writes=[pst[pb]], sig=(k == 15))
                    pb2 = 6 + (ts % 2)
                    for k in range(16):
                        kb.op("pe", lambda e, k=k, ts=ts, pb2=pb2: e.matmul(ps[pb2][:, 0:140], lhsT=hT[:, k, ts * 128:(ts + 1) * 128], rhs=wb[:, k, 1538:1678],
                                                                            start=(k == 0), stop=(k == 15)),
                              reads=[t_wb, t_hT[k]], writes=[pst[pb2]], sig=(k == 15))
                    ob = ts % 2
                    kb.op("dve", lambda e, pb=pb, ob=ob: e.tensor_copy(out=otm[ob][:, 0:256], in_=ps[pb][:, 0:256]), reads=[pst[pb]], writes=[t_otm[ob]])
                    kb.op("act", lambda e, pb=pb, ob=ob: e.activation(out=otm[ob][:, 256:512], in_=ps[pb][:, 256:512], func=AF.Sigmoid),
                          reads=[pst[pb]], writes=[t_otm[ob]])
                    SK = os.environ.get('SKIP', '')
                    if 'P' not in SK:
                        kb.op("dve", lambda e, ob=ob: e.tensor_tensor(out=otm[ob][:, 256:512], in0=otm[ob][:, 256:512], in1=mng[:], op=ALU.mult),
                              reads=[t_otm[ob], t_mng], writes=[t_otm[ob]])
                    if 'S' not in SK:
                        kb.dma(s_v[tt:tt + 128, :], otm[ob][:, 0:256], reads=[t_otm[ob]], writes=[t_scr["v"]])
                        kb.dma(s_go[tt:tt + 128, :], otm[ob][:, 256:512], reads=[t_otm[ob]], writes=[t_scr["go"]])
                    kb.op("dve", lambda e, pb2=pb2, ob=ob: e.tensor_copy(out=otb[ob][:], in_=ps[pb2][:, 0:128]), reads=[pst[pb2]], writes=[t_otb[ob]])
                    kb.op("act", lambda e, pb2=pb2, ob=ob: e.activation(out=otg[ob][:], in_=ps[pb2][:, 128:140], func=AF.Sigmoid),
                          reads=[pst[pb2]], writes=[t_otg[ob]])
                    if 'V' not in os.environ.get('SKIP', ''):
                        kb.dma(s_vv[tt:tt + 128, :], otb[ob][:], reads=[t_otb[ob]], writes=[t_scr["vv"]])
                    if 'G' not in os.environ.get('SKIP', ''):
                        kb.dma(s_gt[tt:tt + 128, :], otg[ob][:], reads=[t_otg[ob]], writes=[t_scr["gt"]])
            kb.barrier()


    mixT = [nc.dram_tensor("mixT%d" % i, [512, 1024], BF16, kind="Internal").ap() for i in range(8)]; t_mixT = Trk()
    if phases >= 2:
        bif_in = din("bif", [128, 2])
        ut_in = din("ut_f", [128, 128])
        identb_in = din("ident_b", [128, 128], BF16)
        with ExitStack() as p2:
            def sb2(name, shape, dt=F32):
                return p2.enter_context(nc.sbuf_tensor(name, list(shape), dt))
            NCH = 64
            bif = sb2("bif_sb", [128, 2]); t_bif = Trk()
            ut = sb2("ut_sb", [128, 128]); t_ut = Trk()
            identb = sb2("identb", [128, 128], BF16); t_identb = Trk()
            kb.dma(bif[:], bif_in, writes=[t_bif])
            kb.dma(ut[:], ut_in, writes=[t_ut])
            kb.dma(identb[:], identb_in, writes=[t_identb])
            rows = sb2("rows", [64, 2, 128]); t_rows = Trk()
            kb.dma(rows[:, 0, :], s_if[0:1, :].rearrange("o (c p) -> (o c) p", p=128), reads=[t_scr["if"]], writes=[t_rows])
            kb.dma(rows[:, 1, :], s_if[1:2, :].rearrange("o (c p) -> (o c) p", p=128), reads=[t_scr["if"]], writes=[t_rows])
            cols = sb2("cols", [128, 12, 64]); t_cols = Trk()
            nbf = sb2("nbf", [128, 1]); t_nbf = Trk()
            zero64 = sb2("zero64", [128, 128]); t_zero = Trk()
            kb.op("dve", lambda e: e.memset(zero64[:], 0.0), writes=[t_zero])
            kb.op("dve", lambda e: e.tensor_scalar(out=nbf[:], in0=bif[:, 1:2], scalar1=-1.0, scalar2=None, op0=ALU.mult), reads=[t_bif], writes=[t_nbf])
            for w in range(2):
                kb.op("pe", lambda e, w=w: e.matmul(ps[w][:, 0:64], lhsT=rows[:, w, :], rhs=ident[0:64, 0:64], start=True, stop=True),
                      reads=[t_rows, t_ident], writes=[pst[w]])
            kb.op("dve", lambda e: e.tensor_scalar(out=cols[:, 0, :], in0=ps[0][:, 0:64], scalar1=bif[:, 0:1], scalar2=None, op0=ALU.add),
                  reads=[pst[0], t_bif], writes=[t_cols])
            kb.op("act", lambda e: e.activation(out=cols[:, 11, :], in_=ps[1][:, 0:64], func=AF.Exp, scale=-1.0, bias=nbf[:, 0:1]),
                  reads=[pst[1], t_nbf], writes=[t_cols])
            kb.op("act", lambda e: e.activation(out=cols[:, 1, :], in_=cols[:, 11, :], func=AF.Ln, scale=1.0, bias=1.0), reads=[t_cols], writes=[t_cols])
            kb.op("dve", lambda e: e.tensor_scalar(out=cols[:, 1, :], in0=cols[:, 1, :], scalar1=-1.0, scalar2=None, op0=ALU.mult), reads=[t_cols], writes=[t_cols])
            kb.op("pe", lambda e: e.matmul(ps[2][:, 0:64], lhsT=ut[:], rhs=cols[:, 1, :], start=True, stop=True), reads=[t_ut, t_cols], writes=[pst[2]])
            kb.op("pe", lambda e: e.matmul(ps[3][:, 0:64], lhsT=ones_f[:], rhs=cols[:, 1, :], start=True, stop=True), reads=[t_ones_f, t_cols], writes=[pst[3]])
            kb.op("dve", lambda e: e.tensor_copy(out=cols[:, 11, :], in_=ps[3][:, 0:64]), reads=[pst[3]], writes=[t_cols])
            kb.op("dve", lambda e: e.tensor_tensor_scan(out=cols[:, 2, :], data0=cols[:, 11, :], data1=zero64[:, 0:64], initial=0.0, op0=ALU.add, op1=ALU.add),
                  reads=[t_cols, t_zero], writes=[t_cols])
            kb.op("dve", lambda e: e.tensor_tensor(out=cols[:, 2, :], in0=cols[:, 2, :], in1=cols[:, 11, :], op=ALU.subtract), reads=[t_cols], writes=[t_cols])
            kb.op("dve", lambda e: e.tensor_tensor(out=cols[:, 2, :], in0=cols[:, 2, :], in1=ps[2][:, 0:64], op=ALU.add), reads=[t_cols, pst[2]], writes=[t_cols])
            kb.op("dve", lambda e: e.tensor_tensor(out=cols[:, 3, :], in0=cols[:, 0, :], in1=cols[:, 2, :], op=ALU.subtract), reads=[t_cols], writes=[t_cols])
            grow = sb2("grow", [64, 128]); t_grow = Trk()
            cmrow = sb2("cmrow", [64, 128]); t_cmrow = Trk()
            kb.op("pe", lambda e: e.matmul(ps[4][0:64, 0:128], lhsT=cols[:, 3, :], rhs=ident[:, :], start=True, stop=True), reads=[t_cols, t_ident], writes=[pst[4]])
            kb.op("dve", lambda e: e.tensor_copy(out=grow[:], in_=ps[4][0:64, 0:128]), reads=[pst[4]], writes=[t_grow])
            kb.op("dve", lambda e: e.tensor_tensor_scan(out=cmrow[:], data0=grow[:], data1=grow[:], initial=-1e30, op0=ALU.max, op1=ALU.max),
                  reads=[t_grow], writes=[t_cmrow])
            mrow = sb2("mrow", [1, 3, 64]); t_mrow = Trk()
            kb.op("pe", lambda e: e.matmul(ps[5][0:1, 0:64], lhsT=cmrow[:, 127:128], rhs=ident[0:64, 0:64], start=True, stop=True),
                  reads=[t_cmrow, t_ident], writes=[pst[5]])
            kb.op("dve", lambda e: e.tensor_copy(out=mrow[0:1, 0, :], in_=ps[5][0:1, 0:64]), reads=[pst[5]], writes=[t_mrow])
            kb.op("dve", lambda e: e.tensor_tensor_scan(out=mrow[0:1, 1, :], data0=mrow[0:1, 0, :], data1=mrow[0:1, 0, :], initial=0.0, op0=ALU.max, op1=ALU.max),
                  reads=[t_mrow], writes=[t_mrow])
            kb.op("dve", lambda e: e.memset(mrow[0:1, 2, 0:1], 0.0), reads=[t_mrow], writes=[t_mrow])
            kb.op("dve", lambda e: e.tensor_copy(out=mrow[0:1, 2, 1:64], in_=mrow[0:1, 1, 0:63]), reads=[t_mrow], writes=[t_mrow])
            kb.op("pe", lambda e: e.matmul(ps[6][:, 0:128], lhsT=ones_f[0:1, :], rhs=mrow[0:1, 1:3, :].rearrange("o a c -> o (a c)"), start=True, stop=True),
                  reads=[t_ones_f, t_mrow], writes=[pst[6]])
            kb.op("dve", lambda e: e.tensor_copy(out=cols[:, 6, :], in_=ps[6][:, 0:64]), reads=[pst[6]], writes=[t_cols])
            kb.op("dve", lambda e: e.tensor_copy(out=cols[:, 5, :], in_=ps[6][:, 64:128]), reads=[pst[6]], writes=[t_cols])
            kb.op("pe", lambda e: e.matmul(ps[7][:, 0:64], lhsT=cmrow[:, :], rhs=ident[0:64, 0:64], start=True, stop=True), reads=[t_cmrow, t_ident], writes=[pst[7]])
            kb.op("dve", lambda e: e.tensor_tensor(out=cols[:, 4, :], in0=ps[7][:, 0:64], in1=cols[:, 5, :], op=ALU.max), reads=[pst[7], t_cols], writes=[t_cols])
            kb.op("dve", lambda e: e.tensor_tensor(out=cols[:, 11, :], in0=cols[:, 3, :], in1=cols[:, 5, :], op=ALU.subtract), reads=[t_cols], writes=[t_cols])
            kb.op("act", lambda e: e.activation(out=cols[:, 7, :], in_=cols[:, 11, :], func=AF.Exp), reads=[t_cols], writes=[t_cols])
            kb.op("dve", lambda e: e.tensor_scalar(out=cols[:, 7, :], in0=cols[:, 7, :], scalar1=1.0 / 16, scalar2=None, op0=ALU.mult), reads=[t_cols], writes=[t_cols])
            kb.op("dve", lambda e: e.tensor_tensor(out=cols[:, 11, :], in0=cols[:, 5, :], in1=cols[:, 4, :], op=ALU.subtract), reads=[t_cols], writes=[t_cols])
            kb.op("act", lambda e: e.activation(out=cols[:, 8, :], in_=cols[:, 11, :], func=AF.Exp), reads=[t_cols], writes=[t_cols])
            kb.op("dve", lambda e: e.tensor_tensor(out=cols[:, 11, :], in0=cols[:, 2, :], in1=cols[:, 4, :], op=ALU.add), reads=[t_cols], writes=[t_cols])
            kb.op("act", lambda e: e.activation(out=cols[:, 9, :], in_=cols[:, 11, :], func=AF.Exp, scale=-1.0), reads=[t_cols], writes=[t_cols])
            kb.op("dve", lambda e: e.tensor_tensor(out=cols[:, 11, :], in0=cols[:, 5, :], in1=cols[:, 6, :], op=ALU.subtract), reads=[t_cols], writes=[t_cols])
            kb.op("act", lambda e: e.activation(out=cols[:, 10, :], in_=cols[:, 11, :], func=AF.Exp), reads=[t_cols], writes=[t_cols])
            if dbg:
                d_cols = nc.dram_tensor("d_cols", [128, 12, 64], F32, kind="ExternalOutput").ap()
                kb.dma(d_cols, cols[:], reads=[t_cols])

            qT = [sb2("qT%d" % i, [128, 2, 128]) for i in range(2)]; t_qT = [Trk(), Trk()]
            kT = [sb2("kT%d" % i, [128, 2, 128]) for i in range(2)]; t_kT = [Trk(), Trk()]
            va = [sb2("va%d" % i, [128, 257]) for i in range(2)]; t_va = [Trk(), Trk()]
            go = [sb2("go%d" % i, [128, 256]) for i in range(2)]; t_go = [Trk(), Trk()]
            kp = sb2("kp", [128, 256]); t_kp = Trk()
            wT = sb2("wT", [128, 128]); t_wT = Trk()
            St = [sb2("St%d" % i, [128, 2, 257]) for i in range(2)]; t_St = [Trk(), Trk()]
            sc = sb2("sc", [128, 8]); t_sc = Trk()
            junk = sb2("junk", [128, 256]); t_junk = Trk()
            hmf = sb2("hmf", [128, 256], BF16); t_hmf = Trk()
            hmT = [sb2("hmT%d" % i, [128, 2, 128], BF16) for i in range(2)]; t_hmT = [Trk(), Trk()]
            for i in range(2):
                kb.op("dve", lambda e, i=i: e.memset(va[i][:, 256:257], 1.0), writes=[t_va[i]])
            kb.op("dve", lambda e: e.memset(St[0][:], 0.0), writes=[t_St[0]])
            qv = s_q.rearrange("(a p) t -> p a t", p=128)
            kv_ = s_k.rearrange("(a p) t -> p a t", p=128)

            def load_chunk(c):
                bi = c % 2
                kb.dma(qT[bi][:], qv[:, :, c * 128:(c + 1) * 128], reads=[t_scr["q"]], writes=[t_qT[bi]])
                kb.dma(kT[bi][:], kv_[:, :, c * 128:(c + 1) * 128], reads=[t_scr["k"]], writes=[t_kT[bi]])
                kb.dma(va[bi][:, 0:256], s_v[c * 128:(c + 1) * 128, :], reads=[t_scr["v"]], writes=[t_va[bi]])
                kb.dma(go[bi][:], s_go[c * 128:(c + 1) * 128, :], reads=[t_scr["go"]], writes=[t_go[bi]])

            nch = int(os.environ.get("NCH", "64"))
            load_chunk(0)
            for c in range(nch):
                bi = c % 2
                so = St[c % 2]; tso = t_St[c % 2]
                sn = St[(c + 1) % 2]; tsn = t_St[(c + 1) % 2]
                if c + 1 < nch:
                    load_chunk(c + 1)
                for dc in range(2):
                    kb.op("pe", lambda e, dc=dc, bi=bi: e.matmul(ps[0][:, dc * 128:(dc + 1) * 128], lhsT=kT[bi][:, dc, :], rhs=ident[:, :], start=True, stop=True),
                          reads=[t_kT[bi], t_ident], writes=[pst[0]], sig=(dc == 1))
                kb.op("dve", lambda e, c=c: e.tensor_scalar(out=kp[:], in0=ps[0][:, 0:256], scalar1=cols[:, 7, c:c + 1], scalar2=None, op0=ALU.mult),
                      reads=[pst[0], t_cols], writes=[t_kp])
                for dc in range(2):
                    kb.op("pe", lambda e, dc=dc, bi=bi: e.matmul(ps[1][:, 0:128], lhsT=kT[bi][:, dc, :], rhs=qT[bi][:, dc, :], start=(dc == 0), stop=(dc == 1)),
                          reads=[t_kT[bi], t_qT[bi]], writes=[pst[1]], sig=(dc == 1))
                kb.op("dve", lambda e, c=c: e.scalar_tensor_tensor(out=wT[:], in0=ps[1][:, 0:128], scalar=cols[:, 7, c:c + 1], in1=ut[:], op0=ALU.mult, op1=ALU.mult),
                      reads=[pst[1], t_cols, t_ut], writes=[t_wT])
                kb.op("pe", lambda e, bi=bi: e.matmul(ps[2][:, 0:257], lhsT=wT[:], rhs=va[bi][:], start=True, stop=False),
                      reads=[t_wT, t_va[bi]], writes=[pst[2]], sig=False)
                for dc in range(2):
                    kb.op("pe", lambda e, dc=dc, bi=bi, so=so: e.matmul(ps[2][:, 0:257], lhsT=qT[bi][:, dc, :], rhs=so[:, dc, :], start=False, stop=(dc == 1)),
                          reads=[t_qT[bi], tso], writes=[pst[2]], sig=(dc == 1))
                for dc in range(2):
                    pb = 3 + dc
                    kb.op("pe", lambda e, dc=dc, bi=bi, pb=pb: e.matmul(ps[pb][:, 0:257], lhsT=kp[:, dc * 128:(dc + 1) * 128], rhs=va[bi][:], start=True, stop=False),
                          reads=[t_kp, t_va[bi]], writes=[pst[pb]], sig=False)
                    kb.op("pe", lambda e, dc=dc, pb=pb, so=so: e.matmul(ps[pb][:, 0:257], lhsT=ident[:, :], rhs=so[:, dc, :], start=False, stop=True),
                          reads=[t_ident, tso], writes=[pst[pb]])
                    kb.op("act" if dc else "dve",
                          (lambda e, dc=dc, pb=pb, sn=sn, c=c: e.activation(out=sn[:, dc, :], in_=ps[pb][:, 0:257], func=AF.Copy, scale=cols[:, 10, c:c + 1])) if dc else
                          (lambda e, dc=dc, pb=pb, sn=sn, c=c: e.tensor_scalar(out=sn[:, dc, :], in0=ps[pb][:, 0:257], scalar1=cols[:, 10, c:c + 1], scalar2=None, op0=ALU.mult)),
                          reads=[pst[pb], t_cols], writes=[tsn])
                kb.op("act", lambda e, c=c: e.activation(out=sc[:, 0:1], in_=ps[2][:, 256:257], func=AF.Abs, scale=cols[:, 8, c:c + 1]),
                      reads=[pst[2], t_cols], writes=[t_sc])
                kb.op("dve", lambda e, c=c: e.tensor_tensor(out=sc[:, 1:2], in0=sc[:, 0:1], in1=cols[:, 9, c:c + 1], op=ALU.max), reads=[t_sc, t_cols], writes=[t_sc])
                kb.op("dve", lambda e: e.reciprocal(out=sc[:, 2:3], in_=sc[:, 1:2]), reads=[t_sc], writes=[t_sc])
                kb.op("dve", lambda e, c=c: e.tensor_tensor(out=sc[:, 3:4], in0=sc[:, 2:3], in1=cols[:, 8, c:c + 1], op=ALU.mult), reads=[t_sc, t_cols], writes=[t_sc])
                kb.op("act", lambda e: e.activation(out=junk[:], in_=ps[2][:, 0:256], func=AF.Square, accum_out=sc[:, 4:5]), reads=[pst[2]], writes=[t_junk, t_sc])
                kb.op("dve", lambda e: e.scalar_tensor_tensor(out=sc[:, 5:6], in0=sc[:, 3:4], scalar=sc[:, 3:4], in1=sc[:, 4:5], op0=ALU.mult, op1=ALU.mult),
                      reads=[t_sc], writes=[t_sc])
                kb.op("act", lambda e: e.activation(out=sc[:, 5:6], in_=sc[:, 5:6], func=AF.Sqrt, scale=1.0 / 256, bias=EPS), reads=[t_sc], writes=[t_sc])
                kb.op("dve", lambda e: e.reciprocal(out=sc[:, 6:7], in_=sc[:, 5:6]), reads=[t_sc], writes=[t_sc])
                kb.op("dve", lambda e: e.tensor_tensor(out=sc[:, 7:8], in0=sc[:, 6:7], in1=sc[:, 3:4], op=ALU.mult), reads=[t_sc], writes=[t_sc])
                kb.op("dve", lambda e, bi=bi: e.scalar_tensor_tensor(out=hmf[:], in0=ps[2][:, 0:256], scalar=sc[:, 7:8], in1=go[bi][:], op0=ALU.mult, op1=ALU.mult),
                      reads=[pst[2], t_sc, t_go[bi]], writes=[t_hmf])
                ob = c % 2
                for dc in range(2):
                    kb.op("pe", lambda e, dc=dc: e.matmul(ps[5][:, dc * 128:(dc + 1) * 128], lhsT=hmf[:, dc * 128:(dc + 1) * 128], rhs=identb[:, :], start=True, stop=True),
                          reads=[t_hmf, t_identb], writes=[pst[5]], sig=(dc == 1))
                kb.op("act", lambda e, ob=ob: e.activation(out=hmT[ob][:].rearrange("p a t -> p (a t)"), in_=ps[5][:, 0:256], func=AF.Copy), reads=[pst[5]], writes=[t_hmT[ob]])
                kb.dma(mixT[c // 8][0:256, (c % 8) * 128:(c % 8 + 1) * 128].rearrange("(a p) t -> p a t", p=128), hmT[ob][:], reads=[t_hmT[ob]], writes=[t_mixT])
            kb.barrier()

    if phases >= 3:
        tb_sw_in = din("tb_sw", [128, 64, 4])
        tb_c_in = din("tb_c", [128, 64, 4])
        cmask_in = din("cmask", [128, 17, 128], BF16)
        caus_in = din("caus_add", [128, 128], BF16)
        acaus_in = din("acaus_add", [128, 128], BF16)
        ovl_in = din("ovl", [128, 4, 128], BF16)
        expT_in = din("expT", [128, S], BF16)
        tkeep_in = din("t_keep", [128, 255])
        tadd_in = din("t_add", [128, 255])
        w1k_in = din("w1k", [2048, 256]); w1v_in = din("w1v", [2048, 256])
        w2k_in = din("w2k", [256, 64]); w2v_in = din("w2v", [256, 64])
        posk_in = din("posk", [128, 16]); posv_in = din("posv", [128, 16])
        kng0_in = din("kng0", [64, 1])
        with ExitStack() as p3:
            def sb3(name, shape, dt=F32):
                return p3.enter_context(nc.sbuf_tensor(name, list(shape), dt))
            QT = sb3("QT", [128, 4, S], BF16); t_QT = Trk()
            KK = sb3("KK", [128, S], BF16); t_KK = Trk()
            Vs = sb3("Vs", [128, 64, 65], BF16); t_Vs = Trk()
            Vw = sb3("Vw", [128, 64, 65], BF16); t_Vw = Trk()
            expT = sb3("expT_sb", [128, S], BF16); t_expT = Trk()
            tb_sw = sb3("tb_sw_sb", [128, 64, 4]); t_tbsw = Trk()
            tb_c = sb3("tb_c_sb", [128, 64, 4]); t_tbc = Trk()
            cmask = sb3("cmask_sb", [128, 17, 128], BF16); t_cmask = Trk()
            caus = sb3("caus_sb", [128, 4, 128], BF16); t_caus = Trk()
            acaus = sb3("acaus_sb", [128, 4, 128], BF16); t_acaus = Trk()
            ovl = sb3("ovl_sb", [128, 4, 128], BF16); t_ovl = Trk()
            tkeep = sb3("tkeep_sb", [128, 255]); t_tkeep = Trk()
            tadd = sb3("tadd_sb", [128, 255]); t_tadd = Trk()
            gts = sb3("gts", [128, 64, 12]); t_gts = Trk()
            kcT = sb3("kcT", [64, 512], BF16); t_kcT = Trk()
            vca = sb3("vca", [128, 4, 65], BF16); t_vca = Trk()
            identb3 = sb3("identb3", [128, 128], BF16); t_identb3 = Trk()
            aqv = s_aq.rearrange("(h d) t -> d h t", d=64)
            for hh in range(2):
                for h in range(4):
                    kb.dma(QT[hh * 64:(hh + 1) * 64, h, :], aqv[:, h, :], reads=[t_scr["aq"]], writes=[t_QT])
            kb.dma(KK[:, 0:4096], s_kk[:, 0:4096], reads=[t_scr["kk"]], writes=[t_KK])
            kb.dma(KK[:, 4096:S], s_kk[:, 4096:S], reads=[t_scr["kk"]], writes=[t_KK])
            kb.dma(expT[:], expT_in, writes=[t_expT])
            kb.dma(tb_sw[:], tb_sw_in, writes=[t_tbsw]); kb.dma(tb_c[:], tb_c_in, writes=[t_tbc])
            kb.dma(cmask[:], cmask_in, writes=[t_cmask])
            for h4 in range(4):
                kb.dma(caus[:, h4, :], caus_in, writes=[t_caus]); kb.dma(acaus[:, h4, :], acaus_in, writes=[t_acaus])
            kb.dma(ovl[:], ovl_in, writes=[t_ovl]); kb.dma(tkeep[:], tkeep_in, writes=[t_tkeep]); kb.dma(tadd[:], tadd_in, writes=[t_tadd])
            kb.dma(identb3[:], identb_in if phases >= 2 else din("ident_b", [128, 128], BF16), writes=[t_identb3])
            kb.dma(gts[:], s_gt.rearrange("(q p) c -> p q c", p=128), reads=[t_scr["gt"]], writes=[t_gts])
            with ExitStack() as p3a:
                def sb3a(name, shape, dt=F32):
                    return p3a.enter_context(nc.sbuf_tensor(name, list(shape), dt))
                vtmp = sb3a("vtmp", [128, 64, 128], BF16); t_vtmp = Trk()
                kb.dma(vtmp[:], s_vv.rearrange("(q p) c -> p q c", p=128), reads=[t_scr["vv"]], writes=[t_vtmp])
                kb.op("dve", lambda e: e.tensor_copy(out=Vs[:, :, 0:64], in_=vtmp[:, :, 0:64]), reads=[t_vtmp], writes=[t_Vs])
                kb.op("dve", lambda e: e.tensor_copy(out=Vw[:, :, 0:64], in_=vtmp[:, :, 64:128]), reads=[t_vtmp], writes=[t_Vw])
                kb.op("dve", lambda e: e.memset(Vs[:, :, 64:65], 1.0), writes=[t_Vs])
                kb.op("dve", lambda e: e.memset(Vw[:, :, 64:65], 1.0), writes=[t_Vw])
                kb.op("dve", lambda e: e.memset(vca[:, :, 64:65], 1.0), writes=[t_vca])
                w1f = sb3a("w1f", [128, 16, 256]); t_w1f = Trk()
                w1b = sb3a("w1b", [128, 16, 256], BF16); t_w1b = Trk()
                w2f = sb3a("w2f", [128, 2, 64]); t_w2f = Trk()
                w2b = sb3a("w2b", [128, 2, 64], BF16); t_w2b = Trk()
                posf = sb3a("posf", [128, 16]); t_posf = Trk()
                bcol = sb3a("bcol", [128, 2]); t_bcol = Trk()
                kng0 = sb3a("kng0_sb", [64, 1]); t_kng0 = Trk()
                a2f = sb3a("a2f", [128, 2048]); t_a2f = Trk()
                a2b = sb3a("a2b", [128, S], BF16); t_a2b = Trk()
                hid = sb3a("hid", [128, 2, 512], BF16); t_hid = Trk()
                csq = sb3a("csq", [64, 512], BF16); t_csq = Trk()
                crs = sb3a("crs", [64, 512]); t_crs = Trk()
                kb.dma(kng0[:], kng0_in, writes=[t_kng0])
                kb.op("dve", lambda e: e.memset(hid[:], 0.0), writes=[t_hid])
                kb.op("dve", lambda e: e.memset(kcT[:], 0.0), writes=[t_kcT])
                for which in range(2):
                    w1_in = w1k_in if which == 0 else w1v_in
                    w2_in = w2k_in if which == 0 else w2v_in
                    pos_in = posk_in if which == 0 else posv_in
                    r0 = 0 if which == 0 else 64
                    kb.dma(w1f[:, 0:8, :], w1_in.rearrange("(k p) n -> p k n", p=128)[:, 0:8, :], writes=[t_w1f])
                    kb.dma(w1f[:, 8:16, :], w1_in.rearrange("(k p) n -> p k n", p=128)[:, 8:16, :], writes=[t_w1f])
                    kb.dma(w2f[:], w2_in.rearrange("(k p) n -> p k n", p=128), writes=[t_w2f])
                    kb.dma(posf[:], pos_in, writes=[t_posf])
                    kb.op("dve", lambda e: e.tensor_copy(out=w1b[:], in_=w1f[:]), reads=[t_w1f], writes=[t_w1b])
                    kb.op("dve", lambda e: e.tensor_copy(out=w2b[:], in_=w2f[:]), reads=[t_w2f], writes=[t_w2b])
                    for q4 in range(4):
                        c0 = q4 * 2048
                        kb.dma(a2f[0:64, :], s_kv[r0:r0 + 64, c0:c0 + 2048], reads=[t_scr["kv"]], writes=[t_a2f])
                        if q4 < 3:
                            kb.dma(a2f[64:128, :], s_kv[r0:r0 + 64, c0 + 1:c0 + 2049], reads=[t_scr["kv"]], writes=[t_a2f])
                        else:
                            kb.dma(a2f[64:128, 0:2047], s_kv[r0:r0 + 64, c0 + 1:c0 + 2048], reads=[t_scr["kv"]], writes=[t_a2f])
                        kb.op("dve", lambda e, c0=c0: e.tensor_copy(out=a2b[:, c0:c0 + 2048], in_=a2f[:]), reads=[t_a2f], writes=[t_a2b])
                    for hc in range(2):
                        for jj in range(16):
                            kb.op("pe", lambda e, hc=hc, jj=jj: e.matmul(ps[7][:, hc:hc + 1], lhsT=w1f[:, jj, hc * 128:(hc + 1) * 128], rhs=posf[:, jj:jj + 1],
                                                                          start=(jj == 0), stop=(jj == 15)),
                                  reads=[t_w1f, t_posf], writes=[pst[7]], sig=(jj == 15))
                    kb.op("dve", lambda e: e.tensor_copy(out=bcol[:], in_=ps[7][:, 0:2]), reads=[pst[7]], writes=[t_bcol])
                    a2v = a2b[:].rearrange("p (i s) -> p i s", s=16)
                    for hc in range(2):
                        for jj in range(16):
                            j0 = 2 * jj
                            rhs = a2v[:, 0:511, j0] if j0 < 16 else a2v[:, 1:512, j0 - 16]
                            kb.op("pe", lambda e, hc=hc, jj=jj, rhs=rhs: e.matmul(ps[hc][:, 0:511], lhsT=w1b[:, jj, hc * 128:(hc + 1) * 128], rhs=rhs,
                                                                                  start=(jj == 0), stop=(jj == 15)),
                                  reads=[t_w1b, t_a2b], writes=[pst[hc]], sig=(jj == 15))
                        kb.op("act", lambda e, hc=hc: e.activation(out=hid[:, hc, 0:511], in_=ps[hc][:, 0:511], func=AF.Gelu_apprx_tanh, bias=bcol[:, hc:hc + 1]),
                              reads=[pst[hc], t_bcol], writes=[t_hid])
                    if which == 0:
                        for hc in range(2):
                            kb.op("pe", lambda e, hc=hc: e.matmul(ps[2][0:64, 0:511], lhsT=w2b[:, hc, :], rhs=hid[:, hc, 0:511], start=(hc == 0), stop=(hc == 1)),
                                  reads=[t_w2b, t_hid], writes=[pst[2]], sig=(hc == 1))
                        kb.op("act", lambda e: e.activation(out=csq[:, 0:511], in_=ps[2][0:64, 0:511], func=AF.Square), reads=[pst[2]], writes=[t_csq])
                        kb.op("pe", lambda e: e.matmul(ps[3][0:64, 0:511], lhsT=blk64[0:64, 0:64], rhs=csq[:, 0:511], start=True, stop=True),
                              reads=[t_blk64, t_csq], writes=[pst[3]])
                        kb.op("act", lambda e: e.activation(out=crs[:, 0:511], in_=ps[3][0:64, 0:511], func=AF.Sqrt, scale=1.0 / 64, bias=EPS), reads=[pst[3]], writes=[t_crs])
                        kb.op("dve", lambda e: e.reciprocal(out=crs[:, 0:511], in_=crs[:, 0:511]), reads=[t_crs], writes=[t_crs])
                        kb.op("dve", lambda e: e.scalar_tensor_tensor(out=kcT[:, 0:511], in0=ps[2][0:64, 0:511], scalar=kng0[:, 0:1], in1=crs[:, 0:511],
                                                                      op0=ALU.mult, op1=ALU.mult), reads=[pst[2], t_kng0, t_crs], writes=[t_kcT])
                    else:
                        for it in range(4):
                            for hc in range(2):
                                kb.op("pe", lambda e, hc=hc, it=it: e.matmul(ps[4][:, it * 64:(it + 1) * 64], lhsT=hid[:, hc, it * 128:(it + 1) * 128], rhs=w2b[:, hc, :],
                                                                             start=(hc == 0), stop=(hc == 1)),
                                      reads=[t_hid, t_w2b], writes=[pst[4]], sig=(hc == 1 and it == 3))
                        kb.op("dve", lambda e: e.tensor_copy(out=vca[:, :, 0:64], in_=ps[4][:, 0:256].rearrange("p (a d) -> p a d", d=64)), reads=[pst[4]], writes=[t_vca])
                if dbg:
                    d_kcT = nc.dram_tensor("d_kcT", [64, 512], BF16, kind="ExternalOutput").ap()
                    d_vca = nc.dram_tensor("d_vca", [128, 4, 65], BF16, kind="ExternalOutput").ap()
                    kb.dma(d_kcT, kcT[:], reads=[t_kcT]); kb.dma(d_vca, vca[:], reads=[t_vca])
                kb.barrier()

            pT = [sb3("pT%d" % i, [128, 512], BF16) for i in range(3)]; t_pT = [[Trk() for _ in range(4)] for _ in range(3)]
            oT = sb3("oT", [65, 3, 512]); t_oT = Trk()
            zsc = sb3("zsc", [128, 24]); t_zsc = Trk()
            impn = sb3("impn", [128, 128]); t_impn = Trk()
            impw = sb3("impw", [128, 128]); t_impw = Trk()
            m8 = sb3("m8", [128, 16]); t_m8 = Trk()
            selb = sb3("selb", [128, 128], BF16); t_selb = Trk()
            selT = sb3("selT", [128, 4, 128], BF16); t_selT = Trk()
            haf = sb3("haf", [128, 256]); t_haf = Trk()
            hab = sb3("hab", [128, 256], BF16); t_hab = Trk()
            haT = [sb3("haT%d" % i, [128, 2, 128], BF16) for i in range(2)]; t_haT = [Trk(), Trk()]
            if dbg:
                d_imp = nc.dram_tensor("d_imp", [S, 128], F32, kind="ExternalOutput").ap()
                d_sel = nc.dram_tensor("d_sel", [S, 128], BF16, kind="ExternalOutput").ap()
            pcount = [0]
            NEG = -30000.0

            pend = []

            def flush_pairs():
                while pend:
                    pend.pop(0)()

            def tile_pair(lhsK, rhsQ, masks, bias_tab, bidx, Vaug, obank, first, last, imp_it=None):
                i = pcount[0] % 2
                pi = pcount[0] % 3
                pcount[0] += 1
                sps = ps[i]; tsp = pst[i]
                nm = len(masks)
                kb.op("pe", lambda e: e.matmul(sps[:, :], lhsT=lhsK[0], rhs=rhsQ[0], start=True, stop=(nm == 0)),
                      reads=[lhsK[1], rhsQ[1]], writes=[tsp], sig=(nm == 0))
                for mi, (ml, mr, mt) in enumerate(masks):
                    lastm = (mi == nm - 1)
                    if mr.shape[-1] == 512 or len(mr.shape) == 3:
                        kb.op("pe", lambda e, ml=ml, mr=mr, lastm=lastm: e.matmul(sps[:, :], lhsT=ml, rhs=mr, start=False, stop=lastm),
                              reads=mt, writes=[tsp], sig=lastm)
                    else:
                        for h in range(4):
                            lm = (lastm and h == 3)
                            kb.op("pe", lambda e, ml=ml, mr=mr, h=h, lm=lm: e.matmul(sps[:, h * 128:(h + 1) * 128], lhsT=ml, rhs=mr, start=False, stop=lm),
                                  reads=mt, writes=[tsp], sig=lm)
                for h in range(4):
                    kb.op("act", lambda e, h=h: e.activation(out=pT[pi][:, h * 128:(h + 1) * 128], in_=sps[:, h * 128:(h + 1) * 128], func=AF.Exp,
                                                             bias=bias_tab[0][:, bidx, h:h + 1]),
                          reads=[tsp, bias_tab[1]], writes=[t_pT[pi][h]])

                def stage_b():
                    kb.op("pe", lambda e: e.matmul(ps[obank][0:65, :], lhsT=Vaug[0], rhs=pT[pi][:, :], start=first, stop=last),
                          reads=[Vaug[1]] + t_pT[pi], writes=[pst[obank]], sig=last)
                    if imp_it is not None:
                        it, nit = imp_it
                        for h in range(4):
                            kb.op("pe", lambda e, h=h, it=it: e.matmul(ps[5][:, h * 128:(h + 1) * 128], lhsT=pT[pi][:, h * 128:(h + 1) * 128], rhs=ovl[:, it, :],
                                                                       start=(it == 0), stop=(it == nit - 1)),
                                  reads=[t_pT[pi][h], t_ovl], writes=[pst[5]], sig=(it == nit - 1 and h == 3))
                while len(pend) > 1:
                    pend.pop(0)()
                pend.append(stage_b)
                if len(pend) > 1:
                    pend.pop(0)()

            nqt = int(os.environ.get("NQT", "64"))
            for qt in range(nqt):
                q0 = qt * 128
                qv_lo = (QT[0:64, :, q0:q0 + 128], t_QT)
                qv_hi = (QT[64:128, :, q0:q0 + 128], t_QT)
                nit = (8 * qt + 6) // 128 + 1
                for it in range(nit):
                    m = qt - 16 * it
                    masks = []
                    if m <= 16:
                        masks.append((identb3[:, :], cmask[:, m, :], [t_identb3, t_cmask]))
                    tile_pair((kcT[0:64, it * 128:(it + 1) * 128], t_kcT), qv_lo, masks, (tb_c, t_tbc), qt - 16 * it, (vca[:, it, :], t_vca), 2,
                              it == 0, it == nit - 1, imp_it=(it, nit))
                kts = [kt for kt in range(qt - 4, qt + 1) if kt >= 0]
                for n_, kt in enumerate(kts):
                    masks = []
                    if kt == qt:
                        masks.append((identb3[:, :], caus[:, :, :], [t_identb3, t_caus]))
                    if kt == qt - 4:
                        masks.append((identb3[:, :], acaus[:, :, :], [t_identb3, t_acaus]))
                    tile_pair((KK[64:128, kt * 128:(kt + 1) * 128], t_KK), qv_hi, masks, (tb_sw, t_tbsw), qt - kt, (Vw[:, kt, :], t_Vw), 4,
                              n_ == 0, n_ == len(kts) - 1)
                flush_pairs()
                kb.op("dve", lambda e: e.tensor_copy(out=oT[:, 0, :], in_=ps[2][0:65, :]), reads=[pst[2]], writes=[t_oT])
                for h in range(4):
                    kb.op("pe", lambda e, h=h: e.matmul(ps[6][:, h * 65:(h + 1) * 65], lhsT=oT[0:65, 0, h * 128:(h + 1) * 128], rhs=ident[0:65, 0:65], start=True, stop=True),
                          reads=[t_oT, t_ident], writes=[pst[6]], sig=(h == 3))
                kb.op("dve", lambda e: e.tensor_scalar(out=zsc[:, 0:4], in0=ps[6][:, 0:260].rearrange("p (h c) -> p h c", c=65)[:, :, 64], scalar1=1e-30, scalar2=None, op0=ALU.max),
                      reads=[pst[6]], writes=[t_zsc])
                kb.op("dve", lambda e: e.reciprocal(out=zsc[:, 0:4], in_=zsc[:, 0:4]), reads=[t_zsc], writes=[t_zsc])
                kb.op("dve", lambda e: e.tensor_scalar(out=impn[:], in0=ps[5][:, 0:128], scalar1=zsc[:, 0:1], scalar2=None, op0=ALU.mult), reads=[pst[5], t_zsc], writes=[t_impn])
                for h in range(1, 4):
                    kb.op("dve", lambda e, h=h: e.scalar_tensor_tensor(out=impn[:], in0=ps[5][:, h * 128:(h + 1) * 128], scalar=zsc[:, h:h + 1], in1=impn[:], op0=ALU.mult, op1=ALU.add),
                          reads=[pst[5], t_zsc, t_impn], writes=[t_impn])
                if dbg:
                    kb.dma(d_imp[q0:q0 + 128, :], impn[:], reads=[t_impn])
                o0 = 127 - 2 * qt
                kb.op("dve", lambda e, o0=o0: e.tensor_tensor(out=impn[:], in0=impn[:], in1=tkeep[:, o0:o0 + 128], op=ALU.mult), reads=[t_impn, t_tkeep], writes=[t_impn])
                kb.op("dve", lambda e, o0=o0: e.tensor_tensor(out=impn[:], in0=impn[:], in1=tadd[:, o0:o0 + 128], op=ALU.add), reads=[t_impn, t_tadd], writes=[t_impn])
                kb.op("dve", lambda e: e.memset(impn[:, 0:1], 1e4), reads=[t_impn], writes=[t_impn])
                kb.op("dve", lambda e: e.max(out=m8[:, 0:8], in_=impn[:]), reads=[t_impn], writes=[t_m8])
                kb.op("dve", lambda e: e.match_replace(out=impw[:], in_to_replace=m8[:, 0:8], in_values=impn[:], imm_value=-3e38), reads=[t_impn, t_m8], writes=[t_impw])
                kb.op("dve", lambda e: e.max(out=m8[:, 8:16], in_=impw[:]), reads=[t_impw], writes=[t_m8])
                kb.op("dve", lambda e: e.tensor_scalar(out=selb[:], in0=impn[:], scalar1=m8[:, 15:16], scalar2=None, op0=ALU.is_ge), reads=[t_impn, t_m8], writes=[t_selb])
                if dbg:
                    kb.dma(d_sel[q0:q0 + 128, :], selb[:], reads=[t_selb])
                kb.op("pe", lambda e: e.matmul(ps[7][:, 0:128], lhsT=selb[:, :], rhs=identb3[:, :], start=True, stop=True), reads=[t_selb, t_identb3], writes=[pst[7]])
                kb.op("dve", lambda e: e.tensor_scalar(out=selT[:], in0=ps[7][:, 0:128].unsqueeze(1).broadcast_to([128, 4, 128]), scalar1=-1.0, scalar2=-NEG, op0=ALU.add, op1=ALU.mult),
                      reads=[pst[7]], writes=[t_selT])
                for kt in range(qt + 1):
                    if kt == qt:
                        masks = [(identb3[:, :], caus[:, :, :], [t_identb3, t_caus])]
                    else:
                        masks = [(expT[:, kt * 128:(kt + 1) * 128], selT[:, :, :], [t_expT, t_selT])]
                    tile_pair((KK[0:64, kt * 128:(kt + 1) * 128], t_KK), qv_lo, masks, (tb_sw, t_tbsw), qt - kt, (Vs[:, kt, :], t_Vs), 3, kt == 0, kt == qt)
                flush_pairs()
                kb.op("dve", lambda e: e.tensor_copy(out=oT[:, 1, :], in_=ps[3][0:65, :]), reads=[pst[3]], writes=[t_oT])
                kb.op("dve", lambda e: e.tensor_copy(out=oT[:, 2, :], in_=ps[4][0:65, :]), reads=[pst[4]], writes=[t_oT])
                for br in (1, 2):
                    for h in range(4):
                        col = 260 + ((br - 1) * 4 + h) * 65
                        pbk, cc = (6, col) if col + 65 <= 512 else (7, col - 455 + 128)
                        kb.op("pe", lambda e, h=h, br=br, pbk=pbk, cc=cc: e.matmul(ps[pbk][:, cc:cc + 65], lhsT=oT[0:65, br, h * 128:(h + 1) * 128], rhs=ident[0:65, 0:65], start=True, stop=True),
                              reads=[t_oT, t_ident], writes=[pst[pbk]])

                def oslot(br, h):
                    if br == 0:
                        return 6, h * 65
                    col = 260 + ((br - 1) * 4 + h) * 65
                    return (6, col) if col + 65 <= 512 else (7, col - 455 + 128)
                for br in (1, 2):
                    for h in range(4):
                        pbk, cc = oslot(br, h)
                        kb.op("dve", lambda e, br=br, h=h, pbk=pbk, cc=cc: e.tensor_scalar(out=zsc[:, br * 4 + h:br * 4 + h + 1], in0=ps[pbk][:, cc + 64:cc + 65], scalar1=1e-30, scalar2=None, op0=ALU.max),
                              reads=[pst[pbk]], writes=[t_zsc])
                kb.op("dve", lambda e: e.reciprocal(out=zsc[:, 4:12], in_=zsc[:, 4:12]), reads=[t_zsc], writes=[t_zsc])
                for br in range(3):
                    kb.op("dve", lambda e, br=br, qt=qt: e.tensor_tensor(out=zsc[:, 12 + br * 4:16 + br * 4], in0=zsc[:, br * 4:br * 4 + 4],
                                                                         in1=gts[:, qt, :].rearrange("p (h b) -> p h b", b=3)[:, :, br], op=ALU.mult),
                          reads=[t_zsc, t_gts], writes=[t_zsc])
                for h in range(4):
                    for br in range(3):
                        pbk, cc = oslot(br, h)
                        if br == 0:
                            kb.op("dve", lambda e, h=h, br=br, pbk=pbk, cc=cc: e.tensor_scalar(out=haf[:, h * 64:(h + 1) * 64], in0=ps[pbk][:, cc:cc + 64],
                                                                                               scalar1=zsc[:, 12 + br * 4 + h:13 + br * 4 + h], scalar2=None, op0=ALU.mult),
                                  reads=[pst[pbk], t_zsc], writes=[t_haf])
                        else:
                            kb.op("dve", lambda e, h=h, br=br, pbk=pbk, cc=cc: e.scalar_tensor_tensor(out=haf[:, h * 64:(h + 1) * 64], in0=ps[pbk][:, cc:cc + 64],
                                                                                                      scalar=zsc[:, 12 + br * 4 + h:13 + br * 4 + h], in1=haf[:, h * 64:(h + 1) * 64],
                                                                                                      op0=ALU.mult, op1=ALU.add),
                                  reads=[pst[pbk], t_zsc, t_haf], writes=[t_haf])
                kb.op("dve", lambda e: e.tensor_copy(out=hab[:], in_=haf[:]), reads=[t_haf], writes=[t_hab])
                ob = qt % 2
                for dc in range(2):
                    kb.op("pe", lambda e, dc=dc: e.matmul(ps[5][:, dc * 128:(dc + 1) * 128], lhsT=hab[:, dc * 128:(dc + 1) * 128], rhs=identb3[:, :], start=True, stop=True),
                          reads=[t_hab, t_identb3], writes=[pst[5]], sig=(dc == 1))
                kb.op("dve", lambda e, ob=ob: e.tensor_copy(out=haT[ob][:].rearrange("p a t -> p (a t)"), in_=ps[5][:, 0:256]), reads=[pst[5]], writes=[t_haT[ob]])
                kb.dma(mixT[qt // 8][256:512, (qt % 8) * 128:(qt % 8 + 1) * 128].rearrange("(a p) t -> p a t", p=128), haT[ob][:], reads=[t_haT[ob]], writes=[t_mixT])
            kb.barrier()
    if dbg:
        d_mixT = nc.dram_tensor("d_mixT", [512, S], BF16, kind="ExternalOutput").ap()
        for i in range(8):
            kb.dma(d_mixT[:, i * 1024:(i + 1) * 1024], mixT[i], reads=[t_mixT])


    if phases >= 4:
        selm_in = din("selm", [128, 4])
        x_own = din("x_own", [2048, D])
        w_out_in = din("w_out_p", [D, D])
        g2_row = din("g2_row", [1, D])
        wq_in = din("wq", [D, D])
        skT_in = din("skT", [128, 2, 128])
        uT_in = din("uT", [D, 16384])
        v_in = din("v_tab", [16384, D])
        y_out = nc.dram_tensor("y", [2048, D], F32, kind="ExternalOutput").ap()
        mixG = [nc.dram_tensor("mixG%d" % i, [2048, 1024], BF16, kind="Internal").ap() for i in range(8)]; t_mixG = Trk()
        s_x1 = dscr("s_x1", [2048, D]); t_sx1 = Trk()
        s_h2T = dscr("s_h2T", [D, 2048], BF16); t_sh2T = Trk()
        kb.barrier()
        for i in range(8):
            kb.op("pool", lambda e, i=i: e.collective_compute("AllGather", ALU.bypass, replica_groups=[[0, 1, 2, 3], [4, 5, 6, 7]],
                                                              ins=[mixT[i]], outs=[mixG[i]]), reads=[t_mixT], writes=[t_mixG])
        with ExitStack() as p45:
            def sb45(name, shape, dt=F32):
                return p45.enter_context(nc.sbuf_tensor(name, list(shape), dt))
            g2tb = sb45("g2tb", [128, D]); t_g2tb = Trk()
            identb4 = sb45("identb4", [128, 128], BF16); t_identb4 = Trk()
            kb.dma(identb4[:], din("ident_b2", [128, 128], BF16), writes=[t_identb4])
            with ExitStack() as p4:
                def sb4(name, shape, dt=F32):
                    return p4.enter_context(nc.sbuf_tensor(name, list(shape), dt))
                rowst = sb4("rowst", [1, D]); t_rowst = Trk()
                g1b = sb4("g1b", [128, D]); t_g1b = Trk()
                sh2b = sb4("sh2b", [128, D]); t_sh2b = Trk()
                gs2b = sb4("gs2b", [128, D]); t_gs2b = Trk()
                selm = sb4("selm_sb", [128, 4]); t_selm = Trk()
                kb.dma(selm[:], selm_in, writes=[t_selm])

                def bcast_row(src_ap, dst, t_dst, src_reads=()):
                    kb.dma(rowst[:], src_ap, reads=list(src_reads), writes=[t_rowst])
                    for n in range(4):
                        kb.op("pe", lambda e, n=n: e.matmul(ps[n][:, :], lhsT=ones_f[0:1, :], rhs=rowst[0:1, n * 512:(n + 1) * 512], start=True, stop=True),
                              reads=[t_ones_f, t_rowst], writes=[pst[n]])
                        kb.op("dve", lambda e, n=n: e.tensor_copy(out=dst[:, n * 512:(n + 1) * 512], in_=ps[n][:, :]), reads=[pst[n]], writes=[t_dst])
                bcast_row(s_mod[0:1, 2 * D:3 * D], g1b, t_g1b, [t_smod])
                bcast_row(s_mod[0:1, 3 * D:4 * D], sh2b, t_sh2b, [t_smod])
                bcast_row(s_mod[0:1, 5 * D:6 * D], g2tb, t_g2tb, [t_smod])
                bcast_row(s_mod[0:1, 4 * D:5 * D], gs2b, t_gs2b, [t_smod])
                kb.dma(rowst[:], g2_row, writes=[t_rowst])
                for n in range(4):
                    kb.op("pe", lambda e, n=n: e.matmul(ps[n][:, :], lhsT=ones_f[0:1, :], rhs=rowst[0:1, n * 512:(n + 1) * 512], start=True, stop=True),
                          reads=[t_ones_f, t_rowst], writes=[pst[n]])
                    kb.op("dve", lambda e, n=n: e.scalar_tensor_tensor(out=gs2b[:, n * 512:(n + 1) * 512], in0=gs2b[:, n * 512:(n + 1) * 512], scalar=1.0, in1=ps[n][:, :],
                                                                       op0=ALU.add, op1=ALU.mult), reads=[pst[n], t_gs2b], writes=[t_gs2b])
                wo = sb4("wo", [128, 16, D], BF16); t_wo = Trk()
                for fc in range(16):
                    kb.dma(wo[:, fc, :], w_out_in[fc * 128:(fc + 1) * 128, :], writes=[t_wo], q="pool")
                sl = [sb4("sl%d" % i, [128, 16, 128], BF16) for i in range(4)]; t_sl = [Trk() for _ in range(4)]
                mixo = sb4("mixo", [128, 16, 128], BF16); t_mixo = Trk()
                xin = [sb4("xin%d" % i, [128, D]) for i in range(2)]; t_xin = [Trk(), Trk()]
                tmpy = sb4("tmpy", [128, D]); t_tmpy = Trk()
                h2b = sb4("h2b", [128, D], BF16); t_h2b = Trk()
                h2Tt = [sb4("h2Tt%d" % i, [128, 16, 128], BF16) for i in range(2)]; t_h2Tt = [Trk(), Trk()]
                nsc = sb4("nsc", [128, 4]); t_nsc = Trk()
                mgv = [mg.rearrange("(f p) t -> p f t", p=128) for mg in mixG]
                for tt in range(16):
                    bi = tt % 2
                    kb.dma(xin[bi][:], x_own[tt * 128:(tt + 1) * 128, :], writes=[t_xin[bi]])
                    for s_ in range(4):
                        kb.dma(sl[s_][:], mgv[2 * s_ + tt // 8][:, :, (tt % 8) * 128:(tt % 8 + 1) * 128], reads=[t_mixG], writes=[t_sl[s_]])
                    kb.op("dve", lambda e: e.tensor_scalar(out=mixo[:], in0=sl[0][:], scalar1=selm[:, 0:1], scalar2=None, op0=ALU.mult), reads=[t_sl[0], t_selm], writes=[t_mixo])
                    for s_ in range(1, 4):
                        kb.op("dve", lambda e, s_=s_: e.scalar_tensor_tensor(out=mixo[:], in0=sl[s_][:], scalar=selm[:, s_:s_ + 1], in1=mixo[:], op0=ALU.mult, op1=ALU.add),
                              reads=[t_sl[s_], t_selm, t_mixo], writes=[t_mixo])
                    for n in range(4):
                        for fc in range(16):
                            kb.op("pe", lambda e, n=n, fc=fc: e.matmul(ps[n][:, :], lhsT=mixo[:, fc, :], rhs=wo[:, fc, n * 512:(n + 1) * 512], start=(fc == 0), stop=(fc == 15)),
                                  reads=[t_mixo, t_wo], writes=[pst[n]], sig=(fc == 15))
                        kb.op("dve", lambda e, n=n: e.tensor_tensor(out=tmpy[:, n * 512:(n + 1) * 512], in0=ps[n][:, :], in1=g1b[:, n * 512:(n + 1) * 512], op=ALU.mult),
                              reads=[pst[n], t_g1b], writes=[t_tmpy])
                        kb.op("dve", lambda e, n=n, bi=bi: e.tensor_tensor(out=xin[bi][:, n * 512:(n + 1) * 512], in0=xin[bi][:, n * 512:(n + 1) * 512], in1=tmpy[:, n * 512:(n + 1) * 512], op=ALU.add),
                              reads=[t_xin[bi], t_tmpy], writes=[t_xin[bi]])
                    kb.dma(s_x1[tt * 128:(tt + 1) * 128, :], xin[bi][:], reads=[t_xin[bi]], writes=[t_sx1])
                    kb.op("act", lambda e, bi=bi: e.activation(out=tmpy[:], in_=xin[bi][:], func=AF.Square, accum_out=nsc[:, 0:1]), reads=[t_xin[bi]], writes=[t_tmpy, t_nsc])
                    kb.op("act", lambda e: e.activation(out=nsc[:, 1:2], in_=nsc[:, 0:1], func=AF.Sqrt, scale=1.0 / D, bias=EPS), reads=[t_nsc], writes=[t_nsc])
                    kb.op("dve", lambda e: e.reciprocal(out=nsc[:, 2:3], in_=nsc[:, 1:2]), reads=[t_nsc], writes=[t_nsc])
                    kb.op("dve", lambda e, bi=bi: e.scalar_tensor_tensor(out=tmpy[:], in0=xin[bi][:], scalar=nsc[:, 2:3], in1=gs2b[:], op0=ALU.mult, op1=ALU.mult),
                          reads=[t_xin[bi], t_nsc, t_gs2b], writes=[t_tmpy])
                    kb.op("dve", lambda e: e.tensor_tensor(out=h2b[:], in0=tmpy[:], in1=sh2b[:], op=ALU.add), reads=[t_tmpy, t_sh2b], writes=[t_h2b])
                    for qd in range(4):
                        pb = 4 + qd
                        for k in range(4):
                            dc = qd * 4 + k
                            kb.op("pe", lambda e, dc=dc, k=k, pb=pb: e.matmul(ps[pb][:, k * 128:(k + 1) * 128], lhsT=h2b[:, dc * 128:(dc + 1) * 128], rhs=identb4[:, :], start=True, stop=True),
                                  reads=[t_h2b, t_identb4], writes=[pst[pb]], sig=(k == 3))
                        kb.op("act", lambda e, qd=qd, pb=pb, bi=bi: e.activation(out=h2Tt[bi][:, qd * 4:(qd + 1) * 4, :].rearrange("p a t -> p (a t)"), in_=ps[pb][:, :], func=AF.Copy),
                              reads=[pst[pb]], writes=[t_h2Tt[bi]])
                    kb.dma(s_h2T[:, tt * 128:(tt + 1) * 128].rearrange("(a p) t -> p a t", p=128), h2Tt[bi][:], reads=[t_h2Tt[bi]], writes=[t_sh2T])
                kb.barrier()

            if phases >= 5:
                with ExitStack() as p5:
                    def sb5(name, shape, dt=F32):
                        return p5.enter_context(nc.sbuf_tensor(name, list(shape), dt))
                    skT = sb5("skT_sb", [128, 2, 128], BF16); t_skT = Trk()
                    kb.dma(skT[:], skT_in, writes=[t_skT], q="pool")
                    h2g = sb5("h2g", [128, 16, 512], BF16); t_h2g = Trk()
                    s1m = sb5("s1m", [128, 4, 8, 128]); t_s1m = Trk()
                    s2t = sb5("s2t", [128, 4, 8, 128]); t_s2t = Trk()
                    tau = sb5("tau", [128, 4, 8]); t_tau = Trk()
                    Ysb = sb5("Ysb", [128, 4, D]); t_Ysb = [Trk() for _ in range(4)]
                    wqs = [sb5("wqs%d" % i, [128, 16, 128], BF16) for i in range(2)]; t_wqs = [Trk(), Trk()]
                    sct = sb5("sct", [128, 16, 128]); t_sct = Trk()
                    wk = sb5("wk", [128, 256]); t_wk = Trk()
                    tv = sb5("tv", [128, 16, 16]); t_tv = Trk()
                    cand = sb5("cand", [128, 8, 256]); t_cand = Trk()
                    cw2 = sb5("cw2", [128, 256]); t_cw2 = Trk()
                    m24 = sb5("m24", [128, 8, 24]); t_m24 = Trk()
                    hs = sb5("hs", [128, 8, 8]); t_hs = Trk()
                    ex = sb5("ex", [128, 8, 256]); t_ex = Trk()
                    Ub = [sb5("Ub%d" % i, [128, 16, 256], BF16) for i in range(2)]; t_Ub = [Trk(), Trk()]
                    Vb = [sb5("Vb%d" % i, [128, 2, D], BF16) for i in range(2)]; t_Vb = [Trk(), Trk()]
                    Lt = [sb5("Lt0", [128, 8, 256]), cand]; t_Lt = [Trk(), t_cand]
                    etau = sb5("etau", [128, 4, 8]); t_etau = Trk()
                    Wd = [sb5("Wd%d" % i, [128, 256], BF16) for i in range(2)]; t_Wd = [Trk(), Trk()]
                    t_Ex = [[Trk() for _ in range(16)] for _ in range(2)]
                    Wt = [sb5("Wt%d" % i, [128, 8, 256], BF16) for i in range(2)]; t_Wt = [[Trk() for _ in range(8)] for _ in range(2)]
                    glT = [sb5("glT%d" % i, [128, 2, 512], BF16) for i in range(2)]; t_glT = [Trk(), Trk()]
                    GT = [sb5("GT%d" % i, [128, 2, 128], BF16) for i in range(2)]; t_GT = [Trk(), Trk()]
                    x1t = Lt[0][:].rearrange("p h e -> p (h e)"); t_x1t = t_Lt[0]
                    h2v = s_h2T.rearrange("(a p) t -> p a t", p=128)
                    wqv = wq_in.rearrange("(a p) n -> p a n", p=128)
                    uTv = uT_in.rearrange("(a p) e -> p a e", p=128)
                    vv_ = v_in.rearrange("(c p) d -> p c d", p=128)
                    ngrp = int(os.environ.get("NGRP", "4"))
                    net = int(os.environ.get("NET", "64"))
                    for g in range(ngrp):
                        kb.dma(h2g[:, 0:8, :], h2v[:, 0:8, g * 512:(g + 1) * 512], reads=[t_sh2T], writes=[t_h2g])
                        kb.dma(h2g[:, 8:16, :], h2v[:, 8:16, g * 512:(g + 1) * 512], reads=[t_sh2T], writes=[t_h2g])
                        for tl in range(4):
                            pass
                        qall = p5.enter_context(nc.sbuf_tensor("qall%d" % g, [128, 16, 512], BF16)) if g == 0 else qall
                        t_qall = Trk() if g == 0 else t_qall
                        for ch in range(16):
                            wi = ch % 2
                            kb.dma(wqs[wi][:], wqv[:, :, ch * 128:(ch + 1) * 128], writes=[t_wqs[wi]], q="pool")
                            pb = ch % 2
                            for dc in range(16):
                                kb.op("pe", lambda e, dc=dc, wi=wi, pb=pb: e.matmul(ps[pb][:, :], lhsT=wqs[wi][:, dc, :], rhs=h2g[:, dc, :], start=(dc == 0), stop=(dc == 15)),
                                      reads=[t_wqs[wi], t_h2g], writes=[pst[pb]], sig=(dc == 15))
                            kb.op("act", lambda e, ch=ch, pb=pb: e.activation(out=qall[:, ch, :], in_=ps[pb][:, :], func=AF.Copy), reads=[pst[pb]], writes=[t_qall])
                        for tl in range(4):
                            for qd in range(4):
                                pb = 2 + (qd % 2)
                                for k in range(4):
                                    ch = qd * 4 + k
                                    kb.op("pe", lambda e, ch=ch, k=k, pb=pb, tl=tl: e.matmul(ps[pb][:, k * 128:(k + 1) * 128], lhsT=qall[:, ch, tl * 128:(tl + 1) * 128], rhs=skT[:, ch % 2, :],
                                                                                             start=True, stop=True),
                                          reads=[t_qall, t_skT], writes=[pst[pb]], sig=(k == 3))
                                kb.op("dve", lambda e, qd=qd, pb=pb: e.tensor_copy(out=sct[:, qd * 4:(qd + 1) * 4, :].rearrange("p a k -> p (a k)"), in_=ps[pb][:, :]),
                                      reads=[pst[pb]], writes=[t_sct])
                            for ch in range(16):
                                kb.op("dve", lambda e, ch=ch: e.max(out=tv[:, ch, 0:8], in_=sct[:, ch, :]), reads=[t_sct], writes=[t_tv])
                                kb.op("dve", lambda e, ch=ch: e.match_replace(out=wk[:, 0:128], in_to_replace=tv[:, ch, 0:8], in_values=sct[:, ch, :], imm_value=-3e38),
                                      reads=[t_sct, t_tv], writes=[t_wk])
                                kb.op("dve", lambda e, ch=ch: e.max(out=tv[:, ch, 8:16], in_=wk[:, 0:128]), reads=[t_wk], writes=[t_tv])
                            tvv = tv[:].rearrange("p (h two) a -> p h two a", two=2)
                            kb.op("dve", lambda e: e.tensor_tensor(out=cand[:].rearrange("p h (a b) -> p h a b", b=16),
                                                                   in0=tvv[:, :, 0, :].unsqueeze(3).broadcast_to([128, 8, 16, 16]),
                                                                   in1=tvv[:, :, 1, :].unsqueeze(2).broadcast_to([128, 8, 16, 16]), op=ALU.add),
                                  reads=[t_tv], writes=[t_cand])
                            for h in range(8):
                                kb.op("dve", lambda e, h=h: e.max(out=m24[:, h, 0:8], in_=cand[:, h, :]), reads=[t_cand], writes=[t_m24])
                                kb.op("dve", lambda e, h=h: e.match_replace(out=cw2[:], in_to_replace=m24[:, h, 0:8], in_values=cand[:, h, :], imm_value=-3e38),
                                      reads=[t_cand, t_m24], writes=[t_cw2])
                                kb.op("dve", lambda e, h=h: e.max(out=m24[:, h, 8:16], in_=cw2[:]), reads=[t_cw2], writes=[t_m24])
                                kb.op("dve", lambda e, h=h: e.match_replace(out=cw2[:], in_to_replace=m24[:, h, 8:16], in_values=cw2[:], imm_value=-3e38),
                                      reads=[t_cw2, t_m24], writes=[t_cw2])
                                kb.op("dve", lambda e, h=h: e.max(out=m24[:, h, 16:24], in_=cw2[:]), reads=[t_cw2], writes=[t_m24])
                            kb.op("dve", lambda e: e.tensor_copy(out=hs[:, :, 0], in_=m24[:, :, 0]), reads=[t_m24], writes=[t_hs])
                            kb.op("dve", lambda e: e.tensor_tensor(out=hs[:, :, 1], in0=m24[:, :, 15], in1=m24[:, :, 16], op=ALU.add), reads=[t_m24], writes=[t_hs])
                            kb.op("dve", lambda e: e.tensor_scalar(out=hs[:, :, 1], in0=hs[:, :, 1], scalar1=0.5, scalar2=None, op0=ALU.mult), reads=[t_hs], writes=[t_hs])
                            kb.op("dve", lambda e: e.tensor_tensor(out=ex[:], in0=cand[:], in1=hs[:, :, 0:1].broadcast_to([128, 8, 256]), op=ALU.subtract), reads=[t_cand, t_hs], writes=[t_ex])
                            kb.op("act", lambda e: e.activation(out=ex[:], in_=ex[:], func=AF.Exp), reads=[t_ex], writes=[t_ex])
                            kb.op("dve", lambda e: e.tensor_tensor(out=cand[:], in0=cand[:], in1=hs[:, :, 1:2].broadcast_to([128, 8, 256]), op=ALU.is_ge), reads=[t_cand, t_hs], writes=[t_cand])
                            kb.op("dve", lambda e: e.tensor_tensor(out=ex[:], in0=ex[:], in1=cand[:], op=ALU.mult), reads=[t_cand, t_ex], writes=[t_ex])
                            kb.op("dve", lambda e: e.tensor_reduce(out=hs[:, :, 2], in_=ex[:], axis=AX.X, op=ALU.add), reads=[t_ex], writes=[t_hs])
                            kb.op("act", lambda e: e.activation(out=hs[:, :, 3], in_=hs[:, :, 2], func=AF.Ln), reads=[t_hs], writes=[t_hs])
                            kb.op("dve", lambda e: e.tensor_tensor(out=hs[:, :, 4], in0=hs[:, :, 0], in1=hs[:, :, 3], op=ALU.add), reads=[t_hs], writes=[t_hs])
                            sv = sct[:].rearrange("p (h two) k -> p h two k", two=2)
                            kb.op("dve", lambda e, tl=tl: e.tensor_tensor(out=s1m[:, tl, :, :], in0=sv[:, :, 0, :], in1=hs[:, :, 4:5].broadcast_to([128, 8, 128]), op=ALU.subtract),
                                  reads=[t_sct, t_hs], writes=[t_s1m])
                            kb.op("dve", lambda e, tl=tl: e.tensor_copy(out=s2t[:, tl, :, :], in_=sv[:, :, 1, :]), reads=[t_sct], writes=[t_s2t])
                            kb.op("dve", lambda e, tl=tl: e.tensor_tensor(out=tau[:, tl, :], in0=hs[:, :, 1], in1=hs[:, :, 4], op=ALU.subtract), reads=[t_hs], writes=[t_tau])
                            kb.op("act", lambda e, tl=tl: e.activation(out=etau[:, tl, :], in_=tau[:, tl, :], func=AF.Exp), reads=[t_tau], writes=[t_etau])
                        kb.op("dve", lambda e: e.memset(Ysb[:], 0.0), writes=t_Ysb)

                        def load_u(et):
                            bi = et % 2
                            kb.dma(Ub[bi][:, 0:8, :], uTv[:, 0:8, et * 256:(et + 1) * 256], writes=[t_Ub[bi]], q="pool")
                            kb.dma(Ub[bi][:, 8:16, :], uTv[:, 8:16, et * 256:(et + 1) * 256], writes=[t_Ub[bi]], q="pool")

                        def load_v(et):
                            bi = et % 2
                            kb.dma(Vb[bi][:], vv_[:, et * 2:et * 2 + 2, :], writes=[t_Vb[bi]], q="pool")

                        def emit_a(et):
                            bi = et % 2
                            for ec in range(2):
                                for dc in range(16):
                                    kb.op("pe", lambda e, dc=dc, ec=ec, bi=bi: e.matmul(ps[ec][:, :], lhsT=Ub[bi][:, dc, ec * 128:(ec + 1) * 128], rhs=h2g[:, dc, :],
                                                                                        start=(dc == 0), stop=(dc == 15)),
                                          reads=[t_Ub[bi], t_h2g], writes=[pst[ec]], sig=(dc == 15))
                                kb.op("act", lambda e, ec=ec, bi=bi: e.activation(out=glT[bi][:, ec, :], in_=ps[ec][:, :], func=AF.Gelu_apprx_tanh),
                                      reads=[pst[ec]], writes=[t_glT[bi]])
                        load_u(0)
                        if net > 1:
                            load_u(1)
                        load_v(0)
                        emit_a(0)
                        pairs = [(et, tl) for et in range(net) for tl in range(4)]
                        NP = len(pairs)

                        def st_L(n):
                            et, tl = pairs[n]; k = n % 2
                            for h in range(8):
                                for a2 in range(2):
                                    kb.op("act", lambda e, h=h, a2=a2: e.activation(out=Lt[k][:, h, a2 * 128:(a2 + 1) * 128], in_=s2t[:, tl, h, :], func=AF.Exp,
                                                                                    bias=s1m[:, tl, h, 2 * et + a2:2 * et + a2 + 1]),
                                          reads=[t_s2t, t_s1m], writes=[t_Ex[k][h * 2 + a2], t_Lt[k]] if (h == 0 and a2 == 0) else [t_Ex[k][h * 2 + a2]])

                        def st_C(n):
                            et, tl = pairs[n]; k = n % 2
                            for h in range(8):
                                kb.op("dve", lambda e, h=h: e.scalar_tensor_tensor(out=Wt[k][:, h, :], in0=Lt[k][:, h, :], scalar=etau[:, tl, h:h + 1], in1=Lt[k][:, h, :],
                                                                                   op0=ALU.is_ge, op1=ALU.mult),
                                      reads=[t_Ex[k][2 * h], t_Ex[k][2 * h + 1], t_etau, t_Lt[k]], writes=[t_Wt[k][h]])
                            kb.op("dve", lambda e: e.tensor_tensor(out=Wt[k][:, 0:4, :], in0=Wt[k][:, 0:4, :], in1=Wt[k][:, 4:8, :], op=ALU.add), reads=t_Wt[k], writes=t_Wt[k][0:4])
                            kb.op("dve", lambda e: e.tensor_tensor(out=Wt[k][:, 0:2, :], in0=Wt[k][:, 0:2, :], in1=Wt[k][:, 2:4, :], op=ALU.add), reads=t_Wt[k][0:4], writes=t_Wt[k][0:2])
                            kb.op("dve", lambda e: e.tensor_tensor(out=Wd[k][:], in0=Wt[k][:, 0, :], in1=Wt[k][:, 1, :], op=ALU.add), reads=t_Wt[k][0:2], writes=[t_Wd[k]])
                            pw = 2 + k
                            for ec in range(2):
                                kb.op("pe", lambda e, ec=ec: e.matmul(ps[pw][:, ec * 128:(ec + 1) * 128], lhsT=Wd[k][:, ec * 128:(ec + 1) * 128], rhs=identb4[:, :], start=True, stop=True),
                                      reads=[t_Wd[k], t_identb4], writes=[pst[pw]], sig=(ec == 1))

                        def st_G(n):
                            et, tl = pairs[n]; k = n % 2; bi = et % 2; pw = 2 + k
                            if tl == 0:
                                if et + 1 < net:
                                    load_v(et + 1)
                                    emit_a(et + 1)
                                if et + 2 < net:
                                    load_u(et + 2)
                            kb.op("dve", lambda e: e.tensor_tensor(out=GT[k][:], in0=glT[bi][:, :, tl * 128:(tl + 1) * 128],
                                                                   in1=ps[pw][:, 0:256].rearrange("p (a t) -> p a t", t=128), op=ALU.mult),
                                  reads=[t_glT[bi], pst[pw]], writes=[t_GT[k]])
                            for nn in range(4):
                                pb = 4 + nn
                                for ec in range(2):
                                    kb.op("pe", lambda e, ec=ec, nn=nn, pb=pb: e.matmul(ps[pb][:, :], lhsT=GT[k][:, ec, :], rhs=Vb[bi][:, ec, nn * 512:(nn + 1) * 512],
                                                                                        start=(ec == 0), stop=(ec == 1)),
                                          reads=[t_GT[k], t_Vb[bi]], writes=[pst[pb]], sig=(ec == 1))

                        def st_Y(n):
                            et, tl = pairs[n]
                            kb.op("dve", lambda e: e.tensor_tensor(out=Ysb[:, tl, :], in0=Ysb[:, tl, :], in1=psY[:, :], op=ALU.add),
                                  reads=[t_Ysb[tl], pst[4], pst[5], pst[6], pst[7]], writes=[t_Ysb[tl]])
                        for n in range(-2, NP):
                            if 0 <= n:
                                st_G(n)
                            if 0 <= n + 2 < NP:
                                st_L(n + 2)
                            if 0 <= n + 1 < NP:
                                st_C(n + 1)
                            if 0 <= n:
                                st_Y(n)
                        for tl in range(4):
                            r0 = (g * 4 + tl) * 128
                            kb.dma(x1t, s_x1[r0:r0 + 128, :], reads=[t_sx1], writes=[t_x1t])
                            kb.op("dve", lambda e, tl=tl: e.tensor_tensor(out=Ysb[:, tl, :], in0=Ysb[:, tl, :], in1=g2tb[:], op=ALU.mult), reads=[t_Ysb[tl], t_g2tb], writes=[t_Ysb[tl]])
                            kb.op("dve", lambda e, tl=tl: e.tensor_tensor(out=x1t, in0=x1t, in1=Ysb[:, tl, :], op=ALU.add), reads=[t_Ysb[tl], t_x1t], writes=[t_x1t])
                            kb.dma(y_out[r0:r0 + 128, :], x1t, reads=[t_x1t])
                    kb.barrier()

    for _i in range(int(os.environ.get('EXTRA', '0'))):
        kb.dma(s_mod[0:1, 0:128], ident[0:1, :], reads=[t_ident])
    kb.barrier()
    ok, stuck, sems = kb.check()
    if not ok or os.environ.get('KBV'):
        print('SYNC CHECK ok=%s' % ok, stuck, {k: v for k, v in kb.cnt.items()})
    assert ok, 'sync deadlock'
    es.close()
    return nc


def _consts():
    ones = np.ones((128, 128), np.float32)
    blk = np.zeros((128, 128), np.float32)
    blk[:64, :64] = 1.0
    blk[64:, 64:] = 1.0
    ut = np.triu(np.ones((128, 128), np.float32))
    NEG = -30000.0
    il = np.arange(128)
    cmask = np.zeros((128, 17, 128), np.float32)
    for m in range(17):
        ok = (il[None, :] + 128 * m) >= (16 * il[:, None] + 31)
        cmask[:, m, :] = np.where(ok, 0.0, NEG)
    caus = np.where(il[:, None] <= il[None, :], 0.0, NEG).astype(np.float32)
    acaus = np.where(il[:, None] > il[None, :], 0.0, NEG).astype(np.float32)
    i_abs = (np.arange(4)[None, :, None] * 128 + il[:, None, None])
    blkk = np.arange(128)[None, None, :]
    ovl = ((16 * i_abs < 64 * blkk + 64) & (16 * i_abs + 32 > 64 * blkk)).astype(np.float32)
    expT = (np.arange(S)[None, :] // 64 == il[:, None]).astype(np.float32)
    o = np.arange(255)[None, :] - 127
    cur_rel = (il[:, None] >= 64).astype(np.int64)
    rel = o - cur_rel
    t_keep = (rel < -1).astype(np.float32)
    t_add = np.where(rel > 0, -1e4, np.where(rel >= -1, 1e4, 0.0)).astype(np.float32)
    return {"ones_bf": _bf(ones), "blk64_bf": _bf(blk), "ident_f": np.eye(128, dtype=np.float32),
            "ut_f": ut, "ident_b": _bf(np.eye(128, dtype=np.float32)),
            "cmask": _bf(cmask), "caus_add": _bf(caus), "acaus_add": _bf(acaus), "ovl": _bf(ovl), "expT": _bf(expT),
            "t_keep": np.ascontiguousarray(np.broadcast_to(t_keep, (128, 255))).astype(np.float32), "t_add": t_add}


def make_in_maps(inp):
    f = lambda a: np.ascontiguousarray(np.asarray(a, dtype=np.float32))
    x = f(inp["x"]); c = f(inp["c"])
    w_in = f(inp["w_in"])[0]
    conv = f(inp["conv_qk"])[0]
    qn_g = f(inp["qn_g"])[0]; kn_g = f(inp["kn_g"])[0]
    mng = f(inp["mlstm_norm_g"])[0]
    g1 = f(inp["norm1_g"])[0]
    cst = _consts()
    maps = []
    xTs = [np.ascontiguousarray(x[b].T) for b in range(2)]
    w_out = f(inp["w_out"])[0]
    rows = []
    for fc in range(16):
        r, part, half = fc // 4, (fc % 4) // 2, fc % 2
        base = part * 1024 + r * 256 + half * 128
        rows += list(range(base, base + 128))
    w_out_p = np.ascontiguousarray(w_out[rows])
    wq = f(inp["peer_wq"])[0]
    skT = np.ascontiguousarray(f(inp["peer_subkeys"])[0].transpose(2, 0, 1))
    uT = np.ascontiguousarray(f(inp["peer_u"])[0].T)
    vtab = f(inp["peer_v"])[0]
    for core in range(8):
        b, j = divmod(core, 4)
        cols = []
        cols += list(range(j * 256, j * 256 + 256))
        cols += list(range(1024 + j * 256, 1024 + j * 256 + 256))
        AQ = 4096 + 8
        cols += list(range(AQ + j * 256, AQ + j * 256 + 256))
        AKV = AQ + 1024
        for br in (0, 1, 2, 4):
            cols += list(range(AKV + br * 256 + j * 64, AKV + br * 256 + j * 64 + 64))
        cols += [4096 + j, 4096 + 4 + j]
        cols += list(range(2048 + j * 256, 2048 + j * 256 + 256))
        cols += list(range(3072 + j * 256, 3072 + j * 256 + 256))
        for br in (3, 5):
            cols += list(range(AKV + br * 256 + j * 64, AKV + br * 256 + j * 64 + 64))
        AG = AKV + 1536
        cols += list(range(AG + j * 12, AG + j * 12 + 12))
        assert len(cols) == 1678
        wj = np.zeros((D, 1680), np.float32)
        wj[:, :1678] = w_in[:, cols]
        cwt = np.zeros((128, 4, 4), np.float32)
        for t in range(4):
            base = (j * 256 + (t % 2) * 128) if t < 2 else (1024 + j * 256 + (t % 2) * 128)
            cwt[:, t, :] = conv[:, base:base + 128].T
        qkg = np.zeros((128, 4), np.float32)
        qkg[:, 0] = np.tile(qn_g, 2) * 0.125
        qkg[:, 3] = np.concatenate([kn_g[1], kn_g[2]])
        m = {
            "xT": xTs[b], "c_col": np.ascontiguousarray(c[b].reshape(16, 128).T),
            "w_mod": f(inp["w_mod"])[0], "b_mod": f(inp["b_mod"]),
            "g1_col": np.ascontiguousarray(g1.reshape(16, 128).T),
            "w_in": wj, "convw": cwt, "qk_g": qkg,
            "mng_row": np.ascontiguousarray(mng[j * 256:(j + 1) * 256][None, :]),
            "bif": np.ascontiguousarray(np.tile(np.array([[f(inp["b_igate"])[0, j], f(inp["b_fgate"])[0, j]]], np.float32), (128, 1))),
        }
        slopes = np.array([2.0 ** (-8.0 * (4 * j + hh + 1) / 16) for hh in range(4)], np.float64)
        kl = np.arange(128, dtype=np.float64)
        dl = np.arange(64, dtype=np.float64)
        m["tb_sw"] = (slopes[None, None, :] * (kl[:, None, None] - 64 - 128 * dl[None, :, None])).astype(np.float32)
        m["tb_c"] = (slopes[None, None, :] * (16 * kl[:, None, None] - 33 - 128 * dl[None, :, None])).astype(np.float32)
        m["w1k"] = f(inp["cmp_k_w1"])[0]; m["w1v"] = f(inp["cmp_v_w1"])[0]
        m["w2k"] = f(inp["cmp_k_w2"])[0]; m["w2v"] = f(inp["cmp_v_w2"])[0]
        for nm, key in (("posk", "cmp_pos_k"), ("posv", "cmp_pos_v")):
            pos = f(inp[key])[0]
            m[nm] = np.ascontiguousarray(pos.reshape(16, 2, 64).transpose(1, 2, 0).reshape(128, 16))
        m["kng0"] = np.ascontiguousarray(kn_g[0][:, None])
        sm = np.zeros((128, 4), np.float32); sm[:, j] = 1.0
        m["selm"] = sm
        m["x_own"] = np.ascontiguousarray(x[b, j * 2048:(j + 1) * 2048])
        m["w_out_p"] = w_out_p
        m["g2_row"] = f(inp["norm2_g"])
        m["wq"] = wq
        m["skT"] = skT
        m["uT"] = uT
        m["v_tab"] = vtab
        m["ident_b2"] = cst["ident_b"]
        m.update(cst)
        maps.append(m)
    return maps


def kernel(**inputs):
    nc = build()
    maps = make_in_maps(inputs)
    res = run_bass_kernel_spmd(nc, maps, core_ids=list(range(8)))
    out = np.zeros((2, S, D), np.float32)
    for core in range(8):
        b, j = divmod(core, 4)
        out[b, j * 2048:(j + 1) * 2048] = res.results[core]["y"]
    return out
```

```python
import numpy as np
from contextlib import ExitStack
import concourse.bass as bass
import concourse.mybir as mybir
from concourse.bass_utils import run_bass_kernel_spmd

F32 = mybir.dt.float32
BF16 = mybir.dt.bfloat16
AF = mybir.ActivationFunctionType
ALU = mybir.AluOpType
AX = mybir.AxisListType

D = 2048
S = 8192
NB = 16
EPS = 1e-6
import os
NDS = int(os.environ.get("NDS", "24"))


class Trk:
    __slots__ = ("w", "r")

    def __init__(self):
        self.w = None
        self.r = {}


class KB:
    def __init__(self, nc, es):
        self.nc = nc
        self.eng = {"pe": nc.tensor, "act": nc.scalar, "dve": nc.vector, "pool": nc.gpsimd, "sp": nc.sync}
        self.sem = {k: es.enter_context(nc.semaphore("s_" + k)) for k in self.eng}
        self.cnt = {k: 0 for k in self.eng}
        self.seen = {k: {} for k in self.eng}
        self.dsem = [es.enter_context(nc.semaphore("dq%d" % i)) for i in range(NDS)]
        self.duse = [0] * NDS
        self.dnext = 0
        self.ccsem = es.enter_context(nc.semaphore("ccs"))
        self.log = {k: [] for k in self.eng}

    def _wait(self, e, tok):
        if tok is None:
            return
        key, sem, val = tok
        if self.seen[e].get(key, 0) >= val:
            return
        self.eng[e].wait_ge(sem, val)
        self.log[e].append(("w", key, val))
        self.seen[e][key] = val

    def _deps(self, e, reads, writes):
        for b in reads:
            if b.w is not None and not (e == "pe" and b.w[0] == "pe"):
                self._wait(e, b.w)
        for b in writes:
            if b.w is not None and not (e == "pe" and b.w[0] == "pe"):
                self._wait(e, b.w)
            for t in b.r.values():
                if not (e == "pe" and t[0] == "pe"):
                    self._wait(e, t)

    def _mark(self, tok, reads, writes):
        for b in reads:
            b.r[tok[0]] = tok
        for b in writes:
            b.w = tok
            b.r = {}

    def op(self, e, fn, reads=(), writes=(), sig=True):
        self._deps(e, reads, writes)
        inst = fn(self.eng[e])
        if sig:
            self.cnt[e] += 1
            inst.then_inc(self.sem[e], 1)
            self.log[e].append(("i", e, 1))
            tok = (e, self.sem[e], self.cnt[e])
        else:
            tok = (e, self.sem[e], self.cnt[e] + 1)
        self._mark(tok, reads, writes)
        return tok

    def dma(self, out, in_, reads=(), writes=(), q="sp", **kw):
        i = self.dnext
        self.dnext = (i + 1) % NDS
        if self.duse[i] > 0:
            self._wait(q, (("d", i), self.dsem[i], 16 * self.duse[i]))
        self._deps(q, reads, writes)
        inst = self.eng[q].dma_start(out=out, in_=in_, **kw)
        self.duse[i] += 1
        inst.then_inc(self.dsem[i], 16)
        self.log[q].append(("i", ("d", i), 16))
        tok = (("d", i), self.dsem[i], 16 * self.duse[i])
        self._mark(tok, reads, writes)
        return tok

    def check(self):
        sems = {}
        pc = {k: 0 for k in self.log}
        prog = True
        while prog:
            prog = False
            for e, lg in self.log.items():
                while pc[e] < len(lg):
                    kind, key, val = lg[pc[e]]
                    if kind == "w":
                        if sems.get(key, 0) < val:
                            break
                    else:
                        sems[key] = sems.get(key, 0) + val
                    pc[e] += 1
                    prog = True
        stuck = {e: (pc[e], len(lg), lg[pc[e]] if pc[e] < len(lg) else None) for e, lg in self.log.items()}
        ok = all(pc[e] == len(lg) for e, lg in self.log.items())
        return ok, stuck, sems

    def barrier(self):
        for e in self.eng:
            for e2 in self.eng:
                if e2 != e and self.cnt[e2] > 0:
                    self._wait(e, (e2, self.sem[e2], self.cnt[e2]))
            for i in range(NDS):
                if self.duse[i] > 0:
                    self._wait(e, (("d", i), self.dsem[i], 16 * self.duse[i]))


def _bf(a):
    import ml_dtypes
    return np.asarray(a, dtype=np.float32).astype(ml_dtypes.bfloat16)


def build(dbg=False, phases=99, nb=NB, parts='abcde'):
    nc = bass.Bass("TRN2", target_bir_lowering=False)
    es = ExitStack()
    kb = KB(nc, es)

    def din(name, shape, dt=F32):
        return nc.dram_tensor(name, list(shape), dt, kind="ExternalInput").ap()

    def dscr(name, shape, dt=F32):
        return nc.dram_tensor(name, list(shape), dt, kind=("ExternalOutput" if dbg else "Internal")).ap()

    xT = din("xT", [D, S])
    c_col = din("c_col", [128, 16])
    w_mod = din("w_mod", [D, 6 * D])
    b_mod = din("b_mod", [1, 6 * D])
    g1_col = din("g1_col", [128, 16])
    w_in = din("w_in", [D, 1680])
    convw = din("convw", [128, 4, 4])
    qk_g = din("qk_g", [128, 4])
    mng_row = din("mng_row", [1, 256])
    ones_bf = din("ones_bf", [128, 128], BF16)
    blk64_bf = din("blk64_bf", [128, 128], BF16)
    ident_f = din("ident_f", [128, 128])

    s_q = dscr("s_q", [256, S])
    s_k = dscr("s_k", [256, S])
    s_aq = dscr("s_aq", [256, S], BF16)
    s_kv = dscr("s_kv", [128, S])
    s_kk = dscr("s_kk", [128, S], BF16)
    s_if = dscr("s_if", [2, S])
    s_v = dscr("s_v", [S, 256])
    s_go = dscr("s_go", [S, 256])
    s_vv = dscr("s_vv", [S, 128], BF16)
    s_gt = dscr("s_gt", [S, 12])

    ps = []
    pst = []
    for i in range(4):
        ps.append(es.enter_context(nc.psum_tensor("ps%d" % i, [128, 512], F32)))
        pst.append(Trk())
    psY = es.enter_context(nc.psum_tensor("psY", [128, 2048], F32))
    for i in range(4):
        ps.append(psY[:, i * 512:(i + 1) * 512])
        pst.append(Trk())

    def sb(name, shape, dt=F32):
        return es.enter_context(nc.sbuf_tensor(name, list(shape), dt))

    ones_b = sb("ones_b", [128, 128], BF16); t_ones_b = Trk()
    blk64 = sb("blk64", [128, 128], BF16); t_blk64 = Trk()
    ident = sb("ident", [128, 128]); t_ident = Trk()
    ones_f = sb("ones_f", [128, 128]); t_ones_f = Trk()
    kb.dma(ones_b[:], ones_bf, writes=[t_ones_b])
    kb.dma(blk64[:], blk64_bf, writes=[t_blk64])
    kb.dma(ident[:], ident_f, writes=[t_ident])
    kb.op("dve", lambda e: e.memset(ones_f[:], 1.0), writes=[t_ones_f])

    s_mod = dscr("s_mod", [1, 6 * D]); t_smod = Trk()
    csil = sb("csil", [128, 16]); t_csil = Trk()
    ccol = sb("ccol", [128, 16]); t_ccol = Trk()
    g1c = sb("g1c", [128, 16]); t_g1c = Trk()
    gs1 = sb("gs1", [128, 16]); t_gs1 = Trk()
    sh1 = sb("sh1", [128, 16]); t_sh1 = Trk()
    kb.dma(ccol[:], c_col, writes=[t_ccol])
    kb.dma(g1c[:], g1_col, writes=[t_g1c])
    kb.op("act", lambda e: e.activation(out=csil[:], in_=ccol[:], func=AF.Silu), reads=[t_ccol], writes=[t_csil])
    with ExitStack() as p0:
        wm = [p0.enter_context(nc.sbuf_tensor("wm%d" % i, [128, 16, 512], F32)) for i in range(2)]
        modrow = p0.enter_context(nc.sbuf_tensor("modrow", [1, 6 * D], F32)); t_modrow = Trk()
        bmod_sb = p0.enter_context(nc.sbuf_tensor("bmod_sb", [1, 6 * D], F32)); t_bmod = Trk()
        kb.dma(bmod_sb[:], b_mod, writes=[t_bmod])
        t_wm = [Trk(), Trk()]
        wmv = w_mod.rearrange("(k p) n -> p k n", p=128)
        for n in range(24):
            bi = n % 2
            kb.dma(wm[bi][:, 0:8, :], wmv[:, 0:8, n * 512:(n + 1) * 512], writes=[t_wm[bi]])
            kb.dma(wm[bi][:, 8:16, :], wmv[:, 8:16, n * 512:(n + 1) * 512], writes=[t_wm[bi]])
            pb = n % 2
            for k in range(16):
                kb.op("pe", lambda e, k=k, bi=bi, pb=pb: e.matmul(ps[pb][0:1, :], lhsT=csil[:, k:k + 1], rhs=wm[bi][:, k, :],
                                                                  start=(k == 0), stop=(k == 15)),
                      reads=[t_csil, t_wm[bi]], writes=[pst[pb]], sig=(k == 15))
            kb.op("dve", lambda e, n=n, pb=pb: e.tensor_tensor(out=modrow[0:1, n * 512:(n + 1) * 512], in0=ps[pb][0:1, :],
                                                               in1=bmod_sb[0:1, n * 512:(n + 1) * 512], op=ALU.add),
                  reads=[pst[pb], t_bmod], writes=[t_modrow])
        for which, dst, t_dst in ((0, sh1, t_sh1), (1, gs1, t_gs1)):
            for k in range(16):
                off = which * D + k * 128
                kb.op("pe", lambda e, off=off, k=k: e.matmul(ps[2][:, k:k + 1], lhsT=modrow[0:1, off:off + 128], rhs=ones_f[0:1, 0:1],
                                                             start=True, stop=True),
                      reads=[t_modrow, t_ones_f], writes=[pst[2]], sig=(k == 15))
            if which == 0:
                kb.op("dve", lambda e: e.tensor_copy(out=sh1[:], in_=ps[2][:, 0:16]), reads=[pst[2]], writes=[t_sh1])
            else:
                kb.op("dve", lambda e: e.scalar_tensor_tensor(out=gs1[:], in0=ps[2][:, 0:16], scalar=1.0, in1=g1c[:],
                                                              op0=ALU.add, op1=ALU.mult),
                      reads=[pst[2], t_g1c], writes=[t_gs1])
        kb.dma(s_mod, modrow[:], reads=[t_modrow], writes=[t_smod])
        kb.barrier()

    t_scr = {n: Trk() for n in ("q", "k", "aq", "kv", "kk", "if", "v", "go", "vv", "gt")}
    if phases >= 1:
        with ExitStack() as p1:
            def sb1(name, shape, dt=F32):
                return p1.enter_context(nc.sbuf_tensor(name, list(shape), dt))
            wb = sb1("wb", [128, 16, 1680], BF16); t_wb = Trk()
            cw = sb1("cw", [128, 4, 4]); t_cw = Trk()
            qkg = sb1("qkg", [128, 4]); t_qkg = Trk()
            mng = sb1("mng", [128, 256]); t_mng = Trk()
            mngr = sb1("mngr", [1, 256]); t_mngr = Trk()
            kb.dma(cw[:], convw, writes=[t_cw])
            kb.dma(qkg[:], qk_g, writes=[t_qkg])
            kb.dma(mngr[:], mng_row, writes=[t_mngr])
            kb.op("pe", lambda e: e.matmul(ps[3][:, 0:256], lhsT=ones_f[0:1, :], rhs=mngr[0:1, :], start=True, stop=True),
                  reads=[t_ones_f, t_mngr], writes=[pst[3]])
            kb.op("dve", lambda e: e.tensor_copy(out=mng[:], in_=ps[3][:, 0:256]), reads=[pst[3]], writes=[t_mng])
            wst = [sb1("wst%d" % i, [128, 1680]) for i in range(2)]; t_wst = [Trk(), Trk()]
            wiv = w_in.rearrange("(k p) n -> p k n", p=128)
            for k in range(16):
                bi = k % 2
                kb.dma(wst[bi][:], wiv[:, k, :], writes=[t_wst[bi]])
                kb.op("dve", lambda e, k=k, bi=bi: e.tensor_copy(out=wb[:, k, :], in_=wst[bi][:]),
                      reads=[t_wst[bi]], writes=[t_wb])
            xt = [sb1("xt%d" % i, [128, 16, 512]) for i in range(2)]; t_xt = [Trk(), Trk()]
            xsq = sb1("xsq", [128, 16, 512], BF16); t_xsq = [Trk() for _ in range(16)]
            hT = sb1("hT", [128, 16, 512], BF16); t_hT = [Trk() for _ in range(16)]
            rstd = sb1("rstd", [128, 512]); t_rstd = Trk()
            tmp = [sb1("tmp%d" % i, [128, 512]) for i in range(2)]; t_tmp = [Trk(), Trk()]
            zr = [sb1("zr%d" % i, [128, 3 + 512]) for i in range(4)]; t_zr = [Trk() for _ in range(4)]
            acc = [sb1("acc%d" % i, [128, 512]) for i in range(2)]; t_acc = [Trk(), Trk()]
            ofm = [sb1("ofm%d" % i, [128, 512]) for i in range(2)]; t_ofm = [Trk(), Trk()]
            obf = [sb1("obf%d" % i, [128, 512], BF16) for i in range(2)]; t_obf = [Trk(), Trk()]
            sqb = sb1("sqb", [128, 512], BF16); t_sqb = Trk()
            rs2 = sb1("rs2", [128, 512]); t_rs2 = Trk()
            otm = [sb1("otm%d" % i, [128, 512]) for i in range(2)]; t_otm = [Trk(), Trk()]
            otb = [sb1("otb%d" % i, [128, 128], BF16) for i in range(2)]; t_otb = [Trk(), Trk()]
            otg = [sb1("otg%d" % i, [128, 12]) for i in range(2)]; t_otg = [Trk(), Trk()]
            for i in range(4):
                kb.op("dve", lambda e, i=i: e.memset(zr[i][:, 0:3], 0.0), writes=[t_zr[i]])
            xv = xT.rearrange("(k p) t -> p k t", p=128)
            cnt2 = [0]

            def rot2():
                cnt2[0] += 1
                return cnt2[0] % 2

            def load_x(n):
                bi = n % 2
                for h in range(4):
                    kb.dma(xt[bi][:, 4 * h:4 * h + 4, :], xv[:, 4 * h:4 * h + 4, n * 512:(n + 1) * 512], writes=[t_xt[bi]])

            load_x(0)
            for n in range(nb):
                bi = n % 2
                t0 = n * 512
                if n + 1 < nb:
                    load_x(n + 1)
                for k in range(16):
                    kb.op("act" if k % 2 else "dve",
                          (lambda e, k=k, bi=bi: e.activation(out=xsq[:, k, :], in_=xt[bi][:, k, :], func=AF.Square)) if k % 2 else
                          (lambda e, k=k, bi=bi: e.tensor_tensor(out=xsq[:, k, :], in0=xt[bi][:, k, :], in1=xt[bi][:, k, :], op=ALU.mult)),
                          reads=[t_xt[bi]], writes=[t_xsq[k]])
                for k in range(16):
                    kb.op("pe", lambda e, k=k: e.matmul(ps[0][:, :], lhsT=ones_b[:], rhs=xsq[:, k, :], start=(k == 0), stop=(k == 15)),
                          reads=[t_ones_b, t_xsq[k]], writes=[pst[0]], sig=(k == 15))
                kb.op("act", lambda e: e.activation(out=rstd[:], in_=ps[0][:, :], func=AF.Sqrt, scale=1.0 / D, bias=EPS),
                      reads=[pst[0]], writes=[t_rstd])
                kb.op("dve", lambda e: e.reciprocal(out=rstd[:], in_=rstd[:]), reads=[t_rstd], writes=[t_rstd])
                for k in range(16):
                    tb = k % 2
                    kb.op("dve", lambda e, k=k, tb=tb, bi=bi: e.tensor_tensor(out=tmp[tb][:], in0=xt[bi][:, k, :], in1=rstd[:], op=ALU.mult),
                          reads=[t_xt[bi], t_rstd], writes=[t_tmp[tb]])
                    kb.op("act", lambda e, k=k, tb=tb: e.activation(out=hT[:, k, :], in_=tmp[tb][:], func=AF.Identity,
                                                                    scale=gs1[:, k:k + 1], bias=sh1[:, k:k + 1]),
                          reads=[t_tmp[tb], t_gs1, t_sh1], writes=[t_hT[k]])
                for ct in range(9):
                    if not ({0: 'a', 1: 'a', 2: 'a', 3: 'a', 4: 'b', 5: 'b', 7: 'b', 6: 'c', 8: 'd'}[ct] in parts):
                        continue
                    c0 = ct * 128
                    cn = 128 if ct < 8 else 2
                    pb = 1 + (ct % 2)
                    for k in range(16):
                        kb.op("pe", lambda e, k=k, c0=c0, cn=cn, pb=pb: e.matmul(ps[pb][0:cn, :], lhsT=wb[:, k, c0:c0 + cn], rhs=hT[:, k, :],
                                                                                 start=(k == 0), stop=(k == 15)),
                              reads=[t_wb, t_hT[k]], writes=[pst[pb]], sig=(k == 15))
                    if ct < 4:
                        z = zr[ct]; tz = t_zr[ct]
                        kb.op("act", lambda e, z=z, pb=pb: e.activation(out=z[:, 3:515], in_=ps[pb][:, :], func=AF.Copy),
                              reads=[pst[pb]], writes=[tz])
                        ab = rot2()
                        a = acc[ab]; ta = t_acc[ab]
                        kb.op("dve", lambda e, z=z, a=a, ct=ct: e.tensor_scalar(out=a[:], in0=z[:, 3:515], scalar1=cw[:, ct, 3:4], scalar2=None,
                                                                                 op0=ALU.mult), reads=[tz, t_cw], writes=[ta])
                        for j in range(3):
                            kb.op("dve", lambda e, z=z, a=a, ct=ct, j=j: e.scalar_tensor_tensor(out=a[:], in0=z[:, j:j + 512], scalar=cw[:, ct, j:j + 1],
                                                                                               in1=a[:], op0=ALU.mult, op1=ALU.add),
                                  reads=[tz, t_cw, ta], writes=[ta])
                        ob = rot2()
                        kb.op("act", lambda e, a=a, ob=ob: e.activation(out=ofm[ob][:], in_=a[:], func=AF.Silu), reads=[ta], writes=[t_ofm[ob]])
                        dst = s_q if ct < 2 else s_k
                        r0 = (ct % 2) * 128
                        kb.dma(dst[r0:r0 + 128, t0:t0 + 512], ofm[ob][:], reads=[t_ofm[ob]], writes=[t_scr["q" if ct < 2 else "k"]])
                        kb.op("dve", lambda e, z=z: e.tensor_copy(out=z[:, 0:3], in_=z[:, 512:515]), reads=[tz], writes=[tz])
                    elif ct in (4, 5, 7):
                        kb.op("act", lambda e, pb=pb: e.activation(out=sqb[:], in_=ps[pb][:, :], func=AF.Square), reads=[pst[pb]], writes=[t_sqb])
                        kb.op("pe", lambda e: e.matmul(ps[3][:, :], lhsT=blk64[:], rhs=sqb[:], start=True, stop=True),
                              reads=[t_blk64, t_sqb], writes=[pst[3]])
                        kb.op("act", lambda e: e.activation(out=rs2[:], in_=ps[3][:, :], func=AF.Sqrt, scale=1.0 / 64, bias=EPS),
                              reads=[pst[3]], writes=[t_rs2])
                        kb.op("dve", lambda e: e.reciprocal(out=rs2[:], in_=rs2[:]), reads=[t_rs2], writes=[t_rs2])
                        ob = rot2()
                        gcol = 0 if ct in (4, 5) else 3
                        kb.op("dve", lambda e, pb=pb, ob=ob, gcol=gcol: e.scalar_tensor_tensor(out=obf[ob][:], in0=ps[pb][:, :], scalar=qkg[:, gcol:gcol + 1],
                                                                                              in1=rs2[:], op0=ALU.mult, op1=ALU.mult),
                              reads=[pst[pb], t_rs2, t_qkg], writes=[t_obf[ob]])
                        if ct == 7:
                            kb.dma(s_kk[:, t0:t0 + 512], obf[ob][:], reads=[t_obf[ob]], writes=[t_scr["kk"]])
                        else:
                            r0 = (ct - 4) * 128
                            kb.dma(s_aq[r0:r0 + 128, t0:t0 + 512], obf[ob][:], reads=[t_obf[ob]], writes=[t_scr["aq"]])
                    elif ct == 6:
                        ob = rot2()
                        kb.op("act", lambda e, pb=pb, ob=ob: e.activation(out=ofm[ob][:], in_=ps[pb][:, :], func=AF.Copy), reads=[pst[pb]], writes=[t_ofm[ob]])
                        kb.dma(s_kv[:, t0:t0 + 512], ofm[ob][:], reads=[t_ofm[ob]], writes=[t_scr["kv"]])
                    else:
                        ob = rot2()
                        kb.op("act", lambda e, pb=pb, ob=ob: e.activation(out=ofm[ob][0:2, :], in_=ps[pb][0:2, :], func=AF.Copy), reads=[pst[pb]], writes=[t_ofm[ob]])
                        kb.dma(s_if[:, t0:t0 + 512], ofm[ob][0:2, :], reads=[t_ofm[ob]], writes=[t_scr["if"]])
                for ts in range(int(os.environ.get('NTS', '4')) if 'e' in parts else 0):
                    tt = t0 + ts * 128
                    pb = 4 + (ts % 2)
                    for k in range(16):
                        kb.op("pe", lambda e, k=k, ts=ts, pb=pb: e.matmul(ps[pb][:, :], lhsT=hT[:, k, ts * 128:(ts + 1) * 128], rhs=wb[:, k, 1026:1538],
                                                                          start=(k == 0), stop=(k == 15)),
                              reads=[t_wb, t_hT[k]], writes=[pst[pb]], sig=(k == 15))
                    pb2 = 6 + (ts % 2)
                    for k in range(16):
                        kb.op("pe", lambda e, k=k, ts=ts, pb2=pb2: e.matmul(ps[pb2][:, 0:140], lhsT=hT[:, k, ts * 128:(ts + 1) * 128], rhs=wb[:, k, 1538:1678],
                                                                            start=(k == 0), stop=(k == 15)),
                              reads=[t_wb, t_hT[k]], writes=[pst[pb2]], sig=(k == 15))
                    ob = ts % 2
                    kb.op("dve", lambda e, pb=pb, ob=ob: e.tensor_copy(out=otm[ob][:, 0:256], in_=ps[pb][:, 0:256]), reads=[pst[pb]], writes=[t_otm[ob]])
                    kb.op("act", lambda e, pb=pb, ob=ob: e.activation(out=otm[ob][:, 256:512], in_=ps[pb][:, 256:512], func=AF.Sigmoid),
                          reads=[pst[pb]], writes=[t_otm[ob]])
                    SK = os.environ.get('SKIP', '')
                    if 'P' not in SK:
                        kb.op("dve", lambda e, ob=ob: e.tensor_tensor(out=otm[ob][:, 256:512], in0=otm[ob][:, 256:512], in1=mng[:], op=ALU.mult),
                              reads=[t_otm[ob], t_mng], writes=[t_otm[ob]])
                    if 'S' not in SK:
                        kb.dma(s_v[tt:tt + 128, :], otm[ob][:, 0:256], reads=[t_otm[ob]], writes=[t_scr["v"]])
                        kb.dma(s_go[tt:tt + 128, :], otm[ob][:, 256:512], reads=[t_otm[ob]], writes=[t_scr["go"]])
                    kb.op("dve", lambda e, pb2=pb2, ob=ob: e.tensor_copy(out=otb[ob][:], in_=ps[pb2][:, 0:128]), reads=[pst[pb2]], writes=[t_otb[ob]])
                    kb.op("act", lambda e, pb2=pb2, ob=ob: e.activation(out=otg[ob][:], in_=ps[pb2][:, 128:140], func=AF.Sigmoid),
                          reads=[pst[pb2]], writes=[t_otg[ob]])
                    if 'V' not in os.environ.get('SKIP', ''):
                        kb.dma(s_vv[tt:tt + 128, :], otb[ob][:], reads=[t_otb[ob]], writes=[t_scr["vv"]])
                    if 'G' not in os.environ.get('SKIP', ''):
                        kb.dma(s_gt[tt:tt + 128, :], otg[ob][:], reads=[t_otg[ob]], writes=[t_scr["gt"]])
            kb.barrier()


    mixT = [nc.dram_tensor("mixT%d" % i, [512, 1024], BF16, kind="Internal").ap() for i in range(8)]; t_mixT = Trk()
    if phases >= 2:
        bif_in = din("bif", [128, 2])
        ut_in = din("ut_f", [128, 128])
        identb_in = din("ident_b", [128, 128], BF16)
        with ExitStack() as p2:
            def sb2(name, shape, dt=F32):
                return p2.enter_context(nc.sbuf_tensor(name, list(shape), dt))
            NCH = 64
            bif = sb2("bif_sb", [128, 2]); t_bif = Trk()
            ut = sb2("ut_sb", [128, 128]); t_ut = Trk()
            identb = sb2("identb", [128, 128], BF16); t_identb = Trk()
            kb.dma(bif[:], bif_in, writes=[t_bif])
            kb.dma(ut[:], ut_in, writes=[t_ut])
            kb.dma(identb[:], identb_in, writes=[t_identb])
            rows = sb2("rows", [64, 2, 128]); t_rows = Trk()
            kb.dma(rows[:, 0, :], s_if[0:1, :].rearrange("o (c p) -> (o c) p", p=128), reads=[t_scr["if"]], writes=[t_rows])
            kb.dma(rows[:, 1, :], s_if[1:2, :].rearrange("o (c p) -> (o c) p", p=128), reads=[t_scr["if"]], writes=[t_rows])
            cols = sb2("cols", [128, 12, 64]); t_cols = Trk()
            nbf = sb2("nbf", [128, 1]); t_nbf = Trk()
            zero64 = sb2("zero64", [128, 128]); t_zero = Trk()
            kb.op("dve", lambda e: e.memset(zero64[:], 0.0), writes=[t_zero])
            kb.op("dve", lambda e: e.tensor_scalar(out=nbf[:], in0=bif[:, 1:2], scalar1=-1.0, scalar2=None, op0=ALU.mult), reads=[t_bif], writes=[t_nbf])
            for w in range(2):
                kb.op("pe", lambda e, w=w: e.matmul(ps[w][:, 0:64], lhsT=rows[:, w, :], rhs=ident[0:64, 0:64], start=True, stop=True),
                      reads=[t_rows, t_ident], writes=[pst[w]])
            kb.op("dve", lambda e: e.tensor_scalar(out=cols[:, 0, :], in0=ps[0][:, 0:64], scalar1=bif[:, 0:1], scalar2=None, op0=ALU.add),
                  reads=[pst[0], t_bif], writes=[t_cols])
            kb.op("act", lambda e: e.activation(out=cols[:, 11, :], in_=ps[1][:, 0:64], func=AF.Exp, scale=-1.0, bias=nbf[:, 0:1]),
                  reads=[pst[1], t_nbf], writes=[t_cols])
            kb.op("act", lambda e: e.activation(out=cols[:, 1, :], in_=cols[:, 11, :], func=AF.Ln, scale=1.0, bias=1.0), reads=[t_cols], writes=[t_cols])
            kb.op("dve", lambda e: e.tensor_scalar(out=cols[:, 1, :], in0=cols[:, 1, :], scalar1=-1.0, scalar2=None, op0=ALU.mult), reads=[t_cols], writes=[t_cols])
            kb.op("pe", lambda e: e.matmul(ps[2][:, 0:64], lhsT=ut[:], rhs=cols[:, 1, :], start=True, stop=True), reads=[t_ut, t_cols], writes=[pst[2]])
            kb.op("pe", lambda e: e.matmul(ps[3][:, 0:64], lhsT=ones_f[:], rhs=cols[:, 1, :], start=True, stop=True), reads=[t_ones_f, t_cols], writes=[pst[3]])
            kb.op("dve", lambda e: e.tensor_copy(out=cols[:, 11, :], in_=ps[3][:, 0:64]), reads=[pst[3]], writes=[t_cols])
            kb.op("dve", lambda e: e.tensor_tensor_scan(out=cols[:, 2, :], data0=cols[:, 11, :], data1=zero64[:, 0:64], initial=0.0, op0=ALU.add, op1=ALU.add),
                  reads=[t_cols, t_zero], writes=[t_cols])
            kb.op("dve", lambda e: e.tensor_tensor(out=cols[:, 2, :], in0=cols[:, 2, :], in1=cols[:, 11, :], op=ALU.subtract), reads=[t_cols], writes=[t_cols])
            kb.op("dve", lambda e: e.tensor_tensor(out=cols[:, 2, :], in0=cols[:, 2, :], in1=ps[2][:, 0:64], op=ALU.add), reads=[t_cols, pst[2]], writes=[t_cols])
            kb.op("dve", lambda e: e.tensor_tensor(out=cols[:, 3, :], in0=cols[:, 0, :], in1=cols[:, 2, :], op=ALU.subtract), reads=[t_cols], writes=[t_cols])
            grow = sb2("grow", [64, 128]); t_grow = Trk()
            cmrow = sb2("cmrow", [64, 128]); t_cmrow = Trk()
            kb.op("pe", lambda e: e.matmul(ps[4][0:64, 0:128], lhsT=cols[:, 3, :], rhs=ident[:, :], start=True, stop=True), reads=[t_cols, t_ident], writes=[pst[4]])
            kb.op("dve", lambda e: e.tensor_copy(out=grow[:], in_=ps[4][0:64, 0:128]), reads=[pst[4]], writes=[t_grow])
            kb.op("dve", lambda e: e.tensor_tensor_scan(out=cmrow[:], data0=grow[:], data1=grow[:], initial=-1e30, op0=ALU.max, op1=ALU.max),
                  reads=[t_grow], writes=[t_cmrow])
            mrow = sb2("mrow", [1, 3, 64]); t_mrow = Trk()
            kb.op("pe", lambda e: e.matmul(ps[5][0:1, 0:64], lhsT=cmrow[:, 127:128], rhs=ident[0:64, 0:64], start=True, stop=True),
                  reads=[t_cmrow, t_ident], writes=[pst[5]])
            kb.op("dve", lambda e: e.tensor_copy(out=mrow[0:1, 0, :], in_=ps[5][0:1, 0:64]), reads=[pst[5]], writes=[t_mrow])
            kb.op("dve", lambda e: e.tensor_tensor_scan(out=mrow[0:1, 1, :], data0=mrow[0:1, 0, :], data1=mrow[0:1, 0, :], initial=0.0, op0=ALU.max, op1=ALU.max),
                  reads=[t_mrow], writes=[t_mrow])
            kb.op("dve", lambda e: e.memset(mrow[0:1, 2, 0:1], 0.0), reads=[t_mrow], writes=[t_mrow])
            kb.op("dve", lambda e: e.tensor_copy(out=mrow[0:1, 2, 1:64], in_=mrow[0:1, 1, 0:63]), reads=[t_mrow], writes=[t_mrow])
            kb.op("pe", lambda e: e.matmul(ps[6][:, 0:128], lhsT=ones_f[0:1, :], rhs=mrow[0:1, 1:3, :].rearrange("o a c -> o (a c)"), start=True, stop=True),
                  reads=[t_ones_f, t_mrow], writes=[pst[6]])
            kb.op("dve", lambda e: e.tensor_copy(out=cols[:, 6, :], in_=ps[6][:, 0:64]), reads=[pst[6]], writes=[t_cols])
            kb.op("dve", lambda e: e.tensor_copy(out=cols[:, 5, :], in_=ps[6][:, 64:128]), reads=[pst[6]], writes=[t_cols])
            kb.op("pe", lambda e: e.matmul(ps[7][:, 0:64], lhsT=cmrow[:, :], rhs=ident[0:64, 0:64], start=True, stop=True), reads=[t_cmrow, t_ident], writes=[pst[7]])
            kb.op("dve", lambda e: e.tensor_tensor(out=cols[:, 4, :], in0=ps[7][:, 0:64], in1=cols[:, 5, :], op=ALU.max), reads=[pst[7], t_cols], writes=[t_cols])
            kb.op("dve", lambda e: e.tensor_tensor(out=cols[:, 11, :], in0=cols[:, 3, :], in1=cols[:, 5, :], op=ALU.subtract), reads=[t_cols], writes=[t_cols])
            kb.op("act", lambda e: e.activation(out=cols[:, 7, :], in_=cols[:, 11, :], func=AF.Exp), reads=[t_cols], writes=[t_cols])
            kb.op("dve", lambda e: e.tensor_scalar(out=cols[:, 7, :], in0=cols[:, 7, :], scalar1=1.0 / 16, scalar2=None, op0=ALU.mult), reads=[t_cols], writes=[t_cols])
            kb.op("dve", lambda e: e.tensor_tensor(out=cols[:, 11, :], in0=cols[:, 5, :], in1=cols[:, 4, :], op=ALU.subtract), reads=[t_cols], writes=[t_cols])
            kb.op("act", lambda e: e.activation(out=cols[:, 8, :], in_=cols[:, 11, :], func=AF.Exp), reads=[t_cols], writes=[t_cols])
            kb.op("dve", lambda e: e.tensor_tensor(out=cols[:, 11, :], in0=cols[:, 2, :], in1=cols[:, 4, :], op=ALU.add), reads=[t_cols], writes=[t_cols])
            kb.op("act", lambda e: e.activation(out=cols[:, 9, :], in_=cols[:, 11, :], func=AF.Exp, scale=-1.0), reads=[t_cols], writes=[t_cols])
            kb.op("dve", lambda e: e.tensor_tensor(out=cols[:, 11, :], in0=cols[:, 5, :], in1=cols[:, 6, :], op=ALU.subtract), reads=[t_cols], writes=[t_cols])
            kb.op("act", lambda e: e.activation(out=cols[:, 10, :], in_=cols[:, 11, :], func=AF.Exp), reads=[t_cols], writes=[t_cols])
            if dbg:
                d_cols = nc.dram_tensor("d_cols", [128, 12, 64], F32, kind="ExternalOutput").ap()
                kb.dma(d_cols, cols[:], reads=[t_cols])

            qT = [sb2("qT%d" % i, [128, 2, 128]) for i in range(2)]; t_qT = [Trk(), Trk()]
            kT = [sb2("kT%d" % i, [128, 2, 128]) for i in range(2)]; t_kT = [Trk(), Trk()]
            va = [sb2("va%d" % i, [128, 257]) for i in range(2)]; t_va = [Trk(), Trk()]
            go = [sb2("go%d" % i, [128, 256]) for i in range(2)]; t_go = [Trk(), Trk()]
            kp = sb2("kp", [128, 256]); t_kp = Trk()
            wT = sb2("wT", [128, 128]); t_wT = Trk()
            St = [sb2("St%d" % i, [128, 2, 257]) for i in range(2)]; t_St = [Trk(), Trk()]
            sc = sb2("sc", [128, 8]); t_sc = Trk()
            junk = sb2("junk", [128, 256]); t_junk = Trk()
            hmf = sb2("hmf", [128, 256], BF16); t_hmf = Trk()
            hmT = [sb2("hmT%d" % i, [128, 2, 128], BF16) for i in range(2)]; t_hmT = [Trk(), Trk()]
            for i in range(2):
                kb.op("dve", lambda e, i=i: e.memset(va[i][:, 256:257], 1.0), writes=[t_va[i]])
            kb.op("dve", lambda e: e.memset(St[0][:], 0.0), writes=[t_St[0]])
            qv = s_q.rearrange("(a p) t -> p a t", p=128)
            kv_ = s_k.rearrange("(a p) t -> p a t", p=128)

            def load_chunk(c):
                bi = c % 2
                kb.dma(qT[bi][:], qv[:, :, c * 128:(c + 1) * 128], reads=[t_scr["q"]], writes=[t_qT[bi]])
                kb.dma(kT[bi][:], kv_[:, :, c * 128:(c + 1) * 128], reads=[t_scr["k"]], writes=[t_kT[bi]])
                kb.dma(va[bi][:, 0:256], s_v[c * 128:(c + 1) * 128, :], reads=[t_scr["v"]], writes=[t_va[bi]])
                kb.dma(go[bi][:], s_go[c * 128:(c + 1) * 128, :], reads=[t_scr["go"]], writes=[t_go[bi]])

            nch = int(os.environ.get("NCH", "64"))
            load_chunk(0)
            for c in range(nch):
                bi = c % 2
                so = St[c % 2]; tso = t_St[c % 2]
                sn = St[(c + 1) % 2]; tsn = t_St[(c + 1) % 2]
                if c + 1 < nch:
                    load_chunk(c + 1)
                for dc in range(2):
                    kb.op("pe", lambda e, dc=dc, bi=bi: e.matmul(ps[0][:, dc * 128:(dc + 1) * 128], lhsT=kT[bi][:, dc, :], rhs=ident[:, :], start=True, stop=True),
                          reads=[t_kT[bi], t_ident], writes=[pst[0]], sig=(dc == 1))
                kb.op("dve", lambda e, c=c: e.tensor_scalar(out=kp[:], in0=ps[0][:, 0:256], scalar1=cols[:, 7, c:c + 1], scalar2=None, op0=ALU.mult),
                      reads=[pst[0], t_cols], writes=[t_kp])
                for dc in range(2):
                    kb.op("pe", lambda e, dc=dc, bi=bi: e.matmul(ps[1][:, 0:128], lhsT=kT[bi][:, dc, :], rhs=qT[bi][:, dc, :], start=(dc == 0), stop=(dc == 1)),
                          reads=[t_kT[bi], t_qT[bi]], writes=[pst[1]], sig=(dc == 1))
                kb.op("dve", lambda e, c=c: e.scalar_tensor_tensor(out=wT[:], in0=ps[1][:, 0:128], scalar=cols[:, 7, c:c + 1], in1=ut[:], op0=ALU.mult, op1=ALU.mult),
                      reads=[pst[1], t_cols, t_ut], writes=[t_wT])
                kb.op("pe", lambda e, bi=bi: e.matmul(ps[2][:, 0:257], lhsT=wT[:], rhs=va[bi][:], start=True, stop=False),
                      reads=[t_wT, t_va[bi]], writes=[pst[2]], sig=False)
                for dc in range(2):
                    kb.op("pe", lambda e, dc=dc, bi=bi, so=so: e.matmul(ps[2][:, 0:257], lhsT=qT[bi][:, dc, :], rhs=so[:, dc, :], start=False, stop=(dc == 1)),
                          reads=[t_qT[bi], tso], writes=[pst[2]], sig=(dc == 1))
                for dc in range(2):
                    pb = 3 + dc
                    kb.op("pe", lambda e, dc=dc, bi=bi, pb=pb: e.matmul(ps[pb][:, 0:257], lhsT=kp[:, dc * 128:(dc + 1) * 128], rhs=va[bi][:], start=True, stop=False),
                          reads=[t_kp, t_va[bi]], writes=[pst[pb]], sig=False)
                    kb.op("pe", lambda e, dc=dc, pb=pb, so=so: e.matmul(ps[pb][:, 0:257], lhsT=ident[:, :], rhs=so[:, dc, :], start=False, stop=True),
                          reads=[t_ident, tso], writes=[pst[pb]])
                    kb.op("act" if dc else "dve",
                          (lambda e, dc=dc, pb=pb, sn=sn, c=c: e.activation(out=sn[:, dc, :], in_=ps[pb][:, 0:257], func=AF.Copy, scale=cols[:, 10, c:c + 1])) if dc else
                          (lambda e, dc=dc, pb=pb, sn=sn, c=c: e.tensor_scalar(out=sn[:, dc, :], in0=ps[pb][:, 0:257], scalar1=cols[:, 10, c:c + 1], scalar2=None, op0=ALU.mult)),
                          reads=[pst[pb], t_cols], writes=[tsn])
                kb.op("act", lambda e, c=c: e.activation(out=sc[:, 0:1], in_=ps[2][:, 256:257], func=AF.Abs, scale=cols[:, 8, c:c + 1]),
                      reads=[pst[2], t_cols], writes=[t_sc])
                kb.op("dve", lambda e, c=c: e.tensor_tensor(out=sc[:, 1:2], in0=sc[:, 0:1], in1=cols[:, 9, c:c + 1], op=ALU.max), reads=[t_sc, t_cols], writes=[t_sc])
                kb.op("dve", lambda e: e.reciprocal(out=sc[:, 2:3], in_=sc[:, 1:2]), reads=[t_sc], writes=[t_sc])
                kb.op("dve", lambda e, c=c: e.tensor_tensor(out=sc[:, 3:4], in0=sc[:, 2:3], in1=cols[:, 8, c:c + 1], op=ALU.mult), reads=[t_sc, t_cols], writes=[t_sc])
                kb.op("act", lambda e: e.activation(out=junk[:], in_=ps[2][:, 0:256], func=AF.Square, accum_out=sc[:, 4:5]), reads=[pst[2]], writes=[t_junk, t_sc])
                kb.op("dve", lambda e: e.scalar_tensor_tensor(out=sc[:, 5:6], in0=sc[:, 3:4], scalar=sc[:, 3:4], in1=sc[:, 4:5], op0=ALU.mult, op1=ALU.mult),
                      reads=[t_sc], writes=[t_sc])
                kb.op("act", lambda e: e.activation(out=sc[:, 5:6], in_=sc[:, 5:6], func=AF.Sqrt, scale=1.0 / 256, bias=EPS), reads=[t_sc], writes=[t_sc])
                kb.op("dve", lambda e: e.reciprocal(out=sc[:, 6:7], in_=sc[:, 5:6]), reads=[t_sc], writes=[t_sc])
                kb.op("dve", lambda e: e.tensor_tensor(out=sc[:, 7:8], in0=sc[:, 6:7], in1=sc[:, 3:4], op=ALU.mult), reads=[t_sc], writes=[t_sc])
                kb.op("dve", lambda e, bi=bi: e.scalar_tensor_tensor(out=hmf[:], in0=ps[2][:, 0:256], scalar=sc[:, 7:8], in1=go[bi][:], op0=ALU.mult, op1=ALU.mult),
                      reads=[pst[2], t_sc, t_go[bi]], writes=[t_hmf])
                ob = c % 2
                for dc in range(2):
                    kb.op("pe", lambda e, dc=dc: e.matmul(ps[5][:, dc * 128:(dc + 1) * 128], lhsT=hmf[:, dc * 128:(dc + 1) * 128], rhs=identb[:, :], start=True, stop=True),
                          reads=[t_hmf, t_identb], writes=[pst[5]], sig=(dc == 1))
                kb.op("act", lambda e, ob=ob: e.activation(out=hmT[ob][:].rearrange("p a t -> p (a t)"), in_=ps[5][:, 0:256], func=AF.Copy), reads=[pst[5]], writes=[t_hmT[ob]])
                kb.dma(mixT[c // 8][0:256, (c % 8) * 128:(c % 8 + 1) * 128].rearrange("(a p) t -> p a t", p=128), hmT[ob][:], reads=[t_hmT[ob]], writes=[t_mixT])
            kb.barrier()

    if phases >= 3:
        tb_sw_in = din("tb_sw", [128, 64, 4])
        tb_c_in = din("tb_c", [128, 64, 4])
        cmask_in = din("cmask", [128, 17, 128], BF16)
        caus_in = din("caus_add", [128, 128], BF16)
        acaus_in = din("acaus_add", [128, 128], BF16)
        ovl_in = din("ovl", [128, 4, 128], BF16)
        expT_in = din("expT", [128, S], BF16)
        tkeep_in = din("t_keep", [128, 255])
        tadd_in = din("t_add", [128, 255])
        w1k_in = din("w1k", [2048, 256]); w1v_in = din("w1v", [2048, 256])
        w2k_in = din("w2k", [256, 64]); w2v_in = din("w2v", [256, 64])
        posk_in = din("posk", [128, 16]); posv_in = din("posv", [128, 16])
        kng0_in = din("kng0", [64, 1])
        with ExitStack() as p3:
            def sb3(name, shape, dt=F32):
                return p3.enter_context(nc.sbuf_tensor(name, list(shape), dt))
            QT = sb3("QT", [128, 4, S], BF16); t_QT = Trk()
            KK = sb3("KK", [128, S], BF16); t_KK = Trk()
            Vs = sb3("Vs", [128, 64, 65], BF16); t_Vs = Trk()
            Vw = sb3("Vw", [128, 64, 65], BF16); t_Vw = Trk()
            expT = sb3("expT_sb", [128, S], BF16); t_expT = Trk()
            tb_sw = sb3("tb_sw_sb", [128, 64, 4]); t_tbsw = Trk()
            tb_c = sb3("tb_c_sb", [128, 64, 4]); t_tbc = Trk()
            cmask = sb3("cmask_sb", [128, 17, 128], BF16); t_cmask = Trk()
            caus = sb3("caus_sb", [128, 4, 128], BF16); t_caus = Trk()
            acaus = sb3("acaus_sb", [128, 4, 128], BF16); t_acaus = Trk()
            ovl = sb3("ovl_sb", [128, 4, 128], BF16); t_ovl = Trk()
            tkeep = sb3("tkeep_sb", [128, 255]); t_tkeep = Trk()
            tadd = sb3("tadd_sb", [128, 255]); t_tadd = Trk()
            gts = sb3("gts", [128, 64, 12]); t_gts = Trk()
            kcT = sb3("kcT", [64, 512], BF16); t_kcT = Trk()
            vca = sb3("vca", [128, 4, 65], BF16); t_vca = Trk()
            identb3 = sb3("identb3", [128, 128], BF16); t_identb3 = Trk()
            aqv = s_aq.rearrange("(h d) t -> d h t", d=64)
            for hh in range(2):
                for h in range(4):
                    kb.dma(QT[hh * 64:(hh + 1) * 64, h, :], aqv[:, h, :], reads=[t_scr["aq"]], writes=[t_QT])
            kb.dma(KK[:, 0:4096], s_kk[:, 0:4096], reads=[t_scr["kk"]], writes=[t_KK])
            kb.dma(KK[:, 4096:S], s_kk[:, 4096:S], reads=[t_scr["kk"]], writes=[t_KK])
            kb.dma(expT[:], expT_in, writes=[t_expT])
            kb.dma(tb_sw[:], tb_sw_in, writes=[t_tbsw]); kb.dma(tb_c[:], tb_c_in, writes=[t_tbc])
            kb.dma(cmask[:], cmask_in, writes=[t_cmask])
            for h4 in range(4):
                kb.dma(caus[:, h4, :], caus_in, writes=[t_caus]); kb.dma(acaus[:, h4, :], acaus_in, writes=[t_acaus])
            kb.dma(ovl[:], ovl_in, writes=[t_ovl]); kb.dma(tkeep[:], tkeep_in, writes=[t_tkeep]); kb.dma(tadd[:], tadd_in, writes=[t_tadd])
            kb.dma(identb3[:], identb_in if phases >= 2 else din("ident_b", [128, 128], BF16), writes=[t_identb3])
            kb.dma(gts[:], s_gt.rearrange("(q p) c -> p q c", p=128), reads=[t_scr["gt"]], writes=[t_gts])
            with ExitStack() as p3a:
                def sb3a(name, shape, dt=F32):
                    return p3a.enter_context(nc.sbuf_tensor(name, list(shape), dt))
                vtmp = sb3a("vtmp", [128, 64, 128], BF16); t_vtmp = Trk()
                kb.dma(vtmp[:], s_vv.rearrange("(q p) c -> p q c", p=128), reads=[t_scr["vv"]], writes=[t_vtmp])
                kb.op("dve", lambda e: e.tensor_copy(out=Vs[:, :, 0:64], in_=vtmp[:, :, 0:64]), reads=[t_vtmp], writes=[t_Vs])
                kb.op("dve", lambda e: e.tensor_copy(out=Vw[:, :, 0:64], in_=vtmp[:, :, 64:128]), reads=[t_vtmp], writes=[t_Vw])
                kb.op("dve", lambda e: e.memset(Vs[:, :, 64:65], 1.0), writes=[t_Vs])
                kb.op("dve", lambda e: e.memset(Vw[:, :, 64:65], 1.0), writes=[t_Vw])
                kb.op("dve", lambda e: e.memset(vca[:, :, 64:65], 1.0), writes=[t_vca])
                w1f = sb3a("w1f", [128, 16, 256]); t_w1f = Trk()
                w1b = sb3a("w1b", [128, 16, 256], BF16); t_w1b = Trk()
                w2f = sb3a("w2f", [128, 2, 64]); t_w2f = Trk()
                w2b = sb3a("w2b", [128, 2, 64], BF16); t_w2b = Trk()
                posf = sb3a("posf", [128, 16]); t_posf = Trk()
                bcol = sb3a("bcol", [128, 2]); t_bcol = Trk()
                kng0 = sb3a("kng0_sb", [64, 1]); t_kng0 = Trk()
                a2f = sb3a("a2f", [128, 2048]); t_a2f = Trk()
                a2b = sb3a("a2b", [128, S], BF16); t_a2b = Trk()
                hid = sb3a("hid", [128, 2, 512], BF16); t_hid = Trk()
                csq = sb3a("csq", [64, 512], BF16); t_csq = Trk()
                crs = sb3a("crs", [64, 512]); t_crs = Trk()
                kb.dma(kng0[:], kng0_in, writes=[t_kng0])
                kb.op("dve", lambda e: e.memset(hid[:], 0.0), writes=[t_hid])
                kb.op("dve", lambda e: e.memset(kcT[:], 0.0), writes=[t_kcT])
                for which in range(2):
                    w1_in = w1k_in if which == 0 else w1v_in
                    w2_in = w2k_in if which == 0 else w2v_in
                    pos_in = posk_in if which == 0 else posv_in
                    r0 = 0 if which == 0 else 64
                    kb.dma(w1f[:, 0:8, :], w1_in.rearrange("(k p) n -> p k n", p=128)[:, 0:8, :], writes=[t_w1f])
                    kb.dma(w1f[:, 8:16, :], w1_in.rearrange("(k p) n -> p k n", p=128)[:, 8:16, :], writes=[t_w1f])
                    kb.dma(w2f[:], w2_in.rearrange("(k p) n -> p k n", p=128), writes=[t_w2f])
                    kb.dma(posf[:], pos_in, writes=[t_posf])
                    kb.op("dve", lambda e: e.tensor_copy(out=w1b[:], in_=w1f[:]), reads=[t_w1f], writes=[t_w1b])
                    kb.op("dve", lambda e: e.tensor_copy(out=w2b[:], in_=w2f[:]), reads=[t_w2f], writes=[t_w2b])
                    for q4 in range(4):
                        c0 = q4 * 2048
                        kb.dma(a2f[0:64, :], s_kv[r0:r0 + 64, c0:c0 + 2048], reads=[t_scr["kv"]], writes=[t_a2f])
                        if q4 < 3:
                            kb.dma(a2f[64:128, :], s_kv[r0:r0 + 64, c0 + 1:c0 + 2049], reads=[t_scr["kv"]], writes=[t_a2f])
                        else:
                            kb.dma(a2f[64:128, 0:2047], s_kv[r0:r0 + 64, c0 + 1:c0 + 2048], reads=[t_scr["kv"]], writes=[t_a2f])
                        kb.op("dve", lambda e, c0=c0: e.tensor_copy(out=a2b[:, c0:c0 + 2048], in_=a2f[:]), reads=[t_a2f], writes=[t_a2b])
                    for hc in range(2):
                        for jj in range(16):
                            kb.op("pe", lambda e, hc=hc, jj=jj: e.matmul(ps[7][:, hc:hc + 1], lhsT=w1f[:, jj, hc * 128:(hc + 1) * 128], rhs=posf[:, jj:jj + 1],
                                                                          start=(jj == 0), stop=(jj == 15)),
                                  reads=[t_w1f, t_posf], writes=[pst[7]], sig=(jj == 15))
                    kb.op("dve", lambda e: e.tensor_copy(out=bcol[:], in_=ps[7][:, 0:2]), reads=[pst[7]], writes=[t_bcol])
                    a2v = a2b[:].rearrange("p (i s) -> p i s", s=16)
                    for hc in range(2):
                        for jj in range(16):
                            j0 = 2 * jj
                            rhs = a2v[:, 0:511, j0] if j0 < 16 else a2v[:, 1:512, j0 - 16]
                            kb.op("pe", lambda e, hc=hc, jj=jj, rhs=rhs: e.matmul(ps[hc][:, 0:511], lhsT=w1b[:, jj, hc * 128:(hc + 1) * 128], rhs=rhs,
                                                                                  start=(jj == 0), stop=(jj == 15)),
                                  reads=[t_w1b, t_a2b], writes=[pst[hc]], sig=(jj == 15))
                        kb.op("act", lambda e, hc=hc: e.activation(out=hid[:, hc, 0:511], in_=ps[hc][:, 0:511], func=AF.Gelu_apprx_tanh, bias=bcol[:, hc:hc + 1]),
                              reads=[pst[hc], t_bcol], writes=[t_hid])
                    if which == 0:
                        for hc in range(2):
                            kb.op("pe", lambda e, hc=hc: e.matmul(ps[2][0:64, 0:511], lhsT=w2b[:, hc, :], rhs=hid[:, hc, 0:511], start=(hc == 0), stop=(hc == 1)),
                                  reads=[t_w2b, t_hid], writes=[pst[2]], sig=(hc == 1))
                        kb.op("act", lambda e: e.activation(out=csq[:, 0:511], in_=ps[2][0:64, 0:511], func=AF.Square), reads=[pst[2]], writes=[t_csq])
                        kb.op("pe", lambda e: e.matmul(ps[3][0:64, 0:511], lhsT=blk64[0:64, 0:64], rhs=csq[:, 0:511], start=True, stop=True),
                              reads=[t_blk64, t_csq], writes=[pst[3]])
                        kb.op("act", lambda e: e.activation(out=crs[:, 0:511], in_=ps[3][0:64, 0:511], func=AF.Sqrt, scale=1.0 / 64, bias=EPS), reads=[pst[3]], writes=[t_crs])
                        kb.op("dve", lambda e: e.reciprocal(out=crs[:, 0:511], in_=crs[:, 0:511]), reads=[t_crs], writes=[t_crs])
                        kb.op("dve", lambda e: e.scalar_tensor_tensor(out=kcT[:, 0:511], in0=ps[2][0:64, 0:511], scalar=kng0[:, 0:1], in1=crs[:, 0:511],
                                                                      op0=ALU.mult, op1=ALU.mult), reads=[pst[2], t_kng0, t_crs], writes=[t_kcT])
                    else:
                        for it in range(4):
                            for hc in range(2):
                                kb.op("pe", lambda e, hc=hc, it=it: e.matmul(ps[4][:, it * 64:(it + 1) * 64], lhsT=hid[:, hc, it * 128:(it + 1) * 128], rhs=w2b[:, hc, :],
                                                                             start=(hc == 0), stop=(hc == 1)),
                                      reads=[t_hid, t_w2b], writes=[pst[4]], sig=(hc == 1 and it == 3))
                        kb.op("dve", lambda e: e.tensor_copy(out=vca[:, :, 0:64], in_=ps[4][:, 0:256].rearrange("p (a d) -> p a d", d=64)), reads=[pst[4]], writes=[t_vca])
                if dbg:
                    d_kcT = nc.dram_tensor("d_kcT", [64, 512], BF16, kind="ExternalOutput").ap()
                    d_vca = nc.dram_tensor("d_vca", [128, 4, 65], BF16, kind="ExternalOutput").ap()
                    kb.dma(d_kcT, kcT[:], reads=[t_kcT]); kb.dma(d_vca, vca[:], reads=[t_vca])
                kb.barrier()

            pT = [sb3("pT%d" % i, [128, 512], BF16) for i in range(3)]; t_pT = [[Trk() for _ in range(4)] for _ in range(3)]
            oT = sb3("oT", [65, 3, 512]); t_oT = Trk()
            zsc = sb3("zsc", [128, 24]); t_zsc = Trk()
            impn = sb3("impn", [128, 128]); t_impn = Trk()
            impw = sb3("impw", [128, 128]); t_impw = Trk()
            m8 = sb3("m8", [128, 16]); t_m8 = Trk()
            selb = sb3("selb", [128, 128], BF16); t_selb = Trk()
            selT = sb3("selT", [128, 4, 128], BF16); t_selT = Trk()
            haf = sb3("haf", [128, 256]); t_haf = Trk()
            hab = sb3("hab", [128, 256], BF16); t_hab = Trk()
            haT = [sb3("haT%d" % i, [128, 2, 128], BF16) for i in range(2)]; t_haT = [Trk(), Trk()]
            if dbg:
                d_imp = nc.dram_tensor("d_imp", [S, 128], F32, kind="ExternalOutput").ap()
                d_sel = nc.dram_tensor("d_sel", [S, 128], BF16, kind="ExternalOutput").ap()
            pcount = [0]
            NEG = -30000.0

            pend = []

            def flush_pairs():
                while pend:
                    pend.pop(0)()

            def tile_pair(lhsK, rhsQ, masks, bias_tab, bidx, Vaug, obank, first, last, imp_it=None):
                i = pcount[0] % 2
                pi = pcount[0] % 3
                pcount[0] += 1
                sps = ps[i]; tsp = pst[i]
                nm = len(masks)
                kb.op("pe", lambda e: e.matmul(sps[:, :], lhsT=lhsK[0], rhs=rhsQ[0], start=True, stop=(nm == 0)),
                      reads=[lhsK[1], rhsQ[1]], writes=[tsp], sig=(nm == 0))
                for mi, (ml, mr, mt) in enumerate(masks):
                    lastm = (mi == nm - 1)
                    if mr.shape[-1] == 512 or len(mr.shape) == 3:
                        kb.op("pe", lambda e, ml=ml, mr=mr, lastm=lastm: e.matmul(sps[:, :], lhsT=ml, rhs=mr, start=False, stop=lastm),
                              reads=mt, writes=[tsp], sig=lastm)
                    else:
                        for h in range(4):
                            lm = (lastm and h == 3)
                            kb.op("pe", lambda e, ml=ml, mr=mr, h=h, lm=lm: e.matmul(sps[:, h * 128:(h + 1) * 128], lhsT=ml, rhs=mr, start=False, stop=lm),
                                  reads=mt, writes=[tsp], sig=lm)
                for h in range(4):
                    kb.op("act", lambda e, h=h: e.activation(out=pT[pi][:, h * 128:(h + 1) * 128], in_=sps[:, h * 128:(h + 1) * 128], func=AF.Exp,
                                                             bias=bias_tab[0][:, bidx, h:h + 1]),
                          reads=[tsp, bias_tab[1]], writes=[t_pT[pi][h]])

                def stage_b():
                    kb.op("pe", lambda e: e.matmul(ps[obank][0:65, :], lhsT=Vaug[0], rhs=pT[pi][:, :], start=first, stop=last),
                          reads=[Vaug[1]] + t_pT[pi], writes=[pst[obank]], sig=last)
                    if imp_it is not None:
                        it, nit = imp_it
                        for h in range(4):
                            kb.op("pe", lambda e, h=h, it=it: e.matmul(ps[5][:, h * 128:(h + 1) * 128], lhsT=pT[pi][:, h * 128:(h + 1) * 128], rhs=ovl[:, it, :],
                                                                       start=(it == 0), stop=(it == nit - 1)),
                                  reads=[t_pT[pi][h], t_ovl], writes=[pst[5]], sig=(it == nit - 1 and h == 3))
                while len(pend) > 1:
                    pend.pop(0)()
                pend.append(stage_b)
                if len(pend) > 1:
                    pend.pop(0)()

            nqt = int(os.environ.get("NQT", "64"))
            for qt in range(nqt):
                q0 = qt * 128
                qv_lo = (QT[0:64, :, q0:q0 + 128], t_QT)
                qv_hi = (QT[64:128, :, q0:q0 + 128], t_QT)
                nit = (8 * qt + 6) // 128 + 1
                for it in range(nit):
                    m = qt - 16 * it
                    masks = []
                    if m <= 16:
                        masks.append((identb3[:, :], cmask[:, m, :], [t_identb3, t_cmask]))
                    tile_pair((kcT[0:64, it * 128:(it + 1) * 128], t_kcT), qv_lo, masks, (tb_c, t_tbc), qt - 16 * it, (vca[:, it, :], t_vca), 2,
                              it == 0, it == nit - 1, imp_it=(it, nit))
                kts = [kt for kt in range(qt - 4, qt + 1) if kt >= 0]
                for n_, kt in enumerate(kts):
                    masks = []
                    if kt == qt:
                        masks.append((identb3[:, :], caus[:, :, :], [t_identb3, t_caus]))
                    if kt == qt - 4:
                        masks.append((identb3[:, :], acaus[:, :, :], [t_identb3, t_acaus]))
                    tile_pair((KK[64:128, kt * 128:(kt + 1) * 128], t_KK), qv_hi, masks, (tb_sw, t_tbsw), qt - kt, (Vw[:, kt, :], t_Vw), 4,
                              n_ == 0, n_ == len(kts) - 1)
                flush_pairs()
                kb.op("dve", lambda e: e.tensor_copy(out=oT[:, 0, :], in_=ps[2][0:65, :]), reads=[pst[2]], writes=[t_oT])
                for h in range(4):
                    kb.op("pe", lambda e, h=h: e.matmul(ps[6][:, h * 65:(h + 1) * 65], lhsT=oT[0:65, 0, h * 128:(h + 1) * 128], rhs=ident[0:65, 0:65], start=True, stop=True),
                          reads=[t_oT, t_ident], writes=[pst[6]], sig=(h == 3))
                kb.op("dve", lambda e: e.tensor_scalar(out=zsc[:, 0:4], in0=ps[6][:, 0:260].rearrange("p (h c) -> p h c", c=65)[:, :, 64], scalar1=1e-30, scalar2=None, op0=ALU.max),
                      reads=[pst[6]], writes=[t_zsc])
                kb.op("dve", lambda e: e.reciprocal(out=zsc[:, 0:4], in_=zsc[:, 0:4]), reads=[t_zsc], writes=[t_zsc])
                kb.op("dve", lambda e: e.tensor_scalar(out=impn[:], in0=ps[5][:, 0:128], scalar1=zsc[:, 0:1], scalar2=None, op0=ALU.mult), reads=[pst[5], t_zsc], writes=[t_impn])
                for h in range(1, 4):
                    kb.op("dve", lambda e, h=h: e.scalar_tensor_tensor(out=impn[:], in0=ps[5][:, h * 128:(h + 1) * 128], scalar=zsc[:, h:h + 1], in1=impn[:], op0=ALU.mult, op1=ALU.add),
                          reads=[pst[5], t_zsc, t_impn], writes=[t_impn])
                if dbg:
                    kb.dma(d_imp[q0:q0 + 128, :], impn[:], reads=[t_impn])
                o0 = 127 - 2 * qt
                kb.op("dve", lambda e, o0=o0: e.tensor_tensor(out=impn[:], in0=impn[:], in1=tkeep[:, o0:o0 + 128], op=ALU.mult), reads=[t_impn, t_tkeep], writes=[t_impn])
                kb.op("dve", lambda e, o0=o0: e.tensor_tensor(out=impn[:], in0=impn[:], in1=tadd[:, o0:o0 + 128], op=ALU.add), reads=[t_impn, t_tadd], writes=[t_impn])
                kb.op("dve", lambda e: e.memset(impn[:, 0:1], 1e4), reads=[t_impn], writes=[t_impn])
                kb.op("dve", lambda e: e.max(out=m8[:, 0:8], in_=impn[:]), reads=[t_impn], writes=[t_m8])
                kb.op("dve", lambda e: e.match_replace(out=impw[:], in_to_replace=m8[:, 0:8], in_values=impn[:], imm_value=-3e38), reads=[t_impn, t_m8], writes=[t_impw])
                kb.op("dve", lambda e: e.max(out=m8[:, 8:16], in_=impw[:]), reads=[t_impw], writes=[t_m8])
                kb.op("dve", lambda e: e.tensor_scalar(out=selb[:], in0=impn[:], scalar1=m8[:, 15:16], scalar2=None, op0=ALU.is_ge), reads=[t_impn, t_m8], writes=[t_selb])
                if dbg:
                    kb.dma(d_sel[q0:q0 + 128, :], selb[:], reads=[t_selb])
                kb.op("pe", lambda e: e.matmul(ps[7][:, 0:128], lhsT=selb[:, :], rhs=identb3[:, :], start=True, stop=True), reads=[t_selb, t_identb3], writes=[pst[7]])
                kb.op("dve", lambda e: e.tensor_scalar(out=selT[:], in0=ps[7][:, 0:128].unsqueeze(1).broadcast_to([128, 4, 128]), scalar1=-1.0, scalar2=-NEG, op0=ALU.add, op1=ALU.mult),
                      reads=[pst[7]], writes=[t_selT])
                for kt in range(qt + 1):
                    if kt == qt:
                        masks = [(identb3[:, :], caus[:, :, :], [t_identb3, t_caus])]
                    else:
                        masks = [(expT[:, kt * 128:(kt + 1) * 128], selT[:, :, :], [t_expT, t_selT])]
                    tile_pair((KK[0:64, kt * 128:(kt + 1) * 128], t_KK), qv_lo, masks, (tb_sw, t_tbsw), qt - kt, (Vs[:, kt, :], t_Vs), 3, kt == 0, kt == qt)
                flush_pairs()
                kb.op("dve", lambda e: e.tensor_copy(out=oT[:, 1, :], in_=ps[3][0:65, :]), reads=[pst[3]], writes=[t_oT])
                kb.op("dve", lambda e: e.tensor_copy(out=oT[:, 2, :], in_=ps[4][0:65, :]), reads=[pst[4]], writes=[t_oT])
                for br in (1, 2):
                    for h in range(4):
                        col = 260 + ((br - 1) * 4 + h) * 65
                        pbk, cc = (6, col) if col + 65 <= 512 else (7, col - 455 + 128)
                        kb.op("pe", lambda e, h=h, br=br, pbk=pbk, cc=cc: e.matmul(ps[pbk][:, cc:cc + 65], lhsT=oT[0:65, br, h * 128:(h + 1) * 128], rhs=ident[0:65, 0:65], start=True, stop=True),
                              reads=[t_oT, t_ident], writes=[pst[pbk]])

                def oslot(br, h):
                    if br == 0:
                        return 6, h * 65
                    col = 260 + ((br - 1) * 4 + h) * 65
                    return (6, col) if col + 65 <= 512 else (7, col - 455 + 128)
                for br in (1, 2):
                    for h in range(4):
                        pbk, cc = oslot(br, h)
                        kb.op("dve", lambda e, br=br, h=h, pbk=pbk, cc=cc: e.tensor_scalar(out=zsc[:, br * 4 + h:br * 4 + h + 1], in0=ps[pbk][:, cc + 64:cc + 65], scalar1=1e-30, scalar2=None, op0=ALU.max),
                              reads=[pst[pbk]], writes=[t_zsc])
                kb.op("dve", lambda e: e.reciprocal(out=zsc[:, 4:12], in_=zsc[:, 4:12]), reads=[t_zsc], writes=[t_zsc])
                for br in range(3):
                    kb.op("dve", lambda e, br=br, qt=qt: e.tensor_tensor(out=zsc[:, 12 + br * 4:16 + br * 4], in0=zsc[:, br * 4:br * 4 + 4],
                                                                         in1=gts[:, qt, :].rearrange("p (h b) -> p h b", b=3)[:, :, br], op=ALU.mult),
                          reads=[t_zsc, t_gts], writes=[t_zsc])
                for h in range(4):
                    for br in range(3):
                        pbk, cc = oslot(br, h)
                        if br == 0:
                            kb.op("dve", lambda e, h=h, br=br, pbk=pbk, cc=cc: e.tensor_scalar(out=haf[:, h * 64:(h + 1) * 64], in0=ps[pbk][:, cc:cc + 64],
                                                                                               scalar1=zsc[:, 12 + br * 4 + h:13 + br * 4 + h], scalar2=None, op0=ALU.mult),
                                  reads=[pst[pbk], t_zsc], writes=[t_haf])
                        else:
                            kb.op("dve", lambda e, h=h, br=br, pbk=pbk, cc=cc: e.scalar_tensor_tensor(out=haf[:, h * 64:(h + 1) * 64], in0=ps[pbk][:, cc:cc + 64],
                                                                                                      scalar=zsc[:, 12 + br * 4 + h:13 + br * 4 + h], in1=haf[:, h * 64:(h + 1) * 64],
                                                                                                      op0=ALU.mult, op1=ALU.add),
                                  reads=[pst[pbk], t_zsc, t_haf], writes=[t_haf])
                kb.op("dve", lambda e: e.tensor_copy(out=hab[:], in_=haf[:]), reads=[t_haf], writes=[t_hab])
                ob = qt % 2
                for dc in range(2):
                    kb.op("pe", lambda e, dc=dc: e.matmul(ps[5][:, dc * 128:(dc + 1) * 128], lhsT=hab[:, dc * 128:(dc + 1) * 128], rhs=identb3[:, :], start=True, stop=True),
                          reads=[t_hab, t_identb3], writes=[pst[5]], sig=(dc == 1))
                kb.op("dve", lambda e, ob=ob: e.tensor_copy(out=haT[ob][:].rearrange("p a t -> p (a t)"), in_=ps[5][:, 0:256]), reads=[pst[5]], writes=[t_haT[ob]])
                kb.dma(mixT[qt // 8][256:512, (qt % 8) * 128:(qt % 8 + 1) * 128].rearrange("(a p) t -> p a t", p=128), haT[ob][:], reads=[t_haT[ob]], writes=[t_mixT])
            kb.barrier()
    if dbg:
        d_mixT = nc.dram_tensor("d_mixT", [512, S], BF16, kind="ExternalOutput").ap()
        for i in range(8):
            kb.dma(d_mixT[:, i * 1024:(i + 1) * 1024], mixT[i], reads=[t_mixT])


    if phases >= 4:
        selm_in = din("selm", [128, 4])
        x_own = din("x_own", [2048, D])
        w_out_in = din("w_out_p", [D, D])
        g2_row = din("g2_row", [1, D])
        wq_in = din("wq", [D, D])
        skT_in = din("skT", [128, 2, 128])
        uT_in = din("uT", [D, 16384])
        v_in = din("v_tab", [16384, D])
        y_out = nc.dram_tensor("y", [2048, D], F32, kind="ExternalOutput").ap()
        mixG = [nc.dram_tensor("mixG%d" % i, [2048, 1024], BF16, kind="Internal").ap() for i in range(8)]; t_mixG = Trk()
        s_x1 = dscr("s_x1", [2048, D]); t_sx1 = Trk()
        s_h2T = dscr("s_h2T", [D, 2048], BF16); t_sh2T = Trk()
        kb.barrier()
        for i in range(8):
            kb.op("pool", lambda e, i=i: e.collective_compute("AllGather", ALU.bypass, replica_groups=[[0, 1, 2, 3], [4, 5, 6, 7]],
                                                              ins=[mixT[i]], outs=[mixG[i]]), reads=[t_mixT], writes=[t_mixG])
        with ExitStack() as p45:
            def sb45(name, shape, dt=F32):
                return p45.enter_context(nc.sbuf_tensor(name, list(shape), dt))
            g2tb = sb45("g2tb", [128, D]); t_g2tb = Trk()
            identb4 = sb45("identb4", [128, 128], BF16); t_identb4 = Trk()
            kb.dma(identb4[:], din("ident_b2", [128, 128], BF16), writes=[t_identb4])
            with ExitStack() as p4:
                def sb4(name, shape, dt=F32):
                    return p4.enter_context(nc.sbuf_tensor(name, list(shape), dt))
                rowst = sb4("rowst", [1, D]); t_rowst = Trk()
                g1b = sb4("g1b", [128, D]); t_g1b = Trk()
                sh2b = sb4("sh2b", [128, D]); t_sh2b = Trk()
                gs2b = sb4("gs2b", [128, D]); t_gs2b = Trk()
                selm = sb4("selm_sb", [128, 4]); t_selm = Trk()
                kb.dma(selm[:], selm_in, writes=[t_selm])

                def bcast_row(src_ap, dst, t_dst, src_reads=()):
                    kb.dma(rowst[:], src_ap, reads=list(src_reads), writes=[t_rowst])
                    for n in range(4):
                        kb.op("pe", lambda e, n=n: e.matmul(ps[n][:, :], lhsT=ones_f[0:1, :], rhs=rowst[0:1, n * 512:(n + 1) * 512], start=True, stop=True),
                              reads=[t_ones_f, t_rowst], writes=[pst[n]])
                        kb.op("dve", lambda e, n=n: e.tensor_copy(out=dst[:, n * 512:(n + 1) * 512], in_=ps[n][:, :]), reads=[pst[n]], writes=[t_dst])
                bcast_row(s_mod[0:1, 2 * D:3 * D], g1b, t_g1b, [t_smod])
                bcast_row(s_mod[0:1, 3 * D:4 * D], sh2b, t_sh2b, [t_smod])
                bcast_row(s_mod[0:1, 5 * D:6 * D], g2tb, t_g2tb, [t_smod])
                bcast_row(s_mod[0:1, 4 * D:5 * D], gs2b, t_gs2b, [t_smod])
                kb.dma(rowst[:], g2_row, writes=[t_rowst])
                for n in range(4):
                    kb.op("pe", lambda e, n=n: e.matmul(ps[n][:, :], lhsT=ones_f[0:1, :], rhs=rowst[0:1, n * 512:(n + 1) * 512], start=True, stop=True),
                          reads=[t_ones_f, t_rowst], writes=[pst[n]])
                    kb.op("dve", lambda e, n=n: e.scalar_tensor_tensor(out=gs2b[:, n * 512:(n + 1) * 512], in0=gs2b[:, n * 512:(n + 1) * 512], scalar=1.0, in1=ps[n][:, :],
                                                                       op0=ALU.add, op1=ALU.mult), reads=[pst[n], t_gs2b], writes=[t_gs2b])
                wo = sb4("wo", [128, 16, D], BF16); t_wo = Trk()
                for fc in range(16):
                    kb.dma(wo[:, fc, :], w_out_in[fc * 128:(fc + 1) * 128, :], writes=[t_wo], q="pool")
                sl = [sb4("sl%d" % i, [128, 16, 128], BF16) for i in range(4)]; t_sl = [Trk() for _ in range(4)]
                mixo = sb4("mixo", [128, 16, 128], BF16); t_mixo = Trk()
                xin = [sb4("xin%d" % i, [128, D]) for i in range(2)]; t_xin = [Trk(), Trk()]
                tmpy = sb4("tmpy", [128, D]); t_tmpy = Trk()
                h2b = sb4("h2b", [128, D], BF16); t_h2b = Trk()
                h2Tt = [sb4("h2Tt%d" % i, [128, 16, 128], BF16) for i in range(2)]; t_h2Tt = [Trk(), Trk()]
                nsc = sb4("nsc", [128, 4]); t_nsc = Trk()
                mgv = [mg.rearrange("(f p) t -> p f t", p=128) for mg in mixG]
                for tt in range(16):
                    bi = tt % 2
                    kb.dma(xin[bi][:], x_own[tt * 128:(tt + 1) * 128, :], writes=[t_xin[bi]])
                    for s_ in range(4):
                        kb.dma(sl[s_][:], mgv[2 * s_ + tt // 8][:, :, (tt % 8) * 128:(tt % 8 + 1) * 128], reads=[t_mixG], writes=[t_sl[s_]])
                    kb.op("dve", lambda e: e.tensor_scalar(out=mixo[:], in0=sl[0][:], scalar1=selm[:, 0:1], scalar2=None, op0=ALU.mult), reads=[t_sl[0], t_selm], writes=[t_mixo])
                    for s_ in range(1, 4):
                        kb.op("dve", lambda e, s_=s_: e.scalar_tensor_tensor(out=mixo[:], in0=sl[s_][:], scalar=selm[:, s_:s_ + 1], in1=mixo[:], op0=ALU.mult, op1=ALU.add),
                              reads=[t_sl[s_], t_selm, t_mixo], writes=[t_mixo])
                    for n in range(4):
                        for fc in range(16):
                            kb.op("pe", lambda e, n=n, fc=fc: e.matmul(ps[n][:, :], lhsT=mixo[:, fc, :], rhs=wo[:, fc, n * 512:(n + 1) * 512], start=(fc == 0), stop=(fc == 15)),
                                  reads=[t_mixo, t_wo], writes=[pst[n]], sig=(fc == 15))
                        kb.op("dve", lambda e, n=n: e.tensor_tensor(out=tmpy[:, n * 512:(n + 1) * 512], in0=ps[n][:, :], in1=g1b[:, n * 512:(n + 1) * 512], op=ALU.mult),
                              reads=[pst[n], t_g1b], writes=[t_tmpy])
                        kb.op("dve", lambda e, n=n, bi=bi: e.tensor_tensor(out=xin[bi][:, n * 512:(n + 1) * 512], in0=xin[bi][:, n * 512:(n + 1) * 512], in1=tmpy[:, n * 512:(n + 1) * 512], op=ALU.add),
                              reads=[t_xin[bi], t_tmpy], writes=[t_xin[bi]])
                    kb.dma(s_x1[tt * 128:(tt + 1) * 128, :], xin[bi][:], reads=[t_xin[bi]], writes=[t_sx1])
                    kb.op("act", lambda e, bi=bi: e.activation(out=tmpy[:], in_=xin[bi][:], func=AF.Square, accum_out=nsc[:, 0:1]), reads=[t_xin[bi]], writes=[t_tmpy, t_nsc])
                    kb.op("act", lambda e: e.activation(out=nsc[:, 1:2], in_=nsc[:, 0:1], func=AF.Sqrt, scale=1.0 / D, bias=EPS), reads=[t_nsc], writes=[t_nsc])
                    kb.op("dve", lambda e: e.reciprocal(out=nsc[:, 2:3], in_=nsc[:, 1:2]), reads=[t_nsc], writes=[t_nsc])
                    kb.op("dve", lambda e, bi=bi: e.scalar_tensor_tensor(out=tmpy[:], in0=xin[bi][:], scalar=nsc[:, 2:3], in1=gs2b[:], op0=ALU.mult, op1=ALU.mult),
                          reads=[t_xin[bi], t_nsc, t_gs2b], writes=[t_tmpy])
                    kb.op("dve", lambda e: e.tensor_tensor(out=h2b[:], in0=tmpy[:], in1=sh2b[:], op=ALU.add), reads=[t_tmpy, t_sh2b], writes=[t_h2b])
                    for qd in range(4):
                        pb = 4 + qd
                        for k in range(4):
                            dc = qd * 4 + k
                            kb.op("pe", lambda e, dc=dc, k=k, pb=pb: e.matmul(ps[pb][:, k * 128:(k + 1) * 128], lhsT=h2b[:, dc * 128:(dc + 1) * 128], rhs=identb4[:, :], start=True, stop=True),
                                  reads=[t_h2b, t_identb4], writes=[pst[pb]], sig=(k == 3))
                        kb.op("act", lambda e, qd=qd, pb=pb, bi=bi: e.activation(out=h2Tt[bi][:, qd * 4:(qd + 1) * 4, :].rearrange("p a t -> p (a t)"), in_=ps[pb][:, :], func=AF.Copy),
                              reads=[pst[pb]], writes=[t_h2Tt[bi]])
                    kb.dma(s_h2T[:, tt * 128:(tt + 1) * 128].rearrange("(a p) t -> p a t", p=128), h2Tt[bi][:], reads=[t_h2Tt[bi]], writes=[t_sh2T])
                kb.barrier()

            if phases >= 5:
                with ExitStack() as p5:
                    def sb5(name, shape, dt=F32):
                        return p5.enter_context(nc.sbuf_tensor(name, list(shape), dt))
                    skT = sb5("skT_sb", [128, 2, 128], BF16); t_skT = Trk()
                    kb.dma(skT[:], skT_in, writes=[t_skT], q="pool")
                    h2g = sb5("h2g", [128, 16, 512], BF16); t_h2g = Trk()
                    s1m = sb5("s1m", [128, 4, 8, 128]); t_s1m = Trk()
                    s2t = sb5("s2t", [128, 4, 8, 128]); t_s2t = Trk()
                    tau = sb5("tau", [128, 4, 8]); t_tau = Trk()
                    Ysb = sb5("Ysb", [128, 4, D]); t_Ysb = [Trk() for _ in range(4)]
                    wqs = [sb5("wqs%d" % i, [128, 16, 128], BF16) for i in range(2)]; t_wqs = [Trk(), Trk()]
                    sct = sb5("sct", [128, 16, 128]); t_sct = Trk()
                    wk = sb5("wk", [128, 256]); t_wk = Trk()
                    tv = sb5("tv", [128, 16, 16]); t_tv = Trk()
                    cand = sb5("cand", [128, 8, 256]); t_cand = Trk()
                    cw2 = sb5("cw2", [128, 256]); t_cw2 = Trk()
                    m24 = sb5("m24", [128, 8, 24]); t_m24 = Trk()
                    hs = sb5("hs", [128, 8, 8]); t_hs = Trk()
                    ex = sb5("ex", [128, 8, 256]); t_ex = Trk()
                    Ub = [sb5("Ub%d" % i, [128, 16, 256], BF16) for i in range(2)]; t_Ub = [Trk(), Trk()]
                    Vb = [sb5("Vb%d" % i, [128, 2, D], BF16) for i in range(2)]; t_Vb = [Trk(), Trk()]
                    Lt = [sb5("Lt0", [128, 8, 256]), cand]; t_Lt = [Trk(), t_cand]
                    etau = sb5("etau", [128, 4, 8]); t_etau = Trk()
                    Wd = [sb5("Wd%d" % i, [128, 256], BF16) for i in range(2)]; t_Wd = [Trk(), Trk()]
                    t_Ex = [[Trk() for _ in range(16)] for _ in range(2)]
                    Wt = [sb5("Wt%d" % i, [128, 8, 256], BF16) for i in range(2)]; t_Wt = [[Trk() for _ in range(8)] for _ in range(2)]
                    glT = [sb5("glT%d" % i, [128, 2, 512], BF16) for i in range(2)]; t_glT = [Trk(), Trk()]
                    GT = [sb5("GT%d" % i, [128, 2, 128], BF16) for i in range(2)]; t_GT = [Trk(), Trk()]
                    x1t = Lt[0][:].rearrange("p h e -> p (h e)"); t_x1t = t_Lt[0]
                    h2v = s_h2T.rearrange("(a p) t -> p a t", p=128)
                    wqv = wq_in.rearrange("(a p) n -> p a n", p=128)
                    uTv = uT_in.rearrange("(a p) e -> p a e", p=128)
                    vv_ = v_in.rearrange("(c p) d -> p c d", p=128)
                    ngrp = int(os.environ.get("NGRP", "4"))
                    net = int(os.environ.get("NET", "64"))
                    for g in range(ngrp):
                        kb.dma(h2g[:, 0:8, :], h2v[:, 0:8, g * 512:(g + 1) * 512], reads=[t_sh2T], writes=[t_h2g])
                        kb.dma(h2g[:, 8:16, :], h2v[:, 8:16, g * 512:(g + 1) * 512], reads=[t_sh2T], writes=[t_h2g])
                        for tl in range(4):
                            pass
                        qall = p5.enter_context(nc.sbuf_tensor("qall%d" % g, [128, 16, 512], BF16)) if g == 0 else qall
                        t_qall = Trk() if g == 0 else t_qall
                        for ch in range(16):
                            wi = ch % 2
                            kb.dma(wqs[wi][:], wqv[:, :, ch * 128:(ch + 1) * 128], writes=[t_wqs[wi]], q="pool")
                            pb = ch % 2
                            for dc in range(16):
                                kb.op("pe", lambda e, dc=dc, wi=wi, pb=pb: e.matmul(ps[pb][:, :], lhsT=wqs[wi][:, dc, :], rhs=h2g[:, dc, :], start=(dc == 0), stop=(dc == 15)),
                                      reads=[t_wqs[wi], t_h2g], writes=[pst[pb]], sig=(dc == 15))
                            kb.op("act", lambda e, ch=ch, pb=pb: e.activation(out=qall[:, ch, :], in_=ps[pb][:, :], func=AF.Copy), reads=[pst[pb]], writes=[t_qall])
                        for tl in range(4):
                            for qd in range(4):
                                pb = 2 + (qd % 2)
                                for k in range(4):
                                    ch = qd * 4 + k
                                    kb.op("pe", lambda e, ch=ch, k=k, pb=pb, tl=tl: e.matmul(ps[pb][:, k * 128:(k + 1) * 128], lhsT=qall[:, ch, tl * 128:(tl + 1) * 128], rhs=skT[:, ch % 2, :],
                                                                                             start=True, stop=True),
                                          reads=[t_qall, t_skT], writes=[pst[pb]], sig=(k == 3))
                                kb.op("dve", lambda e, qd=qd, pb=pb: e.tensor_copy(out=sct[:, qd * 4:(qd + 1) * 4, :].rearrange("p a k -> p (a k)"), in_=ps[pb][:, :]),
                                      reads=[pst[pb]], writes=[t_sct])
                            for ch in range(16):
                                kb.op("dve", lambda e, ch=ch: e.max(out=tv[:, ch, 0:8], in_=sct[:, ch, :]), reads=[t_sct], writes=[t_tv])
                                kb.op("dve", lambda e, ch=ch: e.match_replace(out=wk[:, 0:128], in_to_replace=tv[:, ch, 0:8], in_values=sct[:, ch, :], imm_value=-3e38),
                                      reads=[t_sct, t_tv], writes=[t_wk])
                                kb.op("dve", lambda e, ch=ch: e.max(out=tv[:, ch, 8:16], in_=wk[:, 0:128]), reads=[t_wk], writes=[t_tv])
                            tvv = tv[:].rearrange("p (h two) a -> p h two a", two=2)
                            kb.op("dve", lambda e: e.tensor_tensor(out=cand[:].rearrange("p h (a b) -> p h a b", b=16),
                                                                   in0=tvv[:, :, 0, :].unsqueeze(3).broadcast_to([128, 8, 16, 16]),
                                                                   in1=tvv[:, :, 1, :].unsqueeze(2).broadcast_to([128, 8, 16, 16]), op=ALU.add),
                                  reads=[t_tv], writes=[t_cand])
                            for h in range(8):
                                kb.op("dve", lambda e, h=h: e.max(out=m24[:, h, 0:8], in_=cand[:, h, :]), reads=[t_cand], writes=[t_m24])
                                kb.op("dve", lambda e, h=h: e.match_replace(out=cw2[:], in_to_replace=m24[:, h, 0:8], in_values=cand[:, h, :], imm_value=-3e38),
                                      reads=[t_cand, t_m24], writes=[t_cw2])
                                kb.op("dve", lambda e, h=h: e.max(out=m24[:, h, 8:16], in_=cw2[:]), reads=[t_cw2], writes=[t_m24])
                                kb.op("dve", lambda e, h=h: e.match_replace(out=cw2[:], in_to_replace=m24[:, h, 8:16], in_values=cw2[:], imm_value=-3e38),
                                      reads=[t_cw2, t_m24], writes=[t_cw2])
                                kb.op("dve", lambda e, h=h: e.max(out=m24[:, h, 16:24], in_=cw2[:]), reads=[t_cw2], writes=[t_m24])
                            kb.op("dve", lambda e: e.tensor_copy(out=hs[:, :, 0], in_=m24[:, :, 0]), reads=[t_m24], writes=[t_hs])
                            kb.op("dve", lambda e: e.tensor_tensor(out=hs[:, :, 1], in0=m24[:, :, 15], in1=m24[:, :, 16], op=ALU.add), reads=[t_m24], writes=[t_hs])
                            kb.op("dve", lambda e: e.tensor_scalar(out=hs[:, :, 1], in0=hs[:, :, 1], scalar1=0.5, scalar2=None, op0=ALU.mult), reads=[t_hs], writes=[t_hs])
                            kb.op("dve", lambda e: e.tensor_tensor(out=ex[:], in0=cand[:], in1=hs[:, :, 0:1].broadcast_to([128, 8, 256]), op=ALU.subtract), reads=[t_cand, t_hs], writes=[t_ex])
                            kb.op("act", lambda e: e.activation(out=ex[:], in_=ex[:], func=AF.Exp), reads=[t_ex], writes=[t_ex])
                            kb.op("dve", lambda e: e.tensor_tensor(out=cand[:], in0=cand[:], in1=hs[:, :, 1:2].broadcast_to([128, 8, 256]), op=ALU.is_ge), reads=[t_cand, t_hs], writes=[t_cand])
                            kb.op("dve", lambda e: e.tensor_tensor(out=ex[:], in0=ex[:], in1=cand[:], op=ALU.mult), reads=[t_cand, t_ex], writes=[t_ex])
                            kb.op("dve", lambda e: e.tensor_reduce(out=hs[:, :, 2], in_=ex[:], axis=AX.X, op=ALU.add), reads=[t_ex], writes=[t_hs])
                            kb.op("act", lambda e: e.activation(out=hs[:, :, 3], in_=hs[:, :, 2], func=AF.Ln), reads=[t_hs], writes=[t_hs])
                            kb.op("dve", lambda e: e.tensor_tensor(out=hs[:, :, 4], in0=hs[:, :, 0], in1=hs[:, :, 3], op=ALU.add), reads=[t_hs], writes=[t_hs])
                            sv = sct[:].rearrange("p (h two) k -> p h two k", two=2)
                            kb.op("dve", lambda e, tl=tl: e.tensor_tensor(out=s1m[:, tl, :, :], in0=sv[:, :, 0, :], in1=hs[:, :, 4:5].broadcast_to([128, 8, 128]), op=ALU.subtract),
                                  reads=[t_sct, t_hs], writes=[t_s1m])
                            kb.op("dve", lambda e, tl=tl: e.tensor_copy(out=s2t[:, tl, :, :], in_=sv[:, :, 1, :]), reads=[t_sct], writes=[t_s2t])
                            kb.op("dve", lambda e, tl=tl: e.tensor_tensor(out=tau[:, tl, :], in0=hs[:, :, 1], in1=hs[:, :, 4], op=ALU.subtract), reads=[t_hs], writes=[t_tau])
                            kb.op("act", lambda e, tl=tl: e.activation(out=etau[:, tl, :], in_=tau[:, tl, :], func=AF.Exp), reads=[t_tau], writes=[t_etau])
                        kb.op("dve", lambda e: e.memset(Ysb[:], 0.0), writes=t_Ysb)

                        def load_u(et):
                            bi = et % 2
                            kb.dma(Ub[bi][:, 0:8, :], uTv[:, 0:8, et * 256:(et + 1) * 256], writes=[t_Ub[bi]], q="pool")
                            kb.dma(Ub[bi][:, 8:16, :], uTv[:, 8:16, et * 256:(et + 1) * 256], writes=[t_Ub[bi]], q="pool")

                        def load_v(et):
                            bi = et % 2
                            kb.dma(Vb[bi][:], vv_[:, et * 2:et * 2 + 2, :], writes=[t_Vb[bi]], q="pool")

                        def emit_a(et):
                            bi = et % 2
                            for ec in range(2):
                                for dc in range(16):
                                    kb.op("pe", lambda e, dc=dc, ec=ec, bi=bi: e.matmul(ps[ec][:, :], lhsT=Ub[bi][:, dc, ec * 128:(ec + 1) * 128], rhs=h2g[:, dc, :],
                                                                                        start=(dc == 0), stop=(dc == 15)),
                                          reads=[t_Ub[bi], t_h2g], writes=[pst[ec]], sig=(dc == 15))
                                kb.op("act", lambda e, ec=ec, bi=bi: e.activation(out=glT[bi][:, ec, :], in_=ps[ec][:, :], func=AF.Gelu_apprx_tanh),
                                      reads=[pst[ec]], writes=[t_glT[bi]])
                        load_u(0)
                        if net > 1:
                            load_u(1)
                        load_v(0)
                        emit_a(0)
                        pairs = [(et, tl) for et in range(net) for tl in range(4)]
                        NP = len(pairs)

                        def st_L(n):
                            et, tl = pairs[n]; k = n % 2
                            for h in range(8):
                                for a2 in range(2):
                                    kb.op("act", lambda e, h=h, a2=a2: e.activation(out=Lt[k][:, h, a2 * 128:(a2 + 1) * 128], in_=s2t[:, tl, h, :], func=AF.Exp,
                                                                                    bias=s1m[:, tl, h, 2 * et + a2:2 * et + a2 + 1]),
                                          reads=[t_s2t, t_s1m], writes=[t_Ex[k][h * 2 + a2], t_Lt[k]] if (h == 0 and a2 == 0) else [t_Ex[k][h * 2 + a2]])

                        def st_C(n):
                            et, tl = pairs[n]; k = n % 2
                            for h in range(8):
                                kb.op("dve", lambda e, h=h: e.scalar_tensor_tensor(out=Wt[k][:, h, :], in0=Lt[k][:, h, :], scalar=etau[:, tl, h:h + 1], in1=Lt[k][:, h, :],
                                                                                   op0=ALU.is_ge, op1=ALU.mult),
                                      reads=[t_Ex[k][2 * h], t_Ex[k][2 * h + 1], t_etau, t_Lt[k]], writes=[t_Wt[k][h]])
                            kb.op("dve", lambda e: e.tensor_tensor(out=Wt[k][:, 0:4, :], in0=Wt[k][:, 0:4, :], in1=Wt[k][:, 4:8, :], op=ALU.add), reads=t_Wt[k], writes=t_Wt[k][0:4])
                            pw = 2 + k
                            for ec in range(2):
                                for h in range(4):
                                    kb.op("pe", lambda e, ec=ec, h=h: e.matmul(ps[pw][:, ec * 128:(ec + 1) * 128], lhsT=Wt[k][:, h, ec * 128:(ec + 1) * 128], rhs=identb4[:, :],
                                                                               start=(h == 0), stop=(h == 3)),
                                          reads=[t_Wt[k][h], t_identb4], writes=[pst[pw]], sig=(ec == 1 and h == 3))

                        def st_G(n):
                            et, tl = pairs[n]; k = n % 2; bi = et % 2; pw = 2 + k
                            if tl == 0:
                                if et + 1 < net:
                                    load_v(et + 1)
                                    emit_a(et + 1)
                                if et + 2 < net:
                                    load_u(et + 2)
                            kb.op("dve", lambda e: e.tensor_tensor(out=GT[k][:], in0=glT[bi][:, :, tl * 128:(tl + 1) * 128],
                                                                   in1=ps[pw][:, 0:256].rearrange("p (a t) -> p a t", t=128), op=ALU.mult),
                                  reads=[t_glT[bi], pst[pw]], writes=[t_GT[k]])
                            for nn in range(4):
                                pb = 4 + nn
                                for ec in range(2):
                                    kb.op("pe", lambda e, ec=ec, nn=nn, pb=pb: e.matmul(ps[pb][:, :], lhsT=GT[k][:, ec, :], rhs=Vb[bi][:, ec, nn * 512:(nn + 1) * 512],
                                                                                        start=(ec == 0), stop=(ec == 1)),
                                          reads=[t_GT[k], t_Vb[bi]], writes=[pst[pb]], sig=(ec == 1))

                        def st_Y(n):
                            et, tl = pairs[n]
                            kb.op("dve", lambda e: e.tensor_tensor(out=Ysb[:, tl, :], in0=Ysb[:, tl, :], in1=psY[:, :], op=ALU.add),
                                  reads=[t_Ysb[tl], pst[4], pst[5], pst[6], pst[7]], writes=[t_Ysb[tl]])
                        for n in range(-2, NP):
                            if 0 <= n:
                                st_G(n)
                            if 0 <= n + 2 < NP:
                                st_L(n + 2)
                            if 0 <= n + 1 < NP:
                                st_C(n + 1)
                            if 0 <= n:
                                st_Y(n)
                        for tl in range(4):
                            r0 = (g * 4 + tl) * 128
                            kb.dma(x1t, s_x1[r0:r0 + 128, :], reads=[t_sx1], writes=[t_x1t])
                            kb.op("dve", lambda e, tl=tl: e.tensor_tensor(out=Ysb[:, tl, :], in0=Ysb[:, tl, :], in1=g2tb[:], op=ALU.mult), reads=[t_Ysb[tl], t_g2tb], writes=[t_Ysb[tl]])
                            kb.op("dve", lambda e, tl=tl: e.tensor_tensor(out=x1t, in0=x1t, in1=Ysb[:, tl, :], op=ALU.add), reads=[t_Ysb[tl], t_x1t], writes=[t_x1t])
                            kb.dma(y_out[r0:r0 + 128, :], x1t, reads=[t_x1t])
                    kb.barrier()

    for _i in range(int(os.environ.get('EXTRA', '0'))):
        kb.dma(s_mod[0:1, 0:128], ident[0:1, :], reads=[t_ident])
    kb.barrier()
    ok, stuck, sems = kb.check()
    if not ok or os.environ.get('KBV'):
        print('SYNC CHECK ok=%s' % ok, stuck, {k: v for k, v in kb.cnt.items()})
    assert ok, 'sync deadlock'
    es.close()
    return nc


def _consts():
    ones = np.ones((128, 128), np.float32)
    blk = np.zeros((128, 128), np.float32)
    blk[:64, :64] = 1.0
    blk[64:, 64:] = 1.0
    ut = np.triu(np.ones((128, 128), np.float32))
    NEG = -30000.0
    il = np.arange(128)
    cmask = np.zeros((128, 17, 128), np.float32)
    for m in range(17):
        ok = (il[None, :] + 128 * m) >= (16 * il[:, None] + 31)
        cmask[:, m, :] = np.where(ok, 0.0, NEG)
    caus = np.where(il[:, None] <= il[None, :], 0.0, NEG).astype(np.float32)
    acaus = np.where(il[:, None] > il[None, :], 0.0, NEG).astype(np.float32)
    i_abs = (np.arange(4)[None, :, None] * 128 + il[:, None, None])
    blkk = np.arange(128)[None, None, :]
    ovl = ((16 * i_abs < 64 * blkk + 64) & (16 * i_abs + 32 > 64 * blkk)).astype(np.float32)
    expT = (np.arange(S)[None, :] // 64 == il[:, None]).astype(np.float32)
    o = np.arange(255)[None, :] - 127
    cur_rel = (il[:, None] >= 64).astype(np.int64)
    rel = o - cur_rel
    t_keep = (rel < -1).astype(np.float32)
    t_add = np.where(rel > 0, -1e4, np.where(rel >= -1, 1e4, 0.0)).astype(np.float32)
    return {"ones_bf": _bf(ones), "blk64_bf": _bf(blk), "ident_f": np.eye(128, dtype=np.float32),
            "ut_f": ut, "ident_b": _bf(np.eye(128, dtype=np.float32)),
            "cmask": _bf(cmask), "caus_add": _bf(caus), "acaus_add": _bf(acaus), "ovl": _bf(ovl), "expT": _bf(expT),
            "t_keep": np.ascontiguousarray(np.broadcast_to(t_keep, (128, 255))).astype(np.float32), "t_add": t_add}


def make_in_maps(inp):
    f = lambda a: np.ascontiguousarray(np.asarray(a, dtype=np.float32))
    x = f(inp["x"]); c = f(inp["c"])
    w_in = f(inp["w_in"])[0]
    conv = f(inp["conv_qk"])[0]
    qn_g = f(inp["qn_g"])[0]; kn_g = f(inp["kn_g"])[0]
    mng = f(inp["mlstm_norm_g"])[0]
    g1 = f(inp["norm1_g"])[0]
    cst = _consts()
    maps = []
    xTs = [np.ascontiguousarray(x[b].T) for b in range(2)]
    w_out = f(inp["w_out"])[0]
    rows = []
    for fc in range(16):
        r, part, half = fc // 4, (fc % 4) // 2, fc % 2
        base = part * 1024 + r * 256 + half * 128
        rows += list(range(base, base + 128))
    w_out_p = np.ascontiguousarray(w_out[rows])
    wq = f(inp["peer_wq"])[0]
    skT = np.ascontiguousarray(f(inp["peer_subkeys"])[0].transpose(2, 0, 1))
    uT = np.ascontiguousarray(f(inp["peer_u"])[0].T)
    vtab = f(inp["peer_v"])[0]
    for core in range(8):
        b, j = divmod(core, 4)
        cols = []
        cols += list(range(j * 256, j * 256 + 256))
        cols += list(range(1024 + j * 256, 1024 + j * 256 + 256))
        AQ = 4096 + 8
        cols += list(range(AQ + j * 256, AQ + j * 256 + 256))
        AKV = AQ + 1024
        for br in (0, 1, 2, 4):
            cols += list(range(AKV + br * 256 + j * 64, AKV + br * 256 + j * 64 + 64))
        cols += [4096 + j, 4096 + 4 + j]
        cols += list(range(2048 + j * 256, 2048 + j * 256 + 256))
        cols += list(range(3072 + j * 256, 3072 + j * 256 + 256))
        for br in (3, 5):
            cols += list(range(AKV + br * 256 + j * 64, AKV + br * 256 + j * 64 + 64))
        AG = AKV + 1536
        cols += list(range(AG + j * 12, AG + j * 12 + 12))
        assert len(cols) == 1678
        wj = np.zeros((D, 1680), np.float32)
        wj[:, :1678] = w_in[:, cols]
        cwt = np.zeros((128, 4, 4), np.float32)
        for t in range(4):
            base = (j * 256 + (t % 2) * 128) if t < 2 else (1024 + j * 256 + (t % 2) * 128)
            cwt[:, t, :] = conv[:, base:base + 128].T
        qkg = np.zeros((128, 4), np.float32)
        qkg[:, 0] = np.tile(qn_g, 2) * 0.125
        qkg[:, 3] = np.concatenate([kn_g[1], kn_g[2]])
        m = {
            "xT": xTs[b], "c_col": np.ascontiguousarray(c[b].reshape(16, 128).T),
            "w_mod": f(inp["w_mod"])[0], "b_mod": f(inp["b_mod"]),
            "g1_col": np.ascontiguousarray(g1.reshape(16, 128).T),
            "w_in": wj, "convw": cwt, "qk_g": qkg,
            "mng_row": np.ascontiguousarray(mng[j * 256:(j + 1) * 256][None, :]),
            "bif": np.ascontiguousarray(np.tile(np.array([[f(inp["b_igate"])[0, j], f(inp["b_fgate"])[0, j]]], np.float32), (128, 1))),
        }
        slopes = np.array([2.0 ** (-8.0 * (4 * j + hh + 1) / 16) for hh in range(4)], np.float64)
        kl = np.arange(128, dtype=np.float64)
        dl = np.arange(64, dtype=np.float64)
        m["tb_sw"] = (slopes[None, None, :] * (kl[:, None, None] - 64 - 128 * dl[None, :, None])).astype(np.float32)
        m["tb_c"] = (slopes[None, None, :] * (16 * kl[:, None, None] - 33 - 128 * dl[None, :, None])).astype(np.float32)
        m["w1k"] = f(inp["cmp_k_w1"])[0]; m["w1v"] = f(inp["cmp_v_w1"])[0]
        m["w2k"] = f(inp["cmp_k_w2"])[0]; m["w2v"] = f(inp["cmp_v_w2"])[0]
        for nm, key in (("posk", "cmp_pos_k"), ("posv", "cmp_pos_v")):
            pos = f(inp[key])[0]
            m[nm] = np.ascontiguousarray(pos.reshape(16, 2, 64).transpose(1, 2, 0).reshape(128, 16))
        m["kng0"] = np.ascontiguousarray(kn_g[0][:, None])
        sm = np.zeros((128, 4), np.float32); sm[:, j] = 1.0
        m["selm"] = sm
        m["x_own"] = np.ascontiguousarray(x[b, j * 2048:(j + 1) * 2048])
        m["w_out_p"] = w_out_p
        m["g2_row"] = f(inp["norm2_g"])
        m["wq"] = wq
        m["skT"] = skT
        m["uT"] = uT
        m["v_tab"] = vtab
        m["ident_b2"] = cst["ident_b"]
        m.update(cst)
        maps.append(m)
    return maps


def kernel(**inputs):
    nc = build()
    maps = make_in_maps(inputs)
    res = run_bass_kernel_spmd(nc, maps, core_ids=list(range(8)))
    out = np.zeros((2, S, D), np.float32)
    for core in range(8):
        b, j = divmod(core, 4)
        out[b, j * 2048:(j + 1) * 2048] = res.results[core]["y"]
    return out
```

```python
import numpy as np
from contextlib import ExitStack
import concourse.bass as bass
import concourse.mybir as mybir
from concourse.bass_utils import run_bass_kernel_spmd

F32 = mybir.dt.float32
BF16 = mybir.dt.bfloat16
AF = mybir.ActivationFunctionType
ALU = mybir.AluOpType
AX = mybir.AxisListType

D = 2048
S = 8192
NB = 16
EPS = 1e-6
import os
NDS = int(os.environ.get("NDS", "24"))


class Trk:
    __slots__ = ("w", "r")

    def __init__(self):
        self.w = None
        self.r = {}


class KB:
    def __init__(self, nc, es):
        self.nc = nc
        self.eng = {"pe": nc.tensor, "act": nc.scalar, "dve": nc.vector, "pool": nc.gpsimd, "sp": nc.sync}
        self.sem = {k: es.enter_context(nc.semaphore("s_" + k)) for k in self.eng}
        self.cnt = {k: 0 for k in self.eng}
        self.seen = {k: {} for k in self.eng}
        self.dsem = [es.enter_context(nc.semaphore("dq%d" % i)) for i in range(NDS)]
        self.duse = [0] * NDS
        self.dnext = 0
        self.ccsem = es.enter_context(nc.semaphore("ccs"))
        self.log = {k: [] for k in self.eng}

    def _wait(self, e, tok):
        if tok is None:
            return
        key, sem, val = tok
        if self.seen[e].get(key, 0) >= val:
            return
        self.eng[e].wait_ge(sem, val)
        self.log[e].append(("w", key, val))
        self.seen[e][key] = val

    def _deps(self, e, reads, writes):
        for b in reads:
            if b.w is not None and not (e == "pe" and b.w[0] == "pe"):
                self._wait(e, b.w)
        for b in writes:
            if b.w is not None and not (e == "pe" and b.w[0] == "pe"):
                self._wait(e, b.w)
            for t in b.r.values():
                if not (e == "pe" and t[0] == "pe"):
                    self._wait(e, t)

    def _mark(self, tok, reads, writes):
        for b in reads:
            b.r[tok[0]] = tok
        for b in writes:
            b.w = tok
            b.r = {}

    def op(self, e, fn, reads=(), writes=(), sig=True):
        self._deps(e, reads, writes)
        inst = fn(self.eng[e])
        if sig:
            self.cnt[e] += 1
            inst.then_inc(self.sem[e], 1)
            self.log[e].append(("i", e, 1))
            tok = (e, self.sem[e], self.cnt[e])
        else:
            tok = (e, self.sem[e], self.cnt[e] + 1)
        self._mark(tok, reads, writes)
        return tok

    def dma(self, out, in_, reads=(), writes=(), q="sp", **kw):
        i = self.dnext
        self.dnext = (i + 1) % NDS
        if self.duse[i] > 0:
            self._wait(q, (("d", i), self.dsem[i], 16 * self.duse[i]))
        self._deps(q, reads, writes)
        inst = self.eng[q].dma_start(out=out, in_=in_, **kw)
        self.duse[i] += 1
        inst.then_inc(self.dsem[i], 16)
        self.log[q].append(("i", ("d", i), 16))
        tok = (("d", i), self.dsem[i], 16 * self.duse[i])
        self._mark(tok, reads, writes)
        return tok

    def check(self):
        sems = {}
        pc = {k: 0 for k in self.log}
        prog = True
        while prog:
            prog = False
            for e, lg in self.log.items():
                while pc[e] < len(lg):
                    kind, key, val = lg[pc[e]]
                    if kind == "w":
                        if sems.get(key, 0) < val:
                            break
                    else:
                        sems[key] = sems.get(key, 0) + val
                    pc[e] += 1
                    prog = True
        stuck = {e: (pc[e], len(lg), lg[pc[e]] if pc[e] < len(lg) else None) for e, lg in self.log.items()}
        ok = all(pc[e] == len(lg) for e, lg in self.log.items())
        return ok, stuck, sems

    def barrier(self):
        for e in self.eng:
            for e2 in self.eng:
                if e2 != e and self.cnt[e2] > 0:
                    self._wait(e, (e2, self.sem[e2], self.cnt[e2]))
            for i in range(NDS):
                if self.duse[i] > 0:
                    self._wait(e, (("d", i), self.dsem[i], 16 * self.duse[i]))


def _bf(a):
    import ml_dtypes
    return np.asarray(a, dtype=np.float32).astype(ml_dtypes.bfloat16)


def build(dbg=False, phases=99, nb=NB, parts='abcde'):
    nc = bass.Bass("TRN2", target_bir_lowering=False)
    es = ExitStack()
    kb = KB(nc, es)

    def din(name, shape, dt=F32):
        return nc.dram_tensor(name, list(shape), dt, kind="ExternalInput").ap()

    def dscr(name, shape, dt=F32):
        return nc.dram_tensor(name, list(shape), dt, kind=("ExternalOutput" if dbg else "Internal")).ap()

    xT = din("xT", [D, S])
    c_col = din("c_col", [128, 16])
    w_mod = din("w_mod", [D, 6 * D])
    b_mod = din("b_mod", [1, 6 * D])
    g1_col = din("g1_col", [128, 16])
    w_in = din("w_in", [D, 1680])
    convw = din("convw", [128, 4, 4])
    qk_g = din("qk_g", [128, 4])
    mng_row = din("mng_row", [1, 256])
    ones_bf = din("ones_bf", [128, 128], BF16)
    blk64_bf = din("blk64_bf", [128, 128], BF16)
    ident_f = din("ident_f", [128, 128])

    s_q = dscr("s_q", [256, S])
    s_k = dscr("s_k", [256, S])
    s_aq = dscr("s_aq", [256, S], BF16)
    s_kv = dscr("s_kv", [128, S])
    s_kk = dscr("s_kk", [128, S], BF16)
    s_if = dscr("s_if", [2, S])
    s_v = dscr("s_v", [S, 256])
    s_go = dscr("s_go", [S, 256])
    s_vv = dscr("s_vv", [S, 128], BF16)
    s_gt = dscr("s_gt", [S, 12])

    ps = []
    pst = []
    for i in range(4):
        ps.append(es.enter_context(nc.psum_tensor("ps%d" % i, [128, 512], F32)))
        pst.append(Trk())
    psY = es.enter_context(nc.psum_tensor("psY", [128, 2048], F32))
    for i in range(4):
        ps.append(psY[:, i * 512:(i + 1) * 512])
        pst.append(Trk())

    def sb(name, shape, dt=F32):
        return es.enter_context(nc.sbuf_tensor(name, list(shape), dt))

    ones_b = sb("ones_b", [128, 128], BF16); t_ones_b = Trk()
    blk64 = sb("blk64", [128, 128], BF16); t_blk64 = Trk()
    ident = sb("ident", [128, 128]); t_ident = Trk()
    ones_f = sb("ones_f", [128, 128]); t_ones_f = Trk()
    kb.dma(ones_b[:], ones_bf, writes=[t_ones_b])
    kb.dma(blk64[:], blk64_bf, writes=[t_blk64])
    kb.dma(ident[:], ident_f, writes=[t_ident])
    kb.op("dve", lambda e: e.memset(ones_f[:], 1.0), writes=[t_ones_f])

    s_mod = dscr("s_mod", [1, 6 * D]); t_smod = Trk()
    csil = sb("csil", [128, 16]); t_csil = Trk()
    ccol = sb("ccol", [128, 16]); t_ccol = Trk()
    g1c = sb("g1c", [128, 16]); t_g1c = Trk()
    gs1 = sb("gs1", [128, 16]); t_gs1 = Trk()
    sh1 = sb("sh1", [128, 16]); t_sh1 = Trk()
    kb.dma(ccol[:], c_col, writes=[t_ccol])
    kb.dma(g1c[:], g1_col, writes=[t_g1c])
    kb.op("act", lambda e: e.activation(out=csil[:], in_=ccol[:], func=AF.Silu), reads=[t_ccol], writes=[t_csil])
    with ExitStack() as p0:
        wm = [p0.enter_context(nc.sbuf_tensor("wm%d" % i, [128, 16, 512], F32)) for i in range(2)]
        modrow = p0.enter_context(nc.sbuf_tensor("modrow", [1, 6 * D], F32)); t_modrow = Trk()
        bmod_sb = p0.enter_context(nc.sbuf_tensor("bmod_sb", [1, 6 * D], F32)); t_bmod = Trk()
        kb.dma(bmod_sb[:], b_mod, writes=[t_bmod])
        t_wm = [Trk(), Trk()]
        wmv = w_mod.rearrange("(k p) n -> p k n", p=128)
        for n in range(24):
            bi = n % 2
            kb.dma(wm[bi][:, 0:8, :], wmv[:, 0:8, n * 512:(n + 1) * 512], writes=[t_wm[bi]])
            kb.dma(wm[bi][:, 8:16, :], wmv[:, 8:16, n * 512:(n + 1) * 512], writes=[t_wm[bi]])
            pb = n % 2
            for k in range(16):
                kb.op("pe", lambda e, k=k, bi=bi, pb=pb: e.matmul(ps[pb][0:1, :], lhsT=csil[:, k:k + 1], rhs=wm[bi][:, k, :],
                                                                  start=(k == 0), stop=(k == 15)),
                      reads=[t_csil, t_wm[bi]], writes=[pst[pb]], sig=(k == 15))
            kb.op("dve", lambda e, n=n, pb=pb: e.tensor_tensor(out=modrow[0:1, n * 512:(n + 1) * 512], in0=ps[pb][0:1, :],
                                                               in1=bmod_sb[0:1, n * 512:(n + 1) * 512], op=ALU.add),
                  reads=[pst[pb], t_bmod], writes=[t_modrow])
        for which, dst, t_dst in ((0, sh1, t_sh1), (1, gs1, t_gs1)):
            for k in range(16):
                off = which * D + k * 128
                kb.op("pe", lambda e, off=off, k=k: e.matmul(ps[2][:, k:k + 1], lhsT=modrow[0:1, off:off + 128], rhs=ones_f[0:1, 0:1],
                                                             start=True, stop=True),
                      reads=[t_modrow, t_ones_f], writes=[pst[2]], sig=(k == 15))
            if which == 0:
                kb.op("dve", lambda e: e.tensor_copy(out=sh1[:], in_=ps[2][:, 0:16]), reads=[pst[2]], writes=[t_sh1])
            else:
                kb.op("dve", lambda e: e.scalar_tensor_tensor(out=gs1[:], in0=ps[2][:, 0:16], scalar=1.0, in1=g1c[:],
                                                              op0=ALU.add, op1=ALU.mult),
                      reads=[pst[2], t_g1c], writes=[t_gs1])
        kb.dma(s_mod, modrow[:], reads=[t_modrow], writes=[t_smod])
        kb.barrier()

    t_scr = {n: Trk() for n in ("q", "k", "aq", "kv", "kk", "if", "v", "go", "vv", "gt")}
    if phases >= 1:
        with ExitStack() as p1:
            def sb1(name, shape, dt=F32):
                return p1.enter_context(nc.sbuf_tensor(name, list(shape), dt))
            wb = sb1("wb", [128, 16, 1680], BF16); t_wb = Trk()
            cw = sb1("cw", [128, 4, 4]); t_cw = Trk()
            qkg = sb1("qkg", [128, 4]); t_qkg = Trk()
            mng = sb1("mng", [128, 256]); t_mng = Trk()
            mngr = sb1("mngr", [1, 256]); t_mngr = Trk()
            kb.dma(cw[:], convw, writes=[t_cw])
            kb.dma(qkg[:], qk_g, writes=[t_qkg])
            kb.dma(mngr[:], mng_row, writes=[t_mngr])
            kb.op("pe", lambda e: e.matmul(ps[3][:, 0:256], lhsT=ones_f[0:1, :], rhs=mngr[0:1, :], start=True, stop=True),
                  reads=[t_ones_f, t_mngr], writes=[pst[3]])
            kb.op("dve", lambda e: e.tensor_copy(out=mng[:], in_=ps[3][:, 0:256]), reads=[pst[3]], writes=[t_mng])
            wst = [sb1("wst%d" % i, [128, 1680]) for i in range(2)]; t_wst = [Trk(), Trk()]
            wiv = w_in.rearrange("(k p) n -> p k n", p=128)
            for k in range(16):
                bi = k % 2
                kb.dma(wst[bi][:], wiv[:, k, :], writes=[t_wst[bi]])
                kb.op("dve", lambda e, k=k, bi=bi: e.tensor_copy(out=wb[:, k, :], in_=wst[bi][:]),
                      reads=[t_wst[bi]], writes=[t_wb])
            xt = [sb1("xt%d" % i, [128, 16, 512]) for i in range(2)]; t_xt = [Trk(), Trk()]
            xsq = sb1("xsq", [128, 16, 512], BF16); t_xsq = [Trk() for _ in range(16)]
            hT = sb1("hT", [128, 16, 512], BF16); t_hT = [Trk() for _ in range(16)]
            rstd = sb1("rstd", [128, 512]); t_rstd = Trk()
            tmp = [sb1("tmp%d" % i, [128, 512]) for i in range(2)]; t_tmp = [Trk(), Trk()]
            zr = [sb1("zr%d" % i, [128, 3 + 512]) for i in range(4)]; t_zr = [Trk() for _ in range(4)]
            acc = [sb1("acc%d" % i, [128, 512]) for i in range(2)]; t_acc = [Trk(), Trk()]
            ofm = [sb1("ofm%d" % i, [128, 512]) for i in range(2)]; t_ofm = [Trk(), Trk()]
            obf = [sb1("obf%d" % i, [128, 512], BF16) for i in range(2)]; t_obf = [Trk(), Trk()]
            sqb = sb1("sqb", [128, 512], BF16); t_sqb = Trk()
            rs2 = sb1("rs2", [128, 512]); t_rs2 = Trk()
            otm = [sb1("otm%d" % i, [128, 512]) for i in range(2)]; t_otm = [Trk(), Trk()]
            otb = [sb1("otb%d" % i, [128, 128], BF16) for i in range(2)]; t_otb = [Trk(), Trk()]
            otg = [sb1("otg%d" % i, [128, 12]) for i in range(2)]; t_otg = [Trk(), Trk()]
            for i in range(4):
                kb.op("dve", lambda e, i=i: e.memset(zr[i][:, 0:3], 0.0), writes=[t_zr[i]])
            xv = xT.rearrange("(k p) t -> p k t", p=128)
            cnt2 = [0]

            def rot2():
                cnt2[0] += 1
                return cnt2[0] % 2

            def load_x(n):
                bi = n % 2
                for h in range(4):
                    kb.dma(xt[bi][:, 4 * h:4 * h + 4, :], xv[:, 4 * h:4 * h + 4, n * 512:(n + 1) * 512], writes=[t_xt[bi]])

            load_x(0)
            for n in range(nb):
                bi = n % 2
                t0 = n * 512
                if n + 1 < nb:
                    load_x(n + 1)
                for k in range(16):
                    kb.op("act" if k % 2 else "dve",
                          (lambda e, k=k, bi=bi: e.activation(out=xsq[:, k, :], in_=xt[bi][:, k, :], func=AF.Square)) if k % 2 else
                          (lambda e, k=k, bi=bi: e.tensor_tensor(out=xsq[:, k, :], in0=xt[bi][:, k, :], in1=xt[bi][:, k, :], op=ALU.mult)),
                          reads=[t_xt[bi]], writes=[t_xsq[k]])
                for k in range(16):
                    kb.op("pe", lambda e, k=k: e.matmul(ps[0][:, :], lhsT=ones_b[:], rhs=xsq[:, k, :], start=(k == 0), stop=(k == 15)),
                          reads=[t_ones_b, t_xsq[k]], writes=[pst[0]], sig=(k == 15))
                kb.op("act", lambda e: e.activation(out=rstd[:], in_=ps[0][:, :], func=AF.Sqrt, scale=1.0 / D, bias=EPS),
                      reads=[pst[0]], writes=[t_rstd])
                kb.op("dve", lambda e: e.reciprocal(out=rstd[:], in_=rstd[:]), reads=[t_rstd], writes=[t_rstd])
                for k in range(16):
                    tb = k % 2
                    kb.op("dve", lambda e, k=k, tb=tb, bi=bi: e.tensor_tensor(out=tmp[tb][:], in0=xt[bi][:, k, :], in1=rstd[:], op=ALU.mult),
                          reads=[t_xt[bi], t_rstd], writes=[t_tmp[tb]])
                    kb.op("act", lambda e, k=k, tb=tb: e.activation(out=hT[:, k, :], in_=tmp[tb][:], func=AF.Identity,
                                                                    scale=gs1[:, k:k + 1], bias=sh1[:, k:k + 1]),
                          reads=[t_tmp[tb], t_gs1, t_sh1], writes=[t_hT[k]])
                for ct in range(9):
                    if not ({0: 'a', 1: 'a', 2: 'a', 3: 'a', 4: 'b', 5: 'b', 7: 'b', 6: 'c', 8: 'd'}[ct] in parts):
                        continue
                    c0 = ct * 128
                    cn = 128 if ct < 8 else 2
                    pb = 1 + (ct % 2)
                    for k in range(16):
                        kb.op("pe", lambda e, k=k, c0=c0, cn=cn, pb=pb: e.matmul(ps[pb][0:cn, :], lhsT=wb[:, k, c0:c0 + cn], rhs=hT[:, k, :],
                                                                                 start=(k == 0), stop=(k == 15)),
                              reads=[t_wb, t_hT[k]], writes=[pst[pb]], sig=(k == 15))
                    if ct < 4:
                        z = zr[ct]; tz = t_zr[ct]
                        kb.op("act", lambda e, z=z, pb=pb: e.activation(out=z[:, 3:515], in_=ps[pb][:, :], func=AF.Copy),
                              reads=[pst[pb]], writes=[tz])
                        ab = rot2()
                        a = acc[ab]; ta = t_acc[ab]
                        kb.op("dve", lambda e, z=z, a=a, ct=ct: e.tensor_scalar(out=a[:], in0=z[:, 3:515], scalar1=cw[:, ct, 3:4], scalar2=None,
                                                                                 op0=ALU.mult), reads=[tz, t_cw], writes=[ta])
                        for j in range(3):
                            kb.op("dve", lambda e, z=z, a=a, ct=ct, j=j: e.scalar_tensor_tensor(out=a[:], in0=z[:, j:j + 512], scalar=cw[:, ct, j:j + 1],
                                                                                               in1=a[:], op0=ALU.mult, op1=ALU.add),
                                  reads=[tz, t_cw, ta], writes=[ta])
                        ob = rot2()
                        kb.op("act", lambda e, a=a, ob=ob: e.activation(out=ofm[ob][:], in_=a[:], func=AF.Silu), reads=[ta], writes=[t_ofm[ob]])
                        dst = s_q if ct < 2 else s_k
                        r0 = (ct % 2) * 128
                        kb.dma(dst[r0:r0 + 128, t0:t0 + 512], ofm[ob][:], reads=[t_ofm[ob]], writes=[t_scr["q" if ct < 2 else "k"]])
                        kb.op("dve", lambda e, z=z: e.tensor_copy(out=z[:, 0:3], in_=z[:, 512:515]), reads=[tz], writes=[tz])
                    elif ct in (4, 5, 7):
                        kb.op("act", lambda e, pb=pb: e.activation(out=sqb[:], in_=ps[pb][:, :], func=AF.Square), reads=[pst[pb]], writes=[t_sqb])
                        kb.op("pe", lambda e: e.matmul(ps[3][:, :], lhsT=blk64[:], rhs=sqb[:], start=True, stop=True),
                              reads=[t_blk64, t_sqb], writes=[pst[3]])
                        kb.op("act", lambda e: e.activation(out=rs2[:], in_=ps[3][:, :], func=AF.Sqrt, scale=1.0 / 64, bias=EPS),
                              reads=[pst[3]], writes=[t_rs2])
                        kb.op("dve", lambda e: e.reciprocal(out=rs2[:], in_=rs2[:]), reads=[t_rs2], writes=[t_rs2])
                        ob = rot2()
                        gcol = 0 if ct in (4, 5) else 3
                        kb.op("dve", lambda e, pb=pb, ob=ob, gcol=gcol: e.scalar_tensor_tensor(out=obf[ob][:], in0=ps[pb][:, :], scalar=qkg[:, gcol:gcol + 1],
                                                                                              in1=rs2[:], op0=ALU.mult, op1=ALU.mult),
                              reads=[pst[pb], t_rs2, t_qkg], writes=[t_obf[ob]])
                        if ct == 7:
                            kb.dma(s_kk[:, t0:t0 + 512], obf[ob][:], reads=[t_obf[ob]], writes=[t_scr["kk"]])
                        else:
                            r0 = (ct - 4) * 128
                            kb.dma(s_aq[r0:r0 + 128, t0:t0 + 512], obf[ob][:], reads=[t_obf[ob]], writes=[t_scr["aq"]])
                    elif ct == 6:
                        ob = rot2()
                        kb.op("act", lambda e, pb=pb, ob=ob: e.activation(out=ofm[ob][:], in_=ps[pb][:, :], func=AF.Copy), reads=[pst[pb]], writes=[t_ofm[ob]])
                        kb.dma(s_kv[:, t0:t0 + 512], ofm[ob][:], reads=[t_ofm[ob]], writes=[t_scr["kv"]])
                    else:
                        ob = rot2()
                        kb.op("act", lambda e, pb=pb, ob=ob: e.activation(out=ofm[ob][0:2, :], in_=ps[pb][0:2, :], func=AF.Copy), reads=[pst[pb]], writes=[t_ofm[ob]])
                        kb.dma(s_if[:, t0:t0 + 512], ofm[ob][0:2, :], reads=[t_ofm[ob]], writes=[t_scr["if"]])
                for ts in range(int(os.environ.get('NTS', '4')) if 'e' in parts else 0):
                    tt = t0 + ts * 128
                    pb = 4 + (ts % 2)
                    for k in range(16):
                        kb.op("pe", lambda e, k=k, ts=ts, pb=pb: e.matmul(ps[pb][:, :], lhsT=hT[:, k, ts * 128:(ts + 1) * 128], rhs=wb[:, k, 1026:1538],
                                                                          start=(k == 0), stop=(k == 15)),
                              reads=[t_wb, t_hT[k]], writes=[pst[pb]], sig=(k == 15))
                    pb2 = 6 + (ts % 2)
                    for k in range(16):
                        kb.op("pe", lambda e, k=k, ts=ts, pb2=pb2: e.matmul(ps[pb2][:, 0:140], lhsT=hT[:, k, ts * 128:(ts + 1) * 128], rhs=wb[:, k, 1538:1678],
                                                                            start=(k == 0), stop=(k == 15)),
                              reads=[t_wb, t_hT[k]], writes=[pst[pb2]], sig=(k == 15))
                    ob = ts % 2
                    kb.op("dve", lambda e, pb=pb, ob=ob: e.tensor_copy(out=otm[ob][:, 0:256], in_=ps[pb][:, 0:256]), reads=[pst[pb]], writes=[t_otm[ob]])
                    kb.op("act", lambda e, pb=pb, ob=ob: e.activation(out=otm[ob][:, 256:512], in_=ps[pb][:, 256:512], func=AF.Sigmoid),
                          reads=[pst[pb]], writes=[t_otm[ob]])
                    SK = os.environ.get('SKIP', '')
                    if 'P' not in SK:
                        kb.op("dve", lambda e, ob=ob: e.tensor_tensor(out=otm[ob][:, 256:512], in0=otm[ob][:, 256:512], in1=mng[:], op=ALU.mult),
                              reads=[t_otm[ob], t_mng], writes=[t_otm[ob]])
                    if 'S' not in SK:
                        kb.dma(s_v[tt:tt + 128, :], otm[ob][:, 0:256], reads=[t_otm[ob]], writes=[t_scr["v"]])
                        kb.dma(s_go[tt:tt + 128, :], otm[ob][:, 256:512], reads=[t_otm[ob]], writes=[t_scr["go"]])
                    kb.op("dve", lambda e, pb2=pb2, ob=ob: e.tensor_copy(out=otb[ob][:], in_=ps[pb2][:, 0:128]), reads=[pst[pb2]], writes=[t_otb[ob]])
                    kb.op("act", lambda e, pb2=pb2, ob=ob: e.activation(out=otg[ob][:], in_=ps[pb2][:, 128:140], func=AF.Sigmoid),
                          reads=[pst[pb2]], writes=[t_otg[ob]])
                    if 'V' not in os.environ.get('SKIP', ''):
                        kb.dma(s_vv[tt:tt + 128, :], otb[ob][:], reads=[t_otb[ob]], writes=[t_scr["vv"]])
                    if 'G' not in os.environ.get('SKIP', ''):
                        kb.dma(s_gt[tt:tt + 128, :], otg[ob][:], reads=[t_otg[ob]], writes=[t_scr["gt"]])
            kb.barrier()


    mixT = [nc.dram_tensor("mixT%d" % i, [512, 1024], BF16, kind="Internal").ap() for i in range(8)]; t_mixT = [Trk() for _ in range(8)]
    mixG = [nc.dram_tensor("mixG%d" % i, [2048, 1024], BF16, kind="Internal").ap() for i in range(8)]; t_mixG = [Trk() for _ in range(8)]

    def gather_chunk(i):
        if phases >= 4:
            kb.op("pool", lambda e: e.collective_compute("AllGather", ALU.bypass, replica_groups=[[0, 1, 2, 3], [4, 5, 6, 7]],
                                                         ins=[mixT[i]], outs=[mixG[i]]), reads=[t_mixT[i]], writes=[t_mixG[i]])
    if phases >= 2:
        bif_in = din("bif", [128, 2])
        ut_in = din("ut_f", [128, 128])
        identb_in = din("ident_b", [128, 128], BF16)
        with ExitStack() as p2:
            def sb2(name, shape, dt=F32):
                return p2.enter_context(nc.sbuf_tensor(name, list(shape), dt))
            NCH = 64
            bif = sb2("bif_sb", [128, 2]); t_bif = Trk()
            ut = sb2("ut_sb", [128, 128]); t_ut = Trk()
            identb = sb2("identb", [128, 128], BF16); t_identb = Trk()
            kb.dma(bif[:], bif_in, writes=[t_bif])
            kb.dma(ut[:], ut_in, writes=[t_ut])
            kb.dma(identb[:], identb_in, writes=[t_identb])
            rows = sb2("rows", [64, 2, 128]); t_rows = Trk()
            kb.dma(rows[:, 0, :], s_if[0:1, :].rearrange("o (c p) -> (o c) p", p=128), reads=[t_scr["if"]], writes=[t_rows])
            kb.dma(rows[:, 1, :], s_if[1:2, :].rearrange("o (c p) -> (o c) p", p=128), reads=[t_scr["if"]], writes=[t_rows])
            cols = sb2("cols", [128, 12, 64]); t_cols = Trk()
            nbf = sb2("nbf", [128, 1]); t_nbf = Trk()
            zero64 = sb2("zero64", [128, 128]); t_zero = Trk()
            kb.op("dve", lambda e: e.memset(zero64[:], 0.0), writes=[t_zero])
            kb.op("dve", lambda e: e.tensor_scalar(out=nbf[:], in0=bif[:, 1:2], scalar1=-1.0, scalar2=None, op0=ALU.mult), reads=[t_bif], writes=[t_nbf])
            for w in range(2):
                kb.op("pe", lambda e, w=w: e.matmul(ps[w][:, 0:64], lhsT=rows[:, w, :], rhs=ident[0:64, 0:64], start=True, stop=True),
                      reads=[t_rows, t_ident], writes=[pst[w]])
            kb.op("dve", lambda e: e.tensor_scalar(out=cols[:, 0, :], in0=ps[0][:, 0:64], scalar1=bif[:, 0:1], scalar2=None, op0=ALU.add),
                  reads=[pst[0], t_bif], writes=[t_cols])
            kb.op("act", lambda e: e.activation(out=cols[:, 11, :], in_=ps[1][:, 0:64], func=AF.Exp, scale=-1.0, bias=nbf[:, 0:1]),
                  reads=[pst[1], t_nbf], writes=[t_cols])
            kb.op("act", lambda e: e.activation(out=cols[:, 1, :], in_=cols[:, 11, :], func=AF.Ln, scale=1.0, bias=1.0), reads=[t_cols], writes=[t_cols])
            kb.op("dve", lambda e: e.tensor_scalar(out=cols[:, 1, :], in0=cols[:, 1, :], scalar1=-1.0, scalar2=None, op0=ALU.mult), reads=[t_cols], writes=[t_cols])
            kb.op("pe", lambda e: e.matmul(ps[2][:, 0:64], lhsT=ut[:], rhs=cols[:, 1, :], start=True, stop=True), reads=[t_ut, t_cols], writes=[pst[2]])
            kb.op("pe", lambda e: e.matmul(ps[3][:, 0:64], lhsT=ones_f[:], rhs=cols[:, 1, :], start=True, stop=True), reads=[t_ones_f, t_cols], writes=[pst[3]])
            kb.op("dve", lambda e: e.tensor_copy(out=cols[:, 11, :], in_=ps[3][:, 0:64]), reads=[pst[3]], writes=[t_cols])
            kb.op("dve", lambda e: e.tensor_tensor_scan(out=cols[:, 2, :], data0=cols[:, 11, :], data1=zero64[:, 0:64], initial=0.0, op0=ALU.add, op1=ALU.add),
                  reads=[t_cols, t_zero], writes=[t_cols])
            kb.op("dve", lambda e: e.tensor_tensor(out=cols[:, 2, :], in0=cols[:, 2, :], in1=cols[:, 11, :], op=ALU.subtract), reads=[t_cols], writes=[t_cols])
            kb.op("dve", lambda e: e.tensor_tensor(out=cols[:, 2, :], in0=cols[:, 2, :], in1=ps[2][:, 0:64], op=ALU.add), reads=[t_cols, pst[2]], writes=[t_cols])
            kb.op("dve", lambda e: e.tensor_tensor(out=cols[:, 3, :], in0=cols[:, 0, :], in1=cols[:, 2, :], op=ALU.subtract), reads=[t_cols], writes=[t_cols])
            grow = sb2("grow", [64, 128]); t_grow = Trk()
            cmrow = sb2("cmrow", [64, 128]); t_cmrow = Trk()
            kb.op("pe", lambda e: e.matmul(ps[4][0:64, 0:128], lhsT=cols[:, 3, :], rhs=ident[:, :], start=True, stop=True), reads=[t_cols, t_ident], writes=[pst[4]])
            kb.op("dve", lambda e: e.tensor_copy(out=grow[:], in_=ps[4][0:64, 0:128]), reads=[pst[4]], writes=[t_grow])
            kb.op("dve", lambda e: e.tensor_tensor_scan(out=cmrow[:], data0=grow[:], data1=grow[:], initial=-1e30, op0=ALU.max, op1=ALU.max),
                  reads=[t_grow], writes=[t_cmrow])
            mrow = sb2("mrow", [1, 3, 64]); t_mrow = Trk()
            kb.op("pe", lambda e: e.matmul(ps[5][0:1, 0:64], lhsT=cmrow[:, 127:128], rhs=ident[0:64, 0:64], start=True, stop=True),
                  reads=[t_cmrow, t_ident], writes=[pst[5]])
            kb.op("dve", lambda e: e.tensor_copy(out=mrow[0:1, 0, :], in_=ps[5][0:1, 0:64]), reads=[pst[5]], writes=[t_mrow])
            kb.op("dve", lambda e: e.tensor_tensor_scan(out=mrow[0:1, 1, :], data0=mrow[0:1, 0, :], data1=mrow[0:1, 0, :], initial=0.0, op0=ALU.max, op1=ALU.max),
                  reads=[t_mrow], writes=[t_mrow])
            kb.op("dve", lambda e: e.memset(mrow[0:1, 2, 0:1], 0.0), reads=[t_mrow], writes=[t_mrow])
            kb.op("dve", lambda e: e.tensor_copy(out=mrow[0:1, 2, 1:64], in_=mrow[0:1, 1, 0:63]), reads=[t_mrow], writes=[t_mrow])
            kb.op("pe", lambda e: e.matmul(ps[6][:, 0:128], lhsT=ones_f[0:1, :], rhs=mrow[0:1, 1:3, :].rearrange("o a c -> o (a c)"), start=True, stop=True),
                  reads=[t_ones_f, t_mrow], writes=[pst[6]])
            kb.op("dve", lambda e: e.tensor_copy(out=cols[:, 6, :], in_=ps[6][:, 0:64]), reads=[pst[6]], writes=[t_cols])
            kb.op("dve", lambda e: e.tensor_copy(out=cols[:, 5, :], in_=ps[6][:, 64:128]), reads=[pst[6]], writes=[t_cols])
            kb.op("pe", lambda e: e.matmul(ps[7][:, 0:64], lhsT=cmrow[:, :], rhs=ident[0:64, 0:64], start=True, stop=True), reads=[t_cmrow, t_ident], writes=[pst[7]])
            kb.op("dve", lambda e: e.tensor_tensor(out=cols[:, 4, :], in0=ps[7][:, 0:64], in1=cols[:, 5, :], op=ALU.max), reads=[pst[7], t_cols], writes=[t_cols])
            kb.op("dve", lambda e: e.tensor_tensor(out=cols[:, 11, :], in0=cols[:, 3, :], in1=cols[:, 5, :], op=ALU.subtract), reads=[t_cols], writes=[t_cols])
            kb.op("act", lambda e: e.activation(out=cols[:, 7, :], in_=cols[:, 11, :], func=AF.Exp), reads=[t_cols], writes=[t_cols])
            kb.op("dve", lambda e: e.tensor_scalar(out=cols[:, 7, :], in0=cols[:, 7, :], scalar1=1.0 / 16, scalar2=None, op0=ALU.mult), reads=[t_cols], writes=[t_cols])
            kb.op("dve", lambda e: e.tensor_tensor(out=cols[:, 11, :], in0=cols[:, 5, :], in1=cols[:, 4, :], op=ALU.subtract), reads=[t_cols], writes=[t_cols])
            kb.op("act", lambda e: e.activation(out=cols[:, 8, :], in_=cols[:, 11, :], func=AF.Exp), reads=[t_cols], writes=[t_cols])
            kb.op("dve", lambda e: e.tensor_tensor(out=cols[:, 11, :], in0=cols[:, 2, :], in1=cols[:, 4, :], op=ALU.add), reads=[t_cols], writes=[t_cols])
            kb.op("act", lambda e: e.activation(out=cols[:, 9, :], in_=cols[:, 11, :], func=AF.Exp, scale=-1.0), reads=[t_cols], writes=[t_cols])
            kb.op("dve", lambda e: e.tensor_tensor(out=cols[:, 11, :], in0=cols[:, 5, :], in1=cols[:, 6, :], op=ALU.subtract), reads=[t_cols], writes=[t_cols])
            kb.op("act", lambda e: e.activation(out=cols[:, 10, :], in_=cols[:, 11, :], func=AF.Exp), reads=[t_cols], writes=[t_cols])
            if dbg:
                d_cols = nc.dram_tensor("d_cols", [128, 12, 64], F32, kind="ExternalOutput").ap()
                kb.dma(d_cols, cols[:], reads=[t_cols])

            qT = [sb2("qT%d" % i, [128, 2, 128]) for i in range(2)]; t_qT = [Trk(), Trk()]
            kT = [sb2("kT%d" % i, [128, 2, 128]) for i in range(2)]; t_kT = [Trk(), Trk()]
            va = [sb2("va%d" % i, [128, 257]) for i in range(2)]; t_va = [Trk(), Trk()]
            go = [sb2("go%d" % i, [128, 256]) for i in range(2)]; t_go = [Trk(), Trk()]
            kp = sb2("kp", [128, 256]); t_kp = Trk()
            wT = sb2("wT", [128, 128]); t_wT = Trk()
            St = [sb2("St%d" % i, [128, 2, 257]) for i in range(2)]; t_St = [Trk(), Trk()]
            sc = sb2("sc", [128, 8]); t_sc = Trk()
            junk = sb2("junk", [128, 256]); t_junk = Trk()
            hmf = sb2("hmf", [128, 256], BF16); t_hmf = Trk()
            hmT = [sb2("hmT%d" % i, [128, 2, 128], BF16) for i in range(2)]; t_hmT = [Trk(), Trk()]
            for i in range(2):
                kb.op("dve", lambda e, i=i: e.memset(va[i][:, 256:257], 1.0), writes=[t_va[i]])
            kb.op("dve", lambda e: e.memset(St[0][:], 0.0), writes=[t_St[0]])
            qv = s_q.rearrange("(a p) t -> p a t", p=128)
            kv_ = s_k.rearrange("(a p) t -> p a t", p=128)

            def load_chunk(c):
                bi = c % 2
                kb.dma(qT[bi][:], qv[:, :, c * 128:(c + 1) * 128], reads=[t_scr["q"]], writes=[t_qT[bi]])
                kb.dma(kT[bi][:], kv_[:, :, c * 128:(c + 1) * 128], reads=[t_scr["k"]], writes=[t_kT[bi]])
                kb.dma(va[bi][:, 0:256], s_v[c * 128:(c + 1) * 128, :], reads=[t_scr["v"]], writes=[t_va[bi]])
                kb.dma(go[bi][:], s_go[c * 128:(c + 1) * 128, :], reads=[t_scr["go"]], writes=[t_go[bi]])

            nch = int(os.environ.get("NCH", "64"))
            load_chunk(0)
            for c in range(nch):
                bi = c % 2
                so = St[c % 2]; tso = t_St[c % 2]
                sn = St[(c + 1) % 2]; tsn = t_St[(c + 1) % 2]
                if c + 1 < nch:
                    load_chunk(c + 1)
                for dc in range(2):
                    kb.op("pe", lambda e, dc=dc, bi=bi: e.matmul(ps[0][:, dc * 128:(dc + 1) * 128], lhsT=kT[bi][:, dc, :], rhs=ident[:, :], start=True, stop=True),
                          reads=[t_kT[bi], t_ident], writes=[pst[0]], sig=(dc == 1))
                kb.op("dve", lambda e, c=c: e.tensor_scalar(out=kp[:], in0=ps[0][:, 0:256], scalar1=cols[:, 7, c:c + 1], scalar2=None, op0=ALU.mult),
                      reads=[pst[0], t_cols], writes=[t_kp])
                for dc in range(2):
                    kb.op("pe", lambda e, dc=dc, bi=bi: e.matmul(ps[1][:, 0:128], lhsT=kT[bi][:, dc, :], rhs=qT[bi][:, dc, :], start=(dc == 0), stop=(dc == 1)),
                          reads=[t_kT[bi], t_qT[bi]], writes=[pst[1]], sig=(dc == 1))
                kb.op("dve", lambda e, c=c: e.scalar_tensor_tensor(out=wT[:], in0=ps[1][:, 0:128], scalar=cols[:, 7, c:c + 1], in1=ut[:], op0=ALU.mult, op1=ALU.mult),
                      reads=[pst[1], t_cols, t_ut], writes=[t_wT])
                kb.op("pe", lambda e, bi=bi: e.matmul(ps[2][:, 0:257], lhsT=wT[:], rhs=va[bi][:], start=True, stop=False),
                      reads=[t_wT, t_va[bi]], writes=[pst[2]], sig=False)
                for dc in range(2):
                    kb.op("pe", lambda e, dc=dc, bi=bi, so=so: e.matmul(ps[2][:, 0:257], lhsT=qT[bi][:, dc, :], rhs=so[:, dc, :], start=False, stop=(dc == 1)),
                          reads=[t_qT[bi], tso], writes=[pst[2]], sig=(dc == 1))
                for dc in range(2):
                    pb = 3 + dc
                    kb.op("pe", lambda e, dc=dc, bi=bi, pb=pb: e.matmul(ps[pb][:, 0:257], lhsT=kp[:, dc * 128:(dc + 1) * 128], rhs=va[bi][:], start=True, stop=False),
                          reads=[t_kp, t_va[bi]], writes=[pst[pb]], sig=False)
                    kb.op("pe", lambda e, dc=dc, pb=pb, so=so: e.matmul(ps[pb][:, 0:257], lhsT=ident[:, :], rhs=so[:, dc, :], start=False, stop=True),
                          reads=[t_ident, tso], writes=[pst[pb]])
                    kb.op("act" if dc else "dve",
                          (lambda e, dc=dc, pb=pb, sn=sn, c=c: e.activation(out=sn[:, dc, :], in_=ps[pb][:, 0:257], func=AF.Copy, scale=cols[:, 10, c:c + 1])) if dc else
                          (lambda e, dc=dc, pb=pb, sn=sn, c=c: e.tensor_scalar(out=sn[:, dc, :], in0=ps[pb][:, 0:257], scalar1=cols[:, 10, c:c + 1], scalar2=None, op0=ALU.mult)),
                          reads=[pst[pb], t_cols], writes=[tsn])
                kb.op("act", lambda e, c=c: e.activation(out=sc[:, 0:1], in_=ps[2][:, 256:257], func=AF.Abs, scale=cols[:, 8, c:c + 1]),
                      reads=[pst[2], t_cols], writes=[t_sc])
                kb.op("dve", lambda e, c=c: e.tensor_tensor(out=sc[:, 1:2], in0=sc[:, 0:1], in1=cols[:, 9, c:c + 1], op=ALU.max), reads=[t_sc, t_cols], writes=[t_sc])
                kb.op("dve", lambda e: e.reciprocal(out=sc[:, 2:3], in_=sc[:, 1:2]), reads=[t_sc], writes=[t_sc])
                kb.op("dve", lambda e, c=c: e.tensor_tensor(out=sc[:, 3:4], in0=sc[:, 2:3], in1=cols[:, 8, c:c + 1], op=ALU.mult), reads=[t_sc, t_cols], writes=[t_sc])
                kb.op("act", lambda e: e.activation(out=junk[:], in_=ps[2][:, 0:256], func=AF.Square, accum_out=sc[:, 4:5]), reads=[pst[2]], writes=[t_junk, t_sc])
                kb.op("dve", lambda e: e.scalar_tensor_tensor(out=sc[:, 5:6], in0=sc[:, 3:4], scalar=sc[:, 3:4], in1=sc[:, 4:5], op0=ALU.mult, op1=ALU.mult),
                      reads=[t_sc], writes=[t_sc])
                kb.op("act", lambda e: e.activation(out=sc[:, 5:6], in_=sc[:, 5:6], func=AF.Sqrt, scale=1.0 / 256, bias=EPS), reads=[t_sc], writes=[t_sc])
                kb.op("dve", lambda e: e.reciprocal(out=sc[:, 6:7], in_=sc[:, 5:6]), reads=[t_sc], writes=[t_sc])
                kb.op("dve", lambda e: e.tensor_tensor(out=sc[:, 7:8], in0=sc[:, 6:7], in1=sc[:, 3:4], op=ALU.mult), reads=[t_sc], writes=[t_sc])
                kb.op("dve", lambda e, bi=bi: e.scalar_tensor_tensor(out=hmf[:], in0=ps[2][:, 0:256], scalar=sc[:, 7:8], in1=go[bi][:], op0=ALU.mult, op1=ALU.mult),
                      reads=[pst[2], t_sc, t_go[bi]], writes=[t_hmf])
                ob = c % 2
                for dc in range(2):
                    kb.op("pe", lambda e, dc=dc: e.matmul(ps[5][:, dc * 128:(dc + 1) * 128], lhsT=hmf[:, dc * 128:(dc + 1) * 128], rhs=identb[:, :], start=True, stop=True),
                          reads=[t_hmf, t_identb], writes=[pst[5]], sig=(dc == 1))
                kb.op("act", lambda e, ob=ob: e.activation(out=hmT[ob][:].rearrange("p a t -> p (a t)"), in_=ps[5][:, 0:256], func=AF.Copy), reads=[pst[5]], writes=[t_hmT[ob]])
                kb.dma(mixT[c // 8][0:256, (c % 8) * 128:(c % 8 + 1) * 128].rearrange("(a p) t -> p a t", p=128), hmT[ob][:], reads=[t_hmT[ob]], writes=[t_mixT[c // 8]])
            kb.barrier()

    if phases >= 3:
        tb_sw_in = din("tb_sw", [128, 64, 4])
        tb_c_in = din("tb_c", [128, 64, 4])
        cmask_in = din("cmask", [128, 17, 128], BF16)
        caus_in = din("caus_add", [128, 128], BF16)
        acaus_in = din("acaus_add", [128, 128], BF16)
        ovl_in = din("ovl", [128, 4, 128], BF16)
        expT_in = din("expT", [128, S], BF16)
        tkeep_in = din("t_keep", [128, 255])
        tadd_in = din("t_add", [128, 255])
        w1k_in = din("w1k", [2048, 256]); w1v_in = din("w1v", [2048, 256])
        w2k_in = din("w2k", [256, 64]); w2v_in = din("w2v", [256, 64])
        posk_in = din("posk", [128, 16]); posv_in = din("posv", [128, 16])
        kng0_in = din("kng0", [64, 1])
        with ExitStack() as p3:
            def sb3(name, shape, dt=F32):
                return p3.enter_context(nc.sbuf_tensor(name, list(shape), dt))
            QT = sb3("QT", [128, 4, S], BF16); t_QT = Trk()
            KK = sb3("KK", [128, S], BF16); t_KK = Trk()
            Vs = sb3("Vs", [128, 64, 65], BF16); t_Vs = Trk()
            Vw = sb3("Vw", [128, 64, 65], BF16); t_Vw = Trk()
            expT = sb3("expT_sb", [128, S], BF16); t_expT = Trk()
            tb_sw = sb3("tb_sw_sb", [128, 64, 4]); t_tbsw = Trk()
            tb_c = sb3("tb_c_sb", [128, 64, 4]); t_tbc = Trk()
            cmask = sb3("cmask_sb", [128, 17, 128], BF16); t_cmask = Trk()
            caus = sb3("caus_sb", [128, 4, 128], BF16); t_caus = Trk()
            acaus = sb3("acaus_sb", [128, 4, 128], BF16); t_acaus = Trk()
            ovl = sb3("ovl_sb", [128, 4, 128], BF16); t_ovl = Trk()
            tkeep = sb3("tkeep_sb", [128, 255]); t_tkeep = Trk()
            tadd = sb3("tadd_sb", [128, 255]); t_tadd = Trk()
            gts = sb3("gts", [128, 64, 12]); t_gts = Trk()
            kcT = sb3("kcT", [64, 512], BF16); t_kcT = Trk()
            vca = sb3("vca", [128, 4, 65], BF16); t_vca = Trk()
            identb3 = sb3("identb3", [128, 128], BF16); t_identb3 = Trk()
            aqv = s_aq.rearrange("(h d) t -> d h t", d=64)
            for hh in range(2):
                for h in range(4):
                    kb.dma(QT[hh * 64:(hh + 1) * 64, h, :], aqv[:, h, :], reads=[t_scr["aq"]], writes=[t_QT])
            kb.dma(KK[:, 0:4096], s_kk[:, 0:4096], reads=[t_scr["kk"]], writes=[t_KK])
            kb.dma(KK[:, 4096:S], s_kk[:, 4096:S], reads=[t_scr["kk"]], writes=[t_KK])
            kb.dma(expT[:], expT_in, writes=[t_expT])
            kb.dma(tb_sw[:], tb_sw_in, writes=[t_tbsw]); kb.dma(tb_c[:], tb_c_in, writes=[t_tbc])
            kb.dma(cmask[:], cmask_in, writes=[t_cmask])
            for h4 in range(4):
                kb.dma(caus[:, h4, :], caus_in, writes=[t_caus]); kb.dma(acaus[:, h4, :], acaus_in, writes=[t_acaus])
            kb.dma(ovl[:], ovl_in, writes=[t_ovl]); kb.dma(tkeep[:], tkeep_in, writes=[t_tkeep]); kb.dma(tadd[:], tadd_in, writes=[t_tadd])
            kb.dma(identb3[:], identb_in if phases >= 2 else din("ident_b", [128, 128], BF16), writes=[t_identb3])
            kb.dma(gts[:], s_gt.rearrange("(q p) c -> p q c", p=128), reads=[t_scr["gt"]], writes=[t_gts])
            with ExitStack() as p3a:
                def sb3a(name, shape, dt=F32):
                    return p3a.enter_context(nc.sbuf_tensor(name, list(shape), dt))
                vtmp = sb3a("vtmp", [128, 64, 128], BF16); t_vtmp = Trk()
                kb.dma(vtmp[:], s_vv.rearrange("(q p) c -> p q c", p=128), reads=[t_scr["vv"]], writes=[t_vtmp])
                kb.op("dve", lambda e: e.tensor_copy(out=Vs[:, :, 0:64], in_=vtmp[:, :, 0:64]), reads=[t_vtmp], writes=[t_Vs])
                kb.op("dve", lambda e: e.tensor_copy(out=Vw[:, :, 0:64], in_=vtmp[:, :, 64:128]), reads=[t_vtmp], writes=[t_Vw])
                kb.op("dve", lambda e: e.memset(Vs[:, :, 64:65], 1.0), writes=[t_Vs])
                kb.op("dve", lambda e: e.memset(Vw[:, :, 64:65], 1.0), writes=[t_Vw])
                kb.op("dve", lambda e: e.memset(vca[:, :, 64:65], 1.0), writes=[t_vca])
                w1f = sb3a("w1f", [128, 16, 256]); t_w1f = Trk()
                w1b = sb3a("w1b", [128, 16, 256], BF16); t_w1b = Trk()
                w2f = sb3a("w2f", [128, 2, 64]); t_w2f = Trk()
                w2b = sb3a("w2b", [128, 2, 64], BF16); t_w2b = Trk()
                posf = sb3a("posf", [128, 16]); t_posf = Trk()
                bcol = sb3a("bcol", [128, 2]); t_bcol = Trk()
                kng0 = sb3a("kng0_sb", [64, 1]); t_kng0 = Trk()
                a2f = sb3a("a2f", [128, 2048]); t_a2f = Trk()
                a2b = sb3a("a2b", [128, S], BF16); t_a2b = Trk()
                hid = sb3a("hid", [128, 2, 512], BF16); t_hid = Trk()
                csq = sb3a("csq", [64, 512], BF16); t_csq = Trk()
                crs = sb3a("crs", [64, 512]); t_crs = Trk()
                kb.dma(kng0[:], kng0_in, writes=[t_kng0])
                kb.op("dve", lambda e: e.memset(hid[:], 0.0), writes=[t_hid])
                kb.op("dve", lambda e: e.memset(kcT[:], 0.0), writes=[t_kcT])
                for which in range(2):
                    w1_in = w1k_in if which == 0 else w1v_in
                    w2_in = w2k_in if which == 0 else w2v_in
                    pos_in = posk_in if which == 0 else posv_in
                    r0 = 0 if which == 0 else 64
                    kb.dma(w1f[:, 0:8, :], w1_in.rearrange("(k p) n -> p k n", p=128)[:, 0:8, :], writes=[t_w1f])
                    kb.dma(w1f[:, 8:16, :], w1_in.rearrange("(k p) n -> p k n", p=128)[:, 8:16, :], writes=[t_w1f])
                    kb.dma(w2f[:], w2_in.rearrange("(k p) n -> p k n", p=128), writes=[t_w2f])
                    kb.dma(posf[:], pos_in, writes=[t_posf])
                    kb.op("dve", lambda e: e.tensor_copy(out=w1b[:], in_=w1f[:]), reads=[t_w1f], writes=[t_w1b])
                    kb.op("dve", lambda e: e.tensor_copy(out=w2b[:], in_=w2f[:]), reads=[t_w2f], writes=[t_w2b])
                    for q4 in range(4):
                        c0 = q4 * 2048
                        kb.dma(a2f[0:64, :], s_kv[r0:r0 + 64, c0:c0 + 2048], reads=[t_scr["kv"]], writes=[t_a2f])
                        if q4 < 3:
                            kb.dma(a2f[64:128, :], s_kv[r0:r0 + 64, c0 + 1:c0 + 2049], reads=[t_scr["kv"]], writes=[t_a2f])
                        else:
                            kb.dma(a2f[64:128, 0:2047], s_kv[r0:r0 + 64, c0 + 1:c0 + 2048], reads=[t_scr["kv"]], writes=[t_a2f])
                        kb.op("dve", lambda e, c0=c0: e.tensor_copy(out=a2b[:, c0:c0 + 2048], in_=a2f[:]), reads=[t_a2f], writes=[t_a2b])
                    for hc in range(2):
                        for jj in range(16):
                            kb.op("pe", lambda e, hc=hc, jj=jj: e.matmul(ps[7][:, hc:hc + 1], lhsT=w1f[:, jj, hc * 128:(hc + 1) * 128], rhs=posf[:, jj:jj + 1],
                                                                          start=(jj == 0), stop=(jj == 15)),
                                  reads=[t_w1f, t_posf], writes=[pst[7]], sig=(jj == 15))
                    kb.op("dve", lambda e: e.tensor_copy(out=bcol[:], in_=ps[7][:, 0:2]), reads=[pst[7]], writes=[t_bcol])
                    a2v = a2b[:].rearrange("p (i s) -> p i s", s=16)
                    for hc in range(2):
                        for jj in range(16):
                            j0 = 2 * jj
                            rhs = a2v[:, 0:511, j0] if j0 < 16 else a2v[:, 1:512, j0 - 16]
                            kb.op("pe", lambda e, hc=hc, jj=jj, rhs=rhs: e.matmul(ps[hc][:, 0:511], lhsT=w1b[:, jj, hc * 128:(hc + 1) * 128], rhs=rhs,
                                                                                  start=(jj == 0), stop=(jj == 15)),
                                  reads=[t_w1b, t_a2b], writes=[pst[hc]], sig=(jj == 15))
                        kb.op("act", lambda e, hc=hc: e.activation(out=hid[:, hc, 0:511], in_=ps[hc][:, 0:511], func=AF.Gelu_apprx_tanh, bias=bcol[:, hc:hc + 1]),
                              reads=[pst[hc], t_bcol], writes=[t_hid])
                    if which == 0:
                        for hc in range(2):
                            kb.op("pe", lambda e, hc=hc: e.matmul(ps[2][0:64, 0:511], lhsT=w2b[:, hc, :], rhs=hid[:, hc, 0:511], start=(hc == 0), stop=(hc == 1)),
                                  reads=[t_w2b, t_hid], writes=[pst[2]], sig=(hc == 1))
                        kb.op("act", lambda e: e.activation(out=csq[:, 0:511], in_=ps[2][0:64, 0:511], func=AF.Square), reads=[pst[2]], writes=[t_csq])
                        kb.op("pe", lambda e: e.matmul(ps[3][0:64, 0:511], lhsT=blk64[0:64, 0:64], rhs=csq[:, 0:511], start=True, stop=True),
                              reads=[t_blk64, t_csq], writes=[pst[3]])
                        kb.op("act", lambda e: e.activation(out=crs[:, 0:511], in_=ps[3][0:64, 0:511], func=AF.Sqrt, scale=1.0 / 64, bias=EPS), reads=[pst[3]], writes=[t_crs])
                        kb.op("dve", lambda e: e.reciprocal(out=crs[:, 0:511], in_=crs[:, 0:511]), reads=[t_crs], writes=[t_crs])
                        kb.op("dve", lambda e: e.scalar_tensor_tensor(out=kcT[:, 0:511], in0=ps[2][0:64, 0:511], scalar=kng0[:, 0:1], in1=crs[:, 0:511],
                                                                      op0=ALU.mult, op1=ALU.mult), reads=[pst[2], t_kng0, t_crs], writes=[t_kcT])
                    else:
                        for it in range(4):
                            for hc in range(2):
                                kb.op("pe", lambda e, hc=hc, it=it: e.matmul(ps[4][:, it * 64:(it + 1) * 64], lhsT=hid[:, hc, it * 128:(it + 1) * 128], rhs=w2b[:, hc, :],
                                                                             start=(hc == 0), stop=(hc == 1)),
                                      reads=[t_hid, t_w2b], writes=[pst[4]], sig=(hc == 1 and it == 3))
                        kb.op("dve", lambda e: e.tensor_copy(out=vca[:, :, 0:64], in_=ps[4][:, 0:256].rearrange("p (a d) -> p a d", d=64)), reads=[pst[4]], writes=[t_vca])
                if dbg:
                    d_kcT = nc.dram_tensor("d_kcT", [64, 512], BF16, kind="ExternalOutput").ap()
                    d_vca = nc.dram_tensor("d_vca", [128, 4, 65], BF16, kind="ExternalOutput").ap()
                    kb.dma(d_kcT, kcT[:], reads=[t_kcT]); kb.dma(d_vca, vca[:], reads=[t_vca])
                kb.barrier()

            pT = [sb3("pT%d" % i, [128, 512], BF16) for i in range(3)]; t_pT = [[Trk() for _ in range(4)] for _ in range(3)]
            oT = sb3("oT", [65, 3, 512]); t_oT = Trk()
            zsc = sb3("zsc", [128, 24]); t_zsc = Trk()
            impn = sb3("impn", [128, 128]); t_impn = Trk()
            impw = sb3("impw", [128, 128]); t_impw = Trk()
            m8 = sb3("m8", [128, 16]); t_m8 = Trk()
            selb = sb3("selb", [128, 128], BF16); t_selb = Trk()
            selT = sb3("selT", [128, 4, 128], BF16); t_selT = Trk()
            haf = sb3("haf", [128, 256]); t_haf = Trk()
            hab = sb3("hab", [128, 256], BF16); t_hab = Trk()
            haT = [sb3("haT%d" % i, [128, 2, 128], BF16) for i in range(2)]; t_haT = [Trk(), Trk()]
            if dbg:
                d_imp = nc.dram_tensor("d_imp", [S, 128], F32, kind="ExternalOutput").ap()
                d_sel = nc.dram_tensor("d_sel", [S, 128], BF16, kind="ExternalOutput").ap()
            pcount = [0]
            NEG = -30000.0

            pend = []

            def flush_pairs():
                while pend:
                    pend.pop(0)()

            def tile_pair(lhsK, rhsQ, masks, bias_tab, bidx, Vaug, obank, first, last, imp_it=None):
                i = pcount[0] % 2
                pi = pcount[0] % 3
                pcount[0] += 1
                sps = ps[i]; tsp = pst[i]
                nm = len(masks)
                kb.op("pe", lambda e: e.matmul(sps[:, :], lhsT=lhsK[0], rhs=rhsQ[0], start=True, stop=(nm == 0)),
                      reads=[lhsK[1], rhsQ[1]], writes=[tsp], sig=(nm == 0))
                for mi, (ml, mr, mt) in enumerate(masks):
                    lastm = (mi == nm - 1)
                    if mr.shape[-1] == 512 or len(mr.shape) == 3:
                        kb.op("pe", lambda e, ml=ml, mr=mr, lastm=lastm: e.matmul(sps[:, :], lhsT=ml, rhs=mr, start=False, stop=lastm),
                              reads=mt, writes=[tsp], sig=lastm)
                    else:
                        for h in range(4):
                            lm = (lastm and h == 3)
                            kb.op("pe", lambda e, ml=ml, mr=mr, h=h, lm=lm: e.matmul(sps[:, h * 128:(h + 1) * 128], lhsT=ml, rhs=mr, start=False, stop=lm),
                                  reads=mt, writes=[tsp], sig=lm)
                for h in range(4):
                    kb.op("act", lambda e, h=h: e.activation(out=pT[pi][:, h * 128:(h + 1) * 128], in_=sps[:, h * 128:(h + 1) * 128], func=AF.Exp,
                                                             bias=bias_tab[0][:, bidx, h:h + 1]),
                          reads=[tsp, bias_tab[1]], writes=[t_pT[pi][h]])

                def stage_b():
                    kb.op("pe", lambda e: e.matmul(ps[obank][0:65, :], lhsT=Vaug[0], rhs=pT[pi][:, :], start=first, stop=last),
                          reads=[Vaug[1]] + t_pT[pi], writes=[pst[obank]], sig=last)
                    if imp_it is not None:
                        it, nit = imp_it
                        for h in range(4):
                            kb.op("pe", lambda e, h=h, it=it: e.matmul(ps[5][:, h * 128:(h + 1) * 128], lhsT=pT[pi][:, h * 128:(h + 1) * 128], rhs=ovl[:, it, :],
                                                                       start=(it == 0), stop=(it == nit - 1)),
                                  reads=[t_pT[pi][h], t_ovl], writes=[pst[5]], sig=(it == nit - 1 and h == 3))
                while len(pend) > 1:
                    pend.pop(0)()
                pend.append(stage_b)
                if len(pend) > 1:
                    pend.pop(0)()

            nqt = int(os.environ.get("NQT", "64"))
            for qt in range(nqt):
                q0 = qt * 128
                qv_lo = (QT[0:64, :, q0:q0 + 128], t_QT)
                qv_hi = (QT[64:128, :, q0:q0 + 128], t_QT)
                nit = (8 * qt + 6) // 128 + 1
                for it in range(nit):
                    m = qt - 16 * it
                    masks = []
                    if m <= 16:
                        masks.append((identb3[:, :], cmask[:, m, :], [t_identb3, t_cmask]))
                    tile_pair((kcT[0:64, it * 128:(it + 1) * 128], t_kcT), qv_lo, masks, (tb_c, t_tbc), qt - 16 * it, (vca[:, it, :], t_vca), 2,
                              it == 0, it == nit - 1, imp_it=(it, nit))
                kts = [kt for kt in range(qt - 4, qt + 1) if kt >= 0]
                for n_, kt in enumerate(kts):
                    masks = []
                    if kt == qt:
                        masks.append((identb3[:, :], caus[:, :, :], [t_identb3, t_caus]))
                    if kt == qt - 4:
                        masks.append((identb3[:, :], acaus[:, :, :], [t_identb3, t_acaus]))
                    tile_pair((KK[64:128, kt * 128:(kt + 1) * 128], t_KK), qv_hi, masks, (tb_sw, t_tbsw), qt - kt, (Vw[:, kt, :], t_Vw), 4,
                              n_ == 0, n_ == len(kts) - 1)
                flush_pairs()
                kb.op("dve", lambda e: e.tensor_copy(out=oT[:, 0, :], in_=ps[2][0:65, :]), reads=[pst[2]], writes=[t_oT])
                for h in range(4):
                    kb.op("pe", lambda e, h=h: e.matmul(ps[6][:, h * 65:(h + 1) * 65], lhsT=oT[0:65, 0, h * 128:(h + 1) * 128], rhs=ident[0:65, 0:65], start=True, stop=True),
                          reads=[t_oT, t_ident], writes=[pst[6]], sig=(h == 3))
                kb.op("dve", lambda e: e.tensor_scalar(out=zsc[:, 0:4], in0=ps[6][:, 0:260].rearrange("p (h c) -> p h c", c=65)[:, :, 64], scalar1=1e-30, scalar2=None, op0=ALU.max),
                      reads=[pst[6]], writes=[t_zsc])
                kb.op("dve", lambda e: e.reciprocal(out=zsc[:, 0:4], in_=zsc[:, 0:4]), reads=[t_zsc], writes=[t_zsc])
                kb.op("dve", lambda e: e.tensor_scalar(out=impn[:], in0=ps[5][:, 0:128], scalar1=zsc[:, 0:1], scalar2=None, op0=ALU.mult), reads=[pst[5], t_zsc], writes=[t_impn])
                for h in range(1, 4):
                    kb.op("dve", lambda e, h=h: e.scalar_tensor_tensor(out=impn[:], in0=ps[5][:, h * 128:(h + 1) * 128], scalar=zsc[:, h:h + 1], in1=impn[:], op0=ALU.mult, op1=ALU.add),
                          reads=[pst[5], t_zsc, t_impn], writes=[t_impn])
                if dbg:
                    kb.dma(d_imp[q0:q0 + 128, :], impn[:], reads=[t_impn])
                o0 = 127 - 2 * qt
                kb.op("dve", lambda e, o0=o0: e.tensor_tensor(out=impn[:], in0=impn[:], in1=tkeep[:, o0:o0 + 128], op=ALU.mult), reads=[t_impn, t_tkeep], writes=[t_impn])
                kb.op("dve", lambda e, o0=o0: e.tensor_tensor(out=impn[:], in0=impn[:], in1=tadd[:, o0:o0 + 128], op=ALU.add), reads=[t_impn, t_tadd], writes=[t_impn])
                kb.op("dve", lambda e: e.memset(impn[:, 0:1], 1e4), reads=[t_impn], writes=[t_impn])
                kb.op("dve", lambda e: e.max(out=m8[:, 0:8], in_=impn[:]), reads=[t_impn], writes=[t_m8])
                kb.op("dve", lambda e: e.match_replace(out=impw[:], in_to_replace=m8[:, 0:8], in_values=impn[:], imm_value=-3e38), reads=[t_impn, t_m8], writes=[t_impw])
                kb.op("dve", lambda e: e.max(out=m8[:, 8:16], in_=impw[:]), reads=[t_impw], writes=[t_m8])
                kb.op("dve", lambda e: e.tensor_scalar(out=selb[:], in0=impn[:], scalar1=m8[:, 15:16], scalar2=None, op0=ALU.is_ge), reads=[t_impn, t_m8], writes=[t_selb])
                if dbg:
                    kb.dma(d_sel[q0:q0 + 128, :], selb[:], reads=[t_selb])
                kb.op("pe", lambda e: e.matmul(ps[7][:, 0:128], lhsT=selb[:, :], rhs=identb3[:, :], start=True, stop=True), reads=[t_selb, t_identb3], writes=[pst[7]])
                kb.op("dve", lambda e: e.tensor_scalar(out=selT[:], in0=ps[7][:, 0:128].unsqueeze(1).broadcast_to([128, 4, 128]), scalar1=-1.0, scalar2=-NEG, op0=ALU.add, op1=ALU.mult),
                      reads=[pst[7]], writes=[t_selT])
                for kt in range(qt + 1):
                    if kt == qt:
                        masks = [(identb3[:, :], caus[:, :, :], [t_identb3, t_caus])]
                    else:
                        masks = [(expT[:, kt * 128:(kt + 1) * 128], selT[:, :, :], [t_expT, t_selT])]
                    tile_pair((KK[0:64, kt * 128:(kt + 1) * 128], t_KK), qv_lo, masks, (tb_sw, t_tbsw), qt - kt, (Vs[:, kt, :], t_Vs), 3, kt == 0, kt == qt)
                flush_pairs()
                kb.op("dve", lambda e: e.tensor_copy(out=oT[:, 1, :], in_=ps[3][0:65, :]), reads=[pst[3]], writes=[t_oT])
                kb.op("dve", lambda e: e.tensor_copy(out=oT[:, 2, :], in_=ps[4][0:65, :]), reads=[pst[4]], writes=[t_oT])
                for br in (1, 2):
                    for h in range(4):
                        col = 260 + ((br - 1) * 4 + h) * 65
                        pbk, cc = (6, col) if col + 65 <= 512 else (7, col - 455 + 128)
                        kb.op("pe", lambda e, h=h, br=br, pbk=pbk, cc=cc: e.matmul(ps[pbk][:, cc:cc + 65], lhsT=oT[0:65, br, h * 128:(h + 1) * 128], rhs=ident[0:65, 0:65], start=True, stop=True),
                              reads=[t_oT, t_ident], writes=[pst[pbk]])

                def oslot(br, h):
                    if br == 0:
                        return 6, h * 65
                    col = 260 + ((br - 1) * 4 + h) * 65
                    return (6, col) if col + 65 <= 512 else (7, col - 455 + 128)
                for br in (1, 2):
                    for h in range(4):
                        pbk, cc = oslot(br, h)
                        kb.op("dve", lambda e, br=br, h=h, pbk=pbk, cc=cc: e.tensor_scalar(out=zsc[:, br * 4 + h:br * 4 + h + 1], in0=ps[pbk][:, cc + 64:cc + 65], scalar1=1e-30, scalar2=None, op0=ALU.max),
                              reads=[pst[pbk]], writes=[t_zsc])
                kb.op("dve", lambda e: e.reciprocal(out=zsc[:, 4:12], in_=zsc[:, 4:12]), reads=[t_zsc], writes=[t_zsc])
                for br in range(3):
                    kb.op("dve", lambda e, br=br, qt=qt: e.tensor_tensor(out=zsc[:, 12 + br * 4:16 + br * 4], in0=zsc[:, br * 4:br * 4 + 4],
                                                                         in1=gts[:, qt, :].rearrange("p (h b) -> p h b", b=3)[:, :, br], op=ALU.mult),
                          reads=[t_zsc, t_gts], writes=[t_zsc])
                for h in range(4):
                    for br in range(3):
                        pbk, cc = oslot(br, h)
                        if br == 0:
                            kb.op("dve", lambda e, h=h, br=br, pbk=pbk, cc=cc: e.tensor_scalar(out=haf[:, h * 64:(h + 1) * 64], in0=ps[pbk][:, cc:cc + 64],
                                                                                               scalar1=zsc[:, 12 + br * 4 + h:13 + br * 4 + h], scalar2=None, op0=ALU.mult),
                                  reads=[pst[pbk], t_zsc], writes=[t_haf])
                        else:
                            kb.op("dve", lambda e, h=h, br=br, pbk=pbk, cc=cc: e.scalar_tensor_tensor(out=haf[:, h * 64:(h + 1) * 64], in0=ps[pbk][:, cc:cc + 64],
                                                                                                      scalar=zsc[:, 12 + br * 4 + h:13 + br * 4 + h], in1=haf[:, h * 64:(h + 1) * 64],
                                                                                                      op0=ALU.mult, op1=ALU.add),
                                  reads=[pst[pbk], t_zsc, t_haf], writes=[t_haf])
                kb.op("dve", lambda e: e.tensor_copy(out=hab[:], in_=haf[:]), reads=[t_haf], writes=[t_hab])
                ob = qt % 2
                for dc in range(2):
                    kb.op("pe", lambda e, dc=dc: e.matmul(ps[5][:, dc * 128:(dc + 1) * 128], lhsT=hab[:, dc * 128:(dc + 1) * 128], rhs=identb3[:, :], start=True, stop=True),
                          reads=[t_hab, t_identb3], writes=[pst[5]], sig=(dc == 1))
                kb.op("dve", lambda e, ob=ob: e.tensor_copy(out=haT[ob][:].rearrange("p a t -> p (a t)"), in_=ps[5][:, 0:256]), reads=[pst[5]], writes=[t_haT[ob]])
                kb.dma(mixT[qt // 8][256:512, (qt % 8) * 128:(qt % 8 + 1) * 128].rearrange("(a p) t -> p a t", p=128), haT[ob][:], reads=[t_haT[ob]], writes=[t_mixT[qt // 8]])
                if qt % 8 == 7:
                    gather_chunk(qt // 8)
            kb.barrier()
    if dbg:
        d_mixT = nc.dram_tensor("d_mixT", [512, S], BF16, kind="ExternalOutput").ap()
        for i in range(8):
            kb.dma(d_mixT[:, i * 1024:(i + 1) * 1024], mixT[i], reads=[t_mixT[i]])


    if phases >= 4:
        selm_in = din("selm", [128, 4])
        x_own = din("x_own", [2048, D])
        w_out_in = din("w_out_p", [D, D])
        g2_row = din("g2_row", [1, D])
        wq_in = din("wq", [D, D])
        skT_in = din("skT", [128, 2, 128])
        uT_in = din("uT", [D, 16384])
        v_in = din("v_tab", [16384, D])
        y_out = nc.dram_tensor("y", [2048, D], F32, kind="ExternalOutput").ap()

        s_x1 = dscr("s_x1", [2048, D]); t_sx1 = Trk()
        s_h2T = dscr("s_h2T", [D, 2048], BF16); t_sh2T = Trk()
        with ExitStack() as p45:
            def sb45(name, shape, dt=F32):
                return p45.enter_context(nc.sbuf_tensor(name, list(shape), dt))
            g2tb = sb45("g2tb", [128, D]); t_g2tb = Trk()
            identb4 = sb45("identb4", [128, 128], BF16); t_identb4 = Trk()
            kb.dma(identb4[:], din("ident_b2", [128, 128], BF16), writes=[t_identb4])
            with ExitStack() as p4:
                def sb4(name, shape, dt=F32):
                    return p4.enter_context(nc.sbuf_tensor(name, list(shape), dt))
                rowst = sb4("rowst", [1, D]); t_rowst = Trk()
                g1b = sb4("g1b", [128, D]); t_g1b = Trk()
                sh2b = sb4("sh2b", [128, D]); t_sh2b = Trk()
                gs2b = sb4("gs2b", [128, D]); t_gs2b = Trk()
                selm = sb4("selm_sb", [128, 4]); t_selm = Trk()
                kb.dma(selm[:], selm_in, writes=[t_selm])

                def bcast_row(src_ap, dst, t_dst, src_reads=()):
                    kb.dma(rowst[:], src_ap, reads=list(src_reads), writes=[t_rowst])
                    for n in range(4):
                        kb.op("pe", lambda e, n=n: e.matmul(ps[n][:, :], lhsT=ones_f[0:1, :], rhs=rowst[0:1, n * 512:(n + 1) * 512], start=True, stop=True),
                              reads=[t_ones_f, t_rowst], writes=[pst[n]])
                        kb.op("dve", lambda e, n=n: e.tensor_copy(out=dst[:, n * 512:(n + 1) * 512], in_=ps[n][:, :]), reads=[pst[n]], writes=[t_dst])
                bcast_row(s_mod[0:1, 2 * D:3 * D], g1b, t_g1b, [t_smod])
                bcast_row(s_mod[0:1, 3 * D:4 * D], sh2b, t_sh2b, [t_smod])
                bcast_row(s_mod[0:1, 5 * D:6 * D], g2tb, t_g2tb, [t_smod])
                bcast_row(s_mod[0:1, 4 * D:5 * D], gs2b, t_gs2b, [t_smod])
                kb.dma(rowst[:], g2_row, writes=[t_rowst])
                for n in range(4):
                    kb.op("pe", lambda e, n=n: e.matmul(ps[n][:, :], lhsT=ones_f[0:1, :], rhs=rowst[0:1, n * 512:(n + 1) * 512], start=True, stop=True),
                          reads=[t_ones_f, t_rowst], writes=[pst[n]])
                    kb.op("dve", lambda e, n=n: e.scalar_tensor_tensor(out=gs2b[:, n * 512:(n + 1) * 512], in0=gs2b[:, n * 512:(n + 1) * 512], scalar=1.0, in1=ps[n][:, :],
                                                                       op0=ALU.add, op1=ALU.mult), reads=[pst[n], t_gs2b], writes=[t_gs2b])
                wo = sb4("wo", [128, 16, D], BF16); t_wo = Trk()
                for fc in range(16):
                    kb.dma(wo[:, fc, :], w_out_in[fc * 128:(fc + 1) * 128, :], writes=[t_wo], q="pool")
                sl = [sb4("sl%d" % i, [128, 16, 128], BF16) for i in range(4)]; t_sl = [Trk() for _ in range(4)]
                mixo = sb4("mixo", [128, 16, 128], BF16); t_mixo = Trk()
                xin = [sb4("xin%d" % i, [128, D]) for i in range(2)]; t_xin = [Trk(), Trk()]
                tmpy = sb4("tmpy", [128, D]); t_tmpy = Trk()
                h2b = sb4("h2b", [128, D], BF16); t_h2b = Trk()
                h2Tt = [sb4("h2Tt%d" % i, [128, 16, 128], BF16) for i in range(2)]; t_h2Tt = [Trk(), Trk()]
                nsc = sb4("nsc", [128, 4]); t_nsc = Trk()
                mgv = [mg.rearrange("(f p) t -> p f t", p=128) for mg in mixG]
                for tt in range(16):
                    bi = tt % 2
                    kb.dma(xin[bi][:], x_own[tt * 128:(tt + 1) * 128, :], writes=[t_xin[bi]])
                    for s_ in range(4):
                        kb.dma(sl[s_][:], mgv[2 * s_ + tt // 8][:, :, (tt % 8) * 128:(tt % 8 + 1) * 128], reads=[t_mixG[2 * s_ + tt // 8]], writes=[t_sl[s_]])
                    kb.op("dve", lambda e: e.tensor_scalar(out=mixo[:], in0=sl[0][:], scalar1=selm[:, 0:1], scalar2=None, op0=ALU.mult), reads=[t_sl[0], t_selm], writes=[t_mixo])
                    for s_ in range(1, 4):
                        kb.op("dve", lambda e, s_=s_: e.scalar_tensor_tensor(out=mixo[:], in0=sl[s_][:], scalar=selm[:, s_:s_ + 1], in1=mixo[:], op0=ALU.mult, op1=ALU.add),
                              reads=[t_sl[s_], t_selm, t_mixo], writes=[t_mixo])
                    for n in range(4):
                        for fc in range(16):
                            kb.op("pe", lambda e, n=n, fc=fc: e.matmul(ps[n][:, :], lhsT=mixo[:, fc, :], rhs=wo[:, fc, n * 512:(n + 1) * 512], start=(fc == 0), stop=(fc == 15)),
                                  reads=[t_mixo, t_wo], writes=[pst[n]], sig=(fc == 15))
                        kb.op("dve", lambda e, n=n: e.tensor_tensor(out=tmpy[:, n * 512:(n + 1) * 512], in0=ps[n][:, :], in1=g1b[:, n * 512:(n + 1) * 512], op=ALU.mult),
                              reads=[pst[n], t_g1b], writes=[t_tmpy])
                        kb.op("dve", lambda e, n=n, bi=bi: e.tensor_tensor(out=xin[bi][:, n * 512:(n + 1) * 512], in0=xin[bi][:, n * 512:(n + 1) * 512], in1=tmpy[:, n * 512:(n + 1) * 512], op=ALU.add),
                              reads=[t_xin[bi], t_tmpy], writes=[t_xin[bi]])
                    kb.dma(s_x1[tt * 128:(tt + 1) * 128, :], xin[bi][:], reads=[t_xin[bi]], writes=[t_sx1])
                    kb.op("act", lambda e, bi=bi: e.activation(out=tmpy[:], in_=xin[bi][:], func=AF.Square, accum_out=nsc[:, 0:1]), reads=[t_xin[bi]], writes=[t_tmpy, t_nsc])
                    kb.op("act", lambda e: e.activation(out=nsc[:, 1:2], in_=nsc[:, 0:1], func=AF.Sqrt, scale=1.0 / D, bias=EPS), reads=[t_nsc], writes=[t_nsc])
                    kb.op("dve", lambda e: e.reciprocal(out=nsc[:, 2:3], in_=nsc[:, 1:2]), reads=[t_nsc], writes=[t_nsc])
                    kb.op("dve", lambda e, bi=bi: e.scalar_tensor_tensor(out=tmpy[:], in0=xin[bi][:], scalar=nsc[:, 2:3], in1=gs2b[:], op0=ALU.mult, op1=ALU.mult),
                          reads=[t_xin[bi], t_nsc, t_gs2b], writes=[t_tmpy])
                    kb.op("dve", lambda e: e.tensor_tensor(out=h2b[:], in0=tmpy[:], in1=sh2b[:], op=ALU.add), reads=[t_tmpy, t_sh2b], writes=[t_h2b])
                    for qd in range(4):
                        pb = 4 + qd
                        for k in range(4):
                            dc = qd * 4 + k
                            kb.op("pe", lambda e, dc=dc, k=k, pb=pb: e.matmul(ps[pb][:, k * 128:(k + 1) * 128], lhsT=h2b[:, dc * 128:(dc + 1) * 128], rhs=identb4[:, :], start=True, stop=True),
                                  reads=[t_h2b, t_identb4], writes=[pst[pb]], sig=(k == 3))
                        kb.op("act", lambda e, qd=qd, pb=pb, bi=bi: e.activation(out=h2Tt[bi][:, qd * 4:(qd + 1) * 4, :].rearrange("p a t -> p (a t)"), in_=ps[pb][:, :], func=AF.Copy),
                              reads=[pst[pb]], writes=[t_h2Tt[bi]])
                    kb.dma(s_h2T[:, tt * 128:(tt + 1) * 128].rearrange("(a p) t -> p a t", p=128), h2Tt[bi][:], reads=[t_h2Tt[bi]], writes=[t_sh2T])
                kb.barrier()

            if phases >= 5:
                with ExitStack() as p5:
                    def sb5(name, shape, dt=F32):
                        return p5.enter_context(nc.sbuf_tensor(name, list(shape), dt))
                    skT = sb5("skT_sb", [128, 2, 128], BF16); t_skT = Trk()
                    kb.dma(skT[:], skT_in, writes=[t_skT], q="pool")
                    h2g = sb5("h2g", [128, 16, 512], BF16); t_h2g = Trk()
                    s1m = sb5("s1m", [128, 4, 8, 128]); t_s1m = Trk()
                    s2t = sb5("s2t", [128, 4, 8, 128]); t_s2t = Trk()
                    tau = sb5("tau", [128, 4, 8]); t_tau = Trk()
                    Ysb = sb5("Ysb", [128, 4, D]); t_Ysb = [Trk() for _ in range(4)]
                    wqs = [sb5("wqs%d" % i, [128, 16, 128], BF16) for i in range(2)]; t_wqs = [Trk(), Trk()]
                    sct = sb5("sct", [128, 16, 128]); t_sct = Trk()
                    wk = sb5("wk", [128, 256]); t_wk = Trk()
                    tv = sb5("tv", [128, 16, 16]); t_tv = Trk()
                    cand = sb5("cand", [128, 8, 256]); t_cand = Trk()
                    cw2 = sb5("cw2", [128, 256]); t_cw2 = Trk()
                    m24 = sb5("m24", [128, 8, 24]); t_m24 = Trk()
                    hs = sb5("hs", [128, 8, 8]); t_hs = Trk()
                    ex = sb5("ex", [128, 8, 256]); t_ex = Trk()
                    Ub = [sb5("Ub%d" % i, [128, 16, 256], BF16) for i in range(2)]; t_Ub = [Trk(), Trk()]
                    Vb = [sb5("Vb%d" % i, [128, 2, D], BF16) for i in range(2)]; t_Vb = [Trk(), Trk()]
                    Lt = [sb5("Lt0", [128, 8, 256]), cand]; t_Lt = [Trk(), t_cand]
                    Dg = sb5("Dg", [128, 4, 8, 128], BF16); t_Dg = Trk()
                    t_Ex = [[Trk() for _ in range(16)] for _ in range(2)]
                    Wt = [sb5("Wt%d" % i, [128, 8, 256], BF16) for i in range(2)]; t_Wt = [[Trk() for _ in range(8)] for _ in range(2)]
                    glT = [sb5("glT%d" % i, [128, 2, 512], BF16) for i in range(2)]; t_glT = [Trk(), Trk()]
                    GT = [sb5("GT%d" % i, [128, 2, 128], BF16) for i in range(2)]; t_GT = [Trk(), Trk()]
                    x1t = Lt[0][:].rearrange("p h e -> p (h e)"); t_x1t = t_Lt[0]
                    h2v = s_h2T.rearrange("(a p) t -> p a t", p=128)
                    wqv = wq_in.rearrange("(a p) n -> p a n", p=128)
                    uTv = uT_in.rearrange("(a p) e -> p a e", p=128)
                    vv_ = v_in.rearrange("(c p) d -> p c d", p=128)
                    ngrp = int(os.environ.get("NGRP", "4"))
                    net = int(os.environ.get("NET", "64"))
                    for g in range(ngrp):
                        kb.dma(h2g[:, 0:8, :], h2v[:, 0:8, g * 512:(g + 1) * 512], reads=[t_sh2T], writes=[t_h2g])
                        kb.dma(h2g[:, 8:16, :], h2v[:, 8:16, g * 512:(g + 1) * 512], reads=[t_sh2T], writes=[t_h2g])
                        for tl in range(4):
                            pass
                        qall = p5.enter_context(nc.sbuf_tensor("qall%d" % g, [128, 16, 512], BF16)) if g == 0 else qall
                        t_qall = Trk() if g == 0 else t_qall
                        for ch in range(16):
                            wi = ch % 2
                            kb.dma(wqs[wi][:], wqv[:, :, ch * 128:(ch + 1) * 128], writes=[t_wqs[wi]], q="pool")
                            pb = ch % 2
                            for dc in range(16):
                                kb.op("pe", lambda e, dc=dc, wi=wi, pb=pb: e.matmul(ps[pb][:, :], lhsT=wqs[wi][:, dc, :], rhs=h2g[:, dc, :], start=(dc == 0), stop=(dc == 15)),
                                      reads=[t_wqs[wi], t_h2g], writes=[pst[pb]], sig=(dc == 15))
                            kb.op("act", lambda e, ch=ch, pb=pb: e.activation(out=qall[:, ch, :], in_=ps[pb][:, :], func=AF.Copy), reads=[pst[pb]], writes=[t_qall])
                        for tl in range(4):
                            for qd in range(4):
                                pb = 2 + (qd % 2)
                                for k in range(4):
                                    ch = qd * 4 + k
                                    kb.op("pe", lambda e, ch=ch, k=k, pb=pb, tl=tl: e.matmul(ps[pb][:, k * 128:(k + 1) * 128], lhsT=qall[:, ch, tl * 128:(tl + 1) * 128], rhs=skT[:, ch % 2, :],
                                                                                             start=True, stop=True),
                                          reads=[t_qall, t_skT], writes=[pst[pb]], sig=(k == 3))
                                kb.op("dve", lambda e, qd=qd, pb=pb: e.tensor_copy(out=sct[:, qd * 4:(qd + 1) * 4, :].rearrange("p a k -> p (a k)"), in_=ps[pb][:, :]),
                                      reads=[pst[pb]], writes=[t_sct])
                            for ch in range(16):
                                kb.op("dve", lambda e, ch=ch: e.max(out=tv[:, ch, 0:8], in_=sct[:, ch, :]), reads=[t_sct], writes=[t_tv])
                                kb.op("dve", lambda e, ch=ch: e.match_replace(out=wk[:, 0:128], in_to_replace=tv[:, ch, 0:8], in_values=sct[:, ch, :], imm_value=-3e38),
                                      reads=[t_sct, t_tv], writes=[t_wk])
                                kb.op("dve", lambda e, ch=ch: e.max(out=tv[:, ch, 8:16], in_=wk[:, 0:128]), reads=[t_wk], writes=[t_tv])
                            tvv = tv[:].rearrange("p (h two) a -> p h two a", two=2)
                            kb.op("dve", lambda e: e.tensor_tensor(out=cand[:].rearrange("p h (a b) -> p h a b", b=16),
                                                                   in0=tvv[:, :, 0, :].unsqueeze(3).broadcast_to([128, 8, 16, 16]),
                                                                   in1=tvv[:, :, 1, :].unsqueeze(2).broadcast_to([128, 8, 16, 16]), op=ALU.add),
                                  reads=[t_tv], writes=[t_cand])
                            for h in range(8):
                                kb.op("dve", lambda e, h=h: e.max(out=m24[:, h, 0:8], in_=cand[:, h, :]), reads=[t_cand], writes=[t_m24])
                                kb.op("dve", lambda e, h=h: e.match_replace(out=cw2[:], in_to_replace=m24[:, h, 0:8], in_values=cand[:, h, :], imm_value=-3e38),
                                      reads=[t_cand, t_m24], writes=[t_cw2])
                                kb.op("dve", lambda e, h=h: e.max(out=m24[:, h, 8:16], in_=cw2[:]), reads=[t_cw2], writes=[t_m24])
                                kb.op("dve", lambda e, h=h: e.match_replace(out=cw2[:], in_to_replace=m24[:, h, 8:16], in_values=cw2[:], imm_value=-3e38),
                                      reads=[t_cw2, t_m24], writes=[t_cw2])
                                kb.op("dve", lambda e, h=h: e.max(out=m24[:, h, 16:24], in_=cw2[:]), reads=[t_cw2], writes=[t_m24])
                            kb.op("dve", lambda e: e.tensor_copy(out=hs[:, :, 0], in_=m24[:, :, 0]), reads=[t_m24], writes=[t_hs])
                            kb.op("dve", lambda e: e.tensor_tensor(out=hs[:, :, 1], in0=m24[:, :, 15], in1=m24[:, :, 16], op=ALU.add), reads=[t_m24], writes=[t_hs])
                            kb.op("dve", lambda e: e.tensor_scalar(out=hs[:, :, 1], in0=hs[:, :, 1], scalar1=0.5, scalar2=None, op0=ALU.mult), reads=[t_hs], writes=[t_hs])
                            kb.op("dve", lambda e: e.tensor_tensor(out=ex[:], in0=cand[:], in1=hs[:, :, 0:1].broadcast_to([128, 8, 256]), op=ALU.subtract), reads=[t_cand, t_hs], writes=[t_ex])
                            kb.op("act", lambda e: e.activation(out=ex[:], in_=ex[:], func=AF.Exp), reads=[t_ex], writes=[t_ex])
                            kb.op("dve", lambda e: e.tensor_tensor(out=cand[:], in0=cand[:], in1=hs[:, :, 1:2].broadcast_to([128, 8, 256]), op=ALU.is_ge), reads=[t_cand, t_hs], writes=[t_cand])
                            kb.op("dve", lambda e: e.tensor_tensor(out=ex[:], in0=ex[:], in1=cand[:], op=ALU.mult), reads=[t_cand, t_ex], writes=[t_ex])
                            kb.op("dve", lambda e: e.tensor_reduce(out=hs[:, :, 2], in_=ex[:], axis=AX.X, op=ALU.add), reads=[t_ex], writes=[t_hs])
                            kb.op("act", lambda e: e.activation(out=hs[:, :, 3], in_=hs[:, :, 2], func=AF.Ln), reads=[t_hs], writes=[t_hs])
                            kb.op("dve", lambda e: e.tensor_tensor(out=hs[:, :, 4], in0=hs[:, :, 0], in1=hs[:, :, 3], op=ALU.add), reads=[t_hs], writes=[t_hs])
                            sv = sct[:].rearrange("p (h two) k -> p h two k", two=2)
                            kb.op("dve", lambda e, tl=tl: e.tensor_tensor(out=s1m[:, tl, :, :], in0=sv[:, :, 0, :], in1=hs[:, :, 4:5].broadcast_to([128, 8, 128]), op=ALU.subtract),
                                  reads=[t_sct, t_hs], writes=[t_s1m])
                            kb.op("dve", lambda e, tl=tl: e.tensor_copy(out=s2t[:, tl, :, :], in_=sv[:, :, 1, :]), reads=[t_sct], writes=[t_s2t])
                            kb.op("dve", lambda e, tl=tl: e.tensor_tensor(out=tau[:, tl, :], in0=hs[:, :, 1], in1=hs[:, :, 4], op=ALU.subtract), reads=[t_hs], writes=[t_tau])
                            kb.op("dve", lambda e, tl=tl: e.tensor_tensor(out=s1m[:, tl, :, :], in0=s1m[:, tl, :, :], in1=tau[:, tl, :].unsqueeze(2).broadcast_to([128, 8, 128]), op=ALU.subtract),
                                  reads=[t_s1m, t_tau], writes=[t_s1m])
                            kb.op("act", lambda e, tl=tl: e.activation(out=hs[:, :, 5], in_=tau[:, tl, :], func=AF.Exp), reads=[t_tau], writes=[t_hs])
                            for h in range(8):
                                kb.op("dve", lambda e, tl=tl, h=h: e.tensor_scalar(out=Dg[:, tl, h, :], in0=identb4[:, :], scalar1=hs[:, h, 5:6], scalar2=None, op0=ALU.mult),
                                      reads=[t_identb4, t_hs], writes=[t_Dg])
                        kb.op("dve", lambda e: e.memset(Ysb[:], 0.0), writes=t_Ysb)

                        def load_u(et):
                            bi = et % 2
                            kb.dma(Ub[bi][:, 0:8, :], uTv[:, 0:8, et * 256:(et + 1) * 256], writes=[t_Ub[bi]], q="pool")
                            kb.dma(Ub[bi][:, 8:16, :], uTv[:, 8:16, et * 256:(et + 1) * 256], writes=[t_Ub[bi]], q="pool")

                        def load_v(et):
                            bi = et % 2
                            kb.dma(Vb[bi][:], vv_[:, et * 2:et * 2 + 2, :], writes=[t_Vb[bi]], q="pool")

                        def emit_a(et):
                            bi = et % 2
                            for ec in range(2):
                                for dc in range(16):
                                    kb.op("pe", lambda e, dc=dc, ec=ec, bi=bi: e.matmul(ps[ec][:, :], lhsT=Ub[bi][:, dc, ec * 128:(ec + 1) * 128], rhs=h2g[:, dc, :],
                                                                                        start=(dc == 0), stop=(dc == 15)),
                                          reads=[t_Ub[bi], t_h2g], writes=[pst[ec]], sig=(dc == 15))
                                kb.op("act", lambda e, ec=ec, bi=bi: e.activation(out=glT[bi][:, ec, :], in_=ps[ec][:, :], func=AF.Gelu_apprx_tanh),
                                      reads=[pst[ec]], writes=[t_glT[bi]])
                        load_u(0)
                        if net > 1:
                            load_u(1)
                        load_v(0)
                        emit_a(0)
                        pairs = [(et, tl) for et in range(net) for tl in range(4)]
                        NP = len(pairs)

                        def st_L(n):
                            et, tl = pairs[n]; k = n % 2
                            for h in range(8):
                                for a2 in range(2):
                                    kb.op("act", lambda e, h=h, a2=a2: e.activation(out=Lt[k][:, h, a2 * 128:(a2 + 1) * 128], in_=s2t[:, tl, h, :], func=AF.Exp,
                                                                                    bias=s1m[:, tl, h, 2 * et + a2:2 * et + a2 + 1]),
                                          reads=[t_s2t, t_s1m], writes=[t_Ex[k][h * 2 + a2], t_Lt[k]] if (h == 0 and a2 == 0) else [t_Ex[k][h * 2 + a2]])

                        def st_C(n):
                            et, tl = pairs[n]; k = n % 2
                            kb.op("dve", lambda e: e.scalar_tensor_tensor(out=Wt[k][:], in0=Lt[k][:], scalar=1.0, in1=Lt[k][:], op0=ALU.is_ge, op1=ALU.mult),
                                  reads=t_Ex[k] + [t_Lt[k]], writes=t_Wt[k])
                            pw = 2 + k
                            for ec in range(2):
                                for h in range(8):
                                    kb.op("pe", lambda e, ec=ec, h=h: e.matmul(ps[pw][:, ec * 128:(ec + 1) * 128], lhsT=Wt[k][:, h, ec * 128:(ec + 1) * 128], rhs=Dg[:, tl, h, :],
                                                                               start=(h == 0), stop=(h == 7)),
                                          reads=[t_Wt[k][h], t_Dg], writes=[pst[pw]], sig=(ec == 1 and h == 7))

                        def st_G(n):
                            et, tl = pairs[n]; k = n % 2; bi = et % 2; pw = 2 + k
                            if tl == 0:
                                if et + 1 < net:
                                    load_v(et + 1)
                                    emit_a(et + 1)
                                if et + 2 < net:
                                    load_u(et + 2)
                            kb.op("dve", lambda e: e.tensor_tensor(out=GT[k][:], in0=glT[bi][:, :, tl * 128:(tl + 1) * 128],
                                                                   in1=ps[pw][:, 0:256].rearrange("p (a t) -> p a t", t=128), op=ALU.mult),
                                  reads=[t_glT[bi], pst[pw]], writes=[t_GT[k]])
                            for nn in range(4):
                                pb = 4 + nn
                                for ec in range(2):
                                    kb.op("pe", lambda e, ec=ec, nn=nn, pb=pb: e.matmul(ps[pb][:, :], lhsT=GT[k][:, ec, :], rhs=Vb[bi][:, ec, nn * 512:(nn + 1) * 512],
                                                                                        start=(ec == 0), stop=(ec == 1)),
                                          reads=[t_GT[k], t_Vb[bi]], writes=[pst[pb]], sig=(ec == 1))

                        def st_Y(n):
                            et, tl = pairs[n]
                            kb.op("dve", lambda e: e.tensor_tensor(out=Ysb[:, tl, :], in0=Ysb[:, tl, :], in1=psY[:, :], op=ALU.add),
                                  reads=[t_Ysb[tl], pst[4], pst[5], pst[6], pst[7]], writes=[t_Ysb[tl]])
                        for n in range(-2, NP):
                            if 0 <= n:
                                st_G(n)
                            if 0 <= n + 2 < NP:
                                st_L(n + 2)
                            if 0 <= n + 1 < NP:
                                st_C(n + 1)
                            if 0 <= n:
                                st_Y(n)
                        for tl in range(4):
                            r0 = (g * 4 + tl) * 128
                            kb.dma(x1t, s_x1[r0:r0 + 128, :], reads=[t_sx1], writes=[t_x1t])
                            kb.op("dve", lambda e, tl=tl: e.tensor_tensor(out=Ysb[:, tl, :], in0=Ysb[:, tl, :], in1=g2tb[:], op=ALU.mult), reads=[t_Ysb[tl], t_g2tb], writes=[t_Ysb[tl]])
                            kb.op("dve", lambda e, tl=tl: e.tensor_tensor(out=x1t, in0=x1t, in1=Ysb[:, tl, :], op=ALU.add), reads=[t_Ysb[tl], t_x1t], writes=[t_x1t])
                            kb.dma(y_out[r0:r0 + 128, :], x1t, reads=[t_x1t])
                    kb.barrier()

    for _i in range(int(os.environ.get('EXTRA', '0'))):
        kb.dma(s_mod[0:1, 0:128], ident[0:1, :], reads=[t_ident])
    kb.barrier()
    ok, stuck, sems = kb.check()
    if not ok or os.environ.get('KBV'):
        print('SYNC CHECK ok=%s' % ok, stuck, {k: v for k, v in kb.cnt.items()})
    assert ok, 'sync deadlock'
    es.close()
    return nc


def _consts():
    ones = np.ones((128, 128), np.float32)
    blk = np.zeros((128, 128), np.float32)
    blk[:64, :64] = 1.0
    blk[64:, 64:] = 1.0
    ut = np.triu(np.ones((128, 128), np.float32))
    NEG = -30000.0
    il = np.arange(128)
    cmask = np.zeros((128, 17, 128), np.float32)
    for m in range(17):
        ok = (il[None, :] + 128 * m) >= (16 * il[:, None] + 31)
        cmask[:, m, :] = np.where(ok, 0.0, NEG)
    caus = np.where(il[:, None] <= il[None, :], 0.0, NEG).astype(np.float32)
    acaus = np.where(il[:, None] > il[None, :], 0.0, NEG).astype(np.float32)
    i_abs = (np.arange(4)[None, :, None] * 128 + il[:, None, None])
    blkk = np.arange(128)[None, None, :]
    ovl = ((16 * i_abs < 64 * blkk + 64) & (16 * i_abs + 32 > 64 * blkk)).astype(np.float32)
    expT = (np.arange(S)[None, :] // 64 == il[:, None]).astype(np.float32)
    o = np.arange(255)[None, :] - 127
    cur_rel = (il[:, None] >= 64).astype(np.int64)
    rel = o - cur_rel
    t_keep = (rel < -1).astype(np.float32)
    t_add = np.where(rel > 0, -1e4, np.where(rel >= -1, 1e4, 0.0)).astype(np.float32)
    return {"ones_bf": _bf(ones), "blk64_bf": _bf(blk), "ident_f": np.eye(128, dtype=np.float32),
            "ut_f": ut, "ident_b": _bf(np.eye(128, dtype=np.float32)),
            "cmask": _bf(cmask), "caus_add": _bf(caus), "acaus_add": _bf(acaus), "ovl": _bf(ovl), "expT": _bf(expT),
            "t_keep": np.ascontiguousarray(np.broadcast_to(t_keep, (128, 255))).astype(np.float32), "t_add": t_add}


def make_in_maps(inp):
    f = lambda a: np.ascontiguousarray(np.asarray(a, dtype=np.float32))
    x = f(inp["x"]); c = f(inp["c"])
    w_in = f(inp["w_in"])[0]
    conv = f(inp["conv_qk"])[0]
    qn_g = f(inp["qn_g"])[0]; kn_g = f(inp["kn_g"])[0]
    mng = f(inp["mlstm_norm_g"])[0]
    g1 = f(inp["norm1_g"])[0]
    cst = _consts()
    maps = []
    xTs = [np.ascontiguousarray(x[b].T) for b in range(2)]
    w_out = f(inp["w_out"])[0]
    rows = []
    for fc in range(16):
        r, part, half = fc // 4, (fc % 4) // 2, fc % 2
        base = part * 1024 + r * 256 + half * 128
        rows += list(range(base, base + 128))
    w_out_p = np.ascontiguousarray(w_out[rows])
    wq = f(inp["peer_wq"])[0]
    skT = np.ascontiguousarray(f(inp["peer_subkeys"])[0].transpose(2, 0, 1))
    uT = np.ascontiguousarray(f(inp["peer_u"])[0].T)
    vtab = f(inp["peer_v"])[0]
    for core in range(8):
        b, j = divmod(core, 4)
        cols = []
        cols += list(range(j * 256, j * 256 + 256))
        cols += list(range(1024 + j * 256, 1024 + j * 256 + 256))
        AQ = 4096 + 8
        cols += list(range(AQ + j * 256, AQ + j * 256 + 256))
        AKV = AQ + 1024
        for br in (0, 1, 2, 4):
            cols += list(range(AKV + br * 256 + j * 64, AKV + br * 256 + j * 64 + 64))
        cols += [4096 + j, 4096 + 4 + j]
        cols += list(range(2048 + j * 256, 2048 + j * 256 + 256))
        cols += list(range(3072 + j * 256, 3072 + j * 256 + 256))
        for br in (3, 5):
            cols += list(range(AKV + br * 256 + j * 64, AKV + br * 256 + j * 64 + 64))
        AG = AKV + 1536
        cols += list(range(AG + j * 12, AG + j * 12 + 12))
        assert len(cols) == 1678
        wj = np.zeros((D, 1680), np.float32)
        wj[:, :1678] = w_in[:, cols]
        cwt = np.zeros((128, 4, 4), np.float32)
        for t in range(4):
            base = (j * 256 + (t % 2) * 128) if t < 2 else (1024 + j * 256 + (t % 2) * 128)
            cwt[:, t, :] = conv[:, base:base + 128].T
        qkg = np.zeros((128, 4), np.float32)
        qkg[:, 0] = np.tile(qn_g, 2) * 0.125
        qkg[:, 3] = np.concatenate([kn_g[1], kn_g[2]])
        m = {
            "xT": xTs[b], "c_col": np.ascontiguousarray(c[b].reshape(16, 128).T),
            "w_mod": f(inp["w_mod"])[0], "b_mod": f(inp["b_mod"]),
            "g1_col": np.ascontiguousarray(g1.reshape(16, 128).T),
            "w_in": wj, "convw": cwt, "qk_g": qkg,
            "mng_row": np.ascontiguousarray(mng[j * 256:(j + 1) * 256][None, :]),
            "bif": np.ascontiguousarray(np.tile(np.array([[f(inp["b_igate"])[0, j], f(inp["b_fgate"])[0, j]]], np.float32), (128, 1))),
        }
        slopes = np.array([2.0 ** (-8.0 * (4 * j + hh + 1) / 16) for hh in range(4)], np.float64)
        kl = np.arange(128, dtype=np.float64)
        dl = np.arange(64, dtype=np.float64)
        m["tb_sw"] = (slopes[None, None, :] * (kl[:, None, None] - 64 - 128 * dl[None, :, None])).astype(np.float32)
        m["tb_c"] = (slopes[None, None, :] * (16 * kl[:, None, None] - 33 - 128 * dl[None, :, None])).astype(np.float32)
        m["w1k"] = f(inp["cmp_k_w1"])[0]; m["w1v"] = f(inp["cmp_v_w1"])[0]
        m["w2k"] = f(inp["cmp_k_w2"])[0]; m["w2v"] = f(inp["cmp_v_w2"])[0]
        for nm, key in (("posk", "cmp_pos_k"), ("posv", "cmp_pos_v")):
            pos = f(inp[key])[0]
            m[nm] = np.ascontiguousarray(pos.reshape(16, 2, 64).transpose(1, 2, 0).reshape(128, 16))
        m["kng0"] = np.ascontiguousarray(kn_g[0][:, None])
        sm = np.zeros((128, 4), np.float32); sm[:, j] = 1.0
        m["selm"] = sm
        m["x_own"] = np.ascontiguousarray(x[b, j * 2048:(j + 1) * 2048])
        m["w_out_p"] = w_out_p
        m["g2_row"] = f(inp["norm2_g"])
        m["wq"] = wq
        m["skT"] = skT
        m["uT"] = uT
        m["v_tab"] = vtab
        m["ident_b2"] = cst["ident_b"]
        m.update(cst)
        maps.append(m)
    return maps


def kernel(**inputs):
    nc = build()
    maps = make_in_maps(inputs)
    res = run_bass_kernel_spmd(nc, maps, core_ids=list(range(8)))
    out = np.zeros((2, S, D), np.float32)
    for core in range(8):
        b, j = divmod(core, 4)
        out[b, j * 2048:(j + 1) * 2048] = res.results[core]["y"]
    return out
```

```python
import numpy as np
from contextlib import ExitStack
import concourse.bass as bass
import concourse.mybir as mybir
from concourse.bass_utils import run_bass_kernel_spmd

F32 = mybir.dt.float32
BF16 = mybir.dt.bfloat16
AF = mybir.ActivationFunctionType
ALU = mybir.AluOpType
AX = mybir.AxisListType

D = 2048
S = 8192
NB = 16
EPS = 1e-6
import os
NDS = int(os.environ.get("NDS", "24"))


class Trk:
    __slots__ = ("w", "r")

    def __init__(self):
        self.w = None
        self.r = {}


class KB:
    def __init__(self, nc, es):
        self.nc = nc
        self.eng = {"pe": nc.tensor, "act": nc.scalar, "dve": nc.vector, "pool": nc.gpsimd, "sp": nc.sync}
        self.sem = {k: es.enter_context(nc.semaphore("s_" + k)) for k in self.eng}
        self.cnt = {k: 0 for k in self.eng}
        self.seen = {k: {} for k in self.eng}
        self.dsem = [es.enter_context(nc.semaphore("dq%d" % i)) for i in range(NDS)]
        self.duse = [0] * NDS
        self.dnext = 0
        self.ccsem = es.enter_context(nc.semaphore("ccs"))
        self.log = {k: [] for k in self.eng}

    def _wait(self, e, tok):
        if tok is None:
            return
        key, sem, val = tok
        if self.seen[e].get(key, 0) >= val:
            return
        self.eng[e].wait_ge(sem, val)
        self.log[e].append(("w", key, val))
        self.seen[e][key] = val

    def _deps(self, e, reads, writes):
        for b in reads:
            if b.w is not None and not (e == "pe" and b.w[0] == "pe"):
                self._wait(e, b.w)
        for b in writes:
            if b.w is not None and not (e == "pe" and b.w[0] == "pe"):
                self._wait(e, b.w)
            for t in b.r.values():
                if not (e == "pe" and t[0] == "pe"):
                    self._wait(e, t)

    def _mark(self, tok, reads, writes):
        for b in reads:
            b.r[tok[0]] = tok
        for b in writes:
            b.w = tok
            b.r = {}

    def op(self, e, fn, reads=(), writes=(), sig=True):
        self._deps(e, reads, writes)
        inst = fn(self.eng[e])
        if sig:
            self.cnt[e] += 1
            inst.then_inc(self.sem[e], 1)
            self.log[e].append(("i", e, 1))
            tok = (e, self.sem[e], self.cnt[e])
        else:
            tok = (e, self.sem[e], self.cnt[e] + 1)
        self._mark(tok, reads, writes)
        return tok

    def dma(self, out, in_, reads=(), writes=(), q="sp", **kw):
        i = self.dnext
        self.dnext = (i + 1) % NDS
        if self.duse[i] > 0:
            self._wait(q, (("d", i), self.dsem[i], 16 * self.duse[i]))
        self._deps(q, reads, writes)
        inst = self.eng[q].dma_start(out=out, in_=in_, **kw)
        self.duse[i] += 1
        inst.then_inc(self.dsem[i], 16)
        self.log[q].append(("i", ("d", i), 16))
        tok = (("d", i), self.dsem[i], 16 * self.duse[i])
        self._mark(tok, reads, writes)
        return tok

    def check(self):
        sems = {}
        pc = {k: 0 for k in self.log}
        prog = True
        while prog:
            prog = False
            for e, lg in self.log.items():
                while pc[e] < len(lg):
                    kind, key, val = lg[pc[e]]
                    if kind == "w":
                        if sems.get(key, 0) < val:
                            break
                    else:
                        sems[key] = sems.get(key, 0) + val
                    pc[e] += 1
                    prog = True
        stuck = {e: (pc[e], len(lg), lg[pc[e]] if pc[e] < len(lg) else None) for e, lg in self.log.items()}
        ok = all(pc[e] == len(lg) for e, lg in self.log.items())
        return ok, stuck, sems

    def barrier(self):
        for e in self.eng:
            for e2 in self.eng:
                if e2 != e and self.cnt[e2] > 0:
                    self._wait(e, (e2, self.sem[e2], self.cnt[e2]))
            for i in range(NDS):
                if self.duse[i] > 0:
                    self._wait(e, (("d", i), self.dsem[i], 16 * self.duse[i]))


def _bf(a):
    import ml_dtypes
    return np.asarray(a, dtype=np.float32).astype(ml_dtypes.bfloat16)


def build(dbg=False, phases=99, nb=NB, parts='abcde'):
    nc = bass.Bass("TRN2", target_bir_lowering=False)
    es = ExitStack()
    kb = KB(nc, es)

    def din(name, shape, dt=F32):
        return nc.dram_tensor(name, list(shape), dt, kind="ExternalInput").ap()

    def dscr(name, shape, dt=F32):
        return nc.dram_tensor(name, list(shape), dt, kind=("ExternalOutput" if dbg else "Internal")).ap()

    xT = din("xT", [D, S])
    c_col = din("c_col", [128, 16])
    w_mod = din("w_mod", [D, 6 * D])
    b_mod = din("b_mod", [1, 6 * D])
    g1_col = din("g1_col", [128, 16])
    w_in = din("w_in", [D, 1680])
    convw = din("convw", [128, 4, 4])
    qk_g = din("qk_g", [128, 4])
    mng_row = din("mng_row", [1, 256])
    ones_bf = din("ones_bf", [128, 128], BF16)
    blk64_bf = din("blk64_bf", [128, 128], BF16)
    ident_f = din("ident_f", [128, 128])

    s_q = dscr("s_q", [256, S])
    s_k = dscr("s_k", [256, S])
    s_aq = dscr("s_aq", [256, S], BF16)
    s_kv = dscr("s_kv", [128, S])
    s_kk = dscr("s_kk", [128, S], BF16)
    s_if = dscr("s_if", [2, S])
    s_v = dscr("s_v", [S, 256])
    s_go = dscr("s_go", [S, 256])
    s_vv = dscr("s_vv", [S, 128], BF16)
    s_gt = dscr("s_gt", [S, 12])

    ps = []
    pst = []
    for i in range(4):
        ps.append(es.enter_context(nc.psum_tensor("ps%d" % i, [128, 512], F32)))
        pst.append(Trk())
    psY = es.enter_context(nc.psum_tensor("psY", [128, 2048], F32))
    for i in range(4):
        ps.append(psY[:, i * 512:(i + 1) * 512])
        pst.append(Trk())

    def sb(name, shape, dt=F32):
        return es.enter_context(nc.sbuf_tensor(name, list(shape), dt))

    ones_b = sb("ones_b", [128, 128], BF16); t_ones_b = Trk()
    blk64 = sb("blk64", [128, 128], BF16); t_blk64 = Trk()
    ident = sb("ident", [128, 128]); t_ident = Trk()
    ones_f = sb("ones_f", [128, 128]); t_ones_f = Trk()
    kb.dma(ones_b[:], ones_bf, writes=[t_ones_b])
    kb.dma(blk64[:], blk64_bf, writes=[t_blk64])
    kb.dma(ident[:], ident_f, writes=[t_ident])
    kb.op("dve", lambda e: e.memset(ones_f[:], 1.0), writes=[t_ones_f])

    s_mod = dscr("s_mod", [1, 6 * D]); t_smod = Trk()
    csil = sb("csil", [128, 16]); t_csil = Trk()
    ccol = sb("ccol", [128, 16]); t_ccol = Trk()
    g1c = sb("g1c", [128, 16]); t_g1c = Trk()
    gs1 = sb("gs1", [128, 16]); t_gs1 = Trk()
    sh1 = sb("sh1", [128, 16]); t_sh1 = Trk()
    kb.dma(ccol[:], c_col, writes=[t_ccol])
    kb.dma(g1c[:], g1_col, writes=[t_g1c])
    kb.op("act", lambda e: e.activation(out=csil[:], in_=ccol[:], func=AF.Silu), reads=[t_ccol], writes=[t_csil])
    with ExitStack() as p0:
        wm = [p0.enter_context(nc.sbuf_tensor("wm%d" % i, [128, 16, 512], F32)) for i in range(2)]
        modrow = p0.enter_context(nc.sbuf_tensor("modrow", [1, 6 * D], F32)); t_modrow = Trk()
        bmod_sb = p0.enter_context(nc.sbuf_tensor("bmod_sb", [1, 6 * D], F32)); t_bmod = Trk()
        kb.dma(bmod_sb[:], b_mod, writes=[t_bmod])
        t_wm = [Trk(), Trk()]
        wmv = w_mod.rearrange("(k p) n -> p k n", p=128)
        for n in range(24):
            bi = n % 2
            kb.dma(wm[bi][:, 0:8, :], wmv[:, 0:8, n * 512:(n + 1) * 512], writes=[t_wm[bi]])
            kb.dma(wm[bi][:, 8:16, :], wmv[:, 8:16, n * 512:(n + 1) * 512], writes=[t_wm[bi]])
            pb = n % 2
            for k in range(16):
                kb.op("pe", lambda e, k=k, bi=bi, pb=pb: e.matmul(ps[pb][0:1, :], lhsT=csil[:, k:k + 1], rhs=wm[bi][:, k, :],
                                                                  start=(k == 0), stop=(k == 15)),
                      reads=[t_csil, t_wm[bi]], writes=[pst[pb]], sig=(k == 15))
            kb.op("dve", lambda e, n=n, pb=pb: e.tensor_tensor(out=modrow[0:1, n * 512:(n + 1) * 512], in0=ps[pb][0:1, :],
                                                               in1=bmod_sb[0:1, n * 512:(n + 1) * 512], op=ALU.add),
                  reads=[pst[pb], t_bmod], writes=[t_modrow])
        for which, dst, t_dst in ((0, sh1, t_sh1), (1, gs1, t_gs1)):
            for k in range(16):
                off = which * D + k * 128
                kb.op("pe", lambda e, off=off, k=k: e.matmul(ps[2][:, k:k + 1], lhsT=modrow[0:1, off:off + 128], rhs=ones_f[0:1, 0:1],
                                                             start=True, stop=True),
                      reads=[t_modrow, t_ones_f], writes=[pst[2]], sig=(k == 15))
            if which == 0:
                kb.op("dve", lambda e: e.tensor_copy(out=sh1[:], in_=ps[2][:, 0:16]), reads=[pst[2]], writes=[t_sh1])
            else:
                kb.op("dve", lambda e: e.scalar_tensor_tensor(out=gs1[:], in0=ps[2][:, 0:16], scalar=1.0, in1=g1c[:],
                                                              op0=ALU.add, op1=ALU.mult),
                      reads=[pst[2], t_g1c], writes=[t_gs1])
        kb.dma(s_mod, modrow[:], reads=[t_modrow], writes=[t_smod])
        kb.barrier()

    t_scr = {n: Trk() for n in ("q", "k", "aq", "kv", "kk", "if", "v", "go", "vv", "gt")}
    if phases >= 1:
        with ExitStack() as p1:
            def sb1(name, shape, dt=F32):
                return p1.enter_context(nc.sbuf_tensor(name, list(shape), dt))
            wb = sb1("wb", [128, 16, 1680], BF16); t_wb = Trk()
            cw = sb1("cw", [128, 4, 4]); t_cw = Trk()
            qkg = sb1("qkg", [128, 4]); t_qkg = Trk()
            mng = sb1("mng", [128, 256]); t_mng = Trk()
            mngr = sb1("mngr", [1, 256]); t_mngr = Trk()
            kb.dma(cw[:], convw, writes=[t_cw])
            kb.dma(qkg[:], qk_g, writes=[t_qkg])
            kb.dma(mngr[:], mng_row, writes=[t_mngr])
            kb.op("pe", lambda e: e.matmul(ps[3][:, 0:256], lhsT=ones_f[0:1, :], rhs=mngr[0:1, :], start=True, stop=True),
                  reads=[t_ones_f, t_mngr], writes=[pst[3]])
            kb.op("dve", lambda e: e.tensor_copy(out=mng[:], in_=ps[3][:, 0:256]), reads=[pst[3]], writes=[t_mng])
            wst = [sb1("wst%d" % i, [128, 1680]) for i in range(2)]; t_wst = [Trk(), Trk()]
            wiv = w_in.rearrange("(k p) n -> p k n", p=128)
            for k in range(16):
                bi = k % 2
                kb.dma(wst[bi][:], wiv[:, k, :], writes=[t_wst[bi]])
                kb.op("dve", lambda e, k=k, bi=bi: e.tensor_copy(out=wb[:, k, :], in_=wst[bi][:]),
                      reads=[t_wst[bi]], writes=[t_wb])
            xt = [sb1("xt%d" % i, [128, 16, 512]) for i in range(2)]; t_xt = [Trk(), Trk()]
            xsq = sb1("xsq", [128, 16, 512], BF16); t_xsq = [Trk() for _ in range(16)]
            hT = sb1("hT", [128, 16, 512], BF16); t_hT = [Trk() for _ in range(16)]
            rstd = sb1("rstd", [128, 512]); t_rstd = Trk()
            tmp = [sb1("tmp%d" % i, [128, 512]) for i in range(2)]; t_tmp = [Trk(), Trk()]
            zr = [sb1("zr%d" % i, [128, 3 + 512]) for i in range(4)]; t_zr = [Trk() for _ in range(4)]
            acc = [sb1("acc%d" % i, [128, 512]) for i in range(2)]; t_acc = [Trk(), Trk()]
            ofm = [sb1("ofm%d" % i, [128, 512]) for i in range(2)]; t_ofm = [Trk(), Trk()]
            obf = [sb1("obf%d" % i, [128, 512], BF16) for i in range(2)]; t_obf = [Trk(), Trk()]
            sqb = sb1("sqb", [128, 512], BF16); t_sqb = Trk()
            rs2 = sb1("rs2", [128, 512]); t_rs2 = Trk()
            otm = [sb1("otm%d" % i, [128, 512]) for i in range(2)]; t_otm = [Trk(), Trk()]
            otb = [sb1("otb%d" % i, [128, 128], BF16) for i in range(2)]; t_otb = [Trk(), Trk()]
            otg = [sb1("otg%d" % i, [128, 12]) for i in range(2)]; t_otg = [Trk(), Trk()]
            for i in range(4):
                kb.op("dve", lambda e, i=i: e.memset(zr[i][:, 0:3], 0.0), writes=[t_zr[i]])
            xv = xT.rearrange("(k p) t -> p k t", p=128)
            cnt2 = [0]

            def rot2():
                cnt2[0] += 1
                return cnt2[0] % 2

            def load_x(n):
                bi = n % 2
                for h in range(4):
                    kb.dma(xt[bi][:, 4 * h:4 * h + 4, :], xv[:, 4 * h:4 * h + 4, n * 512:(n + 1) * 512], writes=[t_xt[bi]])

            load_x(0)
            for n in range(nb):
                bi = n % 2
                t0 = n * 512
                if n + 1 < nb:
                    load_x(n + 1)
                for k in range(16):
                    kb.op("act" if k % 2 else "dve",
                          (lambda e, k=k, bi=bi: e.activation(out=xsq[:, k, :], in_=xt[bi][:, k, :], func=AF.Square)) if k % 2 else
                          (lambda e, k=k, bi=bi: e.tensor_tensor(out=xsq[:, k, :], in0=xt[bi][:, k, :], in1=xt[bi][:, k, :], op=ALU.mult)),
                          reads=[t_xt[bi]], writes=[t_xsq[k]])
                for k in range(16):
                    kb.op("pe", lambda e, k=k: e.matmul(ps[0][:, :], lhsT=ones_b[:], rhs=xsq[:, k, :], start=(k == 0), stop=(k == 15)),
                          reads=[t_ones_b, t_xsq[k]], writes=[pst[0]], sig=(k == 15))
                kb.op("act", lambda e: e.activation(out=rstd[:], in_=ps[0][:, :], func=AF.Sqrt, scale=1.0 / D, bias=EPS),
                      reads=[pst[0]], writes=[t_rstd])
                kb.op("dve", lambda e: e.reciprocal(out=rstd[:], in_=rstd[:]), reads=[t_rstd], writes=[t_rstd])
                for k in range(16):
                    tb = k % 2
                    kb.op("dve", lambda e, k=k, tb=tb, bi=bi: e.tensor_tensor(out=tmp[tb][:], in0=xt[bi][:, k, :], in1=rstd[:], op=ALU.mult),
                          reads=[t_xt[bi], t_rstd], writes=[t_tmp[tb]])
                    kb.op("act", lambda e, k=k, tb=tb: e.activation(out=hT[:, k, :], in_=tmp[tb][:], func=AF.Identity,
                                                                    scale=gs1[:, k:k + 1], bias=sh1[:, k:k + 1]),
                          reads=[t_tmp[tb], t_gs1, t_sh1], writes=[t_hT[k]])
                for ct in range(9):
                    if not ({0: 'a', 1: 'a', 2: 'a', 3: 'a', 4: 'b', 5: 'b', 7: 'b', 6: 'c', 8: 'd'}[ct] in parts):
                        continue
                    c0 = ct * 128
                    cn = 128 if ct < 8 else 2
                    pb = 1 + (ct % 2)
                    for k in range(16):
                        kb.op("pe", lambda e, k=k, c0=c0, cn=cn, pb=pb: e.matmul(ps[pb][0:cn, :], lhsT=wb[:, k, c0:c0 + cn], rhs=hT[:, k, :],
                                                                                 start=(k == 0), stop=(k == 15)),
                              reads=[t_wb, t_hT[k]], writes=[pst[pb]], sig=(k == 15))
                    if ct < 4:
                        z = zr[ct]; tz = t_zr[ct]
                        kb.op("act", lambda e, z=z, pb=pb: e.activation(out=z[:, 3:515], in_=ps[pb][:, :], func=AF.Copy),
                              reads=[pst[pb]], writes=[tz])
                        ab = rot2()
                        a = acc[ab]; ta = t_acc[ab]
                        kb.op("dve", lambda e, z=z, a=a, ct=ct: e.tensor_scalar(out=a[:], in0=z[:, 3:515], scalar1=cw[:, ct, 3:4], scalar2=None,
                                                                                 op0=ALU.mult), reads=[tz, t_cw], writes=[ta])
                        for j in range(3):
                            kb.op("dve", lambda e, z=z, a=a, ct=ct, j=j: e.scalar_tensor_tensor(out=a[:], in0=z[:, j:j + 512], scalar=cw[:, ct, j:j + 1],
                                                                                               in1=a[:], op0=ALU.mult, op1=ALU.add),
                                  reads=[tz, t_cw, ta], writes=[ta])
                        ob = rot2()
                        kb.op("act", lambda e, a=a, ob=ob: e.activation(out=ofm[ob][:], in_=a[:], func=AF.Silu), reads=[ta], writes=[t_ofm[ob]])
                        dst = s_q if ct < 2 else s_k
                        r0 = (ct % 2) * 128
                        kb.dma(dst[r0:r0 + 128, t0:t0 + 512], ofm[ob][:], reads=[t_ofm[ob]], writes=[t_scr["q" if ct < 2 else "k"]])
                        kb.op("dve", lambda e, z=z: e.tensor_copy(out=z[:, 0:3], in_=z[:, 512:515]), reads=[tz], writes=[tz])
                    elif ct in (4, 5, 7):
                        kb.op("act", lambda e, pb=pb: e.activation(out=sqb[:], in_=ps[pb][:, :], func=AF.Square), reads=[pst[pb]], writes=[t_sqb])
                        kb.op("pe", lambda e: e.matmul(ps[3][:, :], lhsT=blk64[:], rhs=sqb[:], start=True, stop=True),
                              reads=[t_blk64, t_sqb], writes=[pst[3]])
                        kb.op("act", lambda e: e.activation(out=rs2[:], in_=ps[3][:, :], func=AF.Sqrt, scale=1.0 / 64, bias=EPS),
                              reads=[pst[3]], writes=[t_rs2])
                        kb.op("dve", lambda e: e.reciprocal(out=rs2[:], in_=rs2[:]), reads=[t_rs2], writes=[t_rs2])
                        ob = rot2()
                        gcol = 0 if ct in (4, 5) else 3
                        kb.op("dve", lambda e, pb=pb, ob=ob, gcol=gcol: e.scalar_tensor_tensor(out=obf[ob][:], in0=ps[pb][:, :], scalar=qkg[:, gcol:gcol + 1],
                                                                                              in1=rs2[:], op0=ALU.mult, op1=ALU.mult),
                              reads=[pst[pb], t_rs2, t_qkg], writes=[t_obf[ob]])
                        if ct == 7:
                            kb.dma(s_kk[:, t0:t0 + 512], obf[ob][:], reads=[t_obf[ob]], writes=[t_scr["kk"]])
                        else:
                            r0 = (ct - 4) * 128
                            kb.dma(s_aq[r0:r0 + 128, t0:t0 + 512], obf[ob][:], reads=[t_obf[ob]], writes=[t_scr["aq"]])
                    elif ct == 6:
                        ob = rot2()
                        kb.op("act", lambda e, pb=pb, ob=ob: e.activation(out=ofm[ob][:], in_=ps[pb][:, :], func=AF.Copy), reads=[pst[pb]], writes=[t_ofm[ob]])
                        kb.dma(s_kv[:, t0:t0 + 512], ofm[ob][:], reads=[t_ofm[ob]], writes=[t_scr["kv"]])
                    else:
                        ob = rot2()
                        kb.op("act", lambda e, pb=pb, ob=ob: e.activation(out=ofm[ob][0:2, :], in_=ps[pb][0:2, :], func=AF.Copy), reads=[pst[pb]], writes=[t_ofm[ob]])
                        kb.dma(s_if[:, t0:t0 + 512], ofm[ob][0:2, :], reads=[t_ofm[ob]], writes=[t_scr["if"]])
                for ts in range(int(os.environ.get('NTS', '4')) if 'e' in parts else 0):
                    tt = t0 + ts * 128
                    pb = 4 + (ts % 2)
                    for k in range(16):
                        kb.op("pe", lambda e, k=k, ts=ts, pb=pb: e.matmul(ps[pb][:, :], lhsT=hT[:, k, ts * 128:(ts + 1) * 128], rhs=wb[:, k, 1026:1538],
                                                                          start=(k == 0), stop=(k == 15)),
                              reads=[t_wb, t_hT[k]], writes=[pst[pb]], sig=(k == 15))
                    pb2 = 6 + (ts % 2)
                    for k in range(16):
                        kb.op("pe", lambda e, k=k, ts=ts, pb2=pb2: e.matmul(ps[pb2][:, 0:140], lhsT=hT[:, k, ts * 128:(ts + 1) * 128], rhs=wb[:, k, 1538:1678],
                                                                            start=(k == 0), stop=(k == 15)),
                              reads=[t_wb, t_hT[k]], writes=[pst[pb2]], sig=(k == 15))
                    ob = ts % 2
                    kb.op("dve", lambda e, pb=pb, ob=ob: e.tensor_copy(out=otm[ob][:, 0:256], in_=ps[pb][:, 0:256]), reads=[pst[pb]], writes=[t_otm[ob]])
                    kb.op("act", lambda e, pb=pb, ob=ob: e.activation(out=otm[ob][:, 256:512], in_=ps[pb][:, 256:512], func=AF.Sigmoid),
                          reads=[pst[pb]], writes=[t_otm[ob]])
                    SK = os.environ.get('SKIP', '')
                    if 'P' not in SK:
                        kb.op("dve", lambda e, ob=ob: e.tensor_tensor(out=otm[ob][:, 256:512], in0=otm[ob][:, 256:512], in1=mng[:], op=ALU.mult),
                              reads=[t_otm[ob], t_mng], writes=[t_otm[ob]])
                    if 'S' not in SK:
                        kb.dma(s_v[tt:tt + 128, :], otm[ob][:, 0:256], reads=[t_otm[ob]], writes=[t_scr["v"]])
                        kb.dma(s_go[tt:tt + 128, :], otm[ob][:, 256:512], reads=[t_otm[ob]], writes=[t_scr["go"]])
                    kb.op("dve", lambda e, pb2=pb2, ob=ob: e.tensor_copy(out=otb[ob][:], in_=ps[pb2][:, 0:128]), reads=[pst[pb2]], writes=[t_otb[ob]])
                    kb.op("act", lambda e, pb2=pb2, ob=ob: e.activation(out=otg[ob][:], in_=ps[pb2][:, 128:140], func=AF.Sigmoid),
                          reads=[pst[pb2]], writes=[t_otg[ob]])
                    if 'V' not in os.environ.get('SKIP', ''):
                        kb.dma(s_vv[tt:tt + 128, :], otb[ob][:], reads=[t_otb[ob]], writes=[t_scr["vv"]])
                    if 'G' not in os.environ.get('SKIP', ''):
                        kb.dma(s_gt[tt:tt + 128, :], otg[ob][:], reads=[t_otg[ob]], writes=[t_scr["gt"]])
            kb.barrier()


    mixT = [nc.dram_tensor("mixT%d" % i, [512, 1024], BF16, kind="Internal").ap() for i in range(8)]; t_mixT = [Trk() for _ in range(8)]
    mixG = [nc.dram_tensor("mixG%d" % i, [2048, 1024], BF16, kind="Internal").ap() for i in range(8)]; t_mixG = [Trk() for _ in range(8)]

    def gather_chunk(i):
        if phases >= 4:
            kb.op("pool", lambda e: e.collective_compute("AllGather", ALU.bypass, replica_groups=[[0, 1, 2, 3], [4, 5, 6, 7]],
                                                         ins=[mixT[i]], outs=[mixG[i]]), reads=[t_mixT[i]], writes=[t_mixG[i]])
    if phases >= 2:
        bif_in = din("bif", [128, 2])
        ut_in = din("ut_f", [128, 128])
        identb_in = din("ident_b", [128, 128], BF16)
        with ExitStack() as p2:
            def sb2(name, shape, dt=F32):
                return p2.enter_context(nc.sbuf_tensor(name, list(shape), dt))
            NCH = 64
            bif = sb2("bif_sb", [128, 2]); t_bif = Trk()
            ut = sb2("ut_sb", [128, 128]); t_ut = Trk()
            identb = sb2("identb", [128, 128], BF16); t_identb = Trk()
            kb.dma(bif[:], bif_in, writes=[t_bif])
            kb.dma(ut[:], ut_in, writes=[t_ut])
            kb.dma(identb[:], identb_in, writes=[t_identb])
            rows = sb2("rows", [64, 2, 128]); t_rows = Trk()
            kb.dma(rows[:, 0, :], s_if[0:1, :].rearrange("o (c p) -> (o c) p", p=128), reads=[t_scr["if"]], writes=[t_rows])
            kb.dma(rows[:, 1, :], s_if[1:2, :].rearrange("o (c p) -> (o c) p", p=128), reads=[t_scr["if"]], writes=[t_rows])
            cols = sb2("cols", [128, 12, 64]); t_cols = Trk()
            nbf = sb2("nbf", [128, 1]); t_nbf = Trk()
            zero64 = sb2("zero64", [128, 128]); t_zero = Trk()
            kb.op("dve", lambda e: e.memset(zero64[:], 0.0), writes=[t_zero])
            kb.op("dve", lambda e: e.tensor_scalar(out=nbf[:], in0=bif[:, 1:2], scalar1=-1.0, scalar2=None, op0=ALU.mult), reads=[t_bif], writes=[t_nbf])
            for w in range(2):
                kb.op("pe", lambda e, w=w: e.matmul(ps[w][:, 0:64], lhsT=rows[:, w, :], rhs=ident[0:64, 0:64], start=True, stop=True),
                      reads=[t_rows, t_ident], writes=[pst[w]])
            kb.op("dve", lambda e: e.tensor_scalar(out=cols[:, 0, :], in0=ps[0][:, 0:64], scalar1=bif[:, 0:1], scalar2=None, op0=ALU.add),
                  reads=[pst[0], t_bif], writes=[t_cols])
            kb.op("act", lambda e: e.activation(out=cols[:, 11, :], in_=ps[1][:, 0:64], func=AF.Exp, scale=-1.0, bias=nbf[:, 0:1]),
                  reads=[pst[1], t_nbf], writes=[t_cols])
            kb.op("act", lambda e: e.activation(out=cols[:, 1, :], in_=cols[:, 11, :], func=AF.Ln, scale=1.0, bias=1.0), reads=[t_cols], writes=[t_cols])
            kb.op("dve", lambda e: e.tensor_scalar(out=cols[:, 1, :], in0=cols[:, 1, :], scalar1=-1.0, scalar2=None, op0=ALU.mult), reads=[t_cols], writes=[t_cols])
            kb.op("pe", lambda e: e.matmul(ps[2][:, 0:64], lhsT=ut[:], rhs=cols[:, 1, :], start=True, stop=True), reads=[t_ut, t_cols], writes=[pst[2]])
            kb.op("pe", lambda e: e.matmul(ps[3][:, 0:64], lhsT=ones_f[:], rhs=cols[:, 1, :], start=True, stop=True), reads=[t_ones_f, t_cols], writes=[pst[3]])
            kb.op("dve", lambda e: e.tensor_copy(out=cols[:, 11, :], in_=ps[3][:, 0:64]), reads=[pst[3]], writes=[t_cols])
            kb.op("dve", lambda e: e.tensor_tensor_scan(out=cols[:, 2, :], data0=cols[:, 11, :], data1=zero64[:, 0:64], initial=0.0, op0=ALU.add, op1=ALU.add),
                  reads=[t_cols, t_zero], writes=[t_cols])
            kb.op("dve", lambda e: e.tensor_tensor(out=cols[:, 2, :], in0=cols[:, 2, :], in1=cols[:, 11, :], op=ALU.subtract), reads=[t_cols], writes=[t_cols])
            kb.op("dve", lambda e: e.tensor_tensor(out=cols[:, 2, :], in0=cols[:, 2, :], in1=ps[2][:, 0:64], op=ALU.add), reads=[t_cols, pst[2]], writes=[t_cols])
            kb.op("dve", lambda e: e.tensor_tensor(out=cols[:, 3, :], in0=cols[:, 0, :], in1=cols[:, 2, :], op=ALU.subtract), reads=[t_cols], writes=[t_cols])
            grow = sb2("grow", [64, 128]); t_grow = Trk()
            cmrow = sb2("cmrow", [64, 128]); t_cmrow = Trk()
            kb.op("pe", lambda e: e.matmul(ps[4][0:64, 0:128], lhsT=cols[:, 3, :], rhs=ident[:, :], start=True, stop=True), reads=[t_cols, t_ident], writes=[pst[4]])
            kb.op("dve", lambda e: e.tensor_copy(out=grow[:], in_=ps[4][0:64, 0:128]), reads=[pst[4]], writes=[t_grow])
            kb.op("dve", lambda e: e.tensor_tensor_scan(out=cmrow[:], data0=grow[:], data1=grow[:], initial=-1e30, op0=ALU.max, op1=ALU.max),
                  reads=[t_grow], writes=[t_cmrow])
            mrow = sb2("mrow", [1, 3, 64]); t_mrow = Trk()
            kb.op("pe", lambda e: e.matmul(ps[5][0:1, 0:64], lhsT=cmrow[:, 127:128], rhs=ident[0:64, 0:64], start=True, stop=True),
                  reads=[t_cmrow, t_ident], writes=[pst[5]])
            kb.op("dve", lambda e: e.tensor_copy(out=mrow[0:1, 0, :], in_=ps[5][0:1, 0:64]), reads=[pst[5]], writes=[t_mrow])
            kb.op("dve", lambda e: e.tensor_tensor_scan(out=mrow[0:1, 1, :], data0=mrow[0:1, 0, :], data1=mrow[0:1, 0, :], initial=0.0, op0=ALU.max, op1=ALU.max),
                  reads=[t_mrow], writes=[t_mrow])
            kb.op("dve", lambda e: e.memset(mrow[0:1, 2, 0:1], 0.0), reads=[t_mrow], writes=[t_mrow])
            kb.op("dve", lambda e: e.tensor_copy(out=mrow[0:1, 2, 1:64], in_=mrow[0:1, 1, 0:63]), reads=[t_mrow], writes=[t_mrow])
            kb.op("pe", lambda e: e.matmul(ps[6][:, 0:128], lhsT=ones_f[0:1, :], rhs=mrow[0:1, 1:3, :].rearrange("o a c -> o (a c)"), start=True, stop=True),
                  reads=[t_ones_f, t_mrow], writes=[pst[6]])
            kb.op("dve", lambda e: e.tensor_copy(out=cols[:, 6, :], in_=ps[6][:, 0:64]), reads=[pst[6]], writes=[t_cols])
            kb.op("dve", lambda e: e.tensor_copy(out=cols[:, 5, :], in_=ps[6][:, 64:128]), reads=[pst[6]], writes=[t_cols])
            kb.op("pe", lambda e: e.matmul(ps[7][:, 0:64], lhsT=cmrow[:, :], rhs=ident[0:64, 0:64], start=True, stop=True), reads=[t_cmrow, t_ident], writes=[pst[7]])
            kb.op("dve", lambda e: e.tensor_tensor(out=cols[:, 4, :], in0=ps[7][:, 0:64], in1=cols[:, 5, :], op=ALU.max), reads=[pst[7], t_cols], writes=[t_cols])
            kb.op("dve", lambda e: e.tensor_tensor(out=cols[:, 11, :], in0=cols[:, 3, :], in1=cols[:, 5, :], op=ALU.subtract), reads=[t_cols], writes=[t_cols])
            kb.op("act", lambda e: e.activation(out=cols[:, 7, :], in_=cols[:, 11, :], func=AF.Exp), reads=[t_cols], writes=[t_cols])
            kb.op("dve", lambda e: e.tensor_scalar(out=cols[:, 7, :], in0=cols[:, 7, :], scalar1=1.0 / 16, scalar2=None, op0=ALU.mult), reads=[t_cols], writes=[t_cols])
            kb.op("dve", lambda e: e.tensor_tensor(out=cols[:, 11, :], in0=cols[:, 5, :], in1=cols[:, 4, :], op=ALU.subtract), reads=[t_cols], writes=[t_cols])
            kb.op("act", lambda e: e.activation(out=cols[:, 8, :], in_=cols[:, 11, :], func=AF.Exp), reads=[t_cols], writes=[t_cols])
            kb.op("dve", lambda e: e.tensor_tensor(out=cols[:, 11, :], in0=cols[:, 2, :], in1=cols[:, 4, :], op=ALU.add), reads=[t_cols], writes=[t_cols])
            kb.op("act", lambda e: e.activation(out=cols[:, 9, :], in_=cols[:, 11, :], func=AF.Exp, scale=-1.0), reads=[t_cols], writes=[t_cols])
            kb.op("dve", lambda e: e.tensor_tensor(out=cols[:, 11, :], in0=cols[:, 5, :], in1=cols[:, 6, :], op=ALU.subtract), reads=[t_cols], writes=[t_cols])
            kb.op("act", lambda e: e.activation(out=cols[:, 10, :], in_=cols[:, 11, :], func=AF.Exp), reads=[t_cols], writes=[t_cols])
            if dbg:
                d_cols = nc.dram_tensor("d_cols", [128, 12, 64], F32, kind="ExternalOutput").ap()
                kb.dma(d_cols, cols[:], reads=[t_cols])

            qT = [sb2("qT%d" % i, [128, 2, 128]) for i in range(2)]; t_qT = [Trk(), Trk()]
            kT = [sb2("kT%d" % i, [128, 2, 128]) for i in range(2)]; t_kT = [Trk(), Trk()]
            va = [sb2("va%d" % i, [128, 257]) for i in range(2)]; t_va = [Trk(), Trk()]
            go = [sb2("go%d" % i, [128, 256]) for i in range(2)]; t_go = [Trk(), Trk()]
            kp = sb2("kp", [128, 256]); t_kp = Trk()
            wT = sb2("wT", [128, 128]); t_wT = Trk()
            St = [sb2("St%d" % i, [128, 2, 257]) for i in range(2)]; t_St = [Trk(), Trk()]
            sc = sb2("sc", [128, 8]); t_sc = Trk()
            junk = sb2("junk", [128, 256]); t_junk = Trk()
            hmf = sb2("hmf", [128, 256], BF16); t_hmf = Trk()
            hmT = [sb2("hmT%d" % i, [128, 2, 128], BF16) for i in range(2)]; t_hmT = [Trk(), Trk()]
            for i in range(2):
                kb.op("dve", lambda e, i=i: e.memset(va[i][:, 256:257], 1.0), writes=[t_va[i]])
            kb.op("dve", lambda e: e.memset(St[0][:], 0.0), writes=[t_St[0]])
            qv = s_q.rearrange("(a p) t -> p a t", p=128)
            kv_ = s_k.rearrange("(a p) t -> p a t", p=128)

            def load_chunk(c):
                bi = c % 2
                kb.dma(qT[bi][:], qv[:, :, c * 128:(c + 1) * 128], reads=[t_scr["q"]], writes=[t_qT[bi]])
                kb.dma(kT[bi][:], kv_[:, :, c * 128:(c + 1) * 128], reads=[t_scr["k"]], writes=[t_kT[bi]])
                kb.dma(va[bi][:, 0:256], s_v[c * 128:(c + 1) * 128, :], reads=[t_scr["v"]], writes=[t_va[bi]])
                kb.dma(go[bi][:], s_go[c * 128:(c + 1) * 128, :], reads=[t_scr["go"]], writes=[t_go[bi]])

            nch = int(os.environ.get("NCH", "64"))
            load_chunk(0)
            for c in range(nch):
                bi = c % 2
                so = St[c % 2]; tso = t_St[c % 2]
                sn = St[(c + 1) % 2]; tsn = t_St[(c + 1) % 2]
                if c + 1 < nch:
                    load_chunk(c + 1)
                for dc in range(2):
                    kb.op("pe", lambda e, dc=dc, bi=bi: e.matmul(ps[0][:, dc * 128:(dc + 1) * 128], lhsT=kT[bi][:, dc, :], rhs=ident[:, :], start=True, stop=True),
                          reads=[t_kT[bi], t_ident], writes=[pst[0]], sig=(dc == 1))
                kb.op("dve", lambda e, c=c: e.tensor_scalar(out=kp[:], in0=ps[0][:, 0:256], scalar1=cols[:, 7, c:c + 1], scalar2=None, op0=ALU.mult),
                      reads=[pst[0], t_cols], writes=[t_kp])
                for dc in range(2):
                    kb.op("pe", lambda e, dc=dc, bi=bi: e.matmul(ps[1][:, 0:128], lhsT=kT[bi][:, dc, :], rhs=qT[bi][:, dc, :], start=(dc == 0), stop=(dc == 1)),
                          reads=[t_kT[bi], t_qT[bi]], writes=[pst[1]], sig=(dc == 1))
                kb.op("dve", lambda e, c=c: e.scalar_tensor_tensor(out=wT[:], in0=ps[1][:, 0:128], scalar=cols[:, 7, c:c + 1], in1=ut[:], op0=ALU.mult, op1=ALU.mult),
                      reads=[pst[1], t_cols, t_ut], writes=[t_wT])
                kb.op("pe", lambda e, bi=bi: e.matmul(ps[2][:, 0:257], lhsT=wT[:], rhs=va[bi][:], start=True, stop=False),
                      reads=[t_wT, t_va[bi]], writes=[pst[2]], sig=False)
                for dc in range(2):
                    kb.op("pe", lambda e, dc=dc, bi=bi, so=so: e.matmul(ps[2][:, 0:257], lhsT=qT[bi][:, dc, :], rhs=so[:, dc, :], start=False, stop=(dc == 1)),
                          reads=[t_qT[bi], tso], writes=[pst[2]], sig=(dc == 1))
                for dc in range(2):
                    pb = 3 + dc
                    kb.op("pe", lambda e, dc=dc, bi=bi, pb=pb: e.matmul(ps[pb][:, 0:257], lhsT=kp[:, dc * 128:(dc + 1) * 128], rhs=va[bi][:], start=True, stop=False),
                          reads=[t_kp, t_va[bi]], writes=[pst[pb]], sig=False)
                    kb.op("pe", lambda e, dc=dc, pb=pb, so=so: e.matmul(ps[pb][:, 0:257], lhsT=ident[:, :], rhs=so[:, dc, :], start=False, stop=True),
                          reads=[t_ident, tso], writes=[pst[pb]])
                    kb.op("act" if dc else "dve",
                          (lambda e, dc=dc, pb=pb, sn=sn, c=c: e.activation(out=sn[:, dc, :], in_=ps[pb][:, 0:257], func=AF.Copy, scale=cols[:, 10, c:c + 1])) if dc else
                          (lambda e, dc=dc, pb=pb, sn=sn, c=c: e.tensor_scalar(out=sn[:, dc, :], in0=ps[pb][:, 0:257], scalar1=cols[:, 10, c:c + 1], scalar2=None, op0=ALU.mult)),
                          reads=[pst[pb], t_cols], writes=[tsn])
                kb.op("act", lambda e, c=c: e.activation(out=sc[:, 0:1], in_=ps[2][:, 256:257], func=AF.Abs, scale=cols[:, 8, c:c + 1]),
                      reads=[pst[2], t_cols], writes=[t_sc])
                kb.op("dve", lambda e, c=c: e.tensor_tensor(out=sc[:, 1:2], in0=sc[:, 0:1], in1=cols[:, 9, c:c + 1], op=ALU.max), reads=[t_sc, t_cols], writes=[t_sc])
                kb.op("dve", lambda e: e.reciprocal(out=sc[:, 2:3], in_=sc[:, 1:2]), reads=[t_sc], writes=[t_sc])
                kb.op("dve", lambda e, c=c: e.tensor_tensor(out=sc[:, 3:4], in0=sc[:, 2:3], in1=cols[:, 8, c:c + 1], op=ALU.mult), reads=[t_sc, t_cols], writes=[t_sc])
                kb.op("act", lambda e: e.activation(out=junk[:], in_=ps[2][:, 0:256], func=AF.Square, accum_out=sc[:, 4:5]), reads=[pst[2]], writes=[t_junk, t_sc])
                kb.op("dve", lambda e: e.scalar_tensor_tensor(out=sc[:, 5:6], in0=sc[:, 3:4], scalar=sc[:, 3:4], in1=sc[:, 4:5], op0=ALU.mult, op1=ALU.mult),
                      reads=[t_sc], writes=[t_sc])
                kb.op("act", lambda e: e.activation(out=sc[:, 5:6], in_=sc[:, 5:6], func=AF.Sqrt, scale=1.0 / 256, bias=EPS), reads=[t_sc], writes=[t_sc])
                kb.op("dve", lambda e: e.reciprocal(out=sc[:, 6:7], in_=sc[:, 5:6]), reads=[t_sc], writes=[t_sc])
                kb.op("dve", lambda e: e.tensor_tensor(out=sc[:, 7:8], in0=sc[:, 6:7], in1=sc[:, 3:4], op=ALU.mult), reads=[t_sc], writes=[t_sc])
                kb.op("dve", lambda e, bi=bi: e.scalar_tensor_tensor(out=hmf[:], in0=ps[2][:, 0:256], scalar=sc[:, 7:8], in1=go[bi][:], op0=ALU.mult, op1=ALU.mult),
                      reads=[pst[2], t_sc, t_go[bi]], writes=[t_hmf])
                ob = c % 2
                for dc in range(2):
                    kb.op("pe", lambda e, dc=dc: e.matmul(ps[5][:, dc * 128:(dc + 1) * 128], lhsT=hmf[:, dc * 128:(dc + 1) * 128], rhs=identb[:, :], start=True, stop=True),
                          reads=[t_hmf, t_identb], writes=[pst[5]], sig=(dc == 1))
                kb.op("act", lambda e, ob=ob: e.activation(out=hmT[ob][:].rearrange("p a t -> p (a t)"), in_=ps[5][:, 0:256], func=AF.Copy), reads=[pst[5]], writes=[t_hmT[ob]])
                kb.dma(mixT[c // 8][0:256, (c % 8) * 128:(c % 8 + 1) * 128].rearrange("(a p) t -> p a t", p=128), hmT[ob][:], reads=[t_hmT[ob]], writes=[t_mixT[c // 8]])
            kb.barrier()

    if phases >= 3:
        tb_sw_in = din("tb_sw", [128, 64, 4])
        tb_c_in = din("tb_c", [128, 64, 4])
        cmask_in = din("cmask", [128, 17, 128], BF16)
        caus_in = din("caus_add", [128, 128], BF16)
        acaus_in = din("acaus_add", [128, 128], BF16)
        ovl_in = din("ovl", [128, 4, 128], BF16)
        expT_in = din("expT", [128, S], BF16)
        tkeep_in = din("t_keep", [128, 255])
        tadd_in = din("t_add", [128, 255])
        w1k_in = din("w1k", [2048, 256]); w1v_in = din("w1v", [2048, 256])
        w2k_in = din("w2k", [256, 64]); w2v_in = din("w2v", [256, 64])
        posk_in = din("posk", [128, 16]); posv_in = din("posv", [128, 16])
        kng0_in = din("kng0", [64, 1])
        with ExitStack() as p3:
            def sb3(name, shape, dt=F32):
                return p3.enter_context(nc.sbuf_tensor(name, list(shape), dt))
            QT = sb3("QT", [128, 4, S], BF16); t_QT = Trk()
            KK = sb3("KK", [128, S], BF16); t_KK = Trk()
            Vs = sb3("Vs", [128, 64, 65], BF16); t_Vs = Trk()
            Vw = sb3("Vw", [128, 64, 65], BF16); t_Vw = Trk()
            expT = sb3("expT_sb", [128, S], BF16); t_expT = Trk()
            tb_sw = sb3("tb_sw_sb", [128, 64, 4]); t_tbsw = Trk()
            tb_c = sb3("tb_c_sb", [128, 64, 4]); t_tbc = Trk()
            cmask = sb3("cmask_sb", [128, 17, 128], BF16); t_cmask = Trk()
            caus = sb3("caus_sb", [128, 4, 128], BF16); t_caus = Trk()
            acaus = sb3("acaus_sb", [128, 4, 128], BF16); t_acaus = Trk()
            ovl = sb3("ovl_sb", [128, 4, 128], BF16); t_ovl = Trk()
            tkeep = sb3("tkeep_sb", [128, 255]); t_tkeep = Trk()
            tadd = sb3("tadd_sb", [128, 255]); t_tadd = Trk()
            gts = sb3("gts", [128, 64, 12]); t_gts = Trk()
            kcT = sb3("kcT", [64, 512], BF16); t_kcT = Trk()
            vca = sb3("vca", [128, 4, 65], BF16); t_vca = Trk()
            identb3 = sb3("identb3", [128, 128], BF16); t_identb3 = Trk()
            aqv = s_aq.rearrange("(h d) t -> d h t", d=64)
            for hh in range(2):
                for h in range(4):
                    kb.dma(QT[hh * 64:(hh + 1) * 64, h, :], aqv[:, h, :], reads=[t_scr["aq"]], writes=[t_QT])
            kb.dma(KK[:, 0:4096], s_kk[:, 0:4096], reads=[t_scr["kk"]], writes=[t_KK])
            kb.dma(KK[:, 4096:S], s_kk[:, 4096:S], reads=[t_scr["kk"]], writes=[t_KK])
            kb.dma(expT[:], expT_in, writes=[t_expT])
            kb.dma(tb_sw[:], tb_sw_in, writes=[t_tbsw]); kb.dma(tb_c[:], tb_c_in, writes=[t_tbc])
            kb.dma(cmask[:], cmask_in, writes=[t_cmask])
            for h4 in range(4):
                kb.dma(caus[:, h4, :], caus_in, writes=[t_caus]); kb.dma(acaus[:, h4, :], acaus_in, writes=[t_acaus])
            kb.dma(ovl[:], ovl_in, writes=[t_ovl]); kb.dma(tkeep[:], tkeep_in, writes=[t_tkeep]); kb.dma(tadd[:], tadd_in, writes=[t_tadd])
            kb.dma(identb3[:], identb_in if phases >= 2 else din("ident_b", [128, 128], BF16), writes=[t_identb3])
            kb.dma(gts[:], s_gt.rearrange("(q p) c -> p q c", p=128), reads=[t_scr["gt"]], writes=[t_gts])
            with ExitStack() as p3a:
                def sb3a(name, shape, dt=F32):
                    return p3a.enter_context(nc.sbuf_tensor(name, list(shape), dt))
                vtmp = sb3a("vtmp", [128, 64, 128], BF16); t_vtmp = Trk()
                kb.dma(vtmp[:], s_vv.rearrange("(q p) c -> p q c", p=128), reads=[t_scr["vv"]], writes=[t_vtmp])
                kb.op("dve", lambda e: e.tensor_copy(out=Vs[:, :, 0:64], in_=vtmp[:, :, 0:64]), reads=[t_vtmp], writes=[t_Vs])
                kb.op("dve", lambda e: e.tensor_copy(out=Vw[:, :, 0:64], in_=vtmp[:, :, 64:128]), reads=[t_vtmp], writes=[t_Vw])
                kb.op("dve", lambda e: e.memset(Vs[:, :, 64:65], 1.0), writes=[t_Vs])
                kb.op("dve", lambda e: e.memset(Vw[:, :, 64:65], 1.0), writes=[t_Vw])
                kb.op("dve", lambda e: e.memset(vca[:, :, 64:65], 1.0), writes=[t_vca])
                w1f = sb3a("w1f", [128, 16, 256]); t_w1f = Trk()
                w1b = sb3a("w1b", [128, 16, 256], BF16); t_w1b = Trk()
                w2f = sb3a("w2f", [128, 2, 64]); t_w2f = Trk()
                w2b = sb3a("w2b", [128, 2, 64], BF16); t_w2b = Trk()
                posf = sb3a("posf", [128, 16]); t_posf = Trk()
                bcol = sb3a("bcol", [128, 2]); t_bcol = Trk()
                kng0 = sb3a("kng0_sb", [64, 1]); t_kng0 = Trk()
                a2f = sb3a("a2f", [128, 2048]); t_a2f = Trk()
                a2b = sb3a("a2b", [128, S], BF16); t_a2b = Trk()
                hid = sb3a("hid", [128, 2, 512], BF16); t_hid = Trk()
                csq = sb3a("csq", [64, 512], BF16); t_csq = Trk()
                crs = sb3a("crs", [64, 512]); t_crs = Trk()
                kb.dma(kng0[:], kng0_in, writes=[t_kng0])
                kb.op("dve", lambda e: e.memset(hid[:], 0.0), writes=[t_hid])
                kb.op("dve", lambda e: e.memset(kcT[:], 0.0), writes=[t_kcT])
                for which in range(2):
                    w1_in = w1k_in if which == 0 else w1v_in
                    w2_in = w2k_in if which == 0 else w2v_in
                    pos_in = posk_in if which == 0 else posv_in
                    r0 = 0 if which == 0 else 64
                    kb.dma(w1f[:, 0:8, :], w1_in.rearrange("(k p) n -> p k n", p=128)[:, 0:8, :], writes=[t_w1f])
                    kb.dma(w1f[:, 8:16, :], w1_in.rearrange("(k p) n -> p k n", p=128)[:, 8:16, :], writes=[t_w1f])
                    kb.dma(w2f[:], w2_in.rearrange("(k p) n -> p k n", p=128), writes=[t_w2f])
                    kb.dma(posf[:], pos_in, writes=[t_posf])
                    kb.op("dve", lambda e: e.tensor_copy(out=w1b[:], in_=w1f[:]), reads=[t_w1f], writes=[t_w1b])
                    kb.op("dve", lambda e: e.tensor_copy(out=w2b[:], in_=w2f[:]), reads=[t_w2f], writes=[t_w2b])
                    for q4 in range(4):
                        c0 = q4 * 2048
                        kb.dma(a2f[0:64, :], s_kv[r0:r0 + 64, c0:c0 + 2048], reads=[t_scr["kv"]], writes=[t_a2f])
                        if q4 < 3:
                            kb.dma(a2f[64:128, :], s_kv[r0:r0 + 64, c0 + 1:c0 + 2049], reads=[t_scr["kv"]], writes=[t_a2f])
                        else:
                            kb.dma(a2f[64:128, 0:2047], s_kv[r0:r0 + 64, c0 + 1:c0 + 2048], reads=[t_scr["kv"]], writes=[t_a2f])
                        kb.op("dve", lambda e, c0=c0: e.tensor_copy(out=a2b[:, c0:c0 + 2048], in_=a2f[:]), reads=[t_a2f], writes=[t_a2b])
                    for hc in range(2):
                        for jj in range(16):
                            kb.op("pe", lambda e, hc=hc, jj=jj: e.matmul(ps[7][:, hc:hc + 1], lhsT=w1f[:, jj, hc * 128:(hc + 1) * 128], rhs=posf[:, jj:jj + 1],
                                                                          start=(jj == 0), stop=(jj == 15)),
                                  reads=[t_w1f, t_posf], writes=[pst[7]], sig=(jj == 15))
                    kb.op("dve", lambda e: e.tensor_copy(out=bcol[:], in_=ps[7][:, 0:2]), reads=[pst[7]], writes=[t_bcol])
                    a2v = a2b[:].rearrange("p (i s) -> p i s", s=16)
                    for hc in range(2):
                        for jj in range(16):
                            j0 = 2 * jj
                            rhs = a2v[:, 0:511, j0] if j0 < 16 else a2v[:, 1:512, j0 - 16]
                            kb.op("pe", lambda e, hc=hc, jj=jj, rhs=rhs: e.matmul(ps[hc][:, 0:511], lhsT=w1b[:, jj, hc * 128:(hc + 1) * 128], rhs=rhs,
                                                                                  start=(jj == 0), stop=(jj == 15)),
                                  reads=[t_w1b, t_a2b], writes=[pst[hc]], sig=(jj == 15))
                        kb.op("act", lambda e, hc=hc: e.activation(out=hid[:, hc, 0:511], in_=ps[hc][:, 0:511], func=AF.Gelu_apprx_tanh, bias=bcol[:, hc:hc + 1]),
                              reads=[pst[hc], t_bcol], writes=[t_hid])
                    if which == 0:
                        for hc in range(2):
                            kb.op("pe", lambda e, hc=hc: e.matmul(ps[2][0:64, 0:511], lhsT=w2b[:, hc, :], rhs=hid[:, hc, 0:511], start=(hc == 0), stop=(hc == 1)),
                                  reads=[t_w2b, t_hid], writes=[pst[2]], sig=(hc == 1))
                        kb.op("act", lambda e: e.activation(out=csq[:, 0:511], in_=ps[2][0:64, 0:511], func=AF.Square), reads=[pst[2]], writes=[t_csq])
                        kb.op("pe", lambda e: e.matmul(ps[3][0:64, 0:511], lhsT=blk64[0:64, 0:64], rhs=csq[:, 0:511], start=True, stop=True),
                              reads=[t_blk64, t_csq], writes=[pst[3]])
                        kb.op("act", lambda e: e.activation(out=crs[:, 0:511], in_=ps[3][0:64, 0:511], func=AF.Sqrt, scale=1.0 / 64, bias=EPS), reads=[pst[3]], writes=[t_crs])
                        kb.op("dve", lambda e: e.reciprocal(out=crs[:, 0:511], in_=crs[:, 0:511]), reads=[t_crs], writes=[t_crs])
                        kb.op("dve", lambda e: e.scalar_tensor_tensor(out=kcT[:, 0:511], in0=ps[2][0:64, 0:511], scalar=kng0[:, 0:1], in1=crs[:, 0:511],
                                                                      op0=ALU.mult, op1=ALU.mult), reads=[pst[2], t_kng0, t_crs], writes=[t_kcT])
                    else:
                        for it in range(4):
                            for hc in range(2):
                                kb.op("pe", lambda e, hc=hc, it=it: e.matmul(ps[4][:, it * 64:(it + 1) * 64], lhsT=hid[:, hc, it * 128:(it + 1) * 128], rhs=w2b[:, hc, :],
                                                                             start=(hc == 0), stop=(hc == 1)),
                                      reads=[t_hid, t_w2b], writes=[pst[4]], sig=(hc == 1 and it == 3))
                        kb.op("dve", lambda e: e.tensor_copy(out=vca[:, :, 0:64], in_=ps[4][:, 0:256].rearrange("p (a d) -> p a d", d=64)), reads=[pst[4]], writes=[t_vca])
                if dbg:
                    d_kcT = nc.dram_tensor("d_kcT", [64, 512], BF16, kind="ExternalOutput").ap()
                    d_vca = nc.dram_tensor("d_vca", [128, 4, 65], BF16, kind="ExternalOutput").ap()
                    kb.dma(d_kcT, kcT[:], reads=[t_kcT]); kb.dma(d_vca, vca[:], reads=[t_vca])
                kb.barrier()

            pT = [sb3("pT%d" % i, [128, 512], BF16) for i in range(3)]; t_pT = [[Trk() for _ in range(4)] for _ in range(3)]
            oT = sb3("oT", [65, 3, 512]); t_oT = Trk()
            zsc = sb3("zsc", [128, 24]); t_zsc = Trk()
            impn = sb3("impn", [128, 128]); t_impn = Trk()
            impw = sb3("impw", [128, 128]); t_impw = Trk()
            m8 = sb3("m8", [128, 16]); t_m8 = Trk()
            selb = sb3("selb", [128, 128], BF16); t_selb = Trk()
            selT = sb3("selT", [128, 4, 128], BF16); t_selT = Trk()
            haf = sb3("haf", [128, 256]); t_haf = Trk()
            hab = sb3("hab", [128, 256], BF16); t_hab = Trk()
            haT = [sb3("haT%d" % i, [128, 2, 128], BF16) for i in range(2)]; t_haT = [Trk(), Trk()]
            if dbg:
                d_imp = nc.dram_tensor("d_imp", [S, 128], F32, kind="ExternalOutput").ap()
                d_sel = nc.dram_tensor("d_sel", [S, 128], BF16, kind="ExternalOutput").ap()
            pcount = [0]
            NEG = -30000.0

            pend = []

            def flush_pairs():
                while pend:
                    pend.pop(0)()

            def tile_pair(lhsK, rhsQ, masks, bias_tab, bidx, Vaug, obank, first, last, imp_it=None):
                i = pcount[0] % 2
                pi = pcount[0] % 3
                pcount[0] += 1
                sps = ps[i]; tsp = pst[i]
                nm = len(masks)
                kb.op("pe", lambda e: e.matmul(sps[:, :], lhsT=lhsK[0], rhs=rhsQ[0], start=True, stop=(nm == 0)),
                      reads=[lhsK[1], rhsQ[1]], writes=[tsp], sig=(nm == 0))
                for mi, (ml, mr, mt) in enumerate(masks):
                    lastm = (mi == nm - 1)
                    if mr.shape[-1] == 512 or len(mr.shape) == 3:
                        kb.op("pe", lambda e, ml=ml, mr=mr, lastm=lastm: e.matmul(sps[:, :], lhsT=ml, rhs=mr, start=False, stop=lastm),
                              reads=mt, writes=[tsp], sig=lastm)
                    else:
                        for h in range(4):
                            lm = (lastm and h == 3)
                            kb.op("pe", lambda e, ml=ml, mr=mr, h=h, lm=lm: e.matmul(sps[:, h * 128:(h + 1) * 128], lhsT=ml, rhs=mr, start=False, stop=lm),
                                  reads=mt, writes=[tsp], sig=lm)
                for h in range(4):
                    kb.op("act", lambda e, h=h: e.activation(out=pT[pi][:, h * 128:(h + 1) * 128], in_=sps[:, h * 128:(h + 1) * 128], func=AF.Exp,
                                                             bias=bias_tab[0][:, bidx, h:h + 1]),
                          reads=[tsp, bias_tab[1]], writes=[t_pT[pi][h]])

                def stage_b():
                    kb.op("pe", lambda e: e.matmul(ps[obank][0:65, :], lhsT=Vaug[0], rhs=pT[pi][:, :], start=first, stop=last),
                          reads=[Vaug[1]] + t_pT[pi], writes=[pst[obank]], sig=last)
                    if imp_it is not None:
                        it, nit = imp_it
                        for h in range(4):
                            kb.op("pe", lambda e, h=h, it=it: e.matmul(ps[5][:, h * 128:(h + 1) * 128], lhsT=pT[pi][:, h * 128:(h + 1) * 128], rhs=ovl[:, it, :],
                                                                       start=(it == 0), stop=(it == nit - 1)),
                                  reads=[t_pT[pi][h], t_ovl], writes=[pst[5]], sig=(it == nit - 1 and h == 3))
                while len(pend) > 1:
                    pend.pop(0)()
                pend.append(stage_b)
                if len(pend) > 1:
                    pend.pop(0)()

            nqt = int(os.environ.get("NQT", "64"))
            for qt in range(nqt):
                q0 = qt * 128
                qv_lo = (QT[0:64, :, q0:q0 + 128], t_QT)
                qv_hi = (QT[64:128, :, q0:q0 + 128], t_QT)
                nit = (8 * qt + 6) // 128 + 1
                for it in range(nit):
                    m = qt - 16 * it
                    masks = []
                    if m <= 16:
                        masks.append((identb3[:, :], cmask[:, m, :], [t_identb3, t_cmask]))
                    tile_pair((kcT[0:64, it * 128:(it + 1) * 128], t_kcT), qv_lo, masks, (tb_c, t_tbc), qt - 16 * it, (vca[:, it, :], t_vca), 2,
                              it == 0, it == nit - 1, imp_it=(it, nit))
                kts = [kt for kt in range(qt - 4, qt + 1) if kt >= 0]
                for n_, kt in enumerate(kts):
                    masks = []
                    if kt == qt:
                        masks.append((identb3[:, :], caus[:, :, :], [t_identb3, t_caus]))
                    if kt == qt - 4:
                        masks.append((identb3[:, :], acaus[:, :, :], [t_identb3, t_acaus]))
                    tile_pair((KK[64:128, kt * 128:(kt + 1) * 128], t_KK), qv_hi, masks, (tb_sw, t_tbsw), qt - kt, (Vw[:, kt, :], t_Vw), 4,
                              n_ == 0, n_ == len(kts) - 1)
                flush_pairs()
                kb.op("dve", lambda e: e.tensor_copy(out=oT[:, 0, :], in_=ps[2][0:65, :]), reads=[pst[2]], writes=[t_oT])
                for h in range(4):
                    kb.op("pe", lambda e, h=h: e.matmul(ps[6][:, h * 65:(h + 1) * 65], lhsT=oT[0:65, 0, h * 128:(h + 1) * 128], rhs=ident[0:65, 0:65], start=True, stop=True),
                          reads=[t_oT, t_ident], writes=[pst[6]], sig=(h == 3))
                kb.op("dve", lambda e: e.tensor_scalar(out=zsc[:, 0:4], in0=ps[6][:, 0:260].rearrange("p (h c) -> p h c", c=65)[:, :, 64], scalar1=1e-30, scalar2=None, op0=ALU.max),
                      reads=[pst[6]], writes=[t_zsc])
                kb.op("dve", lambda e: e.reciprocal(out=zsc[:, 0:4], in_=zsc[:, 0:4]), reads=[t_zsc], writes=[t_zsc])
                kb.op("dve", lambda e: e.tensor_scalar(out=impn[:], in0=ps[5][:, 0:128], scalar1=zsc[:, 0:1], scalar2=None, op0=ALU.mult), reads=[pst[5], t_zsc], writes=[t_impn])
                for h in range(1, 4):
                    kb.op("dve", lambda e, h=h: e.scalar_tensor_tensor(out=impn[:], in0=ps[5][:, h * 128:(h + 1) * 128], scalar=zsc[:, h:h + 1], in1=impn[:], op0=ALU.mult, op1=ALU.add),
                          reads=[pst[5], t_zsc, t_impn], writes=[t_impn])
                if dbg:
                    kb.dma(d_imp[q0:q0 + 128, :], impn[:], reads=[t_impn])
                o0 = 127 - 2 * qt
                kb.op("dve", lambda e, o0=o0: e.tensor_tensor(out=impn[:], in0=impn[:], in1=tkeep[:, o0:o0 + 128], op=ALU.mult), reads=[t_impn, t_tkeep], writes=[t_impn])
                kb.op("dve", lambda e, o0=o0: e.tensor_tensor(out=impn[:], in0=impn[:], in1=tadd[:, o0:o0 + 128], op=ALU.add), reads=[t_impn, t_tadd], writes=[t_impn])
                kb.op("dve", lambda e: e.memset(impn[:, 0:1], 1e4), reads=[t_impn], writes=[t_impn])
                kb.op("dve", lambda e: e.max(out=m8[:, 0:8], in_=impn[:]), reads=[t_impn], writes=[t_m8])
                kb.op("dve", lambda e: e.match_replace(out=impw[:], in_to_replace=m8[:, 0:8], in_values=impn[:], imm_value=-3e38), reads=[t_impn, t_m8], writes=[t_impw])
                kb.op("dve", lambda e: e.max(out=m8[:, 8:16], in_=impw[:]), reads=[t_impw], writes=[t_m8])
                kb.op("dve", lambda e: e.tensor_scalar(out=selb[:], in0=impn[:], scalar1=m8[:, 15:16], scalar2=None, op0=ALU.is_ge), reads=[t_impn, t_m8], writes=[t_selb])
                if dbg:
                    kb.dma(d_sel[q0:q0 + 128, :], selb[:], reads=[t_selb])
                kb.op("pe", lambda e: e.matmul(ps[7][:, 0:128], lhsT=selb[:, :], rhs=identb3[:, :], start=True, stop=True), reads=[t_selb, t_identb3], writes=[pst[7]])
                kb.op("dve", lambda e: e.tensor_scalar(out=selT[:], in0=ps[7][:, 0:128].unsqueeze(1).broadcast_to([128, 4, 128]), scalar1=-1.0, scalar2=-NEG, op0=ALU.add, op1=ALU.mult),
                      reads=[pst[7]], writes=[t_selT])
                for kt in range(qt + 1):
                    if kt == qt:
                        masks = [(identb3[:, :], caus[:, :, :], [t_identb3, t_caus])]
                    else:
                        masks = [(expT[:, kt * 128:(kt + 1) * 128], selT[:, :, :], [t_expT, t_selT])]
                    tile_pair((KK[0:64, kt * 128:(kt + 1) * 128], t_KK), qv_lo, masks, (tb_sw, t_tbsw), qt - kt, (Vs[:, kt, :], t_Vs), 3, kt == 0, kt == qt)
                flush_pairs()
                kb.op("dve", lambda e: e.tensor_copy(out=oT[:, 1, :], in_=ps[3][0:65, :]), reads=[pst[3]], writes=[t_oT])
                kb.op("dve", lambda e: e.tensor_copy(out=oT[:, 2, :], in_=ps[4][0:65, :]), reads=[pst[4]], writes=[t_oT])
                for br in (1, 2):
                    for h in range(4):
                        col = 260 + ((br - 1) * 4 + h) * 65
                        pbk, cc = (6, col) if col + 65 <= 512 else (7, col - 455 + 128)
                        kb.op("pe", lambda e, h=h, br=br, pbk=pbk, cc=cc: e.matmul(ps[pbk][:, cc:cc + 65], lhsT=oT[0:65, br, h * 128:(h + 1) * 128], rhs=ident[0:65, 0:65], start=True, stop=True),
                              reads=[t_oT, t_ident], writes=[pst[pbk]])

                def oslot(br, h):
                    if br == 0:
                        return 6, h * 65
                    col = 260 + ((br - 1) * 4 + h) * 65
                    return (6, col) if col + 65 <= 512 else (7, col - 455 + 128)
                for br in (1, 2):
                    for h in range(4):
                        pbk, cc = oslot(br, h)
                        kb.op("dve", lambda e, br=br, h=h, pbk=pbk, cc=cc: e.tensor_scalar(out=zsc[:, br * 4 + h:br * 4 + h + 1], in0=ps[pbk][:, cc + 64:cc + 65], scalar1=1e-30, scalar2=None, op0=ALU.max),
                              reads=[pst[pbk]], writes=[t_zsc])
                kb.op("dve", lambda e: e.reciprocal(out=zsc[:, 4:12], in_=zsc[:, 4:12]), reads=[t_zsc], writes=[t_zsc])
                for br in range(3):
                    kb.op("dve", lambda e, br=br, qt=qt: e.tensor_tensor(out=zsc[:, 12 + br * 4:16 + br * 4], in0=zsc[:, br * 4:br * 4 + 4],
                                                                         in1=gts[:, qt, :].rearrange("p (h b) -> p h b", b=3)[:, :, br], op=ALU.mult),
                          reads=[t_zsc, t_gts], writes=[t_zsc])
                for h in range(4):
                    for br in range(3):
                        pbk, cc = oslot(br, h)
                        if br == 0:
                            kb.op("dve", lambda e, h=h, br=br, pbk=pbk, cc=cc: e.tensor_scalar(out=haf[:, h * 64:(h + 1) * 64], in0=ps[pbk][:, cc:cc + 64],
                                                                                               scalar1=zsc[:, 12 + br * 4 + h:13 + br * 4 + h], scalar2=None, op0=ALU.mult),
                                  reads=[pst[pbk], t_zsc], writes=[t_haf])
                        else:
                            kb.op("dve", lambda e, h=h, br=br, pbk=pbk, cc=cc: e.scalar_tensor_tensor(out=haf[:, h * 64:(h + 1) * 64], in0=ps[pbk][:, cc:cc + 64],
                                                                                                      scalar=zsc[:, 12 + br * 4 + h:13 + br * 4 + h], in1=haf[:, h * 64:(h + 1) * 64],
                                                                                                      op0=ALU.mult, op1=ALU.add),
                                  reads=[pst[pbk], t_zsc, t_haf], writes=[t_haf])
                kb.op("dve", lambda e: e.tensor_copy(out=hab[:], in_=haf[:]), reads=[t_haf], writes=[t_hab])
                ob = qt % 2
                for dc in range(2):
                    kb.op("pe", lambda e, dc=dc: e.matmul(ps[5][:, dc * 128:(dc + 1) * 128], lhsT=hab[:, dc * 128:(dc + 1) * 128], rhs=identb3[:, :], start=True, stop=True),
                          reads=[t_hab, t_identb3], writes=[pst[5]], sig=(dc == 1))
                kb.op("dve", lambda e, ob=ob: e.tensor_copy(out=haT[ob][:].rearrange("p a t -> p (a t)"), in_=ps[5][:, 0:256]), reads=[pst[5]], writes=[t_haT[ob]])
                kb.dma(mixT[qt // 8][256:512, (qt % 8) * 128:(qt % 8 + 1) * 128].rearrange("(a p) t -> p a t", p=128), haT[ob][:], reads=[t_haT[ob]], writes=[t_mixT[qt // 8]])
                if qt % 8 == 7:
                    gather_chunk(qt // 8)
            kb.barrier()
    if dbg:
        d_mixT = nc.dram_tensor("d_mixT", [512, S], BF16, kind="ExternalOutput").ap()
        for i in range(8):
            kb.dma(d_mixT[:, i * 1024:(i + 1) * 1024], mixT[i], reads=[t_mixT[i]])


    if phases >= 4:
        selm_in = din("selm", [128, 4])
        x_own = din("x_own", [2048, D])
        w_out_in = din("w_out_p", [D, D])
        g2_row = din("g2_row", [1, D])
        wq_in = din("wq", [D, D])
        skT_in = din("skT", [128, 2, 128])
        uT_in = din("uT", [D, 16384])
        v_in = din("v_tab", [16384, D])
        y_out = nc.dram_tensor("y", [2048, D], F32, kind="ExternalOutput").ap()

        s_x1 = dscr("s_x1", [2048, D]); t_sx1 = Trk()
        s_h2T = dscr("s_h2T", [D, 2048], BF16); t_sh2T = Trk()
        with ExitStack() as p45:
            def sb45(name, shape, dt=F32):
                return p45.enter_context(nc.sbuf_tensor(name, list(shape), dt))
            g2tb = sb45("g2tb", [128, D]); t_g2tb = Trk()
            identb4 = sb45("identb4", [128, 128], BF16); t_identb4 = Trk()
            kb.dma(identb4[:], din("ident_b2", [128, 128], BF16), writes=[t_identb4])
            with ExitStack() as p4:
                def sb4(name, shape, dt=F32):
                    return p4.enter_context(nc.sbuf_tensor(name, list(shape), dt))
                rowst = sb4("rowst", [1, D]); t_rowst = Trk()
                g1b = sb4("g1b", [128, D]); t_g1b = Trk()
                sh2b = sb4("sh2b", [128, D]); t_sh2b = Trk()
                gs2b = sb4("gs2b", [128, D]); t_gs2b = Trk()
                selm = sb4("selm_sb", [128, 4]); t_selm = Trk()
                kb.dma(selm[:], selm_in, writes=[t_selm])

                def bcast_row(src_ap, dst, t_dst, src_reads=()):
                    kb.dma(rowst[:], src_ap, reads=list(src_reads), writes=[t_rowst])
                    for n in range(4):
                        kb.op("pe", lambda e, n=n: e.matmul(ps[n][:, :], lhsT=ones_f[0:1, :], rhs=rowst[0:1, n * 512:(n + 1) * 512], start=True, stop=True),
                              reads=[t_ones_f, t_rowst], writes=[pst[n]])
                        kb.op("dve", lambda e, n=n: e.tensor_copy(out=dst[:, n * 512:(n + 1) * 512], in_=ps[n][:, :]), reads=[pst[n]], writes=[t_dst])
                bcast_row(s_mod[0:1, 2 * D:3 * D], g1b, t_g1b, [t_smod])
                bcast_row(s_mod[0:1, 3 * D:4 * D], sh2b, t_sh2b, [t_smod])
                bcast_row(s_mod[0:1, 5 * D:6 * D], g2tb, t_g2tb, [t_smod])
                bcast_row(s_mod[0:1, 4 * D:5 * D], gs2b, t_gs2b, [t_smod])
                kb.dma(rowst[:], g2_row, writes=[t_rowst])
                for n in range(4):
                    kb.op("pe", lambda e, n=n: e.matmul(ps[n][:, :], lhsT=ones_f[0:1, :], rhs=rowst[0:1, n * 512:(n + 1) * 512], start=True, stop=True),
                          reads=[t_ones_f, t_rowst], writes=[pst[n]])
                    kb.op("dve", lambda e, n=n: e.scalar_tensor_tensor(out=gs2b[:, n * 512:(n + 1) * 512], in0=gs2b[:, n * 512:(n + 1) * 512], scalar=1.0, in1=ps[n][:, :],
                                                                       op0=ALU.add, op1=ALU.mult), reads=[pst[n], t_gs2b], writes=[t_gs2b])
                wo = sb4("wo", [128, 16, D], BF16); t_wo = Trk()
                for fc in range(16):
                    kb.dma(wo[:, fc, :], w_out_in[fc * 128:(fc + 1) * 128, :], writes=[t_wo], q="pool")
                sl2 = [[sb4("sl%d_%d" % (j2, i), [128, 16, 128], BF16) for i in range(4)] for j2 in range(2)]
                t_sl2 = [[Trk() for _ in range(4)] for _ in range(2)]
                mixo = sb4("mixo", [128, 16, 128], BF16); t_mixo = Trk()
                xin = [sb4("xin%d" % i, [128, D]) for i in range(2)]; t_xin = [Trk(), Trk()]
                tmpy = sb4("tmpy", [128, D]); t_tmpy = Trk()
                h2b = sb4("h2b", [128, D], BF16); t_h2b = Trk()
                h2Tt = [sb4("h2Tt%d" % i, [128, 16, 128], BF16) for i in range(2)]; t_h2Tt = [Trk(), Trk()]
                nsc = sb4("nsc", [128, 4]); t_nsc = Trk()
                mgv = [mg.rearrange("(f p) t -> p f t", p=128) for mg in mixG]
                def load_tt(tt):
                    bi = tt % 2
                    kb.dma(xin[bi][:], x_own[tt * 128:(tt + 1) * 128, :], writes=[t_xin[bi]])
                    for s_ in range(4):
                        kb.dma(sl2[bi][s_][:], mgv[2 * s_ + tt // 8][:, :, (tt % 8) * 128:(tt % 8 + 1) * 128], reads=[t_mixG[2 * s_ + tt // 8]], writes=[t_sl2[bi][s_]])
                load_tt(0)
                for tt in range(16):
                    bi = tt % 2
                    sl = sl2[bi]; t_sl = t_sl2[bi]
                    if tt + 1 < 16:
                        load_tt(tt + 1)
                    kb.op("dve", lambda e: e.tensor_scalar(out=mixo[:], in0=sl[0][:], scalar1=selm[:, 0:1], scalar2=None, op0=ALU.mult), reads=[t_sl[0], t_selm], writes=[t_mixo])
                    for s_ in range(1, 4):
                        kb.op("dve", lambda e, s_=s_: e.scalar_tensor_tensor(out=mixo[:], in0=sl[s_][:], scalar=selm[:, s_:s_ + 1], in1=mixo[:], op0=ALU.mult, op1=ALU.add),
                              reads=[t_sl[s_], t_selm, t_mixo], writes=[t_mixo])
                    for n in range(4):
                        for fc in range(16):
                            kb.op("pe", lambda e, n=n, fc=fc: e.matmul(ps[n][:, :], lhsT=mixo[:, fc, :], rhs=wo[:, fc, n * 512:(n + 1) * 512], start=(fc == 0), stop=(fc == 15)),
                                  reads=[t_mixo, t_wo], writes=[pst[n]], sig=(fc == 15))
                        kb.op("dve", lambda e, n=n: e.tensor_tensor(out=tmpy[:, n * 512:(n + 1) * 512], in0=ps[n][:, :], in1=g1b[:, n * 512:(n + 1) * 512], op=ALU.mult),
                              reads=[pst[n], t_g1b], writes=[t_tmpy])
                        kb.op("dve", lambda e, n=n, bi=bi: e.tensor_tensor(out=xin[bi][:, n * 512:(n + 1) * 512], in0=xin[bi][:, n * 512:(n + 1) * 512], in1=tmpy[:, n * 512:(n + 1) * 512], op=ALU.add),
                              reads=[t_xin[bi], t_tmpy], writes=[t_xin[bi]])
                    kb.dma(s_x1[tt * 128:(tt + 1) * 128, :], xin[bi][:], reads=[t_xin[bi]], writes=[t_sx1])
                    kb.op("act", lambda e, bi=bi: e.activation(out=tmpy[:], in_=xin[bi][:], func=AF.Square, accum_out=nsc[:, 0:1]), reads=[t_xin[bi]], writes=[t_tmpy, t_nsc])
                    kb.op("act", lambda e: e.activation(out=nsc[:, 1:2], in_=nsc[:, 0:1], func=AF.Sqrt, scale=1.0 / D, bias=EPS), reads=[t_nsc], writes=[t_nsc])
                    kb.op("dve", lambda e: e.reciprocal(out=nsc[:, 2:3], in_=nsc[:, 1:2]), reads=[t_nsc], writes=[t_nsc])
                    kb.op("dve", lambda e, bi=bi: e.scalar_tensor_tensor(out=tmpy[:], in0=xin[bi][:], scalar=nsc[:, 2:3], in1=gs2b[:], op0=ALU.mult, op1=ALU.mult),
                          reads=[t_xin[bi], t_nsc, t_gs2b], writes=[t_tmpy])
                    kb.op("dve", lambda e: e.tensor_tensor(out=h2b[:], in0=tmpy[:], in1=sh2b[:], op=ALU.add), reads=[t_tmpy, t_sh2b], writes=[t_h2b])
                    for qd in range(4):
                        pb = 4 + qd
                        for k in range(4):
                            dc = qd * 4 + k
                            kb.op("pe", lambda e, dc=dc, k=k, pb=pb: e.matmul(ps[pb][:, k * 128:(k + 1) * 128], lhsT=h2b[:, dc * 128:(dc + 1) * 128], rhs=identb4[:, :], start=True, stop=True),
                                  reads=[t_h2b, t_identb4], writes=[pst[pb]], sig=(k == 3))
                        kb.op("act", lambda e, qd=qd, pb=pb, bi=bi: e.activation(out=h2Tt[bi][:, qd * 4:(qd + 1) * 4, :].rearrange("p a t -> p (a t)"), in_=ps[pb][:, :], func=AF.Copy),
                              reads=[pst[pb]], writes=[t_h2Tt[bi]])
                    kb.dma(s_h2T[:, tt * 128:(tt + 1) * 128].rearrange("(a p) t -> p a t", p=128), h2Tt[bi][:], reads=[t_h2Tt[bi]], writes=[t_sh2T])
                kb.barrier()

            if phases >= 5:
                with ExitStack() as p5:
                    def sb5(name, shape, dt=F32):
                        return p5.enter_context(nc.sbuf_tensor(name, list(shape), dt))
                    skT = sb5("skT_sb", [128, 2, 128], BF16); t_skT = Trk()
                    kb.dma(skT[:], skT_in, writes=[t_skT], q="pool")
                    h2g = sb5("h2g", [128, 16, 512], BF16); t_h2g = Trk()
                    s1m = sb5("s1m", [128, 4, 8, 128]); t_s1m = Trk()
                    s2t = sb5("s2t", [128, 4, 8, 128]); t_s2t = Trk()
                    tau = sb5("tau", [128, 4, 8]); t_tau = Trk()
                    Ysb = sb5("Ysb", [128, 4, D]); t_Ysb = [Trk() for _ in range(4)]
                    wqs = [sb5("wqs%d" % i, [128, 16, 128], BF16) for i in range(2)]; t_wqs = [Trk(), Trk()]
                    sct = sb5("sct", [128, 16, 128]); t_sct = Trk()
                    wk = sb5("wk", [128, 256]); t_wk = Trk()
                    tv = sb5("tv", [128, 16, 16]); t_tv = Trk()
                    cand = sb5("cand", [128, 8, 256]); t_cand = Trk()
                    cw2 = sb5("cw2", [128, 256]); t_cw2 = Trk()
                    m24 = sb5("m24", [128, 8, 24]); t_m24 = Trk()
                    hs = sb5("hs", [128, 8, 8]); t_hs = Trk()
                    ex = sb5("ex", [128, 8, 256]); t_ex = Trk()
                    Ub = [sb5("Ub%d" % i, [128, 16, 256], BF16) for i in range(2)]; t_Ub = [Trk(), Trk()]
                    Vb = [sb5("Vb%d" % i, [128, 2, D], BF16) for i in range(2)]; t_Vb = [Trk(), Trk()]
                    Lt = [sb5("Lt0", [128, 8, 256]), cand]; t_Lt = [Trk(), t_cand]
                    Dg = sb5("Dg", [128, 4, 8, 128], BF16); t_Dg = Trk()
                    t_Ex = [[Trk() for _ in range(16)] for _ in range(2)]
                    Wt = [sb5("Wt%d" % i, [128, 8, 256], BF16) for i in range(2)]; t_Wt = [[Trk() for _ in range(8)] for _ in range(2)]
                    glT = [sb5("glT%d" % i, [128, 2, 512], BF16) for i in range(2)]; t_glT = [Trk(), Trk()]
                    GT = [sb5("GT%d" % i, [128, 2, 128], BF16) for i in range(2)]; t_GT = [Trk(), Trk()]
                    x1t = Lt[0][:].rearrange("p h e -> p (h e)"); t_x1t = t_Lt[0]
                    h2v = s_h2T.rearrange("(a p) t -> p a t", p=128)
                    wqv = wq_in.rearrange("(a p) n -> p a n", p=128)
                    uTv = uT_in.rearrange("(a p) e -> p a e", p=128)
                    vv_ = v_in.rearrange("(c p) d -> p c d", p=128)
                    ngrp = int(os.environ.get("NGRP", "4"))
                    net = int(os.environ.get("NET", "64"))
                    for g in range(ngrp):
                        kb.dma(h2g[:, 0:8, :], h2v[:, 0:8, g * 512:(g + 1) * 512], reads=[t_sh2T], writes=[t_h2g])
                        kb.dma(h2g[:, 8:16, :], h2v[:, 8:16, g * 512:(g + 1) * 512], reads=[t_sh2T], writes=[t_h2g])
                        for tl in range(4):
                            pass
                        qall = p5.enter_context(nc.sbuf_tensor("qall%d" % g, [128, 16, 512], BF16)) if g == 0 else qall
                        t_qall = Trk() if g == 0 else t_qall
                        for ch in range(16):
                            wi = ch % 2
                            kb.dma(wqs[wi][:], wqv[:, :, ch * 128:(ch + 1) * 128], writes=[t_wqs[wi]], q="pool")
                            pb = ch % 2
                            for dc in range(16):
                                kb.op("pe", lambda e, dc=dc, wi=wi, pb=pb: e.matmul(ps[pb][:, :], lhsT=wqs[wi][:, dc, :], rhs=h2g[:, dc, :], start=(dc == 0), stop=(dc == 15)),
                                      reads=[t_wqs[wi], t_h2g], writes=[pst[pb]], sig=(dc == 15))
                            kb.op("act", lambda e, ch=ch, pb=pb: e.activation(out=qall[:, ch, :], in_=ps[pb][:, :], func=AF.Copy), reads=[pst[pb]], writes=[t_qall])
                        for tl in range(4):
                            for qd in range(4):
                                pb = 2 + (qd % 2)
                                for k in range(4):
                                    ch = qd * 4 + k
                                    kb.op("pe", lambda e, ch=ch, k=k, pb=pb, tl=tl: e.matmul(ps[pb][:, k * 128:(k + 1) * 128], lhsT=qall[:, ch, tl * 128:(tl + 1) * 128], rhs=skT[:, ch % 2, :],
                                                                                             start=True, stop=True),
                                          reads=[t_qall, t_skT], writes=[pst[pb]], sig=(k == 3))
                                kb.op("dve", lambda e, qd=qd, pb=pb: e.tensor_copy(out=sct[:, qd * 4:(qd + 1) * 4, :].rearrange("p a k -> p (a k)"), in_=ps[pb][:, :]),
                                      reads=[pst[pb]], writes=[t_sct])
                            for ch in range(16):
                                kb.op("dve", lambda e, ch=ch: e.max(out=tv[:, ch, 0:8], in_=sct[:, ch, :]), reads=[t_sct], writes=[t_tv])
                                kb.op("dve", lambda e, ch=ch: e.match_replace(out=wk[:, 0:128], in_to_replace=tv[:, ch, 0:8], in_values=sct[:, ch, :], imm_value=-3e38),
                                      reads=[t_sct, t_tv], writes=[t_wk])
                                kb.op("dve", lambda e, ch=ch: e.max(out=tv[:, ch, 8:16], in_=wk[:, 0:128]), reads=[t_wk], writes=[t_tv])
                            tvv = tv[:].rearrange("p (h two) a -> p h two a", two=2)
                            kb.op("dve", lambda e: e.tensor_tensor(out=cand[:].rearrange("p h (a b) -> p h a b", b=16),
                                                                   in0=tvv[:, :, 0, :].unsqueeze(3).broadcast_to([128, 8, 16, 16]),
                                                                   in1=tvv[:, :, 1, :].unsqueeze(2).broadcast_to([128, 8, 16, 16]), op=ALU.add),
                                  reads=[t_tv], writes=[t_cand])
                            for h in range(8):
                                kb.op("dve", lambda e, h=h: e.max(out=m24[:, h, 0:8], in_=cand[:, h, :]), reads=[t_cand], writes=[t_m24])
                                kb.op("dve", lambda e, h=h: e.match_replace(out=cw2[:], in_to_replace=m24[:, h, 0:8], in_values=cand[:, h, :], imm_value=-3e38),
                                      reads=[t_cand, t_m24], writes=[t_cw2])
                                kb.op("dve", lambda e, h=h: e.max(out=m24[:, h, 8:16], in_=cw2[:]), reads=[t_cw2], writes=[t_m24])
                                kb.op("dve", lambda e, h=h: e.match_replace(out=cw2[:], in_to_replace=m24[:, h, 8:16], in_values=cw2[:], imm_value=-3e38),
                                      reads=[t_cw2, t_m24], writes=[t_cw2])
                                kb.op("dve", lambda e, h=h: e.max(out=m24[:, h, 16:24], in_=cw2[:]), reads=[t_cw2], writes=[t_m24])
                            kb.op("dve", lambda e: e.tensor_copy(out=hs[:, :, 0], in_=m24[:, :, 0]), reads=[t_m24], writes=[t_hs])
                            kb.op("dve", lambda e: e.tensor_tensor(out=hs[:, :, 1], in0=m24[:, :, 15], in1=m24[:, :, 16], op=ALU.add), reads=[t_m24], writes=[t_hs])
                            kb.op("dve", lambda e: e.tensor_scalar(out=hs[:, :, 1], in0=hs[:, :, 1], scalar1=0.5, scalar2=None, op0=ALU.mult), reads=[t_hs], writes=[t_hs])
                            kb.op("dve", lambda e: e.tensor_tensor(out=ex[:], in0=cand[:], in1=hs[:, :, 0:1].broadcast_to([128, 8, 256]), op=ALU.subtract), reads=[t_cand, t_hs], writes=[t_ex])
                            kb.op("act", lambda e: e.activation(out=ex[:], in_=ex[:], func=AF.Exp), reads=[t_ex], writes=[t_ex])
                            kb.op("dve", lambda e: e.tensor_tensor(out=cand[:], in0=cand[:], in1=hs[:, :, 1:2].broadcast_to([128, 8, 256]), op=ALU.is_ge), reads=[t_cand, t_hs], writes=[t_cand])
                            kb.op("dve", lambda e: e.tensor_tensor(out=ex[:], in0=ex[:], in1=cand[:], op=ALU.mult), reads=[t_cand, t_ex], writes=[t_ex])
                            kb.op("dve", lambda e: e.tensor_reduce(out=hs[:, :, 2], in_=ex[:], axis=AX.X, op=ALU.add), reads=[t_ex], writes=[t_hs])
                            kb.op("act", lambda e: e.activation(out=hs[:, :, 3], in_=hs[:, :, 2], func=AF.Ln), reads=[t_hs], writes=[t_hs])
                            kb.op("dve", lambda e: e.tensor_tensor(out=hs[:, :, 4], in0=hs[:, :, 0], in1=hs[:, :, 3], op=ALU.add), reads=[t_hs], writes=[t_hs])
                            sv = sct[:].rearrange("p (h two) k -> p h two k", two=2)
                            kb.op("dve", lambda e, tl=tl: e.tensor_tensor(out=s1m[:, tl, :, :], in0=sv[:, :, 0, :], in1=hs[:, :, 4:5].broadcast_to([128, 8, 128]), op=ALU.subtract),
                                  reads=[t_sct, t_hs], writes=[t_s1m])
                            kb.op("dve", lambda e, tl=tl: e.tensor_copy(out=s2t[:, tl, :, :], in_=sv[:, :, 1, :]), reads=[t_sct], writes=[t_s2t])
                            kb.op("dve", lambda e, tl=tl: e.tensor_tensor(out=tau[:, tl, :], in0=hs[:, :, 1], in1=hs[:, :, 4], op=ALU.subtract), reads=[t_hs], writes=[t_tau])
                            kb.op("dve", lambda e, tl=tl: e.tensor_tensor(out=s1m[:, tl, :, :], in0=s1m[:, tl, :, :], in1=tau[:, tl, :].unsqueeze(2).broadcast_to([128, 8, 128]), op=ALU.subtract),
                                  reads=[t_s1m, t_tau], writes=[t_s1m])
                            kb.op("act", lambda e, tl=tl: e.activation(out=hs[:, :, 5], in_=tau[:, tl, :], func=AF.Exp), reads=[t_tau], writes=[t_hs])
                            for h in range(8):
                                kb.op("dve", lambda e, tl=tl, h=h: e.tensor_scalar(out=Dg[:, tl, h, :], in0=identb4[:, :], scalar1=hs[:, h, 5:6], scalar2=None, op0=ALU.mult),
                                      reads=[t_identb4, t_hs], writes=[t_Dg])
                        kb.op("dve", lambda e: e.memset(Ysb[:], 0.0), writes=t_Ysb)

                        def load_u(et):
                            bi = et % 2
                            kb.dma(Ub[bi][:, 0:8, :], uTv[:, 0:8, et * 256:(et + 1) * 256], writes=[t_Ub[bi]], q="pool")
                            kb.dma(Ub[bi][:, 8:16, :], uTv[:, 8:16, et * 256:(et + 1) * 256], writes=[t_Ub[bi]], q="pool")

                        def load_v(et):
                            bi = et % 2
                            kb.dma(Vb[bi][:], vv_[:, et * 2:et * 2 + 2, :], writes=[t_Vb[bi]], q="pool")

                        def emit_a(et):
                            bi = et % 2
                            for ec in range(2):
                                for dc in range(16):
                                    kb.op("pe", lambda e, dc=dc, ec=ec, bi=bi: e.matmul(ps[ec][:, :], lhsT=Ub[bi][:, dc, ec * 128:(ec + 1) * 128], rhs=h2g[:, dc, :],
                                                                                        start=(dc == 0), stop=(dc == 15)),
                                          reads=[t_Ub[bi], t_h2g], writes=[pst[ec]], sig=(dc == 15))
                                kb.op("act", lambda e, ec=ec, bi=bi: e.activation(out=glT[bi][:, ec, :], in_=ps[ec][:, :], func=AF.Gelu_apprx_tanh),
                                      reads=[pst[ec]], writes=[t_glT[bi]])
                        load_u(0)
                        if net > 1:
                            load_u(1)
                        load_v(0)
                        emit_a(0)
                        pairs = [(et, tl) for et in range(net) for tl in range(4)]
                        NP = len(pairs)

                        def st_L(n):
                            et, tl = pairs[n]; k = n % 2
                            for h in range(8):
                                for a2 in range(2):
                                    kb.op("act", lambda e, h=h, a2=a2: e.activation(out=Lt[k][:, h, a2 * 128:(a2 + 1) * 128], in_=s2t[:, tl, h, :], func=AF.Exp,
                                                                                    bias=s1m[:, tl, h, 2 * et + a2:2 * et + a2 + 1]),
                                          reads=[t_s2t, t_s1m], writes=[t_Ex[k][h * 2 + a2], t_Lt[k]] if (h == 0 and a2 == 0) else [t_Ex[k][h * 2 + a2]])

                        def st_C(n):
                            et, tl = pairs[n]; k = n % 2
                            kb.op("dve", lambda e: e.scalar_tensor_tensor(out=Wt[k][:], in0=Lt[k][:], scalar=1.0, in1=Lt[k][:], op0=ALU.is_ge, op1=ALU.mult),
                                  reads=t_Ex[k] + [t_Lt[k]], writes=t_Wt[k])
                            pw = 2 + k
                            for ec in range(2):
                                for h in range(8):
                                    kb.op("pe", lambda e, ec=ec, h=h: e.matmul(ps[pw][:, ec * 128:(ec + 1) * 128], lhsT=Wt[k][:, h, ec * 128:(ec + 1) * 128], rhs=Dg[:, tl, h, :],
                                                                               start=(h == 0), stop=(h == 7)),
                                          reads=[t_Wt[k][h], t_Dg], writes=[pst[pw]], sig=(ec == 1 and h == 7))

                        def st_G(n):
                            et, tl = pairs[n]; k = n % 2; bi = et % 2; pw = 2 + k
                            if tl == 0:
                                if et + 1 < net:
                                    load_v(et + 1)
                                    emit_a(et + 1)
                                if et + 2 < net:
                                    load_u(et + 2)
                            kb.op("dve", lambda e: e.tensor_tensor(out=GT[k][:], in0=glT[bi][:, :, tl * 128:(tl + 1) * 128],
                                                                   in1=ps[pw][:, 0:256].rearrange("p (a t) -> p a t", t=128), op=ALU.mult),
                                  reads=[t_glT[bi], pst[pw]], writes=[t_GT[k]])
                            for nn in range(4):
                                pb = 4 + nn
                                for ec in range(2):
                                    kb.op("pe", lambda e, ec=ec, nn=nn, pb=pb: e.matmul(ps[pb][:, :], lhsT=GT[k][:, ec, :], rhs=Vb[bi][:, ec, nn * 512:(nn + 1) * 512],
                                                                                        start=(ec == 0), stop=(ec == 1)),
                                          reads=[t_GT[k], t_Vb[bi]], writes=[pst[pb]], sig=(ec == 1))

                        def st_Y(n):
                            et, tl = pairs[n]
                            kb.op("dve", lambda e: e.tensor_tensor(out=Ysb[:, tl, :], in0=Ysb[:, tl, :], in1=psY[:, :], op=ALU.add),
                                  reads=[t_Ysb[tl], pst[4], pst[5], pst[6], pst[7]], writes=[t_Ysb[tl]])
                        for n in range(-2, NP):
                            if 0 <= n:
                                st_G(n)
                            if 0 <= n + 2 < NP:
                                st_L(n + 2)
                            if 0 <= n + 1 < NP:
                                st_C(n + 1)
                            if 0 <= n:
                                st_Y(n)
                        for tl in range(4):
                            r0 = (g * 4 + tl) * 128
                            kb.dma(x1t, s_x1[r0:r0 + 128, :], reads=[t_sx1], writes=[t_x1t])
                            kb.op("dve", lambda e, tl=tl: e.tensor_tensor(out=Ysb[:, tl, :], in0=Ysb[:, tl, :], in1=g2tb[:], op=ALU.mult), reads=[t_Ysb[tl], t_g2tb], writes=[t_Ysb[tl]])
                            kb.op("dve", lambda e, tl=tl: e.tensor_tensor(out=x1t, in0=x1t, in1=Ysb[:, tl, :], op=ALU.add), reads=[t_Ysb[tl], t_x1t], writes=[t_x1t])
                            kb.dma(y_out[r0:r0 + 128, :], x1t, reads=[t_x1t])
                    kb.barrier()

    for _i in range(int(os.environ.get('EXTRA', '0'))):
        kb.dma(s_mod[0:1, 0:128], ident[0:1, :], reads=[t_ident])
    kb.barrier()
    ok, stuck, sems = kb.check()
    if not ok or os.environ.get('KBV'):
        print('SYNC CHECK ok=%s' % ok, stuck, {k: v for k, v in kb.cnt.items()})
    assert ok, 'sync deadlock'
    es.close()
    return nc


def _consts():
    ones = np.ones((128, 128), np.float32)
    blk = np.zeros((128, 128), np.float32)
    blk[:64, :64] = 1.0
    blk[64:, 64:] = 1.0
    ut = np.triu(np.ones((128, 128), np.float32))
    NEG = -30000.0
    il = np.arange(128)
    cmask = np.zeros((128, 17, 128), np.float32)
    for m in range(17):
        ok = (il[None, :] + 128 * m) >= (16 * il[:, None] + 31)
        cmask[:, m, :] = np.where(ok, 0.0, NEG)
    caus = np.where(il[:, None] <= il[None, :], 0.0, NEG).astype(np.float32)
    acaus = np.where(il[:, None] > il[None, :], 0.0, NEG).astype(np.float32)
    i_abs = (np.arange(4)[None, :, None] * 128 + il[:, None, None])
    blkk = np.arange(128)[None, None, :]
    ovl = ((16 * i_abs < 64 * blkk + 64) & (16 * i_abs + 32 > 64 * blkk)).astype(np.float32)
    expT = (np.arange(S)[None, :] // 64 == il[:, None]).astype(np.float32)
    o = np.arange(255)[None, :] - 127
    cur_rel = (il[:, None] >= 64).astype(np.int64)
    rel = o - cur_rel
    t_keep = (rel < -1).astype(np.float32)
    t_add = np.where(rel > 0, -1e4, np.where(rel >= -1, 1e4, 0.0)).astype(np.float32)
    return {"ones_bf": _bf(ones), "blk64_bf": _bf(blk), "ident_f": np.eye(128, dtype=np.float32),
            "ut_f": ut, "ident_b": _bf(np.eye(128, dtype=np.float32)),
            "cmask": _bf(cmask), "caus_add": _bf(caus), "acaus_add": _bf(acaus), "ovl": _bf(ovl), "expT": _bf(expT),
            "t_keep": np.ascontiguousarray(np.broadcast_to(t_keep, (128, 255))).astype(np.float32), "t_add": t_add}


def make_in_maps(inp):
    f = lambda a: np.ascontiguousarray(np.asarray(a, dtype=np.float32))
    x = f(inp["x"]); c = f(inp["c"])
    w_in = f(inp["w_in"])[0]
    conv = f(inp["conv_qk"])[0]
    qn_g = f(inp["qn_g"])[0]; kn_g = f(inp["kn_g"])[0]
    mng = f(inp["mlstm_norm_g"])[0]
    g1 = f(inp["norm1_g"])[0]
    cst = _consts()
    maps = []
    xTs = [np.ascontiguousarray(x[b].T) for b in range(2)]
    w_out = f(inp["w_out"])[0]
    rows = []
    for fc in range(16):
        r, part, half = fc // 4, (fc % 4) // 2, fc % 2
        base = part * 1024 + r * 256 + half * 128
        rows += list(range(base, base + 128))
    w_out_p = np.ascontiguousarray(w_out[rows])
    wq = f(inp["peer_wq"])[0]
    skT = np.ascontiguousarray(f(inp["peer_subkeys"])[0].transpose(2, 0, 1))
    uT = np.ascontiguousarray(f(inp["peer_u"])[0].T)
    vtab = f(inp["peer_v"])[0]
    for core in range(8):
        b, j = divmod(core, 4)
        cols = []
        cols += list(range(j * 256, j * 256 + 256))
        cols += list(range(1024 + j * 256, 1024 + j * 256 + 256))
        AQ = 4096 + 8
        cols += list(range(AQ + j * 256, AQ + j * 256 + 256))
        AKV = AQ + 1024
        for br in (0, 1, 2, 4):
            cols += list(range(AKV + br * 256 + j * 64, AKV + br * 256 + j * 64 + 64))
        cols += [4096 + j, 4096 + 4 + j]
        cols += list(range(2048 + j * 256, 2048 + j * 256 + 256))
        cols += list(range(3072 + j * 256, 3072 + j * 256 + 256))
        for br in (3, 5):
            cols += list(range(AKV + br * 256 + j * 64, AKV + br * 256 + j * 64 + 64))
        AG = AKV + 1536
        cols += list(range(AG + j * 12, AG + j * 12 + 12))
        assert len(cols) == 1678
        wj = np.zeros((D, 1680), np.float32)
        wj[:, :1678] = w_in[:, cols]
        cwt = np.zeros((128, 4, 4), np.float32)
        for t in range(4):
            base = (j * 256 + (t % 2) * 128) if t < 2 else (1024 + j * 256 + (t % 2) * 128)
            cwt[:, t, :] = conv[:, base:base + 128].T
        qkg = np.zeros((128, 4), np.float32)
        qkg[:, 0] = np.tile(qn_g, 2) * 0.125
        qkg[:, 3] = np.concatenate([kn_g[1], kn_g[2]])
        m = {
            "xT": xTs[b], "c_col": np.ascontiguousarray(c[b].reshape(16, 128).T),
            "w_mod": f(inp["w_mod"])[0], "b_mod": f(inp["b_mod"]),
            "g1_col": np.ascontiguousarray(g1.reshape(16, 128).T),
            "w_in": wj, "convw": cwt, "qk_g": qkg,
            "mng_row": np.ascontiguousarray(mng[j * 256:(j + 1) * 256][None, :]),
            "bif": np.ascontiguousarray(np.tile(np.array([[f(inp["b_igate"])[0, j], f(inp["b_fgate"])[0, j]]], np.float32), (128, 1))),
        }
        slopes = np.array([2.0 ** (-8.0 * (4 * j + hh + 1) / 16) for hh in range(4)], np.float64)
        kl = np.arange(128, dtype=np.float64)
        dl = np.arange(64, dtype=np.float64)
        m["tb_sw"] = (slopes[None, None, :] * (kl[:, None, None] - 64 - 128 * dl[None, :, None])).astype(np.float32)
        m["tb_c"] = (slopes[None, None, :] * (16 * kl[:, None, None] - 33 - 128 * dl[None, :, None])).astype(np.float32)
        m["w1k"] = f(inp["cmp_k_w1"])[0]; m["w1v"] = f(inp["cmp_v_w1"])[0]
        m["w2k"] = f(inp["cmp_k_w2"])[0]; m["w2v"] = f(inp["cmp_v_w2"])[0]
        for nm, key in (("posk", "cmp_pos_k"), ("posv", "cmp_pos_v")):
            pos = f(inp[key])[0]
            m[nm] = np.ascontiguousarray(pos.reshape(16, 2, 64).transpose(1, 2, 0).reshape(128, 16))
        m["kng0"] = np.ascontiguousarray(kn_g[0][:, None])
        sm = np.zeros((128, 4), np.float32); sm[:, j] = 1.0
        m["selm"] = sm
        m["x_own"] = np.ascontiguousarray(x[b, j * 2048:(j + 1) * 2048])
        m["w_out_p"] = w_out_p
        m["g2_row"] = f(inp["norm2_g"])
        m["wq"] = wq
        m["skT"] = skT
        m["uT"] = uT
        m["v_tab"] = vtab
        m["ident_b2"] = cst["ident_b"]
        m.update(cst)
        maps.append(m)
    return maps


def kernel(**inputs):
    nc = build()
    maps = make_in_maps(inputs)
    res = run_bass_kernel_spmd(nc, maps, core_ids=list(range(8)))
    out = np.zeros((2, S, D), np.float32)
    for core in range(8):
        b, j = divmod(core, 4)
        out[b, j * 2048:(j + 1) * 2048] = res.results[core]["y"]
    return out
```

```python
import numpy as np
from contextlib import ExitStack
import concourse.bass as bass
import concourse.mybir as mybir
from concourse.bass_utils import run_bass_kernel_spmd

F32 = mybir.dt.float32
BF16 = mybir.dt.bfloat16
AF = mybir.ActivationFunctionType
ALU = mybir.AluOpType
AX = mybir.AxisListType

D = 2048
S = 8192
NB = 16
EPS = 1e-6
import os
NDS = int(os.environ.get("NDS", "24"))


class Trk:
    __slots__ = ("w", "r")

    def __init__(self):
        self.w = None
        self.r = {}


class KB:
    def __init__(self, nc, es):
        self.nc = nc
        self.eng = {"pe": nc.tensor, "act": nc.scalar, "dve": nc.vector, "pool": nc.gpsimd, "sp": nc.sync}
        self.sem = {k: es.enter_context(nc.semaphore("s_" + k)) for k in self.eng}
        self.cnt = {k: 0 for k in self.eng}
        self.seen = {k: {} for k in self.eng}
        self.dsem = [es.enter_context(nc.semaphore("dq%d" % i)) for i in range(NDS)]
        self.duse = [0] * NDS
        self.dnext = 0
        self.ccsem = es.enter_context(nc.semaphore("ccs"))
        self.log = {k: [] for k in self.eng}

    def _wait(self, e, tok):
        if tok is None:
            return
        key, sem, val = tok
        if self.seen[e].get(key, 0) >= val:
            return
        self.eng[e].wait_ge(sem, val)
        self.log[e].append(("w", key, val))
        self.seen[e][key] = val

    def _deps(self, e, reads, writes):
        for b in reads:
            if b.w is not None and not (e == "pe" and b.w[0] == "pe"):
                self._wait(e, b.w)
        for b in writes:
            if b.w is not None and not (e == "pe" and b.w[0] == "pe"):
                self._wait(e, b.w)
            for t in b.r.values():
                if not (e == "pe" and t[0] == "pe"):
                    self._wait(e, t)

    def _mark(self, tok, reads, writes):
        for b in reads:
            b.r[tok[0]] = tok
        for b in writes:
            b.w = tok
            b.r = {}

    def op(self, e, fn, reads=(), writes=(), sig=True):
        self._deps(e, reads, writes)
        inst = fn(self.eng[e])
        if sig:
            self.cnt[e] += 1
            inst.then_inc(self.sem[e], 1)
            self.log[e].append(("i", e, 1))
            tok = (e, self.sem[e], self.cnt[e])
        else:
            tok = (e, self.sem[e], self.cnt[e] + 1)
        self._mark(tok, reads, writes)
        return tok

    def dma(self, out, in_, reads=(), writes=(), q="sp", **kw):
        i = self.dnext
        self.dnext = (i + 1) % NDS
        if self.duse[i] > 0:
            self._wait(q, (("d", i), self.dsem[i], 16 * self.duse[i]))
        self._deps(q, reads, writes)
        inst = self.eng[q].dma_start(out=out, in_=in_, **kw)
        self.duse[i] += 1
        inst.then_inc(self.dsem[i], 16)
        self.log[q].append(("i", ("d", i), 16))
        tok = (("d", i), self.dsem[i], 16 * self.duse[i])
        self._mark(tok, reads, writes)
        return tok

    def check(self):
        sems = {}
        pc = {k: 0 for k in self.log}
        prog = True
        while prog:
            prog = False
            for e, lg in self.log.items():
                while pc[e] < len(lg):
                    kind, key, val = lg[pc[e]]
                    if kind == "w":
                        if sems.get(key, 0) < val:
                            break
                    else:
                        sems[key] = sems.get(key, 0) + val
                    pc[e] += 1
                    prog = True
        stuck = {e: (pc[e], len(lg), lg[pc[e]] if pc[e] < len(lg) else None) for e, lg in self.log.items()}
        ok = all(pc[e] == len(lg) for e, lg in self.log.items())
        return ok, stuck, sems

    def barrier(self):
        for e in self.eng:
            for e2 in self.eng:
                if e2 != e and self.cnt[e2] > 0:
                    self._wait(e, (e2, self.sem[e2], self.cnt[e2]))
            for i in range(NDS):
                if self.duse[i] > 0:
                    self._wait(e, (("d", i), self.dsem[i], 16 * self.duse[i]))


def _bf(a):
    import ml_dtypes
    return np.asarray(a, dtype=np.float32).astype(ml_dtypes.bfloat16)


def build(dbg=False, phases=99, nb=NB, parts='abcde'):
    nc = bass.Bass("TRN2", target_bir_lowering=False)
    es = ExitStack()
    kb = KB(nc, es)

    def din(name, shape, dt=F32):
        return nc.dram_tensor(name, list(shape), dt, kind="ExternalInput").ap()

    def dscr(name, shape, dt=F32):
        return nc.dram_tensor(name, list(shape), dt, kind=("ExternalOutput" if dbg else "Internal")).ap()

    xT = din("xT", [D, S])
    c_col = din("c_col", [128, 16])
    w_mod = din("w_mod", [D, 6 * D])
    b_mod = din("b_mod", [1, 6 * D])
    g1_col = din("g1_col", [128, 16])
    w_in = din("w_in", [D, 1680])
    convw = din("convw", [128, 4, 4])
    qk_g = din("qk_g", [128, 4])
    mng_row = din("mng_row", [1, 256])
    ones_bf = din("ones_bf", [128, 128], BF16)
    blk64_bf = din("blk64_bf", [128, 128], BF16)
    ident_f = din("ident_f", [128, 128])

    s_q = dscr("s_q", [256, S])
    s_k = dscr("s_k", [256, S])
    s_aq = dscr("s_aq", [256, S], BF16)
    s_kv = dscr("s_kv", [128, S])
    s_kk = dscr("s_kk", [128, S], BF16)
    s_if = dscr("s_if", [2, S])
    s_v = dscr("s_v", [S, 256])
    s_go = dscr("s_go", [S, 256])
    s_vv = dscr("s_vv", [S, 128], BF16)
    s_gt = dscr("s_gt", [S, 12])

    ps = []
    pst = []
    for i in range(4):
        ps.append(es.enter_context(nc.psum_tensor("ps%d" % i, [128, 512], F32)))
        pst.append(Trk())
    psY = es.enter_context(nc.psum_tensor("psY", [128, 2048], F32))
    for i in range(4):
        ps.append(psY[:, i * 512:(i + 1) * 512])
        pst.append(Trk())

    def sb(name, shape, dt=F32):
        return es.enter_context(nc.sbuf_tensor(name, list(shape), dt))

    ones_b = sb("ones_b", [128, 128], BF16); t_ones_b = Trk()
    blk64 = sb("blk64", [128, 128], BF16); t_blk64 = Trk()
    ident = sb("ident", [128, 128]); t_ident = Trk()
    ones_f = sb("ones_f", [128, 128]); t_ones_f = Trk()
    kb.dma(ones_b[:], ones_bf, writes=[t_ones_b])
    kb.dma(blk64[:], blk64_bf, writes=[t_blk64])
    kb.dma(ident[:], ident_f, writes=[t_ident])
    kb.op("dve", lambda e: e.memset(ones_f[:], 1.0), writes=[t_ones_f])

    s_mod = dscr("s_mod", [1, 6 * D]); t_smod = Trk()
    csil = sb("csil", [128, 16]); t_csil = Trk()
    ccol = sb("ccol", [128, 16]); t_ccol = Trk()
    g1c = sb("g1c", [128, 16]); t_g1c = Trk()
    gs1 = sb("gs1", [128, 16]); t_gs1 = Trk()
    sh1 = sb("sh1", [128, 16]); t_sh1 = Trk()
    kb.dma(ccol[:], c_col, writes=[t_ccol])
    kb.dma(g1c[:], g1_col, writes=[t_g1c])
    kb.op("act", lambda e: e.activation(out=csil[:], in_=ccol[:], func=AF.Silu), reads=[t_ccol], writes=[t_csil])
    with ExitStack() as p0:
        wm = [p0.enter_context(nc.sbuf_tensor("wm%d" % i, [128, 16, 512], F32)) for i in range(2)]
        modrow = p0.enter_context(nc.sbuf_tensor("modrow", [1, 6 * D], F32)); t_modrow = Trk()
        bmod_sb = p0.enter_context(nc.sbuf_tensor("bmod_sb", [1, 6 * D], F32)); t_bmod = Trk()
        kb.dma(bmod_sb[:], b_mod, writes=[t_bmod])
        t_wm = [Trk(), Trk()]
        wmv = w_mod.rearrange("(k p) n -> p k n", p=128)
        for n in range(24):
            bi = n % 2
            kb.dma(wm[bi][:, 0:8, :], wmv[:, 0:8, n * 512:(n + 1) * 512], writes=[t_wm[bi]])
            kb.dma(wm[bi][:, 8:16, :], wmv[:, 8:16, n * 512:(n + 1) * 512], writes=[t_wm[bi]])
            pb = n % 2
            for k in range(16):
                kb.op("pe", lambda e, k=k, bi=bi, pb=pb: e.matmul(ps[pb][0:1, :], lhsT=csil[:, k:k + 1], rhs=wm[bi][:, k, :],
                                                                  start=(k == 0), stop=(k == 15)),
                      reads=[t_csil, t_wm[bi]], writes=[pst[pb]], sig=(k == 15))
            kb.op("dve", lambda e, n=n, pb=pb: e.tensor_tensor(out=modrow[0:1, n * 512:(n + 1) * 512], in0=ps[pb][0:1, :],
                                                               in1=bmod_sb[0:1, n * 512:(n + 1) * 512], op=ALU.add),
                  reads=[pst[pb], t_bmod], writes=[t_modrow])
        for which, dst, t_dst in ((0, sh1, t_sh1), (1, gs1, t_gs1)):
            for k in range(16):
                off = which * D + k * 128
                kb.op("pe", lambda e, off=off, k=k: e.matmul(ps[2][:, k:k + 1], lhsT=modrow[0:1, off:off + 128], rhs=ones_f[0:1, 0:1],
                                                             start=True, stop=True),
                      reads=[t_modrow, t_ones_f], writes=[pst[2]], sig=(k == 15))
            if which == 0:
                kb.op("dve", lambda e: e.tensor_copy(out=sh1[:], in_=ps[2][:, 0:16]), reads=[pst[2]], writes=[t_sh1])
            else:
                kb.op("dve", lambda e: e.scalar_tensor_tensor(out=gs1[:], in0=ps[2][:, 0:16], scalar=1.0, in1=g1c[:],
                                                              op0=ALU.add, op1=ALU.mult),
                      reads=[pst[2], t_g1c], writes=[t_gs1])
        kb.dma(s_mod, modrow[:], reads=[t_modrow], writes=[t_smod])
        kb.barrier()

    t_scr = {n: Trk() for n in ("q", "k", "aq", "kv", "kk", "if", "v", "go", "vv", "gt")}
    if phases >= 1:
        with ExitStack() as p1:
            def sb1(name, shape, dt=F32):
                return p1.enter_context(nc.sbuf_tensor(name, list(shape), dt))
            wb = sb1("wb", [128, 16, 1680], BF16); t_wb = Trk()
            cw = sb1("cw", [128, 4, 4]); t_cw = Trk()
            qkg = sb1("qkg", [128, 4]); t_qkg = Trk()
            mng = sb1("mng", [128, 256]); t_mng = Trk()
            mngr = sb1("mngr", [1, 256]); t_mngr = Trk()
            kb.dma(cw[:], convw, writes=[t_cw])
            kb.dma(qkg[:], qk_g, writes=[t_qkg])
            kb.dma(mngr[:], mng_row, writes=[t_mngr])
            kb.op("pe", lambda e: e.matmul(ps[3][:, 0:256], lhsT=ones_f[0:1, :], rhs=mngr[0:1, :], start=True, stop=True),
                  reads=[t_ones_f, t_mngr], writes=[pst[3]])
            kb.op("dve", lambda e: e.tensor_copy(out=mng[:], in_=ps[3][:, 0:256]), reads=[pst[3]], writes=[t_mng])
            wst = [sb1("wst%d" % i, [128, 1680]) for i in range(2)]; t_wst = [Trk(), Trk()]
            wiv = w_in.rearrange("(k p) n -> p k n", p=128)
            for k in range(16):
                bi = k % 2
                kb.dma(wst[bi][:], wiv[:, k, :], writes=[t_wst[bi]])
                kb.op("dve", lambda e, k=k, bi=bi: e.tensor_copy(out=wb[:, k, :], in_=wst[bi][:]),
                      reads=[t_wst[bi]], writes=[t_wb])
            xt = [sb1("xt%d" % i, [128, 16, 512]) for i in range(2)]; t_xt = [Trk(), Trk()]
            xsq = sb1("xsq", [128, 16, 512], BF16); t_xsq = [Trk() for _ in range(16)]
            hT = sb1("hT", [128, 16, 512], BF16); t_hT = [Trk() for _ in range(16)]
            rstd = sb1("rstd", [128, 512]); t_rstd = Trk()
            tmp = [sb1("tmp%d" % i, [128, 512]) for i in range(2)]; t_tmp = [Trk(), Trk()]
            zr = [sb1("zr%d" % i, [128, 3 + 512]) for i in range(4)]; t_zr = [Trk() for _ in range(4)]
            acc = [sb1("acc%d" % i, [128, 512]) for i in range(2)]; t_acc = [Trk(), Trk()]
            ofm = [sb1("ofm%d" % i, [128, 512]) for i in range(2)]; t_ofm = [Trk(), Trk()]
            obf = [sb1("obf%d" % i, [128, 512], BF16) for i in range(2)]; t_obf = [Trk(), Trk()]
            sqb = sb1("sqb", [128, 512], BF16); t_sqb = Trk()
            rs2 = sb1("rs2", [128, 512]); t_rs2 = Trk()
            otm = [sb1("otm%d" % i, [128, 512]) for i in range(2)]; t_otm = [Trk(), Trk()]
            otb = [sb1("otb%d" % i, [128, 128], BF16) for i in range(2)]; t_otb = [Trk(), Trk()]
            otg = [sb1("otg%d" % i, [128, 12]) for i in range(2)]; t_otg = [Trk(), Trk()]
            for i in range(4):
                kb.op("dve", lambda e, i=i: e.memset(zr[i][:, 0:3], 0.0), writes=[t_zr[i]])
            xv = xT.rearrange("(k p) t -> p k t", p=128)
            cnt2 = [0]

            def rot2():
                cnt2[0] += 1
                return cnt2[0] % 2

            def load_x(n):
                bi = n % 2
                for h in range(4):
                    kb.dma(xt[bi][:, 4 * h:4 * h + 4, :], xv[:, 4 * h:4 * h + 4, n * 512:(n + 1) * 512], writes=[t_xt[bi]])

            load_x(0)
            for n in range(nb):
                bi = n % 2
                t0 = n * 512
                if n + 1 < nb:
                    load_x(n + 1)
                for k in range(16):
                    kb.op("act" if k % 2 else "dve",
                          (lambda e, k=k, bi=bi: e.activation(out=xsq[:, k, :], in_=xt[bi][:, k, :], func=AF.Square)) if k % 2 else
                          (lambda e, k=k, bi=bi: e.tensor_tensor(out=xsq[:, k, :], in0=xt[bi][:, k, :], in1=xt[bi][:, k, :], op=ALU.mult)),
                          reads=[t_xt[bi]], writes=[t_xsq[k]])
                for k in range(16):
                    kb.op("pe", lambda e, k=k: e.matmul(ps[0][:, :], lhsT=ones_b[:], rhs=xsq[:, k, :], start=(k == 0), stop=(k == 15)),
                          reads=[t_ones_b, t_xsq[k]], writes=[pst[0]], sig=(k == 15))
                kb.op("act", lambda e: e.activation(out=rstd[:], in_=ps[0][:, :], func=AF.Sqrt, scale=1.0 / D, bias=EPS),
                      reads=[pst[0]], writes=[t_rstd])
                kb.op("dve", lambda e: e.reciprocal(out=rstd[:], in_=rstd[:]), reads=[t_rstd], writes=[t_rstd])
                for k in range(16):
                    tb = k % 2
                    kb.op("dve", lambda e, k=k, tb=tb, bi=bi: e.tensor_tensor(out=tmp[tb][:], in0=xt[bi][:, k, :], in1=rstd[:], op=ALU.mult),
                          reads=[t_xt[bi], t_rstd], writes=[t_tmp[tb]])
                    kb.op("act", lambda e, k=k, tb=tb: e.activation(out=hT[:, k, :], in_=tmp[tb][:], func=AF.Identity,
                                                                    scale=gs1[:, k:k + 1], bias=sh1[:, k:k + 1]),
                          reads=[t_tmp[tb], t_gs1, t_sh1], writes=[t_hT[k]])
                for ct in range(9):
                    if not ({0: 'a', 1: 'a', 2: 'a', 3: 'a', 4: 'b', 5: 'b', 7: 'b', 6: 'c', 8: 'd'}[ct] in parts):
                        continue
                    c0 = ct * 128
                    cn = 128 if ct < 8 else 2
                    pb = (1, 2, 0)[ct % 3]
                    for k in range(16):
                        kb.op("pe", lambda e, k=k, c0=c0, cn=cn, pb=pb: e.matmul(ps[pb][0:cn, :], lhsT=wb[:, k, c0:c0 + cn], rhs=hT[:, k, :],
                                                                                 start=(k == 0), stop=(k == 15)),
                              reads=[t_wb, t_hT[k]], writes=[pst[pb]], sig=(k == 15))
                    if ct < 4:
                        z = zr[ct]; tz = t_zr[ct]
                        kb.op("act", lambda e, z=z, pb=pb: e.activation(out=z[:, 3:515], in_=ps[pb][:, :], func=AF.Copy),
                              reads=[pst[pb]], writes=[tz])
                        ab = rot2()
                        a = acc[ab]; ta = t_acc[ab]
                        kb.op("dve", lambda e, z=z, a=a, ct=ct: e.tensor_scalar(out=a[:], in0=z[:, 3:515], scalar1=cw[:, ct, 3:4], scalar2=None,
                                                                                 op0=ALU.mult), reads=[tz, t_cw], writes=[ta])
                        for j in range(3):
                            kb.op("dve", lambda e, z=z, a=a, ct=ct, j=j: e.scalar_tensor_tensor(out=a[:], in0=z[:, j:j + 512], scalar=cw[:, ct, j:j + 1],
                                                                                               in1=a[:], op0=ALU.mult, op1=ALU.add),
                                  reads=[tz, t_cw, ta], writes=[ta])
                        ob = rot2()
                        kb.op("act", lambda e, a=a, ob=ob: e.activation(out=ofm[ob][:], in_=a[:], func=AF.Silu), reads=[ta], writes=[t_ofm[ob]])
                        dst = s_q if ct < 2 else s_k
                        r0 = (ct % 2) * 128
                        kb.dma(dst[r0:r0 + 128, t0:t0 + 512], ofm[ob][:], reads=[t_ofm[ob]], writes=[t_scr["q" if ct < 2 else "k"]])
                        kb.op("dve", lambda e, z=z: e.tensor_copy(out=z[:, 0:3], in_=z[:, 512:515]), reads=[tz], writes=[tz])
                    elif ct in (4, 5, 7):
                        kb.op("act", lambda e, pb=pb: e.activation(out=sqb[:], in_=ps[pb][:, :], func=AF.Square), reads=[pst[pb]], writes=[t_sqb])
                        kb.op("pe", lambda e: e.matmul(ps[3][:, :], lhsT=blk64[:], rhs=sqb[:], start=True, stop=True),
                              reads=[t_blk64, t_sqb], writes=[pst[3]])
                        kb.op("act", lambda e: e.activation(out=rs2[:], in_=ps[3][:, :], func=AF.Sqrt, scale=1.0 / 64, bias=EPS),
                              reads=[pst[3]], writes=[t_rs2])
                        kb.op("dve", lambda e: e.reciprocal(out=rs2[:], in_=rs2[:]), reads=[t_rs2], writes=[t_rs2])
                        ob = rot2()
                        gcol = 0 if ct in (4, 5) else 3
                        kb.op("dve", lambda e, pb=pb, ob=ob, gcol=gcol: e.scalar_tensor_tensor(out=obf[ob][:], in0=ps[pb][:, :], scalar=qkg[:, gcol:gcol + 1],
                                                                                              in1=rs2[:], op0=ALU.mult, op1=ALU.mult),
                              reads=[pst[pb], t_rs2, t_qkg], writes=[t_obf[ob]])
                        if ct == 7:
                            kb.dma(s_kk[:, t0:t0 + 512], obf[ob][:], reads=[t_obf[ob]], writes=[t_scr["kk"]])
                        else:
                            r0 = (ct - 4) * 128
                            kb.dma(s_aq[r0:r0 + 128, t0:t0 + 512], obf[ob][:], reads=[t_obf[ob]], writes=[t_scr["aq"]])
                    elif ct == 6:
                        ob = rot2()
                        kb.op("act", lambda e, pb=pb, ob=ob: e.activation(out=ofm[ob][:], in_=ps[pb][:, :], func=AF.Copy), reads=[pst[pb]], writes=[t_ofm[ob]])
                        kb.dma(s_kv[:, t0:t0 + 512], ofm[ob][:], reads=[t_ofm[ob]], writes=[t_scr["kv"]])
                    else:
                        ob = rot2()
                        kb.op("act", lambda e, pb=pb, ob=ob: e.activation(out=ofm[ob][0:2, :], in_=ps[pb][0:2, :], func=AF.Copy), reads=[pst[pb]], writes=[t_ofm[ob]])
                        kb.dma(s_if[:, t0:t0 + 512], ofm[ob][0:2, :], reads=[t_ofm[ob]], writes=[t_scr["if"]])
                for ts in range(int(os.environ.get('NTS', '4')) if 'e' in parts else 0):
                    tt = t0 + ts * 128
                    pb = 4 + (ts % 2)
                    for k in range(16):
                        kb.op("pe", lambda e, k=k, ts=ts, pb=pb: e.matmul(ps[pb][:, :], lhsT=hT[:, k, ts * 128:(ts + 1) * 128], rhs=wb[:, k, 1026:1538],
                                                                          start=(k == 0), stop=(k == 15)),
                              reads=[t_wb, t_hT[k]], writes=[pst[pb]], sig=(k == 15))
                    pb2 = 6 + (ts % 2)
                    for k in range(16):
                        kb.op("pe", lambda e, k=k, ts=ts, pb2=pb2: e.matmul(ps[pb2][:, 0:140], lhsT=hT[:, k, ts * 128:(ts + 1) * 128], rhs=wb[:, k, 1538:1678],
                                                                            start=(k == 0), stop=(k == 15)),
                              reads=[t_wb, t_hT[k]], writes=[pst[pb2]], sig=(k == 15))
                    ob = ts % 2
                    kb.op("dve", lambda e, pb=pb, ob=ob: e.tensor_copy(out=otm[ob][:, 0:256], in_=ps[pb][:, 0:256]), reads=[pst[pb]], writes=[t_otm[ob]])
                    kb.op("act", lambda e, pb=pb, ob=ob: e.activation(out=otm[ob][:, 256:512], in_=ps[pb][:, 256:512], func=AF.Sigmoid),
                          reads=[pst[pb]], writes=[t_otm[ob]])
                    SK = os.environ.get('SKIP', '')
                    if 'P' not in SK:
                        kb.op("dve", lambda e, ob=ob: e.tensor_tensor(out=otm[ob][:, 256:512], in0=otm[ob][:, 256:512], in1=mng[:], op=ALU.mult),
                              reads=[t_otm[ob], t_mng], writes=[t_otm[ob]])
                    if 'S' not in SK:
                        kb.dma(s_v[tt:tt + 128, :], otm[ob][:, 0:256], reads=[t_otm[ob]], writes=[t_scr["v"]])
                        kb.dma(s_go[tt:tt + 128, :], otm[ob][:, 256:512], reads=[t_otm[ob]], writes=[t_scr["go"]])
                    kb.op("dve", lambda e, pb2=pb2, ob=ob: e.tensor_copy(out=otb[ob][:], in_=ps[pb2][:, 0:128]), reads=[pst[pb2]], writes=[t_otb[ob]])
                    kb.op("act", lambda e, pb2=pb2, ob=ob: e.activation(out=otg[ob][:], in_=ps[pb2][:, 128:140], func=AF.Sigmoid),
                          reads=[pst[pb2]], writes=[t_otg[ob]])
                    if 'V' not in os.environ.get('SKIP', ''):
                        kb.dma(s_vv[tt:tt + 128, :], otb[ob][:], reads=[t_otb[ob]], writes=[t_scr["vv"]])
                    if 'G' not in os.environ.get('SKIP', ''):
                        kb.dma(s_gt[tt:tt + 128, :], otg[ob][:], reads=[t_otg[ob]], writes=[t_scr["gt"]])
            kb.barrier()


    mixT = [nc.dram_tensor("mixT%d" % i, [512, 1024], BF16, kind="Internal").ap() for i in range(8)]; t_mixT = [Trk() for _ in range(8)]
    mixG = [nc.dram_tensor("mixG%d" % i, [2048, 1024], BF16, kind="Internal").ap() for i in range(8)]; t_mixG = [Trk() for _ in range(8)]

    def gather_chunk(i):
        if phases >= 4:
            kb.op("pool", lambda e: e.collective_compute("AllGather", ALU.bypass, replica_groups=[[0, 1, 2, 3], [4, 5, 6, 7]],
                                                         ins=[mixT[i]], outs=[mixG[i]]), reads=[t_mixT[i]], writes=[t_mixG[i]])
    if phases >= 2:
        bif_in = din("bif", [128, 2])
        ut_in = din("ut_f", [128, 128])
        identb_in = din("ident_b", [128, 128], BF16)
        with ExitStack() as p2:
            def sb2(name, shape, dt=F32):
                return p2.enter_context(nc.sbuf_tensor(name, list(shape), dt))
            NCH = 64
            bif = sb2("bif_sb", [128, 2]); t_bif = Trk()
            ut = sb2("ut_sb", [128, 128]); t_ut = Trk()
            identb = sb2("identb", [128, 128], BF16); t_identb = Trk()
            kb.dma(bif[:], bif_in, writes=[t_bif])
            kb.dma(ut[:], ut_in, writes=[t_ut])
            kb.dma(identb[:], identb_in, writes=[t_identb])
            rows = sb2("rows", [64, 2, 128]); t_rows = Trk()
            kb.dma(rows[:, 0, :], s_if[0:1, :].rearrange("o (c p) -> (o c) p", p=128), reads=[t_scr["if"]], writes=[t_rows])
            kb.dma(rows[:, 1, :], s_if[1:2, :].rearrange("o (c p) -> (o c) p", p=128), reads=[t_scr["if"]], writes=[t_rows])
            cols = sb2("cols", [128, 12, 64]); t_cols = Trk()
            nbf = sb2("nbf", [128, 1]); t_nbf = Trk()
            zero64 = sb2("zero64", [128, 128]); t_zero = Trk()
            kb.op("dve", lambda e: e.memset(zero64[:], 0.0), writes=[t_zero])
            kb.op("dve", lambda e: e.tensor_scalar(out=nbf[:], in0=bif[:, 1:2], scalar1=-1.0, scalar2=None, op0=ALU.mult), reads=[t_bif], writes=[t_nbf])
            for w in range(2):
                kb.op("pe", lambda e, w=w: e.matmul(ps[w][:, 0:64], lhsT=rows[:, w, :], rhs=ident[0:64, 0:64], start=True, stop=True),
                      reads=[t_rows, t_ident], writes=[pst[w]])
            kb.op("dve", lambda e: e.tensor_scalar(out=cols[:, 0, :], in0=ps[0][:, 0:64], scalar1=bif[:, 0:1], scalar2=None, op0=ALU.add),
                  reads=[pst[0], t_bif], writes=[t_cols])
            kb.op("act", lambda e: e.activation(out=cols[:, 11, :], in_=ps[1][:, 0:64], func=AF.Exp, scale=-1.0, bias=nbf[:, 0:1]),
                  reads=[pst[1], t_nbf], writes=[t_cols])
            kb.op("act", lambda e: e.activation(out=cols[:, 1, :], in_=cols[:, 11, :], func=AF.Ln, scale=1.0, bias=1.0), reads=[t_cols], writes=[t_cols])
            kb.op("dve", lambda e: e.tensor_scalar(out=cols[:, 1, :], in0=cols[:, 1, :], scalar1=-1.0, scalar2=None, op0=ALU.mult), reads=[t_cols], writes=[t_cols])
            kb.op("pe", lambda e: e.matmul(ps[2][:, 0:64], lhsT=ut[:], rhs=cols[:, 1, :], start=True, stop=True), reads=[t_ut, t_cols], writes=[pst[2]])
            kb.op("pe", lambda e: e.matmul(ps[3][:, 0:64], lhsT=ones_f[:], rhs=cols[:, 1, :], start=True, stop=True), reads=[t_ones_f, t_cols], writes=[pst[3]])
            kb.op("dve", lambda e: e.tensor_copy(out=cols[:, 11, :], in_=ps[3][:, 0:64]), reads=[pst[3]], writes=[t_cols])
            kb.op("dve", lambda e: e.tensor_tensor_scan(out=cols[:, 2, :], data0=cols[:, 11, :], data1=zero64[:, 0:64], initial=0.0, op0=ALU.add, op1=ALU.add),
                  reads=[t_cols, t_zero], writes=[t_cols])
            kb.op("dve", lambda e: e.tensor_tensor(out=cols[:, 2, :], in0=cols[:, 2, :], in1=cols[:, 11, :], op=ALU.subtract), reads=[t_cols], writes=[t_cols])
            kb.op("dve", lambda e: e.tensor_tensor(out=cols[:, 2, :], in0=cols[:, 2, :], in1=ps[2][:, 0:64], op=ALU.add), reads=[t_cols, pst[2]], writes=[t_cols])
            kb.op("dve", lambda e: e.tensor_tensor(out=cols[:, 3, :], in0=cols[:, 0, :], in1=cols[:, 2, :], op=ALU.subtract), reads=[t_cols], writes=[t_cols])
            grow = sb2("grow", [64, 128]); t_grow = Trk()
            cmrow = sb2("cmrow", [64, 128]); t_cmrow = Trk()
            kb.op("pe", lambda e: e.matmul(ps[4][0:64, 0:128], lhsT=cols[:, 3, :], rhs=ident[:, :], start=True, stop=True), reads=[t_cols, t_ident], writes=[pst[4]])
            kb.op("dve", lambda e: e.tensor_copy(out=grow[:], in_=ps[4][0:64, 0:128]), reads=[pst[4]], writes=[t_grow])
            kb.op("dve", lambda e: e.tensor_tensor_scan(out=cmrow[:], data0=grow[:], data1=grow[:], initial=-1e30, op0=ALU.max, op1=ALU.max),
                  reads=[t_grow], writes=[t_cmrow])
            mrow = sb2("mrow", [1, 3, 64]); t_mrow = Trk()
            kb.op("pe", lambda e: e.matmul(ps[5][0:1, 0:64], lhsT=cmrow[:, 127:128], rhs=ident[0:64, 0:64], start=True, stop=True),
                  reads=[t_cmrow, t_ident], writes=[pst[5]])
            kb.op("dve", lambda e: e.tensor_copy(out=mrow[0:1, 0, :], in_=ps[5][0:1, 0:64]), reads=[pst[5]], writes=[t_mrow])
            kb.op("dve", lambda e: e.tensor_tensor_scan(out=mrow[0:1, 1, :], data0=mrow[0:1, 0, :], data1=mrow[0:1, 0, :], initial=0.0, op0=ALU.max, op1=ALU.max),
                  reads=[t_mrow], writes=[t_mrow])
            kb.op("dve", lambda e: e.memset(mrow[0:1, 2, 0:1], 0.0), reads=[t_mrow], writes=[t_mrow])
            kb.op("dve", lambda e: e.tensor_copy(out=mrow[0:1, 2, 1:64], in_=mrow[0:1, 1, 0:63]), reads=[t_mrow], writes=[t_mrow])
            kb.op("pe", lambda e: e.matmul(ps[6][:, 0:128], lhsT=ones_f[0:1, :], rhs=mrow[0:1, 1:3, :].rearrange("o a c -> o (a c)"), start=True, stop=True),
                  reads=[t_ones_f, t_mrow], writes=[pst[6]])
            kb.op("dve", lambda e: e.tensor_copy(out=cols[:, 6, :], in_=ps[6][:, 0:64]), reads=[pst[6]], writes=[t_cols])
            kb.op("dve", lambda e: e.tensor_copy(out=cols[:, 5, :], in_=ps[6][:, 64:128]), reads=[pst[6]], writes=[t_cols])
            kb.op("pe", lambda e: e.matmul(ps[7][:, 0:64], lhsT=cmrow[:, :], rhs=ident[0:64, 0:64], start=True, stop=True), reads=[t_cmrow, t_ident], writes=[pst[7]])
            kb.op("dve", lambda e: e.tensor_tensor(out=cols[:, 4, :], in0=ps[7][:, 0:64], in1=cols[:, 5, :], op=ALU.max), reads=[pst[7], t_cols], writes=[t_cols])
            kb.op("dve", lambda e: e.tensor_tensor(out=cols[:, 11, :], in0=cols[:, 3, :], in1=cols[:, 5, :], op=ALU.subtract), reads=[t_cols], writes=[t_cols])
            kb.op("act", lambda e: e.activation(out=cols[:, 7, :], in_=cols[:, 11, :], func=AF.Exp), reads=[t_cols], writes=[t_cols])
            kb.op("dve", lambda e: e.tensor_scalar(out=cols[:, 7, :], in0=cols[:, 7, :], scalar1=1.0 / 16, scalar2=None, op0=ALU.mult), reads=[t_cols], writes=[t_cols])
            kb.op("dve", lambda e: e.tensor_tensor(out=cols[:, 11, :], in0=cols[:, 5, :], in1=cols[:, 4, :], op=ALU.subtract), reads=[t_cols], writes=[t_cols])
            kb.op("act", lambda e: e.activation(out=cols[:, 8, :], in_=cols[:, 11, :], func=AF.Exp), reads=[t_cols], writes=[t_cols])
            kb.op("dve", lambda e: e.tensor_tensor(out=cols[:, 11, :], in0=cols[:, 2, :], in1=cols[:, 4, :], op=ALU.add), reads=[t_cols], writes=[t_cols])
            kb.op("act", lambda e: e.activation(out=cols[:, 9, :], in_=cols[:, 11, :], func=AF.Exp, scale=-1.0), reads=[t_cols], writes=[t_cols])
            kb.op("dve", lambda e: e.tensor_tensor(out=cols[:, 11, :], in0=cols[:, 5, :], in1=cols[:, 6, :], op=ALU.subtract), reads=[t_cols], writes=[t_cols])
            kb.op("act", lambda e: e.activation(out=cols[:, 10, :], in_=cols[:, 11, :], func=AF.Exp), reads=[t_cols], writes=[t_cols])
            if dbg:
                d_cols = nc.dram_tensor("d_cols", [128, 12, 64], F32, kind="ExternalOutput").ap()
                kb.dma(d_cols, cols[:], reads=[t_cols])

            qT = [sb2("qT%d" % i, [128, 2, 128]) for i in range(2)]; t_qT = [Trk(), Trk()]
            kT = [sb2("kT%d" % i, [128, 2, 128]) for i in range(2)]; t_kT = [Trk(), Trk()]
            va = [sb2("va%d" % i, [128, 257]) for i in range(2)]; t_va = [Trk(), Trk()]
            go = [sb2("go%d" % i, [128, 256]) for i in range(2)]; t_go = [Trk(), Trk()]
            kp = sb2("kp", [128, 256]); t_kp = Trk()
            wT = sb2("wT", [128, 128]); t_wT = Trk()
            St = [sb2("St%d" % i, [128, 2, 257]) for i in range(2)]; t_St = [Trk(), Trk()]
            sc = sb2("sc", [128, 8]); t_sc = Trk()
            junk = sb2("junk", [128, 256]); t_junk = Trk()
            hmf = sb2("hmf", [128, 256], BF16); t_hmf = Trk()
            hmT = [sb2("hmT%d" % i, [128, 2, 128], BF16) for i in range(2)]; t_hmT = [Trk(), Trk()]
            for i in range(2):
                kb.op("dve", lambda e, i=i: e.memset(va[i][:, 256:257], 1.0), writes=[t_va[i]])
            kb.op("dve", lambda e: e.memset(St[0][:], 0.0), writes=[t_St[0]])
            qv = s_q.rearrange("(a p) t -> p a t", p=128)
            kv_ = s_k.rearrange("(a p) t -> p a t", p=128)

            def load_chunk(c):
                bi = c % 2
                kb.dma(qT[bi][:], qv[:, :, c * 128:(c + 1) * 128], reads=[t_scr["q"]], writes=[t_qT[bi]])
                kb.dma(kT[bi][:], kv_[:, :, c * 128:(c + 1) * 128], reads=[t_scr["k"]], writes=[t_kT[bi]])
                kb.dma(va[bi][:, 0:256], s_v[c * 128:(c + 1) * 128, :], reads=[t_scr["v"]], writes=[t_va[bi]])
                kb.dma(go[bi][:], s_go[c * 128:(c + 1) * 128, :], reads=[t_scr["go"]], writes=[t_go[bi]])

            nch = int(os.environ.get("NCH", "64"))
            load_chunk(0)
            for c in range(nch):
                bi = c % 2
                so = St[c % 2]; tso = t_St[c % 2]
                sn = St[(c + 1) % 2]; tsn = t_St[(c + 1) % 2]
                if c + 1 < nch:
                    load_chunk(c + 1)
                for dc in range(2):
                    kb.op("pe", lambda e, dc=dc, bi=bi: e.matmul(ps[0][:, dc * 128:(dc + 1) * 128], lhsT=kT[bi][:, dc, :], rhs=ident[:, :], start=True, stop=True),
                          reads=[t_kT[bi], t_ident], writes=[pst[0]], sig=(dc == 1))
                kb.op("dve", lambda e, c=c: e.tensor_scalar(out=kp[:], in0=ps[0][:, 0:256], scalar1=cols[:, 7, c:c + 1], scalar2=None, op0=ALU.mult),
                      reads=[pst[0], t_cols], writes=[t_kp])
                for dc in range(2):
                    kb.op("pe", lambda e, dc=dc, bi=bi: e.matmul(ps[1][:, 0:128], lhsT=kT[bi][:, dc, :], rhs=qT[bi][:, dc, :], start=(dc == 0), stop=(dc == 1)),
                          reads=[t_kT[bi], t_qT[bi]], writes=[pst[1]], sig=(dc == 1))
                kb.op("dve", lambda e, c=c: e.scalar_tensor_tensor(out=wT[:], in0=ps[1][:, 0:128], scalar=cols[:, 7, c:c + 1], in1=ut[:], op0=ALU.mult, op1=ALU.mult),
                      reads=[pst[1], t_cols, t_ut], writes=[t_wT])
                kb.op("pe", lambda e, bi=bi: e.matmul(ps[2][:, 0:257], lhsT=wT[:], rhs=va[bi][:], start=True, stop=False),
                      reads=[t_wT, t_va[bi]], writes=[pst[2]], sig=False)
                for dc in range(2):
                    kb.op("pe", lambda e, dc=dc, bi=bi, so=so: e.matmul(ps[2][:, 0:257], lhsT=qT[bi][:, dc, :], rhs=so[:, dc, :], start=False, stop=(dc == 1)),
                          reads=[t_qT[bi], tso], writes=[pst[2]], sig=(dc == 1))
                for dc in range(2):
                    pb = 3 + dc
                    kb.op("pe", lambda e, dc=dc, bi=bi, pb=pb: e.matmul(ps[pb][:, 0:257], lhsT=kp[:, dc * 128:(dc + 1) * 128], rhs=va[bi][:], start=True, stop=False),
                          reads=[t_kp, t_va[bi]], writes=[pst[pb]], sig=False)
                    kb.op("pe", lambda e, dc=dc, pb=pb, so=so: e.matmul(ps[pb][:, 0:257], lhsT=ident[:, :], rhs=so[:, dc, :], start=False, stop=True),
                          reads=[t_ident, tso], writes=[pst[pb]])
                    kb.op("act" if dc else "dve",
                          (lambda e, dc=dc, pb=pb, sn=sn, c=c: e.activation(out=sn[:, dc, :], in_=ps[pb][:, 0:257], func=AF.Copy, scale=cols[:, 10, c:c + 1])) if dc else
                          (lambda e, dc=dc, pb=pb, sn=sn, c=c: e.tensor_scalar(out=sn[:, dc, :], in0=ps[pb][:, 0:257], scalar1=cols[:, 10, c:c + 1], scalar2=None, op0=ALU.mult)),
                          reads=[pst[pb], t_cols], writes=[tsn])
                kb.op("act", lambda e, c=c: e.activation(out=sc[:, 0:1], in_=ps[2][:, 256:257], func=AF.Abs, scale=cols[:, 8, c:c + 1]),
                      reads=[pst[2], t_cols], writes=[t_sc])
                kb.op("dve", lambda e, c=c: e.tensor_tensor(out=sc[:, 1:2], in0=sc[:, 0:1], in1=cols[:, 9, c:c + 1], op=ALU.max), reads=[t_sc, t_cols], writes=[t_sc])
                kb.op("dve", lambda e: e.reciprocal(out=sc[:, 2:3], in_=sc[:, 1:2]), reads=[t_sc], writes=[t_sc])
                kb.op("dve", lambda e, c=c: e.tensor_tensor(out=sc[:, 3:4], in0=sc[:, 2:3], in1=cols[:, 8, c:c + 1], op=ALU.mult), reads=[t_sc, t_cols], writes=[t_sc])
                kb.op("act", lambda e: e.activation(out=junk[:], in_=ps[2][:, 0:256], func=AF.Square, accum_out=sc[:, 4:5]), reads=[pst[2]], writes=[t_junk, t_sc])
                kb.op("dve", lambda e: e.scalar_tensor_tensor(out=sc[:, 5:6], in0=sc[:, 3:4], scalar=sc[:, 3:4], in1=sc[:, 4:5], op0=ALU.mult, op1=ALU.mult),
                      reads=[t_sc], writes=[t_sc])
                kb.op("act", lambda e: e.activation(out=sc[:, 5:6], in_=sc[:, 5:6], func=AF.Sqrt, scale=1.0 / 256, bias=EPS), reads=[t_sc], writes=[t_sc])
                kb.op("dve", lambda e: e.reciprocal(out=sc[:, 6:7], in_=sc[:, 5:6]), reads=[t_sc], writes=[t_sc])
                kb.op("dve", lambda e: e.tensor_tensor(out=sc[:, 7:8], in0=sc[:, 6:7], in1=sc[:, 3:4], op=ALU.mult), reads=[t_sc], writes=[t_sc])
                kb.op("dve", lambda e, bi=bi: e.scalar_tensor_tensor(out=hmf[:], in0=ps[2][:, 0:256], scalar=sc[:, 7:8], in1=go[bi][:], op0=ALU.mult, op1=ALU.mult),
                      reads=[pst[2], t_sc, t_go[bi]], writes=[t_hmf])
                ob = c % 2
                for dc in range(2):
                    kb.op("pe", lambda e, dc=dc: e.matmul(ps[5][:, dc * 128:(dc + 1) * 128], lhsT=hmf[:, dc * 128:(dc + 1) * 128], rhs=identb[:, :], start=True, stop=True),
                          reads=[t_hmf, t_identb], writes=[pst[5]], sig=(dc == 1))
                kb.op("act", lambda e, ob=ob: e.activation(out=hmT[ob][:].rearrange("p a t -> p (a t)"), in_=ps[5][:, 0:256], func=AF.Copy), reads=[pst[5]], writes=[t_hmT[ob]])
                kb.dma(mixT[c // 8][0:256, (c % 8) * 128:(c % 8 + 1) * 128].rearrange("(a p) t -> p a t", p=128), hmT[ob][:], reads=[t_hmT[ob]], writes=[t_mixT[c // 8]])
            kb.barrier()

    if phases >= 3:
        tb_sw_in = din("tb_sw", [128, 64, 4])
        tb_c_in = din("tb_c", [128, 64, 4])
        cmask_in = din("cmask", [128, 17, 128], BF16)
        caus_in = din("caus_add", [128, 128], BF16)
        acaus_in = din("acaus_add", [128, 128], BF16)
        ovl_in = din("ovl", [128, 4, 128], BF16)
        expT_in = din("expT", [128, S], BF16)
        tkeep_in = din("t_keep", [128, 255])
        tadd_in = din("t_add", [128, 255])
        w1k_in = din("w1k", [2048, 256]); w1v_in = din("w1v", [2048, 256])
        w2k_in = din("w2k", [256, 64]); w2v_in = din("w2v", [256, 64])
        posk_in = din("posk", [128, 16]); posv_in = din("posv", [128, 16])
        kng0_in = din("kng0", [64, 1])
        with ExitStack() as p3:
            def sb3(name, shape, dt=F32):
                return p3.enter_context(nc.sbuf_tensor(name, list(shape), dt))
            QT = sb3("QT", [128, 4, S], BF16); t_QT = Trk()
            KK = sb3("KK", [128, S], BF16); t_KK = Trk()
            Vs = sb3("Vs", [128, 64, 65], BF16); t_Vs = Trk()
            Vw = sb3("Vw", [128, 64, 65], BF16); t_Vw = Trk()
            expT = sb3("expT_sb", [128, S], BF16); t_expT = Trk()
            tb_sw = sb3("tb_sw_sb", [128, 64, 4]); t_tbsw = Trk()
            tb_c = sb3("tb_c_sb", [128, 64, 4]); t_tbc = Trk()
            cmask = sb3("cmask_sb", [128, 17, 128], BF16); t_cmask = Trk()
            caus = sb3("caus_sb", [128, 4, 128], BF16); t_caus = Trk()
            acaus = sb3("acaus_sb", [128, 4, 128], BF16); t_acaus = Trk()
            ovl = sb3("ovl_sb", [128, 4, 128], BF16); t_ovl = Trk()
            tkeep = sb3("tkeep_sb", [128, 255]); t_tkeep = Trk()
            tadd = sb3("tadd_sb", [128, 255]); t_tadd = Trk()
            gts = sb3("gts", [128, 64, 12]); t_gts = Trk()
            kcT = sb3("kcT", [64, 512], BF16); t_kcT = Trk()
            vca = sb3("vca", [128, 4, 65], BF16); t_vca = Trk()
            identb3 = sb3("identb3", [128, 128], BF16); t_identb3 = Trk()
            aqv = s_aq.rearrange("(h d) t -> d h t", d=64)
            for hh in range(2):
                for h in range(4):
                    kb.dma(QT[hh * 64:(hh + 1) * 64, h, :], aqv[:, h, :], reads=[t_scr["aq"]], writes=[t_QT])
            kb.dma(KK[:, 0:4096], s_kk[:, 0:4096], reads=[t_scr["kk"]], writes=[t_KK])
            kb.dma(KK[:, 4096:S], s_kk[:, 4096:S], reads=[t_scr["kk"]], writes=[t_KK])
            kb.dma(expT[:], expT_in, writes=[t_expT])
            kb.dma(tb_sw[:], tb_sw_in, writes=[t_tbsw]); kb.dma(tb_c[:], tb_c_in, writes=[t_tbc])
            kb.dma(cmask[:], cmask_in, writes=[t_cmask])
            for h4 in range(4):
                kb.dma(caus[:, h4, :], caus_in, writes=[t_caus]); kb.dma(acaus[:, h4, :], acaus_in, writes=[t_acaus])
            kb.dma(ovl[:], ovl_in, writes=[t_ovl]); kb.dma(tkeep[:], tkeep_in, writes=[t_tkeep]); kb.dma(tadd[:], tadd_in, writes=[t_tadd])
            kb.dma(identb3[:], identb_in if phases >= 2 else din("ident_b", [128, 128], BF16), writes=[t_identb3])
            kb.dma(gts[:], s_gt.rearrange("(q p) c -> p q c", p=128), reads=[t_scr["gt"]], writes=[t_gts])
            with ExitStack() as p3a:
                def sb3a(name, shape, dt=F32):
                    return p3a.enter_context(nc.sbuf_tensor(name, list(shape), dt))
                vtmp = sb3a("vtmp", [128, 64, 128], BF16); t_vtmp = Trk()
                kb.dma(vtmp[:], s_vv.rearrange("(q p) c -> p q c", p=128), reads=[t_scr["vv"]], writes=[t_vtmp])
                kb.op("dve", lambda e: e.tensor_copy(out=Vs[:, :, 0:64], in_=vtmp[:, :, 0:64]), reads=[t_vtmp], writes=[t_Vs])
                kb.op("dve", lambda e: e.tensor_copy(out=Vw[:, :, 0:64], in_=vtmp[:, :, 64:128]), reads=[t_vtmp], writes=[t_Vw])
                kb.op("dve", lambda e: e.memset(Vs[:, :, 64:65], 1.0), writes=[t_Vs])
                kb.op("dve", lambda e: e.memset(Vw[:, :, 64:65], 1.0), writes=[t_Vw])
                kb.op("dve", lambda e: e.memset(vca[:, :, 64:65], 1.0), writes=[t_vca])
                w1f = sb3a("w1f", [128, 16, 256]); t_w1f = Trk()
                w1b = sb3a("w1b", [128, 16, 256], BF16); t_w1b = Trk()
                w2f = sb3a("w2f", [128, 2, 64]); t_w2f = Trk()
                w2b = sb3a("w2b", [128, 2, 64], BF16); t_w2b = Trk()
                posf = sb3a("posf", [128, 16]); t_posf = Trk()
                bcol = sb3a("bcol", [128, 2]); t_bcol = Trk()
                kng0 = sb3a("kng0_sb", [64, 1]); t_kng0 = Trk()
                a2f = sb3a("a2f", [128, 2048]); t_a2f = Trk()
                a2b = sb3a("a2b", [128, S], BF16); t_a2b = Trk()
                hid = sb3a("hid", [128, 2, 512], BF16); t_hid = Trk()
                csq = sb3a("csq", [64, 512], BF16); t_csq = Trk()
                crs = sb3a("crs", [64, 512]); t_crs = Trk()
                kb.dma(kng0[:], kng0_in, writes=[t_kng0])
                kb.op("dve", lambda e: e.memset(hid[:], 0.0), writes=[t_hid])
                kb.op("dve", lambda e: e.memset(kcT[:], 0.0), writes=[t_kcT])
                for which in range(2):
                    w1_in = w1k_in if which == 0 else w1v_in
                    w2_in = w2k_in if which == 0 else w2v_in
                    pos_in = posk_in if which == 0 else posv_in
                    r0 = 0 if which == 0 else 64
                    kb.dma(w1f[:, 0:8, :], w1_in.rearrange("(k p) n -> p k n", p=128)[:, 0:8, :], writes=[t_w1f])
                    kb.dma(w1f[:, 8:16, :], w1_in.rearrange("(k p) n -> p k n", p=128)[:, 8:16, :], writes=[t_w1f])
                    kb.dma(w2f[:], w2_in.rearrange("(k p) n -> p k n", p=128), writes=[t_w2f])
                    kb.dma(posf[:], pos_in, writes=[t_posf])
                    kb.op("dve", lambda e: e.tensor_copy(out=w1b[:], in_=w1f[:]), reads=[t_w1f], writes=[t_w1b])
                    kb.op("dve", lambda e: e.tensor_copy(out=w2b[:], in_=w2f[:]), reads=[t_w2f], writes=[t_w2b])
                    for q4 in range(4):
                        c0 = q4 * 2048
                        kb.dma(a2f[0:64, :], s_kv[r0:r0 + 64, c0:c0 + 2048], reads=[t_scr["kv"]], writes=[t_a2f])
                        if q4 < 3:
                            kb.dma(a2f[64:128, :], s_kv[r0:r0 + 64, c0 + 1:c0 + 2049], reads=[t_scr["kv"]], writes=[t_a2f])
                        else:
                            kb.dma(a2f[64:128, 0:2047], s_kv[r0:r0 + 64, c0 + 1:c0 + 2048], reads=[t_scr["kv"]], writes=[t_a2f])
                        kb.op("dve", lambda e, c0=c0: e.tensor_copy(out=a2b[:, c0:c0 + 2048], in_=a2f[:]), reads=[t_a2f], writes=[t_a2b])
                    for hc in range(2):
                        for jj in range(16):
                            kb.op("pe", lambda e, hc=hc, jj=jj: e.matmul(ps[7][:, hc:hc + 1], lhsT=w1f[:, jj, hc * 128:(hc + 1) * 128], rhs=posf[:, jj:jj + 1],
                                                                          start=(jj == 0), stop=(jj == 15)),
                                  reads=[t_w1f, t_posf], writes=[pst[7]], sig=(jj == 15))
                    kb.op("dve", lambda e: e.tensor_copy(out=bcol[:], in_=ps[7][:, 0:2]), reads=[pst[7]], writes=[t_bcol])
                    a2v = a2b[:].rearrange("p (i s) -> p i s", s=16)
                    for hc in range(2):
                        for jj in range(16):
                            j0 = 2 * jj
                            rhs = a2v[:, 0:511, j0] if j0 < 16 else a2v[:, 1:512, j0 - 16]
                            kb.op("pe", lambda e, hc=hc, jj=jj, rhs=rhs: e.matmul(ps[hc][:, 0:511], lhsT=w1b[:, jj, hc * 128:(hc + 1) * 128], rhs=rhs,
                                                                                  start=(jj == 0), stop=(jj == 15)),
                                  reads=[t_w1b, t_a2b], writes=[pst[hc]], sig=(jj == 15))
                        kb.op("act", lambda e, hc=hc: e.activation(out=hid[:, hc, 0:511], in_=ps[hc][:, 0:511], func=AF.Gelu_apprx_tanh, bias=bcol[:, hc:hc + 1]),
                              reads=[pst[hc], t_bcol], writes=[t_hid])
                    if which == 0:
                        for hc in range(2):
                            kb.op("pe", lambda e, hc=hc: e.matmul(ps[2][0:64, 0:511], lhsT=w2b[:, hc, :], rhs=hid[:, hc, 0:511], start=(hc == 0), stop=(hc == 1)),
                                  reads=[t_w2b, t_hid], writes=[pst[2]], sig=(hc == 1))
                        kb.op("act", lambda e: e.activation(out=csq[:, 0:511], in_=ps[2][0:64, 0:511], func=AF.Square), reads=[pst[2]], writes=[t_csq])
                        kb.op("pe", lambda e: e.matmul(ps[3][0:64, 0:511], lhsT=blk64[0:64, 0:64], rhs=csq[:, 0:511], start=True, stop=True),
                              reads=[t_blk64, t_csq], writes=[pst[3]])
                        kb.op("act", lambda e: e.activation(out=crs[:, 0:511], in_=ps[3][0:64, 0:511], func=AF.Sqrt, scale=1.0 / 64, bias=EPS), reads=[pst[3]], writes=[t_crs])
                        kb.op("dve", lambda e: e.reciprocal(out=crs[:, 0:511], in_=crs[:, 0:511]), reads=[t_crs], writes=[t_crs])
                        kb.op("dve", lambda e: e.scalar_tensor_tensor(out=kcT[:, 0:511], in0=ps[2][0:64, 0:511], scalar=kng0[:, 0:1], in1=crs[:, 0:511],
                                                                      op0=ALU.mult, op1=ALU.mult), reads=[pst[2], t_kng0, t_crs], writes=[t_kcT])
                    else:
                        for it in range(4):
                            for hc in range(2):
                                kb.op("pe", lambda e, hc=hc, it=it: e.matmul(ps[4][:, it * 64:(it + 1) * 64], lhsT=hid[:, hc, it * 128:(it + 1) * 128], rhs=w2b[:, hc, :],
                                                                             start=(hc == 0), stop=(hc == 1)),
                                      reads=[t_hid, t_w2b], writes=[pst[4]], sig=(hc == 1 and it == 3))
                        kb.op("dve", lambda e: e.tensor_copy(out=vca[:, :, 0:64], in_=ps[4][:, 0:256].rearrange("p (a d) -> p a d", d=64)), reads=[pst[4]], writes=[t_vca])
                if dbg:
                    d_kcT = nc.dram_tensor("d_kcT", [64, 512], BF16, kind="ExternalOutput").ap()
                    d_vca = nc.dram_tensor("d_vca", [128, 4, 65], BF16, kind="ExternalOutput").ap()
                    kb.dma(d_kcT, kcT[:], reads=[t_kcT]); kb.dma(d_vca, vca[:], reads=[t_vca])
                kb.barrier()

            pT = [sb3("pT%d" % i, [128, 512], BF16) for i in range(3)]; t_pT = [[Trk() for _ in range(4)] for _ in range(3)]
            oT = sb3("oT", [65, 3, 512]); t_oT = Trk()
            zsc = sb3("zsc", [128, 24]); t_zsc = Trk()
            impn = sb3("impn", [128, 128]); t_impn = Trk()
            impw = sb3("impw", [128, 128]); t_impw = Trk()
            m8 = sb3("m8", [128, 16]); t_m8 = Trk()
            selb = sb3("selb", [128, 128], BF16); t_selb = Trk()
            selT = sb3("selT", [128, 4, 128], BF16); t_selT = Trk()
            haf = sb3("haf", [128, 256]); t_haf = Trk()
            hab = sb3("hab", [128, 256], BF16); t_hab = Trk()
            haT = [sb3("haT%d" % i, [128, 2, 128], BF16) for i in range(2)]; t_haT = [Trk(), Trk()]
            if dbg:
                d_imp = nc.dram_tensor("d_imp", [S, 128], F32, kind="ExternalOutput").ap()
                d_sel = nc.dram_tensor("d_sel", [S, 128], BF16, kind="ExternalOutput").ap()
            pcount = [0]
            NEG = -30000.0

            pend = []

            def flush_pairs():
                while pend:
                    pend.pop(0)()

            def tile_pair(lhsK, rhsQ, masks, bias_tab, bidx, Vaug, obank, first, last, imp_it=None):
                i = pcount[0] % 2
                pi = pcount[0] % 3
                pcount[0] += 1
                sps = ps[i]; tsp = pst[i]
                nm = len(masks)
                kb.op("pe", lambda e: e.matmul(sps[:, :], lhsT=lhsK[0], rhs=rhsQ[0], start=True, stop=(nm == 0)),
                      reads=[lhsK[1], rhsQ[1]], writes=[tsp], sig=(nm == 0))
                for mi, (ml, mr, mt) in enumerate(masks):
                    lastm = (mi == nm - 1)
                    if mr.shape[-1] == 512 or len(mr.shape) == 3:
                        kb.op("pe", lambda e, ml=ml, mr=mr, lastm=lastm: e.matmul(sps[:, :], lhsT=ml, rhs=mr, start=False, stop=lastm),
                              reads=mt, writes=[tsp], sig=lastm)
                    else:
                        for h in range(4):
                            lm = (lastm and h == 3)
                            kb.op("pe", lambda e, ml=ml, mr=mr, h=h, lm=lm: e.matmul(sps[:, h * 128:(h + 1) * 128], lhsT=ml, rhs=mr, start=False, stop=lm),
                                  reads=mt, writes=[tsp], sig=lm)
                for h in range(4):
                    kb.op("act", lambda e, h=h: e.activation(out=pT[pi][:, h * 128:(h + 1) * 128], in_=sps[:, h * 128:(h + 1) * 128], func=AF.Exp,
                                                             bias=bias_tab[0][:, bidx, h:h + 1]),
                          reads=[tsp, bias_tab[1]], writes=[t_pT[pi][h]])

                def stage_b():
                    kb.op("pe", lambda e: e.matmul(ps[obank][0:65, :], lhsT=Vaug[0], rhs=pT[pi][:, :], start=first, stop=last),
                          reads=[Vaug[1]] + t_pT[pi], writes=[pst[obank]], sig=last)
                    if imp_it is not None:
                        it, nit = imp_it
                        for h in range(4):
                            kb.op("pe", lambda e, h=h, it=it: e.matmul(ps[5][:, h * 128:(h + 1) * 128], lhsT=pT[pi][:, h * 128:(h + 1) * 128], rhs=ovl[:, it, :],
                                                                       start=(it == 0), stop=(it == nit - 1)),
                                  reads=[t_pT[pi][h], t_ovl], writes=[pst[5]], sig=(it == nit - 1 and h == 3))
                while len(pend) > 1:
                    pend.pop(0)()
                pend.append(stage_b)
                if len(pend) > 1:
                    pend.pop(0)()

            nqt = int(os.environ.get("NQT", "64"))
            for qt in range(nqt):
                q0 = qt * 128
                qv_lo = (QT[0:64, :, q0:q0 + 128], t_QT)
                qv_hi = (QT[64:128, :, q0:q0 + 128], t_QT)
                nit = (8 * qt + 6) // 128 + 1
                for it in range(nit):
                    m = qt - 16 * it
                    masks = []
                    if m <= 16:
                        masks.append((identb3[:, :], cmask[:, m, :], [t_identb3, t_cmask]))
                    tile_pair((kcT[0:64, it * 128:(it + 1) * 128], t_kcT), qv_lo, masks, (tb_c, t_tbc), qt - 16 * it, (vca[:, it, :], t_vca), 2,
                              it == 0, it == nit - 1, imp_it=(it, nit))
                kts = [kt for kt in range(qt - 4, qt + 1) if kt >= 0]
                for n_, kt in enumerate(kts):
                    masks = []
                    if kt == qt:
                        masks.append((identb3[:, :], caus[:, :, :], [t_identb3, t_caus]))
                    if kt == qt - 4:
                        masks.append((identb3[:, :], acaus[:, :, :], [t_identb3, t_acaus]))
                    tile_pair((KK[64:128, kt * 128:(kt + 1) * 128], t_KK), qv_hi, masks, (tb_sw, t_tbsw), qt - kt, (Vw[:, kt, :], t_Vw), 4,
                              n_ == 0, n_ == len(kts) - 1)
                flush_pairs()
                kb.op("dve", lambda e: e.tensor_copy(out=oT[:, 0, :], in_=ps[2][0:65, :]), reads=[pst[2]], writes=[t_oT])
                for h in range(4):
                    kb.op("pe", lambda e, h=h: e.matmul(ps[6][:, h * 65:(h + 1) * 65], lhsT=oT[0:65, 0, h * 128:(h + 1) * 128], rhs=ident[0:65, 0:65], start=True, stop=True),
                          reads=[t_oT, t_ident], writes=[pst[6]], sig=(h == 3))
                kb.op("dve", lambda e: e.tensor_scalar(out=zsc[:, 0:4], in0=ps[6][:, 0:260].rearrange("p (h c) -> p h c", c=65)[:, :, 64], scalar1=1e-30, scalar2=None, op0=ALU.max),
                      reads=[pst[6]], writes=[t_zsc])
                kb.op("dve", lambda e: e.reciprocal(out=zsc[:, 0:4], in_=zsc[:, 0:4]), reads=[t_zsc], writes=[t_zsc])
                kb.op("dve", lambda e: e.tensor_scalar(out=impn[:], in0=ps[5][:, 0:128], scalar1=zsc[:, 0:1], scalar2=None, op0=ALU.mult), reads=[pst[5], t_zsc], writes=[t_impn])
                for h in range(1, 4):
                    kb.op("dve", lambda e, h=h: e.scalar_tensor_tensor(out=impn[:], in0=ps[5][:, h * 128:(h + 1) * 128], scalar=zsc[:, h:h + 1], in1=impn[:], op0=ALU.mult, op1=ALU.add),
                          reads=[pst[5], t_zsc, t_impn], writes=[t_impn])
                if dbg:
                    kb.dma(d_imp[q0:q0 + 128, :], impn[:], reads=[t_impn])
                o0 = 127 - 2 * qt
                kb.op("dve", lambda e, o0=o0: e.tensor_tensor(out=impn[:], in0=impn[:], in1=tkeep[:, o0:o0 + 128], op=ALU.mult), reads=[t_impn, t_tkeep], writes=[t_impn])
                kb.op("dve", lambda e, o0=o0: e.tensor_tensor(out=impn[:], in0=impn[:], in1=tadd[:, o0:o0 + 128], op=ALU.add), reads=[t_impn, t_tadd], writes=[t_impn])
                kb.op("dve", lambda e: e.memset(impn[:, 0:1], 1e4), reads=[t_impn], writes=[t_impn])
                kb.op("dve", lambda e: e.max(out=m8[:, 0:8], in_=impn[:]), reads=[t_impn], writes=[t_m8])
                kb.op("dve", lambda e: e.match_replace(out=impw[:], in_to_replace=m8[:, 0:8], in_values=impn[:], imm_value=-3e38), reads=[t_impn, t_m8], writes=[t_impw])
                kb.op("dve", lambda e: e.max(out=m8[:, 8:16], in_=impw[:]), reads=[t_impw], writes=[t_m8])
                kb.op("dve", lambda e: e.tensor_scalar(out=selb[:], in0=impn[:], scalar1=m8[:, 15:16], scalar2=None, op0=ALU.is_ge), reads=[t_impn, t_m8], writes=[t_selb])
                if dbg:
                    kb.dma(d_sel[q0:q0 + 128, :], selb[:], reads=[t_selb])
                kb.op("pe", lambda e: e.matmul(ps[7][:, 0:128], lhsT=selb[:, :], rhs=identb3[:, :], start=True, stop=True), reads=[t_selb, t_identb3], writes=[pst[7]])
                kb.op("dve", lambda e: e.tensor_scalar(out=selT[:], in0=ps[7][:, 0:128].unsqueeze(1).broadcast_to([128, 4, 128]), scalar1=-1.0, scalar2=-NEG, op0=ALU.add, op1=ALU.mult),
                      reads=[pst[7]], writes=[t_selT])
                for kt in range(qt + 1):
                    if kt == qt:
                        masks = [(identb3[:, :], caus[:, :, :], [t_identb3, t_caus])]
                    else:
                        masks = [(expT[:, kt * 128:(kt + 1) * 128], selT[:, :, :], [t_expT, t_selT])]
                    tile_pair((KK[0:64, kt * 128:(kt + 1) * 128], t_KK), qv_lo, masks, (tb_sw, t_tbsw), qt - kt, (Vs[:, kt, :], t_Vs), 3, kt == 0, kt == qt)
                flush_pairs()
                kb.op("dve", lambda e: e.tensor_copy(out=oT[:, 1, :], in_=ps[3][0:65, :]), reads=[pst[3]], writes=[t_oT])
                kb.op("dve", lambda e: e.tensor_copy(out=oT[:, 2, :], in_=ps[4][0:65, :]), reads=[pst[4]], writes=[t_oT])
                for br in (1, 2):
                    for h in range(4):
                        col = 260 + ((br - 1) * 4 + h) * 65
                        pbk, cc = (6, col) if col + 65 <= 512 else (7, col - 455 + 128)
                        kb.op("pe", lambda e, h=h, br=br, pbk=pbk, cc=cc: e.matmul(ps[pbk][:, cc:cc + 65], lhsT=oT[0:65, br, h * 128:(h + 1) * 128], rhs=ident[0:65, 0:65], start=True, stop=True),
                              reads=[t_oT, t_ident], writes=[pst[pbk]])

                def oslot(br, h):
                    if br == 0:
                        return 6, h * 65
                    col = 260 + ((br - 1) * 4 + h) * 65
                    return (6, col) if col + 65 <= 512 else (7, col - 455 + 128)
                for br in (1, 2):
                    for h in range(4):
                        pbk, cc = oslot(br, h)
                        kb.op("dve", lambda e, br=br, h=h, pbk=pbk, cc=cc: e.tensor_scalar(out=zsc[:, br * 4 + h:br * 4 + h + 1], in0=ps[pbk][:, cc + 64:cc + 65], scalar1=1e-30, scalar2=None, op0=ALU.max),
                              reads=[pst[pbk]], writes=[t_zsc])
                kb.op("dve", lambda e: e.reciprocal(out=zsc[:, 4:12], in_=zsc[:, 4:12]), reads=[t_zsc], writes=[t_zsc])
                for br in range(3):
                    kb.op("dve", lambda e, br=br, qt=qt: e.tensor_tensor(out=zsc[:, 12 + br * 4:16 + br * 4], in0=zsc[:, br * 4:br * 4 + 4],
                                                                         in1=gts[:, qt, :].rearrange("p (h b) -> p h b", b=3)[:, :, br], op=ALU.mult),
                          reads=[t_zsc, t_gts], writes=[t_zsc])
                for h in range(4):
                    for br in range(3):
                        pbk, cc = oslot(br, h)
                        if br == 0:
                            kb.op("dve", lambda e, h=h, br=br, pbk=pbk, cc=cc: e.tensor_scalar(out=haf[:, h * 64:(h + 1) * 64], in0=ps[pbk][:, cc:cc + 64],
                                                                                               scalar1=zsc[:, 12 + br * 4 + h:13 + br * 4 + h], scalar2=None, op0=ALU.mult),
                                  reads=[pst[pbk], t_zsc], writes=[t_haf])
                        else:
                            kb.op("dve", lambda e, h=h, br=br, pbk=pbk, cc=cc: e.scalar_tensor_tensor(out=haf[:, h * 64:(h + 1) * 64], in0=ps[pbk][:, cc:cc + 64],
                                                                                                      scalar=zsc[:, 12 + br * 4 + h:13 + br * 4 + h], in1=haf[:, h * 64:(h + 1) * 64],
                                                                                                      op0=ALU.mult, op1=ALU.add),
                                  reads=[pst[pbk], t_zsc, t_haf], writes=[t_haf])
                kb.op("dve", lambda e: e.tensor_copy(out=hab[:], in_=haf[:]), reads=[t_haf], writes=[t_hab])
                ob = qt % 2
                for dc in range(2):
                    kb.op("pe", lambda e, dc=dc: e.matmul(ps[5][:, dc * 128:(dc + 1) * 128], lhsT=hab[:, dc * 128:(dc + 1) * 128], rhs=identb3[:, :], start=True, stop=True),
                          reads=[t_hab, t_identb3], writes=[pst[5]], sig=(dc == 1))
                kb.op("dve", lambda e, ob=ob: e.tensor_copy(out=haT[ob][:].rearrange("p a t -> p (a t)"), in_=ps[5][:, 0:256]), reads=[pst[5]], writes=[t_haT[ob]])
                kb.dma(mixT[qt // 8][256:512, (qt % 8) * 128:(qt % 8 + 1) * 128].rearrange("(a p) t -> p a t", p=128), haT[ob][:], reads=[t_haT[ob]], writes=[t_mixT[qt // 8]])
                if qt % 8 == 7:
                    gather_chunk(qt // 8)
            kb.barrier()
    if dbg:
        d_mixT = nc.dram_tensor("d_mixT", [512, S], BF16, kind="ExternalOutput").ap()
        for i in range(8):
            kb.dma(d_mixT[:, i * 1024:(i + 1) * 1024], mixT[i], reads=[t_mixT[i]])


    if phases >= 4:
        selm_in = din("selm", [128, 4])
        x_own = din("x_own", [2048, D])
        w_out_in = din("w_out_p", [D, D])
        g2_row = din("g2_row", [1, D])
        wq_in = din("wq", [D, D])
        skT_in = din("skT", [128, 2, 128])
        uT_in = din("uT", [D, 16384])
        v_in = din("v_tab", [16384, D])
        y_out = nc.dram_tensor("y", [2048, D], F32, kind="ExternalOutput").ap()

        s_x1 = dscr("s_x1", [2048, D]); t_sx1 = Trk()
        s_h2T = dscr("s_h2T", [D, 2048], BF16); t_sh2T = Trk()
        with ExitStack() as p45:
            def sb45(name, shape, dt=F32):
                return p45.enter_context(nc.sbuf_tensor(name, list(shape), dt))
            g2tb = sb45("g2tb", [128, D]); t_g2tb = Trk()
            identb4 = sb45("identb4", [128, 128], BF16); t_identb4 = Trk()
            kb.dma(identb4[:], din("ident_b2", [128, 128], BF16), writes=[t_identb4])
            with ExitStack() as p4:
                def sb4(name, shape, dt=F32):
                    return p4.enter_context(nc.sbuf_tensor(name, list(shape), dt))
                rowst = sb4("rowst", [1, D]); t_rowst = Trk()
                g1b = sb4("g1b", [128, D]); t_g1b = Trk()
                sh2b = sb4("sh2b", [128, D]); t_sh2b = Trk()
                gs2b = sb4("gs2b", [128, D]); t_gs2b = Trk()
                selm = sb4("selm_sb", [128, 4]); t_selm = Trk()
                kb.dma(selm[:], selm_in, writes=[t_selm])

                def bcast_row(src_ap, dst, t_dst, src_reads=()):
                    kb.dma(rowst[:], src_ap, reads=list(src_reads), writes=[t_rowst])
                    for n in range(4):
                        kb.op("pe", lambda e, n=n: e.matmul(ps[n][:, :], lhsT=ones_f[0:1, :], rhs=rowst[0:1, n * 512:(n + 1) * 512], start=True, stop=True),
                              reads=[t_ones_f, t_rowst], writes=[pst[n]])
                        kb.op("dve", lambda e, n=n: e.tensor_copy(out=dst[:, n * 512:(n + 1) * 512], in_=ps[n][:, :]), reads=[pst[n]], writes=[t_dst])
                bcast_row(s_mod[0:1, 2 * D:3 * D], g1b, t_g1b, [t_smod])
                bcast_row(s_mod[0:1, 3 * D:4 * D], sh2b, t_sh2b, [t_smod])
                bcast_row(s_mod[0:1, 5 * D:6 * D], g2tb, t_g2tb, [t_smod])
                bcast_row(s_mod[0:1, 4 * D:5 * D], gs2b, t_gs2b, [t_smod])
                kb.dma(rowst[:], g2_row, writes=[t_rowst])
                for n in range(4):
                    kb.op("pe", lambda e, n=n: e.matmul(ps[n][:, :], lhsT=ones_f[0:1, :], rhs=rowst[0:1, n * 512:(n + 1) * 512], start=True, stop=True),
                          reads=[t_ones_f, t_rowst], writes=[pst[n]])
                    kb.op("dve", lambda e, n=n: e.scalar_tensor_tensor(out=gs2b[:, n * 512:(n + 1) * 512], in0=gs2b[:, n * 512:(n + 1) * 512], scalar=1.0, in1=ps[n][:, :],
                                                                       op0=ALU.add, op1=ALU.mult), reads=[pst[n], t_gs2b], writes=[t_gs2b])
                wo = sb4("wo", [128, 16, D], BF16); t_wo = Trk()
                for fc in range(16):
                    kb.dma(wo[:, fc, :], w_out_in[fc * 128:(fc + 1) * 128, :], writes=[t_wo], q="pool")
                sl2 = [[sb4("sl%d_%d" % (j2, i), [128, 16, 128], BF16) for i in range(4)] for j2 in range(2)]
                t_sl2 = [[Trk() for _ in range(4)] for _ in range(2)]
                mixo = sb4("mixo", [128, 16, 128], BF16); t_mixo = Trk()
                xin = [sb4("xin%d" % i, [128, D]) for i in range(2)]; t_xin = [Trk(), Trk()]
                tmpy = sb4("tmpy", [128, D]); t_tmpy = Trk()
                h2b = sb4("h2b", [128, D], BF16); t_h2b = Trk()
                h2Tt = [sb4("h2Tt%d" % i, [128, 16, 128], BF16) for i in range(2)]; t_h2Tt = [Trk(), Trk()]
                nsc = sb4("nsc", [128, 4]); t_nsc = Trk()
                mgv = [mg.rearrange("(f p) t -> p f t", p=128) for mg in mixG]
                def load_tt(tt):
                    bi = tt % 2
                    kb.dma(xin[bi][:], x_own[tt * 128:(tt + 1) * 128, :], writes=[t_xin[bi]])
                    for s_ in range(4):
                        kb.dma(sl2[bi][s_][:], mgv[2 * s_ + tt // 8][:, :, (tt % 8) * 128:(tt % 8 + 1) * 128], reads=[t_mixG[2 * s_ + tt // 8]], writes=[t_sl2[bi][s_]])
                load_tt(0)
                for tt in range(16):
                    bi = tt % 2
                    sl = sl2[bi]; t_sl = t_sl2[bi]
                    if tt + 1 < 16:
                        load_tt(tt + 1)
                    kb.op("dve", lambda e: e.tensor_scalar(out=mixo[:], in0=sl[0][:], scalar1=selm[:, 0:1], scalar2=None, op0=ALU.mult), reads=[t_sl[0], t_selm], writes=[t_mixo])
                    for s_ in range(1, 4):
                        kb.op("dve", lambda e, s_=s_: e.scalar_tensor_tensor(out=mixo[:], in0=sl[s_][:], scalar=selm[:, s_:s_ + 1], in1=mixo[:], op0=ALU.mult, op1=ALU.add),
                              reads=[t_sl[s_], t_selm, t_mixo], writes=[t_mixo])
                    for n in range(4):
                        for fc in range(16):
                            kb.op("pe", lambda e, n=n, fc=fc: e.matmul(ps[n][:, :], lhsT=mixo[:, fc, :], rhs=wo[:, fc, n * 512:(n + 1) * 512], start=(fc == 0), stop=(fc == 15)),
                                  reads=[t_mixo, t_wo], writes=[pst[n]], sig=(fc == 15))
                        kb.op("dve", lambda e, n=n: e.tensor_tensor(out=tmpy[:, n * 512:(n + 1) * 512], in0=ps[n][:, :], in1=g1b[:, n * 512:(n + 1) * 512], op=ALU.mult),
                              reads=[pst[n], t_g1b], writes=[t_tmpy])
                        kb.op("dve", lambda e, n=n, bi=bi: e.tensor_tensor(out=xin[bi][:, n * 512:(n + 1) * 512], in0=xin[bi][:, n * 512:(n + 1) * 512], in1=tmpy[:, n * 512:(n + 1) * 512], op=ALU.add),
                              reads=[t_xin[bi], t_tmpy], writes=[t_xin[bi]])
                    kb.dma(s_x1[tt * 128:(tt + 1) * 128, :], xin[bi][:], reads=[t_xin[bi]], writes=[t_sx1])
                    kb.op("act", lambda e, bi=bi: e.activation(out=tmpy[:], in_=xin[bi][:], func=AF.Square, accum_out=nsc[:, 0:1]), reads=[t_xin[bi]], writes=[t_tmpy, t_nsc])
                    kb.op("act", lambda e: e.activation(out=nsc[:, 1:2], in_=nsc[:, 0:1], func=AF.Sqrt, scale=1.0 / D, bias=EPS), reads=[t_nsc], writes=[t_nsc])
                    kb.op("dve", lambda e: e.reciprocal(out=nsc[:, 2:3], in_=nsc[:, 1:2]), reads=[t_nsc], writes=[t_nsc])
                    kb.op("dve", lambda e, bi=bi: e.scalar_tensor_tensor(out=tmpy[:], in0=xin[bi][:], scalar=nsc[:, 2:3], in1=gs2b[:], op0=ALU.mult, op1=ALU.mult),
                          reads=[t_xin[bi], t_nsc, t_gs2b], writes=[t_tmpy])
                    kb.op("dve", lambda e: e.tensor_tensor(out=h2b[:], in0=tmpy[:], in1=sh2b[:], op=ALU.add), reads=[t_tmpy, t_sh2b], writes=[t_h2b])
                    for qd in range(4):
                        pb = 4 + qd
                        for k in range(4):
                            dc = qd * 4 + k
                            kb.op("pe", lambda e, dc=dc, k=k, pb=pb: e.matmul(ps[pb][:, k * 128:(k + 1) * 128], lhsT=h2b[:, dc * 128:(dc + 1) * 128], rhs=identb4[:, :], start=True, stop=True),
                                  reads=[t_h2b, t_identb4], writes=[pst[pb]], sig=(k == 3))
                        kb.op("act", lambda e, qd=qd, pb=pb, bi=bi: e.activation(out=h2Tt[bi][:, qd * 4:(qd + 1) * 4, :].rearrange("p a t -> p (a t)"), in_=ps[pb][:, :], func=AF.Copy),
                              reads=[pst[pb]], writes=[t_h2Tt[bi]])
                    kb.dma(s_h2T[:, tt * 128:(tt + 1) * 128].rearrange("(a p) t -> p a t", p=128), h2Tt[bi][:], reads=[t_h2Tt[bi]], writes=[t_sh2T])
                kb.barrier()

            if phases >= 5:
                with ExitStack() as p5:
                    def sb5(name, shape, dt=F32):
                        return p5.enter_context(nc.sbuf_tensor(name, list(shape), dt))
                    skT = sb5("skT_sb", [128, 2, 128], BF16); t_skT = Trk()
                    kb.dma(skT[:], skT_in, writes=[t_skT], q="pool")
                    h2g = sb5("h2g", [128, 16, 512], BF16); t_h2g = Trk()
                    s1m = sb5("s1m", [128, 4, 8, 128]); t_s1m = Trk()
                    s2t = sb5("s2t", [128, 4, 8, 128]); t_s2t = Trk()
                    tau = sb5("tau", [128, 4, 8]); t_tau = Trk()
                    Ysb = sb5("Ysb", [128, 4, D]); t_Ysb = [Trk() for _ in range(4)]
                    wqs = [sb5("wqs%d" % i, [128, 16, 128], BF16) for i in range(2)]; t_wqs = [Trk(), Trk()]
                    sct = sb5("sct", [128, 16, 128]); t_sct = Trk()
                    wk = sb5("wk", [128, 256]); t_wk = Trk()
                    tv = sb5("tv", [128, 16, 16]); t_tv = Trk()
                    cand = sb5("cand", [128, 8, 256]); t_cand = Trk()
                    cw2 = sb5("cw2", [128, 256]); t_cw2 = Trk()
                    m24 = sb5("m24", [128, 8, 24]); t_m24 = Trk()
                    hs = sb5("hs", [128, 8, 8]); t_hs = Trk()
                    ex = sb5("ex", [128, 8, 256]); t_ex = Trk()
                    Ub = [sb5("Ub%d" % i, [128, 16, 256], BF16) for i in range(2)]; t_Ub = [Trk(), Trk()]
                    Vb = [sb5("Vb%d" % i, [128, 2, D], BF16) for i in range(2)]; t_Vb = [Trk(), Trk()]
                    Lt = [sb5("Lt0", [128, 8, 256]), cand]; t_Lt = [Trk(), t_cand]
                    Dg = sb5("Dg", [128, 4, 8, 128], BF16); t_Dg = Trk()
                    t_Ex = [[Trk() for _ in range(16)] for _ in range(2)]
                    Wt = [sb5("Wt%d" % i, [128, 8, 256], BF16) for i in range(2)]; t_Wt = [[Trk() for _ in range(8)] for _ in range(2)]
                    glT = [sb5("glT%d" % i, [128, 2, 512], BF16) for i in range(2)]; t_glT = [Trk(), Trk()]
                    GT = [sb5("GT%d" % i, [128, 2, 128], BF16) for i in range(2)]; t_GT = [Trk(), Trk()]
                    x1t = Lt[0][:].rearrange("p h e -> p (h e)"); t_x1t = t_Lt[0]
                    h2v = s_h2T.rearrange("(a p) t -> p a t", p=128)
                    wqv = wq_in.rearrange("(a p) n -> p a n", p=128)
                    uTv = uT_in.rearrange("(a p) e -> p a e", p=128)
                    vv_ = v_in.rearrange("(c p) d -> p c d", p=128)
                    ngrp = int(os.environ.get("NGRP", "4"))
                    net = int(os.environ.get("NET", "64"))
                    for g in range(ngrp):
                        kb.dma(h2g[:, 0:8, :], h2v[:, 0:8, g * 512:(g + 1) * 512], reads=[t_sh2T], writes=[t_h2g])
                        kb.dma(h2g[:, 8:16, :], h2v[:, 8:16, g * 512:(g + 1) * 512], reads=[t_sh2T], writes=[t_h2g])
                        for tl in range(4):
                            pass
                        qall = p5.enter_context(nc.sbuf_tensor("qall%d" % g, [128, 16, 512], BF16)) if g == 0 else qall
                        t_qall = Trk() if g == 0 else t_qall
                        for ch in range(16):
                            wi = ch % 2
                            kb.dma(wqs[wi][:], wqv[:, :, ch * 128:(ch + 1) * 128], writes=[t_wqs[wi]], q="pool")
                            pb = ch % 2
                            for dc in range(16):
                                kb.op("pe", lambda e, dc=dc, wi=wi, pb=pb: e.matmul(ps[pb][:, :], lhsT=wqs[wi][:, dc, :], rhs=h2g[:, dc, :], start=(dc == 0), stop=(dc == 15)),
                                      reads=[t_wqs[wi], t_h2g], writes=[pst[pb]], sig=(dc == 15))
                            kb.op("act", lambda e, ch=ch, pb=pb: e.activation(out=qall[:, ch, :], in_=ps[pb][:, :], func=AF.Copy), reads=[pst[pb]], writes=[t_qall])
                        for tl in range(4):
                            for qd in range(4):
                                pb = 2 + (qd % 2)
                                for k in range(4):
                                    ch = qd * 4 + k
                                    kb.op("pe", lambda e, ch=ch, k=k, pb=pb, tl=tl: e.matmul(ps[pb][:, k * 128:(k + 1) * 128], lhsT=qall[:, ch, tl * 128:(tl + 1) * 128], rhs=skT[:, ch % 2, :],
                                                                                             start=True, stop=True),
                                          reads=[t_qall, t_skT], writes=[pst[pb]], sig=(k == 3))
                                kb.op("dve", lambda e, qd=qd, pb=pb: e.tensor_copy(out=sct[:, qd * 4:(qd + 1) * 4, :].rearrange("p a k -> p (a k)"), in_=ps[pb][:, :]),
                                      reads=[pst[pb]], writes=[t_sct])
                            for ch in range(16):
                                kb.op("dve", lambda e, ch=ch: e.max(out=tv[:, ch, 0:8], in_=sct[:, ch, :]), reads=[t_sct], writes=[t_tv])
                                kb.op("dve", lambda e, ch=ch: e.match_replace(out=wk[:, 0:128], in_to_replace=tv[:, ch, 0:8], in_values=sct[:, ch, :], imm_value=-3e38),
                                      reads=[t_sct, t_tv], writes=[t_wk])
                                kb.op("dve", lambda e, ch=ch: e.max(out=tv[:, ch, 8:16], in_=wk[:, 0:128]), reads=[t_wk], writes=[t_tv])
                            tvv = tv[:].rearrange("p (h two) a -> p h two a", two=2)
                            kb.op("dve", lambda e: e.tensor_tensor(out=cand[:].rearrange("p h (a b) -> p h a b", b=16),
                                                                   in0=tvv[:, :, 0, :].unsqueeze(3).broadcast_to([128, 8, 16, 16]),
                                                                   in1=tvv[:, :, 1, :].unsqueeze(2).broadcast_to([128, 8, 16, 16]), op=ALU.add),
                                  reads=[t_tv], writes=[t_cand])
                            for h in range(8):
                                kb.op("dve", lambda e, h=h: e.max(out=m24[:, h, 0:8], in_=cand[:, h, :]), reads=[t_cand], writes=[t_m24])
                                kb.op("dve", lambda e, h=h: e.match_replace(out=cw2[:], in_to_replace=m24[:, h, 0:8], in_values=cand[:, h, :], imm_value=-3e38),
                                      reads=[t_cand, t_m24], writes=[t_cw2])
                                kb.op("dve", lambda e, h=h: e.max(out=m24[:, h, 8:16], in_=cw2[:]), reads=[t_cw2], writes=[t_m24])
                                kb.op("dve", lambda e, h=h: e.match_replace(out=cw2[:], in_to_replace=m24[:, h, 8:16], in_values=cw2[:], imm_value=-3e38),
                                      reads=[t_cw2, t_m24], writes=[t_cw2])
                                kb.op("dve", lambda e, h=h: e.max(out=m24[:, h, 16:24], in_=cw2[:]), reads=[t_cw2], writes=[t_m24])
                            kb.op("dve", lambda e: e.tensor_copy(out=hs[:, :, 0], in_=m24[:, :, 0]), reads=[t_m24], writes=[t_hs])
                            kb.op("dve", lambda e: e.tensor_tensor(out=hs[:, :, 1], in0=m24[:, :, 15], in1=m24[:, :, 16], op=ALU.add), reads=[t_m24], writes=[t_hs])
                            kb.op("dve", lambda e: e.tensor_scalar(out=hs[:, :, 1], in0=hs[:, :, 1], scalar1=0.5, scalar2=None, op0=ALU.mult), reads=[t_hs], writes=[t_hs])
                            kb.op("dve", lambda e: e.tensor_tensor(out=ex[:], in0=cand[:], in1=hs[:, :, 0:1].broadcast_to([128, 8, 256]), op=ALU.subtract), reads=[t_cand, t_hs], writes=[t_ex])
                            kb.op("act", lambda e: e.activation(out=ex[:], in_=ex[:], func=AF.Exp), reads=[t_ex], writes=[t_ex])
                            kb.op("dve", lambda e: e.tensor_tensor(out=cand[:], in0=cand[:], in1=hs[:, :, 1:2].broadcast_to([128, 8, 256]), op=ALU.is_ge), reads=[t_cand, t_hs], writes=[t_cand])
                            kb.op("dve", lambda e: e.tensor_tensor(out=ex[:], in0=ex[:], in1=cand[:], op=ALU.mult), reads=[t_cand, t_ex], writes=[t_ex])
                            kb.op("dve", lambda e: e.tensor_reduce(out=hs[:, :, 2], in_=ex[:], axis=AX.X, op=ALU.add), reads=[t_ex], writes=[t_hs])
                            kb.op("act", lambda e: e.activation(out=hs[:, :, 3], in_=hs[:, :, 2], func=AF.Ln), reads=[t_hs], writes=[t_hs])
                            kb.op("dve", lambda e: e.tensor_tensor(out=hs[:, :, 4], in0=hs[:, :, 0], in1=hs[:, :, 3], op=ALU.add), reads=[t_hs], writes=[t_hs])
                            sv = sct[:].rearrange("p (h two) k -> p h two k", two=2)
                            kb.op("dve", lambda e, tl=tl: e.tensor_tensor(out=s1m[:, tl, :, :], in0=sv[:, :, 0, :], in1=hs[:, :, 4:5].broadcast_to([128, 8, 128]), op=ALU.subtract),
                                  reads=[t_sct, t_hs], writes=[t_s1m])
                            kb.op("dve", lambda e, tl=tl: e.tensor_copy(out=s2t[:, tl, :, :], in_=sv[:, :, 1, :]), reads=[t_sct], writes=[t_s2t])
                            kb.op("dve", lambda e, tl=tl: e.tensor_tensor(out=tau[:, tl, :], in0=hs[:, :, 1], in1=hs[:, :, 4], op=ALU.subtract), reads=[t_hs], writes=[t_tau])
                            kb.op("dve", lambda e, tl=tl: e.tensor_tensor(out=s1m[:, tl, :, :], in0=s1m[:, tl, :, :], in1=tau[:, tl, :].unsqueeze(2).broadcast_to([128, 8, 128]), op=ALU.subtract),
                                  reads=[t_s1m, t_tau], writes=[t_s1m])
                            kb.op("act", lambda e, tl=tl: e.activation(out=hs[:, :, 5], in_=tau[:, tl, :], func=AF.Exp), reads=[t_tau], writes=[t_hs])
                            for h in range(8):
                                kb.op("dve", lambda e, tl=tl, h=h: e.tensor_scalar(out=Dg[:, tl, h, :], in0=identb4[:, :], scalar1=hs[:, h, 5:6], scalar2=None, op0=ALU.mult),
                                      reads=[t_identb4, t_hs], writes=[t_Dg])
                        kb.op("dve", lambda e: e.memset(Ysb[:], 0.0), writes=t_Ysb)

                        def load_u(et):
                            bi = et % 2
                            kb.dma(Ub[bi][:, 0:8, :], uTv[:, 0:8, et * 256:(et + 1) * 256], writes=[t_Ub[bi]], q="pool")
                            kb.dma(Ub[bi][:, 8:16, :], uTv[:, 8:16, et * 256:(et + 1) * 256], writes=[t_Ub[bi]], q="pool")

                        def load_v(et):
                            bi = et % 2
                            kb.dma(Vb[bi][:], vv_[:, et * 2:et * 2 + 2, :], writes=[t_Vb[bi]], q="pool")

                        def emit_a(et):
                            bi = et % 2
                            for ec in range(2):
                                for dc in range(16):
                                    kb.op("pe", lambda e, dc=dc, ec=ec, bi=bi: e.matmul(ps[ec][:, :], lhsT=Ub[bi][:, dc, ec * 128:(ec + 1) * 128], rhs=h2g[:, dc, :],
                                                                                        start=(dc == 0), stop=(dc == 15)),
                                          reads=[t_Ub[bi], t_h2g], writes=[pst[ec]], sig=(dc == 15))
                                kb.op("act", lambda e, ec=ec, bi=bi: e.activation(out=glT[bi][:, ec, :], in_=ps[ec][:, :], func=AF.Gelu_apprx_tanh),
                                      reads=[pst[ec]], writes=[t_glT[bi]])
                        load_u(0)
                        if net > 1:
                            load_u(1)
                        load_v(0)
                        emit_a(0)
                        pairs = [(et, tl) for et in range(net) for tl in range(4)]
                        NP = len(pairs)

                        def st_L(n):
                            et, tl = pairs[n]; k = n % 2
                            for h in range(8):
                                for a2 in range(2):
                                    kb.op("act", lambda e, h=h, a2=a2: e.activation(out=Lt[k][:, h, a2 * 128:(a2 + 1) * 128], in_=s2t[:, tl, h, :], func=AF.Exp,
                                                                                    bias=s1m[:, tl, h, 2 * et + a2:2 * et + a2 + 1]),
                                          reads=[t_s2t, t_s1m], writes=[t_Ex[k][h * 2 + a2], t_Lt[k]] if (h == 0 and a2 == 0) else [t_Ex[k][h * 2 + a2]])

                        def st_C(n):
                            et, tl = pairs[n]; k = n % 2
                            kb.op("dve", lambda e: e.scalar_tensor_tensor(out=Wt[k][:], in0=Lt[k][:], scalar=1.0, in1=Lt[k][:], op0=ALU.is_ge, op1=ALU.mult),
                                  reads=t_Ex[k] + [t_Lt[k]], writes=t_Wt[k])
                            pw = 2 + k
                            for ec in range(2):
                                for h in range(8):
                                    kb.op("pe", lambda e, ec=ec, h=h: e.matmul(ps[pw][:, ec * 128:(ec + 1) * 128], lhsT=Wt[k][:, h, ec * 128:(ec + 1) * 128], rhs=Dg[:, tl, h, :],
                                                                               start=(h == 0), stop=(h == 7)),
                                          reads=[t_Wt[k][h], t_Dg], writes=[pst[pw]], sig=(ec == 1 and h == 7))

                        def st_G(n):
                            et, tl = pairs[n]; k = n % 2; bi = et % 2; pw = 2 + k
                            if tl == 0:
                                if et + 1 < net:
                                    load_v(et + 1)
                                    emit_a(et + 1)
                                if et + 2 < net:
                                    load_u(et + 2)
                            kb.op("dve", lambda e: e.tensor_tensor(out=GT[k][:], in0=glT[bi][:, :, tl * 128:(tl + 1) * 128],
                                                                   in1=ps[pw][:, 0:256].rearrange("p (a t) -> p a t", t=128), op=ALU.mult),
                                  reads=[t_glT[bi], pst[pw]], writes=[t_GT[k]])
                            for nn in range(4):
                                pb = 4 + nn
                                for ec in range(2):
                                    kb.op("pe", lambda e, ec=ec, nn=nn, pb=pb: e.matmul(ps[pb][:, :], lhsT=GT[k][:, ec, :], rhs=Vb[bi][:, ec, nn * 512:(nn + 1) * 512],
                                                                                        start=(ec == 0), stop=(ec == 1)),
                                          reads=[t_GT[k], t_Vb[bi]], writes=[pst[pb]], sig=(ec == 1))

                        def st_Y(n):
                            et, tl = pairs[n]
                            kb.op("dve", lambda e: e.tensor_tensor(out=Ysb[:, tl, :], in0=Ysb[:, tl, :], in1=psY[:, :], op=ALU.add),
                                  reads=[t_Ysb[tl], pst[4], pst[5], pst[6], pst[7]], writes=[t_Ysb[tl]])
                        for n in range(-2, NP):
                            if 0 <= n:
                                st_G(n)
                            if 0 <= n + 2 < NP:
                                st_L(n + 2)
                            if 0 <= n + 1 < NP:
                                st_C(n + 1)
                            if 0 <= n:
                                st_Y(n)
                        for tl in range(4):
                            r0 = (g * 4 + tl) * 128
                            kb.dma(x1t, s_x1[r0:r0 + 128, :], reads=[t_sx1], writes=[t_x1t])
                            kb.op("dve", lambda e, tl=tl: e.tensor_tensor(out=Ysb[:, tl, :], in0=Ysb[:, tl, :], in1=g2tb[:], op=ALU.mult), reads=[t_Ysb[tl], t_g2tb], writes=[t_Ysb[tl]])
                            kb.op("dve", lambda e, tl=tl: e.tensor_tensor(out=x1t, in0=x1t, in1=Ysb[:, tl, :], op=ALU.add), reads=[t_Ysb[tl], t_x1t], writes=[t_x1t])
                            kb.dma(y_out[r0:r0 + 128, :], x1t, reads=[t_x1t])
                    kb.barrier()

    for _i in range(int(os.environ.get('EXTRA', '0'))):
        kb.dma(s_mod[0:1, 0:128], ident[0:1, :], reads=[t_ident])
    kb.barrier()
    ok, stuck, sems = kb.check()
    if not ok or os.environ.get('KBV'):
        print('SYNC CHECK ok=%s' % ok, stuck, {k: v for k, v in kb.cnt.items()})
    assert ok, 'sync deadlock'
    es.close()
    return nc


def _consts():
    ones = np.ones((128, 128), np.float32)
    blk = np.zeros((128, 128), np.float32)
    blk[:64, :64] = 1.0
    blk[64:, 64:] = 1.0
    ut = np.triu(np.ones((128, 128), np.float32))
    NEG = -30000.0
    il = np.arange(128)
    cmask = np.zeros((128, 17, 128), np.float32)
    for m in range(17):
        ok = (il[None, :] + 128 * m) >= (16 * il[:, None] + 31)
        cmask[:, m, :] = np.where(ok, 0.0, NEG)
    caus = np.where(il[:, None] <= il[None, :], 0.0, NEG).astype(np.float32)
    acaus = np.where(il[:, None] > il[None, :], 0.0, NEG).astype(np.float32)
    i_abs = (np.arange(4)[None, :, None] * 128 + il[:, None, None])
    blkk = np.arange(128)[None, None, :]
    ovl = ((16 * i_abs < 64 * blkk + 64) & (16 * i_abs + 32 > 64 * blkk)).astype(np.float32)
    expT = (np.arange(S)[None, :] // 64 == il[:, None]).astype(np.float32)
    o = np.arange(255)[None, :] - 127
    cur_rel = (il[:, None] >= 64).astype(np.int64)
    rel = o - cur_rel
    t_keep = (rel < -1).astype(np.float32)
    t_add = np.where(rel > 0, -1e4, np.where(rel >= -1, 1e4, 0.0)).astype(np.float32)
    return {"ones_bf": _bf(ones), "blk64_bf": _bf(blk), "ident_f": np.eye(128, dtype=np.float32),
            "ut_f": ut, "ident_b": _bf(np.eye(128, dtype=np.float32)),
            "cmask": _bf(cmask), "caus_add": _bf(caus), "acaus_add": _bf(acaus), "ovl": _bf(ovl), "expT": _bf(expT),
            "t_keep": np.ascontiguousarray(np.broadcast_to(t_keep, (128, 255))).astype(np.float32), "t_add": t_add}


def make_in_maps(inp):
    f = lambda a: np.ascontiguousarray(np.asarray(a, dtype=np.float32))
    x = f(inp["x"]); c = f(inp["c"])
    w_in = f(inp["w_in"])[0]
    conv = f(inp["conv_qk"])[0]
    qn_g = f(inp["qn_g"])[0]; kn_g = f(inp["kn_g"])[0]
    mng = f(inp["mlstm_norm_g"])[0]
    g1 = f(inp["norm1_g"])[0]
    cst = _consts()
    maps = []
    xTs = [np.ascontiguousarray(x[b].T) for b in range(2)]
    w_out = f(inp["w_out"])[0]
    rows = []
    for fc in range(16):
        r, part, half = fc // 4, (fc % 4) // 2, fc % 2
        base = part * 1024 + r * 256 + half * 128
        rows += list(range(base, base + 128))
    w_out_p = np.ascontiguousarray(w_out[rows])
    wq = f(inp["peer_wq"])[0]
    skT = np.ascontiguousarray(f(inp["peer_subkeys"])[0].transpose(2, 0, 1))
    uT = np.ascontiguousarray(f(inp["peer_u"])[0].T)
    vtab = f(inp["peer_v"])[0]
    for core in range(8):
        b, j = divmod(core, 4)
        cols = []
        cols += list(range(j * 256, j * 256 + 256))
        cols += list(range(1024 + j * 256, 1024 + j * 256 + 256))
        AQ = 4096 + 8
        cols += list(range(AQ + j * 256, AQ + j * 256 + 256))
        AKV = AQ + 1024
        for br in (0, 1, 2, 4):
            cols += list(range(AKV + br * 256 + j * 64, AKV + br * 256 + j * 64 + 64))
        cols += [4096 + j, 4096 + 4 + j]
        cols += list(range(2048 + j * 256, 2048 + j * 256 + 256))
        cols += list(range(3072 + j * 256, 3072 + j * 256 + 256))
        for br in (3, 5):
            cols += list(range(AKV + br * 256 + j * 64, AKV + br * 256 + j * 64 + 64))
        AG = AKV + 1536
        cols += list(range(AG + j * 12, AG + j * 12 + 12))
        assert len(cols) == 1678
        wj = np.zeros((D, 1680), np.float32)
        wj[:, :1678] = w_in[:, cols]
        cwt = np.zeros((128, 4, 4), np.float32)
        for t in range(4):
            base = (j * 256 + (t % 2) * 128) if t < 2 else (1024 + j * 256 + (t % 2) * 128)
            cwt[:, t, :] = conv[:, base:base + 128].T
        qkg = np.zeros((128, 4), np.float32)
        qkg[:, 0] = np.tile(qn_g, 2) * 0.125
        qkg[:, 3] = np.concatenate([kn_g[1], kn_g[2]])
        m = {
            "xT": xTs[b], "c_col": np.ascontiguousarray(c[b].reshape(16, 128).T),
            "w_mod": f(inp["w_mod"])[0], "b_mod": f(inp["b_mod"]),
            "g1_col": np.ascontiguousarray(g1.reshape(16, 128).T),
            "w_in": wj, "convw": cwt, "qk_g": qkg,
            "mng_row": np.ascontiguousarray(mng[j * 256:(j + 1) * 256][None, :]),
            "bif": np.ascontiguousarray(np.tile(np.array([[f(inp["b_igate"])[0, j], f(inp["b_fgate"])[0, j]]], np.float32), (128, 1))),
        }
        slopes = np.array([2.0 ** (-8.0 * (4 * j + hh + 1) / 16) for hh in range(4)], np.float64)
        kl = np.arange(128, dtype=np.float64)
        dl = np.arange(64, dtype=np.float64)
        m["tb_sw"] = (slopes[None, None, :] * (kl[:, None, None] - 64 - 128 * dl[None, :, None])).astype(np.float32)
        m["tb_c"] = (slopes[None, None, :] * (16 * kl[:, None, None] - 33 - 128 * dl[None, :, None])).astype(np.float32)
        m["w1k"] = f(inp["cmp_k_w1"])[0]; m["w1v"] = f(inp["cmp_v_w1"])[0]
        m["w2k"] = f(inp["cmp_k_w2"])[0]; m["w2v"] = f(inp["cmp_v_w2"])[0]
        for nm, key in (("posk", "cmp_pos_k"), ("posv", "cmp_pos_v")):
            pos = f(inp[key])[0]
            m[nm] = np.ascontiguousarray(pos.reshape(16, 2, 64).transpose(1, 2, 0).reshape(128, 16))
        m["kng0"] = np.ascontiguousarray(kn_g[0][:, None])
        sm = np.zeros((128, 4), np.float32); sm[:, j] = 1.0
        m["selm"] = sm
        m["x_own"] = np.ascontiguousarray(x[b, j * 2048:(j + 1) * 2048])
        m["w_out_p"] = w_out_p
        m["g2_row"] = f(inp["norm2_g"])
        m["wq"] = wq
        m["skT"] = skT
        m["uT"] = uT
        m["v_tab"] = vtab
        m["ident_b2"] = cst["ident_b"]
        m.update(cst)
        maps.append(m)
    return maps


def kernel(**inputs):
    nc = build()
    maps = make_in_maps(inputs)
    res = run_bass_kernel_spmd(nc, maps, core_ids=list(range(8)))
    out = np.zeros((2, S, D), np.float32)
    for core in range(8):
        b, j = divmod(core, 4)
        out[b, j * 2048:(j + 1) * 2048] = res.results[core]["y"]
    return out
```
